# Optimizing a Trainium2 kernel written in Bass

```python
import math
import jax, jax.numpy as jnp
from jax import lax
import numpy as np

D_MODEL = 1024
BATCH = 2
SEQ = 16384
DEPTH = 1

GRID_W = 64
CTX_LEN = 256
EPS = 1e-6
CHUNK = 128
CONV_K = 5
D_SSD = D_MODEL
SSD_HEAD_DIM = 64
SSD_HEADS = D_SSD // SSD_HEAD_DIM
SSD_STATE = 128
SSD_GROUPS = 2
D_XBC = D_SSD + 2 * SSD_GROUPS * SSD_STATE
D_MLSTM = D_MODEL
MLSTM_DV = 128
MLSTM_HEADS = D_MLSTM // MLSTM_DV
MLSTM_DK = MLSTM_DV // 2
D_QK = 2 * MLSTM_HEADS * MLSTM_DK
N_EXPERTS = 256
TOP_K = 8
N_EXPERT_GROUPS = 8
TOPK_GROUPS = 4
D_EXPERT = D_MODEL // 4
D_SHARED = D_EXPERT
ROUTED_SCALE = 2.5
MOE_BLOCK = 128
IN_PROJ_SIZES = (D_SSD, D_XBC, 2 * SSD_HEADS, D_QK, D_MLSTM, 4 * MLSTM_HEADS, D_MLSTM, 2 * D_MODEL)
D_IN_PROJ = sum(IN_PROJ_SIZES)

kernel_name = 'hybrid_ssd_mlstm_moe_prefix_block'


def rmsnorm(t, g):
    t32 = t.astype(jnp.float32)
    y = t32 * lax.rsqrt(jnp.mean(t32 * t32, axis=-1, keepdims=True) + EPS)
    return (y * g.astype(jnp.float32)).astype(t.dtype)


def modulate(t, g, shift, scale):
    return rmsnorm(t, g) * (1 + scale) + shift


def dwconv_centred(t, w, b, n_seg):
    bsz, length, ch = t.shape
    seg = t.reshape(bsz * n_seg, length // n_seg, ch)
    y = lax.conv_general_dilated(seg, w[:, None, :].astype(t.dtype), window_strides=(1,),
                                 padding=((CONV_K // 2, CONV_K // 2),),
                                 dimension_numbers=('NWC', 'WIO', 'NWC'), feature_group_count=ch)
    return y.reshape(bsz, length, ch) + b


def to_chunks(t):
    bsz, length = t.shape[:2]
    return jnp.moveaxis(t.reshape(bsz, length // CHUNK, CHUNK, *t.shape[2:]), 1, 0)


def from_chunks(t):
    t = jnp.moveaxis(t, 0, 1)
    return t.reshape(t.shape[0], t.shape[1] * t.shape[2], *t.shape[3:])


def ssd_scan(xh, dt, a_neg, bm, cm, s0, with_output):
    bsz, length, n_heads, hd = xh.shape
    n_groups, d_state = bm.shape[2], bm.shape[3]
    hpg = n_heads // n_groups
    f32 = jnp.float32
    xs = (to_chunks(xh.astype(f32).reshape(bsz, length, n_groups, hpg, hd)),
          to_chunks(dt.reshape(bsz, length, n_groups, hpg)),
          to_chunks(bm.astype(f32)), to_chunks(cm.astype(f32)))
    ag = a_neg.reshape(n_groups, hpg)
    causal = jnp.tril(jnp.ones((CHUNK, CHUNK), dtype=bool))

    def step(s, inp):
        xc, dtc, bc, cc = inp
        cum = jnp.cumsum(dtc * ag, axis=1)
        total = cum[:, -1]
        s_new = jnp.exp(total)[..., None, None] * s + jnp.einsum(
            'bkgn,bkgh,bkghp->bghpn', bc, dtc * jnp.exp(total[:, None] - cum), xc)
        if not with_output:
            return s_new, None
        seg = cum[:, :, None] - cum[:, None]
        decay = jnp.exp(jnp.where(causal[None, :, :, None, None], seg, -jnp.inf))
        cb = jnp.einsum('bqgn,bkgn->bqkg', cc, bc)
        y = (jnp.einsum('bqkg,bqkgh,bkgh,bkghp->bqghp', cb, decay, dtc, xc)
             + jnp.einsum('bqgn,bghpn,bqgh->bqghp', cc, s, jnp.exp(cum)))
        return s_new, y

    s_fin, ys = lax.scan(step, s0.reshape(bsz, n_groups, hpg, hd, d_state), xs)
    s_fin = s_fin.reshape(bsz, n_heads, hd, d_state)
    if not with_output:
        return None, s_fin
    return from_chunks(ys).reshape(bsz, length, n_heads, hd), s_fin


def mlstm_scan(q, k, v, ig, lf, state0, with_output):
    f32 = jnp.float32
    xs = (to_chunks(q.astype(f32)), to_chunks(k.astype(f32)), to_chunks(v.astype(f32)), to_chunks(ig), to_chunks(lf))
    causal = jnp.tril(jnp.ones((CHUNK, CHUNK), dtype=bool))

    def step(carry, inp):
        c, n, m = carry
        qc, kc, vc, igc, lfc = inp
        b = jnp.cumsum(lfc, axis=1)
        b_end = b[:, -1]
        g = b_end[:, None] - b + igc
        m_new = jnp.maximum(b_end + m, g.max(axis=1))
        w = jnp.exp(g - m_new[:, None])
        carry_decay = jnp.exp(b_end + m - m_new)
        c_new = carry_decay[..., None, None] * c + jnp.einsum('bkh,bkhv,bkhd->bhvd', w, vc, kc)
        n_new = carry_decay[..., None] * n + jnp.einsum('bkh,bkhd->bhd', w, kc)
        if not with_output:
            return (c_new, n_new, m_new), None
        log_d = jnp.where(causal[None, :, :, None], b[:, :, None] - b[:, None] + igc[:, None], -jnp.inf)
        inter = b + m[:, None]
        m_q = jnp.maximum(log_d.max(axis=2), inter)
        s = jnp.einsum('bqhd,bkhd->bqkh', qc, kc) * jnp.exp(log_d - m_q[:, :, None])
        w_state = jnp.exp(inter - m_q)
        num = jnp.einsum('bqkh,bkhv->bqhv', s, vc) + w_state[..., None] * jnp.einsum('bqhd,bhvd->bqhv', qc, c)
        den = s.sum(axis=2) + w_state * jnp.einsum('bqhd,bhd->bqh', qc, n)
        h = num / jnp.maximum(jnp.abs(den), jnp.exp(-m_q))[..., None]
        return (c_new, n_new, m_new), h

    fin, hs = lax.scan(step, state0, xs)
    if not with_output:
        return None, fin
    return from_chunks(hs), fin


def zero_states(bsz):
    f32 = jnp.float32
    return [(jnp.zeros((bsz, SSD_HEADS, SSD_HEAD_DIM, SSD_STATE), f32),
             (jnp.zeros((bsz, MLSTM_HEADS, MLSTM_DV, MLSTM_DK), f32),
              jnp.zeros((bsz, MLSTM_HEADS, MLSTM_DK), f32),
              jnp.zeros((bsz, MLSTM_HEADS), f32))) for _ in range(2)]


def prepare_mixer_inputs(u, lp, n_seg):
    bsz, length = u.shape[:2]
    f32 = jnp.float32
    proj = u @ lp['w_in']
    split_idx = np.cumsum(IN_PROJ_SIZES)[:-1].tolist()
    z, xbc, dt_raw, qk, v, gates_raw, o_raw, merge_raw = jnp.split(proj, split_idx, axis=-1)
    xbc = jax.nn.silu(dwconv_centred(xbc, lp['conv_xbc_w'], lp['conv_xbc_b'], n_seg))
    xs, bm, cm = jnp.split(xbc, [D_SSD, D_SSD + SSD_GROUPS * SSD_STATE], axis=-1)
    dt = jax.nn.softplus(dt_raw.astype(f32).reshape(bsz, length, 2, SSD_HEADS) + lp['ssd_dt_bias'].astype(f32))
    qk = jax.nn.silu(dwconv_centred(qk, lp['conv_qk_w'], lp['conv_qk_b'], n_seg))
    q, k = jnp.split(qk, 2, axis=-1)
    gates = gates_raw.astype(f32).reshape(bsz, length, 2, 2, MLSTM_HEADS)
    return {
        'z': z,
        'xh': xs.reshape(bsz, length, SSD_HEADS, SSD_HEAD_DIM),
        'bm': bm.reshape(bsz, length, SSD_GROUPS, SSD_STATE),
        'cm': cm.reshape(bsz, length, SSD_GROUPS, SSD_STATE),
        'dt': dt,
        'q': q.reshape(bsz, length, MLSTM_HEADS, MLSTM_DK) * (MLSTM_DK ** -0.5),
        'k': k.reshape(bsz, length, MLSTM_HEADS, MLSTM_DK),
        'v': v.reshape(bsz, length, MLSTM_HEADS, MLSTM_DV),
        'ig': gates[:, :, :, 0] + lp['mlstm_i_bias'].astype(f32),
        'lf': jax.nn.log_sigmoid(gates[:, :, :, 1] + lp['mlstm_f_bias'].astype(f32)),
        'o': jax.nn.sigmoid(o_raw),
        'merge': merge_raw,
    }


def maybe_flip(t, rev):
    return t[:, ::-1] if rev else t


def bidirectional_scans(p, lp, init, with_output):
    a_neg = -jnp.exp(lp['ssd_a_log'].astype(jnp.float32))
    ys_ssd, ys_m, finals = [], [], []
    for d in range(2):
        rev = d == 1
        s0, m0 = init[d]
        y_s, s_fin = ssd_scan(maybe_flip(p['xh'], rev), maybe_flip(p['dt'][:, :, d], rev), a_neg[d],
                              maybe_flip(p['bm'], rev), maybe_flip(p['cm'], rev), s0, with_output)
        h_m, m_fin = mlstm_scan(maybe_flip(p['q'], rev), maybe_flip(p['k'], rev), maybe_flip(p['v'], rev),
                                maybe_flip(p['ig'][:, :, d], rev), maybe_flip(p['lf'][:, :, d], rev), m0, with_output)
        finals.append((s_fin, m_fin))
        if with_output:
            ys_ssd.append(maybe_flip(y_s, rev))
            ys_m.append(maybe_flip(h_m, rev))
    if not with_output:
        return None, None, finals
    return ys_ssd[0] + ys_ssd[1], ys_m[0] + ys_m[1], finals


def merge_branches(p, y_ssd, h_m, lp):
    bsz, length = p['z'].shape[:2]
    dt_in = p['z'].dtype
    y = (y_ssd + lp['ssd_d'].astype(jnp.float32)[:, None] * p['xh'].astype(jnp.float32)).reshape(bsz, length, D_SSD)
    y = rmsnorm(y * jax.nn.silu(p['z'].astype(jnp.float32)), lp['ssd_norm_g']).astype(dt_in)
    h = rmsnorm(h_m, lp['mlstm_norm_g'].reshape(MLSTM_HEADS, MLSTM_DV)).reshape(bsz, length, D_MLSTM).astype(dt_in)
    h = p['o'] * h
    g_ssd, g_m = jnp.split(jax.nn.sigmoid(p['merge']), 2, axis=-1)
    merged = g_ssd * (y @ lp['w_ssd_out']) + g_m * (h @ lp['w_mlstm_out'])
    return merged @ lp['w_out']


def moe_ffn(u, lp):
    bsz, length, dm = u.shape
    t = u.reshape(-1, dm)
    n_tok = t.shape[0]
    scores = jax.nn.sigmoid((t @ lp['router_w']).astype(jnp.float32))
    grouped = (scores + lp['router_bias'].astype(jnp.float32)).reshape(n_tok, N_EXPERT_GROUPS, -1)
    group_score = lax.top_k(grouped, 2)[0].sum(-1)
    _, top_groups = lax.top_k(group_score, TOPK_GROUPS)
    group_mask = jax.nn.one_hot(top_groups, N_EXPERT_GROUPS, dtype=jnp.float32).sum(1) > 0
    sel = jnp.where(group_mask[:, :, None], grouped, -jnp.inf).reshape(n_tok, N_EXPERTS)
    _, top_e = lax.top_k(sel, TOP_K)
    wts = jnp.take_along_axis(scores, top_e, axis=1)
    wts = ROUTED_SCALE * wts / wts.sum(-1, keepdims=True)
    n_assign = n_tok * TOP_K
    flat_e = top_e.reshape(-1)
    order = jnp.argsort(flat_e)
    e_sorted = flat_e[order]
    tok_sorted = (order // TOP_K).astype(jnp.int32)
    w_sorted = wts.reshape(-1)[order]
    counts = jnp.bincount(flat_e, length=N_EXPERTS)
    padded = (counts + MOE_BLOCK - 1) // MOE_BLOCK * MOE_BLOCK
    pad_end = jnp.cumsum(padded)
    pad_start = pad_end - padded
    start = jnp.cumsum(counts) - counts
    dest = pad_start[e_sorted] + jnp.arange(n_assign) - start[e_sorted]
    n_blocks = -(-n_assign // MOE_BLOCK) + N_EXPERTS
    slot_tok = jnp.full((n_blocks * MOE_BLOCK,), n_tok, jnp.int32).at[dest].set(tok_sorted)
    slot_w = jnp.zeros((n_blocks * MOE_BLOCK,), u.dtype).at[dest].set(w_sorted.astype(u.dtype))
    block_expert = jnp.minimum(jnp.searchsorted(pad_end, jnp.arange(n_blocks) * MOE_BLOCK, side='right'), N_EXPERTS - 1)
    t_pad = jnp.concatenate([t, jnp.zeros((1, dm), t.dtype)], axis=0)
    w_gate, w_up, w_down = lp['moe_w_gate'], lp['moe_w_up'], lp['moe_w_down']

    def block(acc, inp):
        tok, wt, e = inp
        xb = t_pad[tok]
        hb = jax.nn.silu(xb @ w_gate[e]) * (xb @ w_up[e])
        return acc.at[tok].add((hb @ w_down[e]) * wt[:, None]), None

    acc, _ = lax.scan(block, jnp.zeros_like(t_pad),
                      (slot_tok.reshape(n_blocks, MOE_BLOCK), slot_w.reshape(n_blocks, MOE_BLOCK), block_expert))
    shared = (jax.nn.silu(t @ lp['shared_w_gate']) * (t @ lp['shared_w_up'])) @ lp['shared_w_down']
    return (acc[:n_tok] + shared).reshape(bsz, length, dm)


def setup_inputs(seed: int = 0) -> dict:
    key = jax.random.key(seed)
    ks = jax.random.split(key, 40)
    nrm = lambda k, shape, s: jax.random.normal(k, shape, jnp.float32) * s
    dt0 = jnp.exp(jax.random.uniform(ks[10], (DEPTH, 2, SSD_HEADS)) * (math.log(0.1) - math.log(0.001)) + math.log(0.001))
    return {
        'x': nrm(ks[0], (BATCH, SEQ, D_MODEL), 1.0),
        'c': nrm(ks[1], (BATCH, D_MODEL), 1.0),
        'ctx': nrm(ks[2], (BATCH, CTX_LEN, D_MODEL), 1.0),
        'c_ctx': nrm(ks[3], (D_MODEL,), 1.0),
        'ada_w': nrm(ks[4], (DEPTH, D_MODEL, 6 * D_MODEL), 0.3 * D_MODEL ** -0.5),
        'ada_b': nrm(ks[5], (DEPTH, 6 * D_MODEL), 0.02),
        'norm_mix_g': 1.0 + nrm(ks[6], (DEPTH, D_MODEL), 0.02),
        'norm_ffn_g': 1.0 + nrm(ks[7], (DEPTH, D_MODEL), 0.02),
        'w_in': nrm(ks[8], (DEPTH, D_MODEL, D_IN_PROJ), D_MODEL ** -0.5),
        'conv_xbc_w': nrm(ks[9], (DEPTH, CONV_K, D_XBC), CONV_K ** -0.5),
        'conv_xbc_b': nrm(ks[11], (DEPTH, D_XBC), 0.02),
        'ssd_dt_bias': dt0 + jnp.log(-jnp.expm1(-dt0)),
        'ssd_a_log': jnp.log(jax.random.uniform(ks[12], (DEPTH, 2, SSD_HEADS), jnp.float32, 1.0, 16.0)),
        'ssd_d': 1.0 + nrm(ks[13], (DEPTH, SSD_HEADS), 0.1),
        'ssd_norm_g': 1.0 + nrm(ks[14], (DEPTH, D_SSD), 0.02),
        'conv_qk_w': nrm(ks[15], (DEPTH, CONV_K, D_QK), CONV_K ** -0.5),
        'conv_qk_b': nrm(ks[16], (DEPTH, D_QK), 0.02),
        'mlstm_i_bias': -1.0 + nrm(ks[17], (DEPTH, 2, MLSTM_HEADS), 0.1),
        'mlstm_f_bias': jnp.linspace(3.0, 6.0, MLSTM_HEADS) + nrm(ks[18], (DEPTH, 2, MLSTM_HEADS), 0.1),
        'mlstm_norm_g': 1.0 + nrm(ks[19], (DEPTH, D_MLSTM), 0.02),
        'w_ssd_out': nrm(ks[20], (DEPTH, D_SSD, D_MODEL), D_SSD ** -0.5),
        'w_mlstm_out': nrm(ks[21], (DEPTH, D_MLSTM, D_MODEL), D_MLSTM ** -0.5),
        'w_out': nrm(ks[22], (DEPTH, D_MODEL, D_MODEL), D_MODEL ** -0.5),
        'router_w': nrm(ks[23], (DEPTH, D_MODEL, N_EXPERTS), D_MODEL ** -0.5),
        'router_bias': nrm(ks[24], (DEPTH, N_EXPERTS), 0.01),
        'moe_w_gate': nrm(ks[25], (DEPTH, N_EXPERTS, D_MODEL, D_EXPERT), D_MODEL ** -0.5),
        'moe_w_up': nrm(ks[26], (DEPTH, N_EXPERTS, D_MODEL, D_EXPERT), D_MODEL ** -0.5),
        'moe_w_down': nrm(ks[27], (DEPTH, N_EXPERTS, D_EXPERT, D_MODEL), D_EXPERT ** -0.5),
        'shared_w_gate': nrm(ks[28], (DEPTH, D_MODEL, D_SHARED), D_MODEL ** -0.5),
        'shared_w_up': nrm(ks[29], (DEPTH, D_MODEL, D_SHARED), D_MODEL ** -0.5),
        'shared_w_down': nrm(ks[30], (DEPTH, D_SHARED, D_MODEL), D_SHARED ** -0.5),
        'norm_final_g': 1.0 + nrm(ks[31], (D_MODEL,), 0.02),
    }


def reference(x, c, ctx, c_ctx, ada_w, ada_b, norm_mix_g, norm_ffn_g, w_in, conv_xbc_w, conv_xbc_b, ssd_dt_bias,
              ssd_a_log, ssd_d, ssd_norm_g, conv_qk_w, conv_qk_b, mlstm_i_bias, mlstm_f_bias, mlstm_norm_g,
              w_ssd_out, w_mlstm_out, w_out, router_w, router_bias, moe_w_gate, moe_w_up, moe_w_down,
              shared_w_gate, shared_w_up, shared_w_down, norm_final_g):
    rows = x.shape[1] // GRID_W
    cond_lat = jax.nn.silu(c)
    cond_ctx = jax.nn.silu(c_ctx)[None]
    for l in range(DEPTH):
        last = l == DEPTH - 1
        lp = {
            'w_in': w_in[l], 'conv_xbc_w': conv_xbc_w[l], 'conv_xbc_b': conv_xbc_b[l],
            'ssd_dt_bias': ssd_dt_bias[l], 'ssd_a_log': ssd_a_log[l], 'ssd_d': ssd_d[l], 'ssd_norm_g': ssd_norm_g[l],
            'conv_qk_w': conv_qk_w[l], 'conv_qk_b': conv_qk_b[l], 'mlstm_i_bias': mlstm_i_bias[l],
            'mlstm_f_bias': mlstm_f_bias[l], 'mlstm_norm_g': mlstm_norm_g[l],
            'w_ssd_out': w_ssd_out[l], 'w_mlstm_out': w_mlstm_out[l], 'w_out': w_out[l],
            'router_w': router_w[l], 'router_bias': router_bias[l],
            'moe_w_gate': moe_w_gate[l], 'moe_w_up': moe_w_up[l], 'moe_w_down': moe_w_down[l],
            'shared_w_gate': shared_w_gate[l], 'shared_w_up': shared_w_up[l], 'shared_w_down': shared_w_down[l],
        }
        mod_lat = jnp.split((cond_lat @ ada_w[l] + ada_b[l])[:, None], 6, axis=-1)
        mod_ctx = jnp.split((cond_ctx @ ada_w[l] + ada_b[l])[:, None], 6, axis=-1)
        p_ctx = prepare_mixer_inputs(modulate(ctx, norm_mix_g[l], mod_ctx[0], mod_ctx[1]), lp, 1)
        y_ssd_c, h_m_c, ctx_states = bidirectional_scans(p_ctx, lp, zero_states(ctx.shape[0]), not last)
        p_lat = prepare_mixer_inputs(modulate(x, norm_mix_g[l], mod_lat[0], mod_lat[1]), lp, rows)
        y_ssd, h_m, _ = bidirectional_scans(p_lat, lp, ctx_states, True)
        x = x + mod_lat[2] * merge_branches(p_lat, y_ssd, h_m, lp)
        x = x + mod_lat[5] * moe_ffn(modulate(x, norm_ffn_g[l], mod_lat[3], mod_lat[4]), lp)
        if not last:
            ctx = ctx + mod_ctx[2] * merge_branches(p_ctx, y_ssd_c, h_m_c, lp)
            ctx = ctx + mod_ctx[5] * moe_ffn(modulate(ctx, norm_ffn_g[l], mod_ctx[3], mod_ctx[4]), lp)
    return rmsnorm(x, norm_final_g)
```

```python
from contextlib import ExitStack
import os
import numpy as np
import concourse.bass as bass
import concourse.mybir as mybir
from concourse.bass_utils import run_bass_kernel_spmd

F32 = mybir.dt.float32
BF16 = mybir.dt.bfloat16
I32 = mybir.dt.int32
ALU = mybir.AluOpType
AF = mybir.ActivationFunctionType
AX = mybir.AxisListType

NCORE = 8
D = 1024
SEG = 4096
NSEGC = 32
CTXL = 256
EPS = 1e-6
BIG = 30000.0
NEXP = 256
CAP = 512
WS = 3648
O_XBC, O_DT, O_QK, O_V, O_GT = 0, 1536, 1568, 2592, 3616


class Buf:
    __slots__ = ("t", "w", "r", "name", "off", "excl")

    def __init__(self, t, name, off=0, excl=False):
        self.t = t
        self.name = name
        self.off = off
        self.excl = excl
        self.w = {}
        self.r = {}

    def __getitem__(self, idx):
        return self.t[idx]


class Ctx:
    ENG = ("pe", "dve", "act", "pool", "sp")
    SAME_WIN = 6

    def __init__(self, nc, es, n_dma_sems=48):
        self.nc = nc
        self.es_stack = [es]
        self.e = {"pe": nc.tensor, "dve": nc.vector, "act": nc.scalar, "pool": nc.gpsimd, "sp": nc.sync}
        self.sem = {k: es.enter_context(nc.semaphore("s_" + k)) for k in self.ENG}
        self.cnt = {k: 0 for k in self.ENG}
        self.seen = {k: {} for k in self.ENG}
        self.dsem = [es.enter_context(nc.semaphore("d%d" % i)) for i in range(n_dma_sems)]
        self.dcnt = [0] * n_dma_sems
        self.dnext = 0
        self.nid = 0
        self.n_inst = 0

    @property
    def es(self):
        return self.es_stack[-1]

    def sb(self, shape, dt=F32, name=None):
        self.nid += 1
        name = "%s_%d" % (name or "sb", self.nid)
        return Buf(self.es.enter_context(self.nc.sbuf_tensor(name, list(shape), dt)), name)

    def ps(self, shape, dt=F32, name=None):
        self.nid += 1
        name = "%s_%d" % (name or "ps", self.nid)
        return Buf(self.es.enter_context(self.nc.psum_tensor(name, list(shape), dt)), name, excl=True)

    def dram(self, shape, dt=F32, name=None):
        self.nid += 1
        name = name or ("dr_%d" % self.nid)
        return Buf(self.nc.dram_tensor(name, list(shape), dt, kind="Internal"), name)

    def _wait(self, eng, key, val):
        if self.seen[eng].get(key, 0) >= val:
            return
        if isinstance(key, int):
            self.e[eng].wait_ge(self.dsem[key], val)
        else:
            if key == eng and (eng == "pe" or val <= self.cnt[eng] - self.SAME_WIN):
                return
            self.e[eng].wait_ge(self.sem[key], val)
        self.seen[eng][key] = val

    def _deps(self, eng, R, W):
        for b in R:
            for k, v in b.w.items():
                self._wait(eng, k, v)
            if b.excl:
                for k, v in b.r.items():
                    if k != eng:
                        self._wait(eng, k, v)
        for b in W:
            for k, v in b.w.items():
                self._wait(eng, k, v)
            for k, v in b.r.items():
                self._wait(eng, k, v)

    def op(self, eng, fn, R=(), W=()):
        self._deps(eng, R, W)
        inst = fn(self.e[eng])
        self.cnt[eng] += 1
        idx = self.cnt[eng]
        inst.then_inc(self.sem[eng], 1)
        self.n_inst += 1
        for b in R:
            b.r[eng] = idx
        for b in W:
            b.w[eng] = idx
        return inst

    def dma(self, fn, R=(), W=(), q="sp"):
        k = self.dnext
        self.dnext = (self.dnext + 1) % len(self.dsem)
        if self.dcnt[k] > 0:
            self._wait(q, k, self.dcnt[k])
        self._deps(q, R, W)
        inst = fn(self.e[q])
        self.dcnt[k] += 16
        inst.then_inc(self.dsem[k], 16)
        self.n_inst += 1
        for b in R:
            b.r[k] = self.dcnt[k]
        for b in W:
            b.w[k] = self.dcnt[k]
        return inst

    def barrier(self):
        for eng in self.ENG:
            for k in self.ENG:
                if k != eng and self.cnt[k] > 0:
                    self._wait(eng, k, self.cnt[k])
            for k in range(len(self.dsem)):
                if self.dcnt[k] > 0:
                    self._wait(eng, k, self.dcnt[k])

    def finish(self, bufs, eng="sp"):
        for b in bufs:
            for k, v in b.w.items():
                self._wait(eng, k, v)


class Scope:
    def __init__(self, C):
        self.C = C

    def __enter__(self):
        self.es = ExitStack()
        self.es.__enter__()
        self.C.es_stack.append(self.es)
        return self

    def __exit__(self, *a):
        self.C.barrier()
        self.C.es_stack.pop()
        return self.es.__exit__(*a)


def bc(buf, col0, n, rep):
    t = buf.t
    Fsz = int(np.prod(t.shape[1:]))
    return bass.AP(t, col0, [[Fsz, t.shape[0]], [1, n], [0, rep]])


def bcp(buf, p0, npart, col0, n, rep):
    t = buf.t
    Fsz = int(np.prod(t.shape[1:]))
    return bass.AP(t, p0 * Fsz + col0, [[Fsz, npart], [1, n], [0, rep]])


def colbc(buf, col, rep):
    t = buf.t
    Fsz = int(np.prod(t.shape[1:]))
    return bass.AP(t, col, [[Fsz, t.shape[0]], [0, rep]])


class Prog:
    def __init__(self, dbg=None, stop_after=None, addD=True):
        self.addD = addD
        self.dbg = dbg or []
        self.stop_after = stop_after
        self.nc = bass.Bass("TRN2", target_bir_lowering=False)
        self.ins = {}
        self.outs = {}

    def inp(self, name, shape, dt=F32):
        b = Buf(self.nc.dram_tensor(name, list(shape), dt, kind="ExternalInput"), name)
        self.ins[name] = b
        return b

    def outp(self, name, shape, dt=F32):
        b = Buf(self.nc.dram_tensor(name, list(shape), dt, kind="ExternalOutput"), name)
        self.outs[name] = b
        return b

    def mm(self, ob, oap, lhsT, rhs, R, start=True, stop=True):
        self.C.op("pe", lambda e: e.matmul(oap, lhsT=lhsT, rhs=rhs, start=start, stop=stop), R=R, W=[ob])

    def tr(self, ob, oap, in_ap, ident_ap, R):
        self.C.op("pe", lambda e: e.transpose(out=oap, in_=in_ap, identity=ident_ap), R=R, W=[ob])

    def gbank(self):
        self._g = (self._g + 1) % len(self.G)
        return self.G[self._g]

    def tbank(self):
        self._t = (self._t + 1) % len(self.T)
        return self.T[self._t]

    def dslot(self):
        self._d = (self._d + 1) % len(self.DS)
        return self.DS[self._d]

    def dump(self, name, buf, ap, shape, dt=F32):
        if name in self.dbg:
            o = self.outp("dbg_" + name, shape, dt)
            self.C.dma(lambda e: e.dma_start(out=o.t.ap() if len(shape) == 0 else o.t[tuple(slice(None) for _ in shape)], in_=ap), R=[buf], W=[o])

    def build(self):
        nc = self.nc
        I = self.inp
        xo = I("xo", [SEG, D]); xpre = I("xpre", [3 * SEG, D]); ctx2 = I("ctx2", [2 * CTXL, D])
        cvec = I("cvec", [128, 16]); ada_w = I("ada_w", [D, 6 * D]); ada_b = I("ada_b", [128, 48])
        gcols = I("gcols", [128, 16])
        wlite = I("wlite", [5 * D, WS]); w_in = I("w_in", [D, 7744])
        cwx = I("cwx", [6 * 128, 60]); cbx = I("cbx", [6 * 128, 12]); cwq = I("cwq", [6 * 128, 40]); cbq = I("cbq", [6 * 128, 8])
        small = I("small", [7 * 128, 48]); flags = I("flags", [128, 8]); ssd_d = I("ssd_d", [128, 16])
        if self.stop_after is None or self.stop_after in ("merge", "route"):
            vecs = I("vecs", [3 * 128, D])
            w_so = I("w_ssd_out", [D, D]); w_mo = I("w_mlstm_out", [D, D]); w_o = I("w_out", [D, D])
            router_w = I("router_w", [D, NEXP]); router_b = I("router_b", [128, NEXP])
            sh_g = I("shared_w_gate", [D, 256]); sh_u = I("shared_w_up", [D, 256]); sh_d = I("shared_w_down", [256, D])
        if self.stop_after is None:
            moe_g = I("moe_w_gate", [NEXP * D, 256]); moe_u = I("moe_w_up", [NEXP * D, 256]); moe_d = I("moe_w_down", [NEXP * 256, D])
        out = self.outp("out", [SEG, D])

        with ExitStack() as es:
            C = self.C = Ctx(nc, es)
            self.bnd = {}
            for nm, val in (("L", NEXP * CAP - 1), ("u2", SEG + 127), ("Y", SEG * 8 - 1)):
                r = es.enter_context(nc.gpsimd.register("bnd_" + nm))
                nc.gpsimd.reg_mov(r, val)
                self.bnd[nm] = r
            self.build_consts()
            self.G = [C.ps([128, 512], F32, "G%d" % i) for i in range(2)]
            self.T = [C.ps([128, 1024], BF16, "T%d" % i) for i in range(2)]
            self.PY = C.ps([128, 512], F32, "PY"); self.PYI = C.ps([128, 512], F32, "PYI")
            self.DS = [C.ps([128, 512], F32, "DS%d" % i) for i in range(2)]
            self._g = self._t = self._d = 0
            self.y_d = [C.dram([SEG, D], F32, "y_d%d" % d) for d in range(2)]
            self.h_d = [C.dram([SEG, D], F32, "h_d%d" % d) for d in range(2)]
            later = self.stop_after is None or self.stop_after in ("merge", "route")
            self.uT_d = C.dram([8 * 128, 4096], BF16, "uT_d") if later else None
            self.x1_d = C.dram([SEG, D], F32, "x1_d")
            self.sh_d = C.dram([SEG, D], F32, "sh_d")
            self.u2_d = C.dram([SEG + 128, D], BF16, "u2_d")
            self.L_d = C.dram([NEXP * CAP, 4], F32, "L_d")
            self.Y_d = C.dram([SEG * 8, D], F32, "Y_d") if self.stop_after is None else None

            self.adaln(cvec, ada_w, ada_b, gcols)
            if self.stop_after == "adaln":
                return self.end()
            self.mixer_scans(xo, xpre, ctx2, wlite, w_in, cwx, cbx, cwq, cbq, small, flags, ssd_d)
            if self.stop_after == "ctx":
                return self.end()
            if self.stop_after == "scans":
                for d in range(2):
                    if "y_d" in self.dbg:
                        self.copy_dram(self.y_d[d], self.outp("dbg_y_d%d" % d, [SEG, D]))
                        self.copy_dram(self.h_d[d], self.outp("dbg_h_d%d" % d, [SEG, D]))
                return self.end()
            self.merge_pass(xo, w_in, vecs, w_so, w_mo, w_o)
            if self.stop_after == "merge":
                self.copy_dram(self.x1_d, self.outp("dbg_x1", [SEG, D]))
                return self.end()
            self.ffn_prep(vecs, router_w, router_b, sh_g, sh_u, sh_d)
            if self.stop_after == "route":
                self.copy_dram(self.x1_d, self.outp("dbg_x1", [SEG, D]))
                self.copy_dram(self.sh_d, self.outp("dbg_sh", [SEG, D]))
                self.copy_dram(self.L_d, self.outp("dbg_L", [NEXP * CAP, 4]), rows=0, flat=(128, NEXP * CAP * 4 // 128))
                return self.end()
            self.experts(moe_g, moe_u, moe_d)
            self.final(vecs, out)
            return self.end()

    def copy_dram(self, src, dst, rows=SEG, flat=None):
        C = self.C
        if flat is not None:
            with Scope(C):
                p, f = flat
                t = C.sb([p, f], F32, "cpf")
                C.dma(lambda e: e.dma_start(out=t[:, :], in_=src.t.ap().rearrange("(p a) n -> p (a n)", p=p)), R=[src], W=[t])
                C.dma(lambda e: e.dma_start(out=dst.t.ap().rearrange("(p a) n -> p (a n)", p=p), in_=t[:, :]), R=[t], W=[dst])
            return
        with Scope(C):
            tl = [C.sb([128, 4, D], F32, "cp") for _ in range(2)]
            for i in range(rows // 512):
                t = tl[i % 2]
                C.dma(lambda e: e.dma_start(out=t[:, :, :], in_=src.t[i * 512:(i + 1) * 512, :].rearrange("(a p) n -> p a n", p=128)), R=[src], W=[t])
                C.dma(lambda e: e.dma_start(out=dst.t[i * 512:(i + 1) * 512, :].rearrange("(a p) n -> p a n", p=128), in_=t[:, :, :]), R=[t], W=[dst])

    def end(self):
        self.C.finish(list(self.outs.values()))
        self.C.barrier()
        return self.nc

    def build_consts(self):
        C = self.C

        def tri(name, cm, pat, op, val=1.0, fill=0.0):
            t = C.sb([128, 128], F32, name)
            C.op("pool", lambda e: e.memset(t[:, :], val), W=[t])
            C.op("pool", lambda e: e.affine_select(out=t[:, :], in_=t[:, :], pattern=[[pat, 128]], compare_op=op, fill=fill, base=0, channel_multiplier=cm), R=[t], W=[t])
            return t

        self.identf = tri("identf", 1, -1, ALU.is_equal)
        self.Uf = tri("Uf", -1, 1, ALU.is_ge)
        self.Lf = tri("Lf", 1, -1, ALU.is_ge)
        self.SUf = tri("SUf", -1, 1, ALU.is_gt)
        self.NEGU = tri("NEGU", 1, -1, ALU.is_gt, val=-BIG)
        self.NEGL = tri("NEGL", -1, 1, ALU.is_gt, val=-BIG)
        self.onesf = C.sb([128, 128], F32, "onesf")
        C.op("pool", lambda e: e.memset(self.onesf[:, :], 1.0), W=[self.onesf])
        self.epsb = C.sb([128, 1], F32, "epsb")
        C.op("pool", lambda e: e.memset(self.epsb[:, :], EPS), W=[self.epsb])

        def tobf(src, name):
            t = C.sb([128, 128], BF16, name)
            C.op("dve", lambda e: e.tensor_copy(out=t[:, :], in_=src[:, :]), R=[src], W=[t])
            return t
        self.identb = tobf(self.identf, "identb")
        self.Ub = tobf(self.Uf, "Ub"); self.Lb = tobf(self.Lf, "Lb"); self.SUb = tobf(self.SUf, "SUb")
        self.onesb = tobf(self.onesf, "onesb")

    def col_to_bcast(self, colbuf, c0, dst):
        C = self.C
        for half in range(2):
            pb = self.gbank()
            for jj in range(4):
                j = half * 4 + jj
                self.mm(pb, pb[:, jj * 128:(jj + 1) * 128], colbc(colbuf, c0 + j, 128), self.identf[:, :], R=[colbuf, self.identf])
            C.op("act", lambda e: e.activation(out=dst[:, half * 512:(half + 1) * 512], in_=pb[:, 0:512], func=AF.Copy), R=[pb], W=[dst])

    def adaln(self, cvec, ada_w, ada_b, gcols):
        C = self.C
        self.mod = C.sb([128, 96], F32, "mod")
        self.gc = C.sb([128, 16], F32, "gc")
        C.dma(lambda e: e.dma_start(out=self.gc[:, :], in_=gcols.t[:, :]), R=[gcols], W=[self.gc])
        with Scope(C):
            cv = C.sb([128, 16], F32, "cv"); sg = C.sb([128, 16], F32, "sg"); sc = C.sb([128, 16], F32, "sc")
            ab = C.sb([128, 48], F32, "ab")
            C.dma(lambda e: e.dma_start(out=cv[:, :], in_=cvec.t[:, :]), R=[cvec], W=[cv])
            C.dma(lambda e: e.dma_start(out=ab[:, :], in_=ada_b.t[:, :]), R=[ada_b], W=[ab])
            C.op("act", lambda e: e.activation(out=sg[:, :], in_=cv[:, :], func=AF.Sigmoid), R=[cv], W=[sg])
            C.op("dve", lambda e: e.tensor_tensor(out=sc[:, :], in0=cv[:, :], in1=sg[:, :], op=ALU.mult), R=[cv, sg], W=[sc])
            wt = [C.sb([128, 6 * D], F32, "adaw%d" % i) for i in range(2)]
            macc = C.sb([128, 96], F32, "macc")
            C.op("dve", lambda e: e.tensor_copy(out=macc[:, :].rearrange("p (j w) -> p j w", w=2), in_=bc(ab, 0, 48, 2)), R=[ab], W=[macc])
            for k in range(8):
                w = wt[k % 2]
                pm = self.gbank()
                for hh in range(2):
                    C.dma(lambda e: e.dma_start(out=w[:, hh * 3072:(hh + 1) * 3072], in_=ada_w.t[k * 128:(k + 1) * 128, hh * 3072:(hh + 1) * 3072]), R=[ada_w], W=[w])
                for j in range(48):
                    self.mm(pm, pm[:, 2 * j:2 * j + 2], w[:, j * 128:(j + 1) * 128], sc[:, 2 * k:2 * k + 2], R=[w, sc], start=True, stop=True)
                C.op("dve", lambda e: e.tensor_tensor(out=macc[:, :], in0=macc[:, :], in1=pm[:, 0:96], op=ALU.add), R=[pm, macc], W=[macc])
            C.op("dve", lambda e: e.tensor_copy(out=self.mod[:, :], in_=macc[:, :]), R=[macc], W=[self.mod])
        if "mod" in self.dbg:
            self.dump_now("mod", self.mod, [128, 96])
        mod3 = self.mod[:, :].rearrange("p (j w) -> p j w", w=2)
        self.cols = C.sb([128, 48], F32, "cols")
        cols = self.cols

        def gs(dst0, scale_j0, which, g0):
            C.op("dve", lambda e: e.scalar_tensor_tensor(out=cols[:, dst0:dst0 + 8], in0=mod3[:, scale_j0:scale_j0 + 8, which], scalar=1.0, in1=self.gc[:, g0:g0 + 8], op0=ALU.add, op1=ALU.mult), R=[self.mod, self.gc], W=[cols])

        def cp(dst0, j0, which):
            C.op("dve", lambda e: e.tensor_copy(out=cols[:, dst0:dst0 + 8], in_=mod3[:, j0:j0 + 8, which]), R=[self.mod], W=[cols])
        gs(0, 8, 0, 0); cp(8, 0, 0); gs(16, 8, 1, 0); cp(24, 0, 1); gs(32, 32, 0, 8); cp(40, 24, 0)
        self.gcol = C.sb([128, 16], F32, "gcol")
        C.op("dve", lambda e: e.tensor_copy(out=self.gcol[:, 0:8], in_=mod3[:, 16:24, 0]), R=[self.mod], W=[self.gcol])
        C.op("dve", lambda e: e.tensor_copy(out=self.gcol[:, 8:16], in_=mod3[:, 40:48, 0]), R=[self.mod], W=[self.gcol])

    def mixer_scans(self, xo, xpre, ctx2, wlite, w_in, cwx, cbx, cwq, cbq, small, flags, ssd_d):
        C = self.C
        with Scope(C):
            K = self.K = type("K", (), {})()
            K.GS = C.sb([128, D], F32, "GS"); K.SH = C.sb([128, D], F32, "SH")
            K.W = C.sb([128, 8, WS], BF16, "Wscan")
            K.cwx = C.sb([128, 60], F32, "cwx"); K.cbx = C.sb([128, 12], F32, "cbx"); K.cwq = C.sb([128, 40], F32, "cwq"); K.cbq = C.sb([128, 8], F32, "cbq")
            K.small = C.sb([128, 48], F32, "small"); K.aneg = C.sb([128, 16], F32, "aneg")
            K.flags = C.sb([128, 8], F32, "flags"); K.Dh = C.sb([128, 16], F32, "Dh")
            C.dma(lambda e: e.dma_start(out=K.flags[:, :], in_=flags.t[:, :]), R=[flags], W=[K.flags])
            C.dma(lambda e: e.dma_start(out=K.Dh[:, :], in_=ssd_d.t[:, :]), R=[ssd_d], W=[K.Dh])
            K.xt = [C.sb([128, D], F32, "xt%d" % i) for i in range(2)]
            K.junk = C.sb([128, D], BF16, "junk"); K.xm = C.sb([128, D], F32, "xm"); K.xn = C.sb([128, D], BF16, "xn")
            K.ss = C.sb([128, 1], F32, "ss"); K.rstd = C.sb([128, 1], F32, "rstd")
            K.uT = C.sb([128, 8, 512], BF16, "uT")
            K.cv = C.sb([128, 12, 512], BF16, "cv"); K.cvq = C.sb([128, 8, 512], BF16, "cvq")
            K.acc = [C.sb([128, 512], F32, "acc%d" % i) for i in range(4)]
            K.raw = [C.sb([128, 512], F32, "raw%d" % i) for i in range(4)]
            K.nconv = 0
            K.Vtok = [C.sb([128, D], BF16, "Vtok%d" % i) for i in range(4)]
            K.SM = [C.sb([128, 48], F32, "SM%d" % i) for i in range(4)]
            K.sm = C.sb([128, 32], F32, "sm"); K.e1 = C.sb([128, 32], F32, "e1")
            K.S = [C.sb([128, 512], F32, "S%d" % g) for g in range(2)]
            K.Sbf = [C.sb([128, 512], BF16, "Sbf%d" % g) for g in range(2)]
            K.Cm = C.sb([128, 4, 128], F32, "Cm"); K.nm = C.sb([128, 4], F32, "nm"); K.mbc = C.sb([128, 8], F32, "mbc")
            K.Cbf = C.sb([128, 4, 128], BF16, "Cbf"); K.nbf = C.sb([128, 4], BF16, "nbf")
            K.sav = []
            for d in range(2):
                K.sav.append(dict(S=[C.sb([128, 512], F32, "SS%d%d" % (d, g)) for g in range(2)], Cm=C.sb([128, 512], F32, "SCm%d" % d),
                                  nm=C.sb([128, 4], F32, "Snm%d" % d), mbc=C.sb([128, 8], F32, "Smbc%d" % d)))
            K.cs = C.sb([128, 48], F32, "cs"); K.Xtok = C.sb([128, D], BF16, "Xtok"); K.BK = C.sb([128, 768], BF16, "BK")
            K.t16 = C.sb([128, 16], F32, "t16"); K.wS = C.sb([128, 16], F32, "wS"); K.expcum = C.sb([128, 16], F32, "expcum"); K.ncum = C.sb([128, 16], F32, "ncum")
            K.exptot = C.sb([128, 16], F32, "exptot")
            K.Xw = C.sb([128, D], BF16, "Xw"); K.Xdt = C.sb([128, D], BF16, "Xdt"); K.XD = C.sb([128, D], BF16, "XD")
            K.CBT = [C.sb([128, 128], BF16, "CBT%d" % g) for g in range(2)]
            K.E = [C.sb([128, 128], BF16, "E%d" % i) for i in range(2)]; K.M = [C.sb([128, 128], BF16, "M%d" % i) for i in range(2)]
            K.yout = [C.sb([128, D], F32, "yout%d" % i) for i in range(2)]; K.hout = [C.sb([128, D], F32, "hout%d" % i) for i in range(2)]
            K.a8 = C.sb([128, 8], F32, "a8"); K.amax = C.sb([8, 1], F32, "amax"); K.dg = C.sb([8, 8], F32, "dg")
            K.Mc = C.sb([128, 8], F32, "Mc"); K.cd = C.sb([128, 8], F32, "cd"); K.w8 = C.sb([128, 8], F32, "w8"); K.w8b = C.sb([128, 8], BF16, "w8b")
            K.t8 = C.sb([128, 8], F32, "t8"); K.fl = C.sb([128, 8], F32, "fl"); K.den = C.sb([128, 8], F32, "den"); K.rc = C.sb([128, 8], F32, "rc")
            K.Vw = C.sb([128, 8, 128], BF16, "Vw"); K.A = [C.sb([128, 128], BF16, "A%d" % h) for h in range(8)]
            K.nchunk = 0

            def P(**kw):
                return type("P", (), kw)()
            passes = [
                P(name="ctxF", src=ctx2, row0=0, n_sc=1, sc_tok=256, rows=256, wsrc=(wlite, 0), slot=0, sset=0, kind="ctx", full=False, rev=False, dd=0, init="zero", save=0, flag=None),
                P(name="ctxB", src=ctx2, row0=CTXL, n_sc=1, sc_tok=256, rows=256, wsrc=(wlite, 1), slot=1, sset=1, kind="ctx", full=False, rev=False, dd=0, init="zero", save=1, flag=None),
            ]
            for j in range(3):
                passes.append(P(name="pre%d" % j, src=xpre, row0=j * SEG, n_sc=8, sc_tok=512, rows=64, wsrc=(wlite, 2 + j), slot=2 + j, sset=2 + j, kind="lat", full=False, rev=False, dd=0, init="blend", save="blend", flag=j))
            passes.append(P(name="ownF", src=xo, row0=0, n_sc=8, sc_tok=512, rows=64, wsrc=(w_in, None), slot=5, sset=5, kind="lat", full=True, rev=False, dd=0, init=0, save=None, flag=None))
            passes.append(P(name="ownB", src=xo, row0=0, n_sc=8, sc_tok=512, rows=64, wsrc=(w_in, None), slot=5, sset=6, kind="lat", full=True, rev=True, dd=1, init=1, save=None, flag=None))
            if self.stop_after == "ctx":
                passes = passes[:2]
            cur_kind = None
            for Pp in passes:
                if Pp.kind != cur_kind:
                    cur_kind = Pp.kind
                    o = 0 if Pp.kind == "lat" else 16
                    self.col_to_bcast(self.cols, o, K.GS)
                    self.col_to_bcast(self.cols, o + 8, K.SH)
                self.run_pass(Pp, cwx, cbx, cwq, cbq, small)
            if "ctxstate" in self.dbg:
                for d in range(2):
                    for g in range(2):
                        self.dump_now("S%d%d" % (d, g), K.sav[d]["S"][g], [128, 512])
                    self.dump_now("Cm%d" % d, K.sav[d]["Cm"], [128, 512])
                    self.dump_now("mbc%d" % d, K.sav[d]["mbc"], [128, 8])
                    self.dump_now("nm%d" % d, K.sav[d]["nm"], [128, 4])

    def dump_now(self, name, buf, shape):
        o = self.outp("dbg_" + name, shape)
        self.C.dma(lambda e: e.dma_start(out=o.t[:, :], in_=buf[:, :]), R=[buf], W=[o])

    def run_pass(self, P, cwx, cbx, cwq, cbq, small):
        C = self.C; K = self.K
        wsrc, wi = P.wsrc
        for k in range(8):
            if wi is None:
                src_ap = wsrc.t[k * 128:(k + 1) * 128, 1024:1024 + WS]
            else:
                src_ap = wsrc.t[wi * D + k * 128: wi * D + (k + 1) * 128, :]
            C.dma(lambda e: e.dma_start(out=K.W[:, k, :], in_=src_ap), R=[wsrc], W=[K.W], q="pool")
        s = P.slot
        C.dma(lambda e: e.dma_start(out=K.cwx[:, :], in_=cwx.t[s * 128:(s + 1) * 128, :]), R=[cwx], W=[K.cwx])
        C.dma(lambda e: e.dma_start(out=K.cbx[:, :], in_=cbx.t[s * 128:(s + 1) * 128, :]), R=[cbx], W=[K.cbx])
        C.dma(lambda e: e.dma_start(out=K.cwq[:, :], in_=cwq.t[s * 128:(s + 1) * 128, :]), R=[cwq], W=[K.cwq])
        C.dma(lambda e: e.dma_start(out=K.cbq[:, :], in_=cbq.t[s * 128:(s + 1) * 128, :]), R=[cbq], W=[K.cbq])
        C.dma(lambda e: e.dma_start(out=K.small[:, :], in_=small.t[P.sset * 128:(P.sset + 1) * 128, :]), R=[small], W=[K.small])
        C.op("act", lambda e: e.activation(out=K.aneg[:, :], in_=K.small[:, 16:32], func=AF.Exp), R=[K.small], W=[K.aneg])
        C.op("dve", lambda e: e.tensor_scalar(out=K.aneg[:, :], in0=K.aneg[:, :], scalar1=-1.0, scalar2=None, op0=ALU.mult), R=[K.aneg], W=[K.aneg])
        st = [(K.S[0], lambda d: K.sav[d]["S"][0], 128), (K.S[1], lambda d: K.sav[d]["S"][1], 128), (K.Cm, lambda d: K.sav[d]["Cm"], 128),
              (K.nm, lambda d: K.sav[d]["nm"], 128), (K.mbc, lambda d: K.sav[d]["mbc"], 128)]

        def flat(b):
            return b[:, :, :].rearrange("p a b -> p (a b)") if len(b.t.shape) == 3 else b[:, :]
        if P.init == "zero":
            for cur, _, _ in st:
                C.op("pool", lambda e: e.memset(flat(cur), 0.0), W=[cur])
        elif P.init == "blend":
            f = K.flags[:, P.flag:P.flag + 1]; nf = K.flags[:, 4 + P.flag:5 + P.flag]
            for cur, sv, _ in st:
                a = sv(0); b = sv(1)
                C.op("dve", lambda e: e.tensor_scalar(out=flat(cur), in0=flat(a), scalar1=f, scalar2=None, op0=ALU.mult), R=[a, K.flags], W=[cur])
                C.op("dve", lambda e: e.scalar_tensor_tensor(out=flat(cur), in0=flat(b), scalar=nf, in1=flat(cur), op0=ALU.mult, op1=ALU.add), R=[b, K.flags, cur], W=[cur])
        else:
            for cur, sv, _ in st:
                a = sv(P.init)
                C.op("dve", lambda e: e.tensor_copy(out=flat(cur), in_=flat(a)), R=[a], W=[cur])
        for g in range(2):
            C.op("act", lambda e: e.activation(out=K.Sbf[g][:, :], in_=K.S[g][:, :], func=AF.Copy), R=[K.S[g]], W=[K.Sbf[g]])
        scs = list(range(P.n_sc))
        if P.rev:
            scs = scs[::-1]
        for sc in scs:
            self.prep_sc(P, sc)
            tiles = list(range(P.sc_tok // 128))
            if P.rev:
                tiles = tiles[::-1]
            for i in tiles:
                self.scan_chunk(P, sc, i)
        if P.save == "blend":
            f = K.flags[:, P.flag:P.flag + 1]; nf = K.flags[:, 4 + P.flag:5 + P.flag]
            for cur, sv, _ in st:
                a = sv(0); b = sv(1)
                C.op("dve", lambda e: e.tensor_scalar(out=flat(a), in0=flat(a), scalar1=nf, scalar2=None, op0=ALU.mult), R=[a, K.flags], W=[a])
                C.op("dve", lambda e: e.scalar_tensor_tensor(out=flat(a), in0=flat(cur), scalar=f, in1=flat(a), op0=ALU.mult, op1=ALU.add), R=[cur, K.flags, a], W=[a])
                C.op("dve", lambda e: e.tensor_scalar(out=flat(b), in0=flat(b), scalar1=f, scalar2=None, op0=ALU.mult), R=[b, K.flags], W=[b])
                C.op("dve", lambda e: e.scalar_tensor_tensor(out=flat(b), in0=flat(cur), scalar=nf, in1=flat(b), op0=ALU.mult, op1=ALU.add), R=[cur, K.flags, b], W=[b])
        elif P.save is not None:
            for cur, sv, _ in st:
                a = sv(P.save)
                C.op("dve", lambda e: e.tensor_copy(out=flat(a), in_=flat(cur)), R=[cur], W=[a])

    def prep_sc(self, P, sc):
        C = self.C; K = self.K
        T = P.sc_tok
        nt = T // 128
        for i in range(nt):
            xt = K.xt[i % 2]
            r0 = P.row0 + sc * T + i * 128
            C.dma(lambda e: e.dma_start(out=xt[:, :], in_=P.src.t[r0:r0 + 128, :]), R=[P.src], W=[xt])
            C.op("pool", lambda e: e.memset(K.ss[:, :], 0.0), W=[K.ss])
            C.op("act", lambda e: e.activation(out=K.junk[:, :], in_=xt[:, :], func=AF.Square, accum_out=K.ss[:, :]), R=[xt, K.ss], W=[K.junk, K.ss])
            C.op("act", lambda e: e.activation(out=K.rstd[:, :], in_=K.ss[:, :], func=AF.Sqrt, scale=1.0 / D, bias=self.epsb[:, :]), R=[K.ss, self.epsb], W=[K.rstd])
            C.op("dve", lambda e: e.reciprocal(out=K.rstd[:, :], in_=K.rstd[:, :]), R=[K.rstd], W=[K.rstd])
            C.op("dve", lambda e: e.scalar_tensor_tensor(out=K.xm[:, :], in0=xt[:, :], scalar=K.rstd[:, 0:1], in1=K.GS[:, :], op0=ALU.mult, op1=ALU.mult), R=[xt, K.rstd, K.GS], W=[K.xm])
            C.op("pool", lambda e: e.tensor_tensor(out=K.xn[:, :], in0=K.xm[:, :], in1=K.SH[:, :], op=ALU.add), R=[K.xm, K.SH], W=[K.xn])
            pt = self.tbank()
            for k in range(8):
                self.tr(pt, pt[:, k * 128:(k + 1) * 128], K.xn[:, k * 128:(k + 1) * 128], self.identb[:, :], R=[K.xn, self.identb])
            C.op("act", lambda e: e.activation(out=K.uT[:, :, i * 128:(i + 1) * 128], in_=pt[:, :].rearrange("p (k n) -> p k n", k=8), func=AF.Copy), R=[pt], W=[K.uT])
        if P.name == "ownB" and self.uT_d is not None:
            C.dma(lambda e: e.dma_start(out=self.uT_d.t[sc * 128:(sc + 1) * 128, :], in_=K.uT[:, :, :].rearrange("p k n -> p (k n)")), R=[K.uT], W=[self.uT_d])
        if "uT" in self.dbg and P.name == "ownF" and sc == 0:
            self.dump_bf("uT", K.uT, K.uT[:, :, :].rearrange("p k n -> p (k n)"), [128, 4096])
        xt_list = list(range(12)) if P.full else list(range(10))
        qt_list = list(range(8)) if P.full else list(range(4, 8))
        jobs = [("x", ct) for ct in xt_list] + [("q", ct) for ct in qt_list]
        for j0 in range(0, len(jobs), 2):
            ctxs = []
            for (kind, ct) in jobs[j0:j0 + 2]:
                off = (O_XBC if kind == "x" else O_QK) + ct * 128
                cw, cb, dst = (K.cwx, K.cbx, K.cv) if kind == "x" else (K.cwq, K.cbq, K.cvq)
                pb = self.gbank()
                for k in range(8):
                    self.mm(pb, pb[:, 0:T], K.W[:, k, off:off + 128], K.uT[:, k, 0:T], R=[K.W, K.uT], start=(k == 0), stop=(k == 7))
                K.nconv += 1
                raw = K.raw[K.nconv % 4]; acc = K.acc[K.nconv % 4]
                C.op("act", lambda e: e.activation(out=raw[:, 0:T], in_=pb[:, 0:T], func=AF.Copy), R=[pb], W=[raw])
                ctxs.append((ct, cw, cb, dst, raw, acc))
            for (ct, cw, cb, dst, raw, acc) in ctxs:
                C.op("dve", lambda e: e.tensor_scalar(out=acc[:, 0:T], in0=raw[:, 0:T], scalar1=cw[:, ct * 5 + 2:ct * 5 + 3], scalar2=cb[:, ct:ct + 1], op0=ALU.mult, op1=ALU.add), R=[raw, cw, cb], W=[acc])
            for j in (0, 1, 3, 4):
                o = j - 2
                lo = max(0, -o); hi = P.rows - max(0, o)
                for (ct, cw, cb, dst, raw, acc) in ctxs:
                    a3 = acc[:, 0:T].rearrange("p (r t) -> p r t", t=P.rows); p3 = raw[:, 0:T].rearrange("p (r t) -> p r t", t=P.rows)
                    C.op("dve", lambda e: e.scalar_tensor_tensor(out=a3[:, :, lo:hi], in0=p3[:, :, lo + o:hi + o], scalar=cw[:, ct * 5 + j:ct * 5 + j + 1], in1=a3[:, :, lo:hi], op0=ALU.mult, op1=ALU.add), R=[raw, cw, acc], W=[acc])
            for (ct, cw, cb, dst, raw, acc) in ctxs:
                C.op("act", lambda e: e.activation(out=dst[:, ct, 0:T], in_=acc[:, 0:T], func=AF.Silu), R=[acc], W=[dst])
        for i in range(nt):
            for n in range(2):
                pb = self.gbank()
                for k in range(8):
                    self.mm(pb, pb[:, 0:512], K.uT[:, k, i * 128:(i + 1) * 128], K.W[:, k, O_V + n * 512:O_V + (n + 1) * 512], R=[K.W, K.uT], start=(k == 0), stop=(k == 7))
                C.op("act", lambda e: e.activation(out=K.Vtok[i][:, n * 512:(n + 1) * 512], in_=pb[:, 0:512], func=AF.Copy), R=[pb], W=[K.Vtok[i]])
            pb = self.gbank()
            dto = O_DT + 16 * P.dd; gto = O_GT + 16 * P.dd
            for k in range(8):
                self.mm(pb, pb[:, 0:16], K.uT[:, k, i * 128:(i + 1) * 128], K.W[:, k, dto:dto + 16], R=[K.W, K.uT], start=(k == 0), stop=(k == 7))
            for k in range(8):
                self.mm(pb, pb[:, 16:32], K.uT[:, k, i * 128:(i + 1) * 128], K.W[:, k, gto:gto + 16], R=[K.W, K.uT], start=(k == 0), stop=(k == 7))
            SM = K.SM[i]
            C.op("dve", lambda e: e.tensor_tensor(out=K.sm[:, 0:16], in0=pb[:, 0:16], in1=K.small[:, 0:16], op=ALU.add), R=[pb, K.small], W=[K.sm])
            C.op("dve", lambda e: e.tensor_tensor(out=K.sm[:, 16:32], in0=pb[:, 16:32], in1=K.small[:, 32:48], op=ALU.add), R=[pb, K.small], W=[K.sm])
            C.op("act", lambda e: e.activation(out=K.e1[:, 0:16], in_=K.sm[:, 0:16], func=AF.Exp), R=[K.sm], W=[K.e1])
            C.op("act", lambda e: e.activation(out=K.e1[:, 16:24], in_=K.sm[:, 24:32], func=AF.Exp, scale=-1.0), R=[K.sm], W=[K.e1])
            C.op("act", lambda e: e.activation(out=SM[:, 24:40], in_=K.e1[:, 0:16], func=AF.Ln, bias=1.0, scale=1.0), R=[K.e1], W=[SM])
            C.op("act", lambda e: e.activation(out=K.e1[:, 24:32], in_=K.e1[:, 16:24], func=AF.Ln, bias=1.0, scale=1.0), R=[K.e1], W=[K.e1])
            C.op("dve", lambda e: e.tensor_scalar(out=SM[:, 16:24], in0=K.e1[:, 24:32], scalar1=-1.0, scalar2=None, op0=ALU.mult), R=[K.e1], W=[SM])
            C.op("dve", lambda e: e.tensor_tensor(out=SM[:, 0:16], in0=SM[:, 24:40], in1=K.aneg[:, :], op=ALU.mult), R=[SM, K.aneg], W=[SM])
            C.op("dve", lambda e: e.tensor_copy(out=SM[:, 40:48], in_=K.sm[:, 16:24]), R=[K.sm], W=[SM])
            if "SM" in self.dbg and P.name == "ownF" and sc == 0 and i == 0:
                self.dump_now("SM", SM, [128, 48])
        if "cv" in self.dbg and P.name == "ownF" and sc == 0:
            self.dump_bf("cv", K.cv, K.cv[:, :, :].rearrange("p k n -> p (k n)"), [128, 12 * 512])
            self.dump_bf("cvq", K.cvq, K.cvq[:, :, :].rearrange("p k n -> p (k n)"), [128, 8 * 512])

    def dump_bf(self, name, buf, ap, shape):
        C = self.C
        o = self.outp("dbg_" + name, shape)
        n = shape[1]
        for c0 in range(0, n, 2048):
            c1 = min(n, c0 + 2048)
            t = C.sb([128, 2048], F32, "dmp")
            C.op("dve", lambda e: e.tensor_copy(out=t[:, 0:c1 - c0], in_=ap[:, c0:c1]), R=[buf], W=[t])
            C.dma(lambda e: e.dma_start(out=o.t[:, c0:c1], in_=t[:, 0:c1 - c0]), R=[t], W=[o])

    def scan_chunk(self, P, sc, i):
        C = self.C; K = self.K
        SKIP = os.environ.get('KSKIP', '').split(',')
        if 'scan' in SKIP:
            return
        tok = slice(i * 128, (i + 1) * 128)
        Uf = self.Lf if P.rev else self.Uf
        Ub = self.Lb if P.rev else self.Ub
        NEGM = self.NEGL if P.rev else self.NEGU
        SM = K.SM[i]
        full = P.full
        K.nchunk += 1
        par = K.nchunk % 2
        sl = self.dslot()
        o = sl.off
        self.mm(sl, sl.t[:, o:o + 24], Uf[:, :], SM[:, 0:24], R=[Uf, SM])
        self.mm(sl, sl.t[:, o + 24:o + 48], self.onesf[:, :], SM[:, 0:24], R=[self.onesf, SM])
        C.op("dve", lambda e: e.tensor_copy(out=K.cs[:, :], in_=sl.t[:, o:o + 48]), R=[sl], W=[K.cs])
        pt = self.tbank()
        for c in range(8):
            self.tr(pt, pt[:, c * 128:(c + 1) * 128], K.cv[:, c, tok], self.identb[:, :], R=[K.cv, self.identb])
        C.op("act", lambda e: e.activation(out=K.Xtok[:, :], in_=pt[:, :], func=AF.Copy), R=[pt], W=[K.Xtok])
        pt = self.tbank()
        for c in range(2):
            self.tr(pt, pt[:, c * 128:(c + 1) * 128], K.cv[:, 8 + c, tok], self.identb[:, :], R=[K.cv, self.identb])
        for c in range(4):
            self.tr(pt, pt[:, 256 + c * 128:256 + (c + 1) * 128], K.cvq[:, 4 + c, tok], self.identb[:, :], R=[K.cvq, self.identb])
        C.op("dve", lambda e: e.tensor_copy(out=K.BK[:, :], in_=pt[:, 0:768]), R=[pt], W=[K.BK])
        C.op("dve", lambda e: e.tensor_tensor(out=K.t16[:, :], in0=K.cs[:, 24:40], in1=K.cs[:, 0:16], op=ALU.subtract), R=[K.cs], W=[K.t16])
        C.op("act", lambda e: e.activation(out=K.t16[:, :], in_=K.t16[:, :], func=AF.Exp), R=[K.t16], W=[K.t16])
        C.op("dve", lambda e: e.tensor_tensor(out=K.wS[:, :], in0=K.t16[:, :], in1=SM[:, 24:40], op=ALU.mult), R=[K.t16, SM], W=[K.wS])
        X3 = K.Xtok[:, :].rearrange("p (h d) -> p h d", h=16)
        C.op("dve", lambda e: e.tensor_tensor(out=K.Xw[:, :].rearrange("p (h d) -> p h d", h=16), in0=X3, in1=bc(K.wS, 0, 16, 64), op=ALU.mult), R=[K.Xtok, K.wS], W=[K.Xw])
        C.op("act", lambda e: e.activation(out=K.exptot[:, :], in_=K.cs[:, 24:40], func=AF.Exp), R=[K.cs], W=[K.exptot])
        if full:
            C.op("act", lambda e: e.activation(out=K.expcum[:, :], in_=K.cs[:, 0:16], func=AF.Exp), R=[K.cs], W=[K.expcum])
            C.op("dve", lambda e: e.tensor_scalar(out=K.ncum[:, :], in0=K.cs[:, 0:16], scalar1=-1.0, scalar2=None, op0=ALU.mult), R=[K.cs], W=[K.ncum])
            C.op("pool", lambda e: e.tensor_tensor(out=K.Xdt[:, :].rearrange("p (h d) -> p h d", h=16), in0=X3, in1=bc(SM, 24, 16, 64), op=ALU.mult), R=[K.Xtok, SM], W=[K.Xdt])
            if P.dd == 0 and self.addD:
                C.op("pool", lambda e: e.tensor_tensor(out=K.XD[:, :].rearrange("p (h d) -> p h d", h=16), in0=X3, in1=bc(K.Dh, 0, 16, 64), op=ALU.mult), R=[K.Xtok, K.Dh], W=[K.XD])
            yout = K.yout[par]
            for g in range(2):
                sl = self.dslot(); o = sl.off
                self.mm(sl, sl.t[:, o:o + 128], K.cv[:, 8 + g, tok], K.cv[:, 10 + g, tok], R=[K.cv])
                C.op("act", lambda e: e.activation(out=K.CBT[g][:, :], in_=sl.t[:, o:o + 128], func=AF.Copy), R=[sl], W=[K.CBT[g]])
                self.mm(self.PYI, self.PYI[:, :], K.cv[:, 10 + g, tok], K.Sbf[g][:, :], R=[K.cv, K.Sbf[g]])
                addD = (P.dd == 0) and self.addD
                if addD:
                    self.mm(self.PY, self.PY[:, :], self.identb[:, :], K.XD[:, g * 512:(g + 1) * 512], R=[self.identb, K.XD], start=True, stop=False)
                pend = None
                for hh in range(8):
                    h = g * 8 + hh
                    sl = self.dslot(); o = sl.off
                    self.mm(sl, sl.t[:, o:o + 128], colbc(SM, h, 128), Uf[:, :], R=[SM, Uf], start=True, stop=False)
                    self.mm(sl, sl.t[:, o:o + 128], self.identf[:, :], NEGM[:, :], R=[self.identf, NEGM], start=False, stop=True)
                    E = K.E[hh % 2]; M = K.M[hh % 2]
                    C.op("act", lambda e: e.activation(out=E[:, :], in_=sl.t[:, o:o + 128], func=AF.Exp, bias=K.ncum[:, h:h + 1], scale=1.0), R=[sl, K.ncum], W=[E])
                    C.op("dve", lambda e: e.tensor_tensor(out=M[:, :], in0=E[:, :], in1=K.CBT[g][:, :], op=ALU.mult), R=[E, K.CBT[g]], W=[M])
                    if pend is not None:
                        pM, phh, ph = pend
                        self.mm(self.PY, self.PY[:, phh * 64:(phh + 1) * 64], pM[:, :], K.Xdt[:, ph * 64:(ph + 1) * 64], R=[pM, K.Xdt], start=(not addD), stop=True)
                    pend = (M, hh, h)
                pM, phh, ph = pend
                self.mm(self.PY, self.PY[:, phh * 64:(phh + 1) * 64], pM[:, :], K.Xdt[:, ph * 64:(ph + 1) * 64], R=[pM, K.Xdt], start=(not addD), stop=True)
                yg = yout[:, g * 512:(g + 1) * 512]
                C.op("dve", lambda e: e.tensor_tensor(out=yg.rearrange("p (h d) -> p h d", h=8), in0=self.PYI[:, :].rearrange("p (h d) -> p h d", h=8), in1=bc(K.expcum, g * 8, 8, 64), op=ALU.mult), R=[self.PYI, K.expcum], W=[yout])
                C.op("dve", lambda e: e.tensor_tensor(out=yg, in0=yg, in1=self.PY[:, :], op=ALU.add), R=[self.PY, yout], W=[yout])
            r0 = sc * P.sc_tok + i * 128
            yd = self.y_d[P.dd]
            C.dma(lambda e: e.dma_start(out=yd.t[r0:r0 + 128, :], in_=yout[:, :]), R=[yout], W=[yd])
        for g in range(2):
            pb = self.gbank()
            self.mm(pb, pb[:, 0:512], K.BK[:, g * 128:(g + 1) * 128], K.Xw[:, g * 512:(g + 1) * 512], R=[K.BK, K.Xw])
            S3 = K.S[g][:, :].rearrange("p (h d) -> p h d", h=8)
            C.op("dve", lambda e: e.tensor_tensor(out=S3, in0=S3, in1=bc(K.exptot, g * 8, 8, 64), op=ALU.mult), R=[K.S[g], K.exptot], W=[K.S[g]])
            C.op("dve", lambda e: e.tensor_tensor(out=K.S[g][:, :], in0=K.S[g][:, :], in1=pb[:, 0:512], op=ALU.add), R=[K.S[g], pb], W=[K.S[g]])
            C.op("act", lambda e: e.activation(out=K.Sbf[g][:, :], in_=K.S[g][:, :], func=AF.Copy), R=[K.S[g]], W=[K.Sbf[g]])
        if 'mlstm' in SKIP:
            return
        C.op("dve", lambda e: e.tensor_tensor(out=K.a8[:, :], in0=SM[:, 40:48], in1=K.cs[:, 16:24], op=ALU.subtract), R=[SM, K.cs], W=[K.a8])
        sl = self.dslot(); o = sl.off
        self.tr(sl, sl.t[0:8, o:o + 128], K.a8[:, 0:8], self.identf[:, :], R=[K.a8, self.identf])
        C.op("dve", lambda e: e.reduce_max(out=K.amax[:, :], in_=sl.t[0:8, o:o + 128], axis=AX.X), R=[sl], W=[K.amax])
        C.op("dve", lambda e: e.tensor_scalar(out=K.dg[:, :], in0=self.identf[0:8, 0:8], scalar1=K.amax[:, 0:1], scalar2=None, op0=ALU.mult), R=[self.identf, K.amax], W=[K.dg])
        sl = self.dslot(); o = sl.off
        self.mm(sl, sl.t[:, o:o + 8], self.onesf[0:8, :], K.dg[:, :], R=[self.onesf, K.dg])
        C.op("dve", lambda e: e.tensor_tensor(out=K.Mc[:, :], in0=K.mbc[:, :], in1=sl.t[:, o:o + 8], op=ALU.max), R=[K.mbc, sl], W=[K.Mc])
        C.op("dve", lambda e: e.tensor_tensor(out=K.cd[:, :], in0=K.mbc[:, :], in1=K.Mc[:, :], op=ALU.subtract), R=[K.mbc, K.Mc], W=[K.cd])
        C.op("act", lambda e: e.activation(out=K.cd[:, :], in_=K.cd[:, :], func=AF.Exp), R=[K.cd], W=[K.cd])
        C.op("dve", lambda e: e.tensor_tensor(out=K.mbc[:, :], in0=K.cs[:, 40:48], in1=K.Mc[:, :], op=ALU.add), R=[K.cs, K.Mc], W=[K.mbc])
        C.op("dve", lambda e: e.tensor_tensor(out=K.w8[:, :], in0=K.a8[:, :], in1=K.Mc[:, :], op=ALU.subtract), R=[K.a8, K.Mc], W=[K.w8])
        C.op("act", lambda e: e.activation(out=K.w8[:, :], in_=K.w8[:, :], func=AF.Exp), R=[K.w8], W=[K.w8])
        C.op("dve", lambda e: e.tensor_copy(out=K.w8b[:, :], in_=K.w8[:, :]), R=[K.w8], W=[K.w8b])
        C.op("pool", lambda e: e.tensor_tensor(out=K.Vw[:, :, :], in0=K.Vtok[i][:, :].rearrange("p (h d) -> p h d", h=8), in1=bc(K.w8, 0, 8, 128), op=ALU.mult), R=[K.Vtok[i], K.w8], W=[K.Vw])
        for hh in range(2):
            ps_ = slice(hh * 64, (hh + 1) * 64)
            cdv = bass.AP(K.cd.t, hh * 64 * 8 + hh, [[8, 64], [2, 4], [0, 128]])
            C.op("dve", lambda e: e.tensor_tensor(out=K.Cm[ps_, :, :], in0=K.Cm[ps_, :, :], in1=cdv, op=ALU.mult), R=[K.Cm, K.cd], W=[K.Cm])
            cdn = bass.AP(K.cd.t, hh * 64 * 8 + hh, [[8, 64], [2, 4]])
            C.op("dve", lambda e: e.tensor_tensor(out=K.nm[ps_, :], in0=K.nm[ps_, :], in1=cdn, op=ALU.mult), R=[K.nm, K.cd], W=[K.nm])
        if full:
            C.op("act", lambda e: e.activation(out=K.Cbf[:, :, :], in_=K.Cm[:, :, :], func=AF.Copy, scale=0.125), R=[K.Cm], W=[K.Cbf])
            C.op("act", lambda e: e.activation(out=K.nbf[:, :], in_=K.nm[:, :], func=AF.Copy, scale=0.125), R=[K.nm], W=[K.nbf])
            C.op("dve", lambda e: e.tensor_tensor(out=K.t8[:, :], in0=K.cs[:, 16:24], in1=K.Mc[:, :], op=ALU.add), R=[K.cs, K.Mc], W=[K.t8])
            C.op("act", lambda e: e.activation(out=K.fl[:, :], in_=K.t8[:, :], func=AF.Exp, scale=-1.0), R=[K.t8], W=[K.fl])
            sden = self.PYI; od = 0
            for h in range(8):
                c = h // 2; pr = (h % 2) * 64
                KT = K.cvq[pr:pr + 64, 4 + c, tok]; QT = K.cvq[pr:pr + 64, c, tok]
                sl = self.dslot(); o = sl.off
                self.mm(sl, sl.t[:, o:o + 128], KT, QT, R=[K.cvq])
                A = K.A[h]
                C.op("dve", lambda e: e.scalar_tensor_tensor(out=A[:, :], in0=sl.t[:, o:o + 128], scalar=0.125, in1=Ub[:, :], op0=ALU.mult, op1=ALU.mult), R=[sl, Ub], W=[A])
                self.mm(sden, sden.t[:, od + h:od + h + 1], A[:, :], K.w8b[:, h:h + 1], R=[A, K.w8b], start=True, stop=False)
                self.mm(sden, sden.t[:, od + h:od + h + 1], QT, K.nbf[pr:pr + 64, c:c + 1], R=[K.cvq, K.nbf], start=False, stop=True)
            C.op("dve", lambda e: e.tensor_copy(out=K.den[:, :], in_=sden.t[:, od:od + 8]), R=[sden], W=[K.den])
            C.op("dve", lambda e: e.scalar_tensor_tensor(out=K.den[:, :], in0=K.den[:, :], scalar=-1.0, in1=K.den[:, :], op0=ALU.mult, op1=ALU.max), R=[K.den], W=[K.den])
            C.op("dve", lambda e: e.tensor_tensor(out=K.den[:, :], in0=K.den[:, :], in1=K.fl[:, :], op=ALU.max), R=[K.den, K.fl], W=[K.den])
            C.op("dve", lambda e: e.reciprocal(out=K.rc[:, :], in_=K.den[:, :]), R=[K.den], W=[K.rc])
            hout = K.hout[par]
            for h in range(8):
                c = h // 2; pr = (h % 2) * 64
                QT = K.cvq[pr:pr + 64, c, tok]
                sl = self.dslot(); o = sl.off
                self.mm(sl, sl.t[:, o:o + 128], K.A[h][:, :], K.Vw[:, h, :], R=[K.A[h], K.Vw], start=True, stop=False)
                self.mm(sl, sl.t[:, o:o + 128], QT, K.Cbf[pr:pr + 64, c, :], R=[K.cvq, K.Cbf], start=False, stop=True)
                C.op("act", lambda e: e.activation(out=hout[:, h * 128:(h + 1) * 128], in_=sl.t[:, o:o + 128], func=AF.Copy, scale=K.rc[:, h:h + 1]), R=[sl, K.rc], W=[hout])
            r0 = sc * P.sc_tok + i * 128
            hd = self.h_d[P.dd]
            C.dma(lambda e: e.dma_start(out=hd.t[r0:r0 + 128, :], in_=hout[:, :]), R=[hout], W=[hd])
        if 'mupd' in SKIP:
            return
        sn2 = self.PY; on = 0
        for h in range(8):
            c = h // 2; hh = h % 2; ps_ = slice(hh * 64, (hh + 1) * 64)
            Kp = K.BK[:, 256 + c * 128:256 + (c + 1) * 128]
            sl = self.dslot(); o = sl.off
            self.mm(sl, sl.t[:, o:o + 128], Kp, K.Vw[:, h, :], R=[K.BK, K.Vw])
            C.op("dve", lambda e: e.tensor_tensor(out=K.Cm[ps_, c, :], in0=K.Cm[ps_, c, :], in1=sl.t[ps_, o:o + 128], op=ALU.add), R=[K.Cm, sl], W=[K.Cm])
            self.mm(sn2, sn2.t[:, on + h:on + h + 1], Kp, K.w8b[:, h:h + 1], R=[K.BK, K.w8b])
        for hh in range(2):
            ps_ = slice(hh * 64, (hh + 1) * 64)
            src = bass.AP(sn2.t, hh * 64 * 512 + on + hh, [[512, 64], [2, 4]])
            C.op("dve", lambda e: e.tensor_tensor(out=K.nm[ps_, :], in0=K.nm[ps_, :], in1=src, op=ALU.add), R=[K.nm, sn2], W=[K.nm])


    def load_w_bf(self, dst, dram, row0, nk, c0, c1, dcol0=0):
        C = self.C
        for k in range(nk):
            C.dma(lambda e: e.dma_start(out=dst[:, k, dcol0:dcol0 + (c1 - c0)], in_=dram.t[row0 + k * 128:row0 + (k + 1) * 128, c0:c1]), R=[dram], W=[dst], q="pool")

    def abank(self):
        self._a = (self._a + 1) % len(self.Gall)
        return self.Gall[self._a]

    def rms_rstd(self, src_ap, srcbuf, junk, ss, rstd, n):
        C = self.C
        C.op("pool", lambda e: e.memset(ss[:, :], 0.0), W=[ss])
        C.op("act", lambda e: e.activation(out=junk[:, :], in_=src_ap, func=AF.Square, accum_out=ss[:, :]), R=[srcbuf, ss], W=[junk, ss])
        C.op("act", lambda e: e.activation(out=rstd[:, :], in_=ss[:, :], func=AF.Sqrt, scale=1.0 / n, bias=self.epsb[:, :]), R=[ss, self.epsb], W=[rstd])
        C.op("dve", lambda e: e.reciprocal(out=rstd[:, :], in_=rstd[:, :]), R=[rstd], W=[rstd])

    def proj(self, lhsT_buf, W, wc0, n512, evac):
        for n in range(n512):
            pb = self.abank()
            for k in range(8):
                self.mm(pb, pb[:, 0:512], lhsT_buf[:, k, :], W[:, k, wc0 + n * 512:wc0 + (n + 1) * 512], R=[lhsT_buf, W], start=(k == 0), stop=(k == 7))
            evac(n, pb)

    def transp8(self, src, dstT):
        C = self.C
        pt = self.tbank()
        for k in range(8):
            self.tr(pt, pt[:, k * 128:(k + 1) * 128], src[:, k * 128:(k + 1) * 128], self.identb[:, :], R=[src, self.identb])
        C.op("act", lambda e: e.activation(out=dstT[:, :, :], in_=pt[:, :].rearrange("p (k n) -> p k n", k=8), func=AF.Copy), R=[pt], W=[dstT])

    def merge_pass(self, xo, w_in, vecs, w_so, w_mo, w_o):
        C = self.C
        self.Gall = self.G + [self.PY, self.PYI] + self.DS
        self._a = 0
        with Scope(C):
            Wz = C.sb([128, 8, 4096], BF16, "Wz")
            self.load_w_bf(Wz, w_in, 0, 8, 0, 1024, 0)
            self.load_w_bf(Wz, w_in, 0, 8, 4672, 7744, 1024)
            Wso = C.sb([128, 8, D], BF16, "Wso"); Wmo = C.sb([128, 8, D], BF16, "Wmo"); Wo = C.sb([128, 8, D], BF16, "Wo")
            self.load_w_bf(Wso, w_so, 0, 8, 0, D); self.load_w_bf(Wmo, w_mo, 0, 8, 0, D); self.load_w_bf(Wo, w_o, 0, 8, 0, D)
            gssd = C.sb([128, D], F32, "gssd"); gml = C.sb([128, D], F32, "gml"); gate = C.sb([128, D], F32, "gate")
            C.dma(lambda e: e.dma_start(out=gssd[:, :], in_=vecs.t[0:128, :]), R=[vecs], W=[gssd])
            C.dma(lambda e: e.dma_start(out=gml[:, :], in_=vecs.t[128:256, :]), R=[vecs], W=[gml])
            self.col_to_bcast(self.gcol, 0, gate)
            xt = C.sb([128, D], F32, "m_xt"); uT = C.sb([128, 8, 128], BF16, "m_uT")
            A1 = C.sb([128, D], F32, "A1"); A2 = C.sb([128, D], F32, "A2"); A3 = C.sb([128, D], F32, "A3"); A4 = C.sb([128, D], F32, "A4")
            Z = C.sb([128, D], F32, "Z"); junk = C.sb([128, D], BF16, "m_junk")
            yn = C.sb([128, D], BF16, "yn"); hn = C.sb([128, D], BF16, "hn"); nT = C.sb([128, 8, 128], BF16, "nT")
            mg = C.sb([128, D], BF16, "mg"); x1 = C.sb([128, D], F32, "x1t")
            ss = C.sb([128, 1], F32, "m_ss"); rstd = C.sb([128, 1], F32, "m_rstd"); ss8 = C.sb([128, 8], F32, "ss8")
            for t in range(32):
                r0 = t * 128
                sc, i = t // 4, t % 4
                C.dma(lambda e: e.dma_start(out=xt[:, :], in_=xo.t[r0:r0 + 128, :]), R=[xo], W=[xt])
                C.dma(lambda e: e.dma_start(out=uT[:, :, :], in_=self.uT_d.t[sc * 128:(sc + 1) * 128, :].rearrange("p (k n) -> p k n", k=8)[:, :, i * 128:(i + 1) * 128]), R=[self.uT_d], W=[uT])
                C.dma(lambda e: e.dma_start(out=A1[:, :], in_=self.y_d[0].t[r0:r0 + 128, :]), R=[self.y_d[0]], W=[A1])
                C.dma(lambda e: e.dma_start(out=A2[:, :], in_=self.y_d[1].t[r0:r0 + 128, :]), R=[self.y_d[1]], W=[A2])
                C.dma(lambda e: e.dma_start(out=A3[:, :], in_=self.h_d[0].t[r0:r0 + 128, :]), R=[self.h_d[0]], W=[A3])
                C.dma(lambda e: e.dma_start(out=A4[:, :], in_=self.h_d[1].t[r0:r0 + 128, :]), R=[self.h_d[1]], W=[A4])
                self.proj(uT, Wz, 0, 2, lambda n, pb: C.op("act", lambda e: e.activation(out=Z[:, n * 512:(n + 1) * 512], in_=pb[:, 0:512], func=AF.Silu), R=[pb], W=[Z]))
                C.op("dve", lambda e: e.tensor_tensor(out=A1[:, :], in0=A1[:, :], in1=A2[:, :], op=ALU.add), R=[A1, A2], W=[A1])
                C.op("dve", lambda e: e.tensor_tensor(out=A1[:, :], in0=A1[:, :], in1=Z[:, :], op=ALU.mult), R=[A1, Z], W=[A1])
                self.rms_rstd(A1[:, :], A1, junk, ss, rstd, D)
                C.op("dve", lambda e: e.scalar_tensor_tensor(out=yn[:, :], in0=A1[:, :], scalar=rstd[:, 0:1], in1=gssd[:, :], op0=ALU.mult, op1=ALU.mult), R=[A1, rstd, gssd], W=[yn])
                self.proj(uT, Wz, 1024, 2, lambda n, pb: C.op("act", lambda e: e.activation(out=Z[:, n * 512:(n + 1) * 512], in_=pb[:, 0:512], func=AF.Sigmoid), R=[pb], W=[Z]))
                C.op("dve", lambda e: e.tensor_tensor(out=A3[:, :], in0=A3[:, :], in1=A4[:, :], op=ALU.add), R=[A3, A4], W=[A3])
                C.op("pool", lambda e: e.tensor_tensor(out=A4[:, :], in0=A3[:, :], in1=A3[:, :], op=ALU.mult), R=[A3], W=[A4])
                C.op("dve", lambda e: e.tensor_reduce(out=ss8[:, :], in_=A4[:, :].rearrange("p (h d) -> p h d", h=8), axis=AX.X, op=ALU.add), R=[A4], W=[ss8])
                C.op("act", lambda e: e.activation(out=ss8[:, :], in_=ss8[:, :], func=AF.Sqrt, scale=1.0 / 128, bias=self.epsb[:, :]), R=[ss8, self.epsb], W=[ss8])
                C.op("dve", lambda e: e.reciprocal(out=ss8[:, :], in_=ss8[:, :]), R=[ss8], W=[ss8])
                C.op("dve", lambda e: e.tensor_tensor(out=A3[:, :].rearrange("p (h d) -> p h d", h=8), in0=A3[:, :].rearrange("p (h d) -> p h d", h=8), in1=bc(ss8, 0, 8, 128), op=ALU.mult), R=[A3, ss8], W=[A3])
                C.op("pool", lambda e: e.tensor_tensor(out=A3[:, :], in0=A3[:, :], in1=gml[:, :], op=ALU.mult), R=[A3, gml], W=[A3])
                C.op("dve", lambda e: e.tensor_tensor(out=hn[:, :], in0=A3[:, :], in1=Z[:, :], op=ALU.mult), R=[A3, Z], W=[hn])
                self.proj(uT, Wz, 2048, 2, lambda n, pb: C.op("act", lambda e: e.activation(out=Z[:, n * 512:(n + 1) * 512], in_=pb[:, 0:512], func=AF.Sigmoid), R=[pb], W=[Z]))
                self.transp8(yn, nT)
                self.proj(nT, Wso, 0, 2, lambda n, pb: C.op("dve", lambda e: e.tensor_tensor(out=A1[:, n * 512:(n + 1) * 512], in0=pb[:, 0:512], in1=Z[:, n * 512:(n + 1) * 512], op=ALU.mult), R=[pb, Z], W=[A1]))
                self.proj(uT, Wz, 3072, 2, lambda n, pb: C.op("act", lambda e: e.activation(out=Z[:, n * 512:(n + 1) * 512], in_=pb[:, 0:512], func=AF.Sigmoid), R=[pb], W=[Z]))
                self.transp8(hn, nT)
                self.proj(nT, Wmo, 0, 2, lambda n, pb: C.op("dve", lambda e: e.tensor_tensor(out=A2[:, n * 512:(n + 1) * 512], in0=pb[:, 0:512], in1=Z[:, n * 512:(n + 1) * 512], op=ALU.mult), R=[pb, Z], W=[A2]))
                C.op("dve", lambda e: e.tensor_tensor(out=mg[:, :], in0=A1[:, :], in1=A2[:, :], op=ALU.add), R=[A1, A2], W=[mg])
                self.transp8(mg, nT)
                self.proj(nT, Wo, 0, 2, lambda n, pb: C.op("dve", lambda e: e.tensor_tensor(out=x1[:, n * 512:(n + 1) * 512], in0=pb[:, 0:512], in1=gate[:, n * 512:(n + 1) * 512], op=ALU.mult), R=[pb, gate], W=[x1]))
                C.op("pool", lambda e: e.tensor_tensor(out=x1[:, :], in0=x1[:, :], in1=xt[:, :], op=ALU.add), R=[x1, xt], W=[x1])
                C.dma(lambda e: e.dma_start(out=self.x1_d.t[r0:r0 + 128, :], in_=x1[:, :]), R=[x1], W=[self.x1_d])

    def ffn_prep(self, vecs, router_w, router_b, sh_g, sh_u, sh_d):
        C = self.C
        with Scope(C):
            if self.Y_d is not None:
                zt = C.sb([128, 2048], F32, "zt")
                C.op("pool", lambda e: e.memset(zt[:, :], 0.0), W=[zt])
                for i in range(SEG * 8 // 256):
                    C.dma(lambda e: e.dma_start(out=self.Y_d.t[i * 256:(i + 1) * 256, :].rearrange("(p a) n -> p (a n)", p=128), in_=zt[:, :]), R=[zt], W=[])
            GS2 = C.sb([128, D], F32, "GS2"); SH2 = C.sb([128, D], F32, "SH2")
            self.col_to_bcast(self.cols, 32, GS2); self.col_to_bcast(self.cols, 40, SH2)
            rw = C.sb([128, 8, NEXP], F32, "rw"); rb = C.sb([128, NEXP], F32, "rb")
            C.dma(lambda e: e.dma_start(out=rw[:, :, :], in_=router_w.t.ap().rearrange("(k p) n -> p k n", p=128)), R=[router_w], W=[rw])
            C.dma(lambda e: e.dma_start(out=rb[:, :], in_=router_b.t[:, :]), R=[router_b], W=[rb])
            Wsgu = C.sb([128, 8, 512], BF16, "Wsgu"); Wsd = C.sb([128, 2, D], BF16, "Wsd")
            self.load_w_bf(Wsgu, sh_g, 0, 8, 0, 256, 0); self.load_w_bf(Wsgu, sh_u, 0, 8, 0, 256, 256); self.load_w_bf(Wsd, sh_d, 0, 2, 0, D)
            iotaE = C.sb([128, NEXP], F32, "iotaE")
            C.op("pool", lambda e: e.iota(iotaE[:, :], pattern=[[1, NEXP]], base=0, channel_multiplier=0, allow_small_or_imprecise_dtypes=True), W=[iotaE])
            C.op("dve", lambda e: e.tensor_scalar(out=iotaE[:, :], in0=iotaE[:, :], scalar1=float(CAP), scalar2=None, op0=ALU.mult), R=[iotaE], W=[iotaE])
            cnt = C.sb([128, NEXP], F32, "cnt")
            C.op("pool", lambda e: e.memset(cnt[:, :], 0.0), W=[cnt])
            SUF = C.sb([128, 2, NEXP], BF16, "SUF")
            C.op("pool", lambda e: e.memset(SUF[:, :, :], 0.0), W=[SUF])
            C.op("dve", lambda e: e.tensor_copy(out=SUF[:, 0, 0:128], in_=self.SUb[:, :]), R=[self.SUb, SUF], W=[SUF])
            C.op("dve", lambda e: e.tensor_copy(out=SUF[:, 0, 128:256], in_=self.onesb[:, :]), R=[self.onesb, SUF], W=[SUF])
            C.op("dve", lambda e: e.tensor_copy(out=SUF[:, 1, 128:256], in_=self.SUb[:, :]), R=[self.SUb, SUF], W=[SUF])
            jv = C.sb([128, 8, NEXP], F32, "jv")
            C.op("pool", lambda e: e.iota(jv[:, :, :], pattern=[[1, 8], [0, NEXP]], base=0, channel_multiplier=0, allow_small_or_imprecise_dtypes=True), W=[jv])
            emT = C.sb([128, 2, 128], BF16, "emT"); jr = C.sb([128, NEXP], F32, "jr")
            Li = C.sb([128, NEXP * CAP // 128, 4], F32, "Li")
            C.op("pool", lambda e: e.memset(Li[:, :, :], 0.0), W=[Li])
            C.op("pool", lambda e: e.memset(Li[:, :, 0:1], float(SEG)), R=[Li], W=[Li])
            C.op("pool", lambda e: e.memset(Li[:, :, 1:2], 1.0e9), R=[Li], W=[Li])
            C.dma(lambda e: e.dma_start(out=self.L_d.t.ap().rearrange("(p a) n -> p a n", p=128), in_=Li[:, :, :]), R=[Li], W=[self.L_d])
            zb = C.sb([128, D], BF16, "zb")
            C.op("pool", lambda e: e.memset(zb[:, :], 0.0), W=[zb])
            C.dma(lambda e: e.dma_start(out=self.u2_d.t[SEG:SEG + 128, :], in_=zb[:, :]), R=[zb], W=[self.u2_d])
            x1 = C.sb([128, D], F32, "f_x1"); junk = C.sb([128, D], BF16, "f_junk"); ss = C.sb([128, 1], F32, "f_ss"); rstd = C.sb([128, 1], F32, "f_rstd")
            u2 = C.sb([128, D], F32, "u2"); u2b = C.sb([128, D], BF16, "u2b")
            u2Tf = C.sb([128, 8, 128], F32, "u2Tf"); u2Tb = C.sb([128, 8, 128], BF16, "u2Tb")
            sc_ = C.sb([128, NEXP], F32, "scores"); gr = C.sb([128, NEXP], F32, "grouped"); sel = C.sb([128, NEXP], F32, "sel")
            m8g = C.sb([128, 8, 8], F32, "m8g"); gs = C.sb([128, 8], F32, "gs"); g8 = C.sb([128, 8], F32, "g8"); pen = C.sb([128, 8], F32, "pen")
            e8 = C.sb([128, 8], F32, "e8"); em = C.sb([128, NEXP], F32, "em"); emb = C.sb([128, NEXP], BF16, "emb")
            Wc = C.sb([128, NEXP], F32, "Wc"); wsum = C.sb([128, 1], F32, "wsum")
            pos = C.sb([128, NEXP], F32, "pos"); dst = C.sb([128, NEXP], F32, "dst"); ov = C.sb([128, NEXP], F32, "ov")
            oh = C.sb([128, 8, NEXP], F32, "oh"); tmp = C.sb([128, 8, NEXP], F32, "ohtmp")
            dj = C.sb([128, 8], F32, "dj"); dji = C.sb([128, 8], I32, "dji"); rowd = C.sb([128, 8, 4], F32, "rowd")
            hs = C.sb([128, 2, 128], BF16, "hs"); sg = C.sb([128, 256], F32, "sgs"); sho = C.sb([128, D], F32, "sho")
            for t in range(32):
                r0 = t * 128
                C.dma(lambda e: e.dma_start(out=x1[:, :], in_=self.x1_d.t[r0:r0 + 128, :]), R=[self.x1_d], W=[x1])
                self.rms_rstd(x1[:, :], x1, junk, ss, rstd, D)
                C.op("dve", lambda e: e.scalar_tensor_tensor(out=u2[:, :], in0=x1[:, :], scalar=rstd[:, 0:1], in1=GS2[:, :], op0=ALU.mult, op1=ALU.mult), R=[x1, rstd, GS2], W=[u2])
                C.op("pool", lambda e: e.tensor_tensor(out=u2[:, :], in0=u2[:, :], in1=SH2[:, :], op=ALU.add), R=[u2, SH2], W=[u2])
                C.op("act", lambda e: e.activation(out=u2b[:, :], in_=u2[:, :], func=AF.Copy), R=[u2], W=[u2b])
                C.dma(lambda e: e.dma_start(out=self.u2_d.t[r0:r0 + 128, :], in_=u2b[:, :]), R=[u2b], W=[self.u2_d])
                for half in range(2):
                    pb = self.abank()
                    for kk in range(4):
                        k = half * 4 + kk
                        self.tr(pb, pb[:, kk * 128:(kk + 1) * 128], u2[:, k * 128:(k + 1) * 128], self.identf[:, :], R=[u2, self.identf])
                    C.op("act", lambda e: e.activation(out=u2Tf[:, half * 4:(half + 1) * 4, :], in_=pb[:, 0:512].rearrange("p (k n) -> p k n", k=4), func=AF.Copy), R=[pb], W=[u2Tf])
                C.op("dve", lambda e: e.tensor_copy(out=u2Tb[:, :, :], in_=u2Tf[:, :, :]), R=[u2Tf], W=[u2Tb])
                pb = self.abank()
                for k in range(8):
                    self.mm(pb, pb[:, 0:NEXP], u2Tf[:, k, :], rw[:, k, :], R=[u2Tf, rw], start=(k == 0), stop=(k == 7))
                C.op("act", lambda e: e.activation(out=sc_[:, :], in_=pb[:, 0:NEXP], func=AF.Sigmoid), R=[pb], W=[sc_])
                C.op("dve", lambda e: e.tensor_tensor(out=gr[:, :], in0=sc_[:, :], in1=rb[:, :], op=ALU.add), R=[sc_, rb], W=[gr])
                for g in range(8):
                    C.op("dve", lambda e: e.max(out=m8g[:, g, :], in_=gr[:, g * 32:(g + 1) * 32]), R=[gr], W=[m8g])
                C.op("dve", lambda e: e.tensor_tensor(out=gs[:, :], in0=m8g[:, :, 0], in1=m8g[:, :, 1], op=ALU.add), R=[m8g], W=[gs])
                C.op("dve", lambda e: e.max(out=g8[:, :], in_=gs[:, :]), R=[gs], W=[g8])
                C.op("dve", lambda e: e.tensor_scalar(out=pen[:, :], in0=gs[:, :], scalar1=g8[:, 3:4], scalar2=None, op0=ALU.is_ge), R=[gs, g8], W=[pen])
                C.op("dve", lambda e: e.tensor_scalar(out=pen[:, :], in0=pen[:, :], scalar1=-1.0, scalar2=1.0e4, op0=ALU.add, op1=ALU.mult), R=[pen], W=[pen])
                C.op("dve", lambda e: e.tensor_tensor(out=sel[:, :].rearrange("p (g e) -> p g e", g=8), in0=gr[:, :].rearrange("p (g e) -> p g e", g=8), in1=bc(pen, 0, 8, 32), op=ALU.add), R=[gr, pen], W=[sel])
                C.op("dve", lambda e: e.max(out=e8[:, :], in_=sel[:, :]), R=[sel], W=[e8])
                C.op("dve", lambda e: e.tensor_scalar(out=em[:, :], in0=sel[:, :], scalar1=e8[:, 7:8], scalar2=None, op0=ALU.is_ge), R=[sel, e8], W=[em])
                C.op("act", lambda e: e.activation(out=emb[:, :], in_=em[:, :], func=AF.Copy), R=[em], W=[emb])
                C.op("dve", lambda e: e.tensor_tensor(out=Wc[:, :], in0=sc_[:, :], in1=em[:, :], op=ALU.mult), R=[sc_, em], W=[Wc])
                C.op("dve", lambda e: e.reduce_sum(out=wsum[:, :], in_=Wc[:, :], axis=AX.X), R=[Wc], W=[wsum])
                C.op("dve", lambda e: e.reciprocal(out=wsum[:, :], in_=wsum[:, :]), R=[wsum], W=[wsum])
                C.op("dve", lambda e: e.tensor_scalar(out=Wc[:, :], in0=Wc[:, :], scalar1=wsum[:, 0:1], scalar2=2.5, op0=ALU.mult, op1=ALU.mult), R=[Wc, wsum], W=[Wc])
                pb = self.abank()
                self.mm(pb, pb[:, 0:NEXP], self.SUb[:, :], emb[:, :], R=[self.SUb, emb])
                C.op("dve", lambda e: e.tensor_tensor(out=pos[:, :], in0=pb[:, 0:NEXP], in1=cnt[:, :], op=ALU.add), R=[pb, cnt], W=[pos])
                pb = self.abank()
                self.mm(pb, pb[:, 0:NEXP], self.onesb[:, :], emb[:, :], R=[self.onesb, emb])
                C.op("dve", lambda e: e.tensor_tensor(out=cnt[:, :], in0=cnt[:, :], in1=pb[:, 0:NEXP], op=ALU.add), R=[pb, cnt], W=[cnt])
                C.op("dve", lambda e: e.tensor_scalar(out=ov[:, :], in0=pos[:, :], scalar1=float(CAP), scalar2=1.0e7, op0=ALU.is_ge, op1=ALU.mult), R=[pos], W=[ov])
                C.op("dve", lambda e: e.tensor_tensor(out=dst[:, :], in0=pos[:, :], in1=iotaE[:, :], op=ALU.add), R=[pos, iotaE], W=[dst])
                C.op("dve", lambda e: e.tensor_tensor(out=dst[:, :], in0=dst[:, :], in1=ov[:, :], op=ALU.add), R=[dst, ov], W=[dst])
                sel_b = bass.AP(sel.t, 0, [[NEXP, 128], [0, 8], [1, NEXP]])
                dst_b = bass.AP(dst.t, 0, [[NEXP, 128], [0, 8], [1, NEXP]])
                wc_b = bass.AP(Wc.t, 0, [[NEXP, 128], [0, 8], [1, NEXP]])
                pt = self.tbank()
                for kc in range(2):
                    self.tr(pt, pt[:, kc * 128:(kc + 1) * 128], emb[:, kc * 128:(kc + 1) * 128], self.identb[:, :], R=[emb, self.identb])
                C.op("act", lambda e: e.activation(out=emT[:, :, :].rearrange("p a b -> p (a b)"), in_=pt[:, 0:256], func=AF.Copy), R=[pt], W=[emT])
                pb = self.abank()
                for kc in range(2):
                    self.mm(pb, pb[:, 0:NEXP], emT[:, kc, :], SUF[:, kc, :], R=[emT, SUF], start=(kc == 0), stop=(kc == 1))
                C.op("act", lambda e: e.activation(out=jr[:, :], in_=pb[:, 0:NEXP], func=AF.Copy), R=[pb], W=[jr])
                C.op("dve", lambda e: e.tensor_tensor(out=dst[:, :], in0=dst[:, :], in1=em[:, :], op=ALU.mult), R=[dst, em], W=[dst])
                jr_b = bass.AP(jr.t, 0, [[NEXP, 128], [0, 8], [1, NEXP]])
                C.op("dve", lambda e: e.tensor_tensor(out=oh[:, :, :], in0=jr_b, in1=jv[:, :, :], op=ALU.is_equal), R=[jr, jv], W=[oh])
                C.op("pool", lambda e: e.tensor_tensor(out=tmp[:, :, :], in0=oh[:, :, :], in1=dst_b, op=ALU.mult), R=[oh, dst], W=[tmp])
                C.op("dve", lambda e: e.tensor_reduce(out=dj[:, :], in_=tmp[:, :, :], axis=AX.X, op=ALU.add), R=[tmp], W=[dj])
                C.op("dve", lambda e: e.tensor_copy(out=dji[:, :], in_=dj[:, :]), R=[dj], W=[dji])
                C.op("pool", lambda e: e.tensor_tensor(out=tmp[:, :, :], in0=oh[:, :, :], in1=wc_b, op=ALU.mult), R=[oh, Wc, tmp], W=[tmp])
                C.op("dve", lambda e: e.tensor_reduce(out=rowd[:, :, 2], in_=tmp[:, :, :], axis=AX.X, op=ALU.add), R=[tmp], W=[rowd])
                C.op("pool", lambda e: e.iota(rowd[:, :, 0], pattern=[[0, 8]], base=r0, channel_multiplier=1, allow_small_or_imprecise_dtypes=True), R=[rowd], W=[rowd])
                C.op("pool", lambda e: e.iota(rowd[:, :, 1], pattern=[[1, 8]], base=r0 * 8, channel_multiplier=8, allow_small_or_imprecise_dtypes=True), R=[rowd], W=[rowd])
                C.op("pool", lambda e: e.memset(rowd[:, :, 3], 0.0), R=[rowd], W=[rowd])
                for j in range(8):
                    C.dma(lambda e: e.indirect_dma_start(out=self.L_d.t[:, :], out_offset=bass.IndirectOffsetOnAxis(ap=dji[:, j:j + 1], axis=0), in_=rowd[:, j, :], in_offset=None,
                                                         bounds_check=self.bnd["L"], oob_is_err=False), R=[rowd, dji, self.L_d], W=[], q="pool")
                pb = self.abank()
                for blk in range(4):
                    for k in range(8):
                        self.mm(pb, pb[:, blk * 128:(blk + 1) * 128], Wsgu[:, k, blk * 128:(blk + 1) * 128], u2Tb[:, k, :], R=[Wsgu, u2Tb], start=(k == 0), stop=(k == 7))
                C.op("act", lambda e: e.activation(out=sg[:, :], in_=pb[:, 0:256], func=AF.Silu), R=[pb], W=[sg])
                C.op("dve", lambda e: e.tensor_tensor(out=hs[:, :, :].rearrange("p a b -> p (a b)"), in0=sg[:, :], in1=pb[:, 256:512], op=ALU.mult), R=[sg, pb], W=[hs])
                for n in range(2):
                    pb = self.abank()
                    for fb in range(2):
                        self.mm(pb, pb[:, 0:512], hs[:, fb, :], Wsd[:, fb, n * 512:(n + 1) * 512], R=[hs, Wsd], start=(fb == 0), stop=(fb == 1))
                    C.op("act", lambda e: e.activation(out=sho[:, n * 512:(n + 1) * 512], in_=pb[:, 0:512], func=AF.Copy), R=[pb], W=[sho])
                C.dma(lambda e: e.dma_start(out=self.sh_d.t[r0:r0 + 128, :], in_=sho[:, :]), R=[sho], W=[self.sh_d])

    def experts(self, moe_g, moe_u, moe_d):
        C = self.C
        NB = CAP // 128
        with Scope(C):
            Wf = [C.sb([128, 6144], F32, "Wf%d" % i) for i in range(3)]
            Wgu = [C.sb([128, 8, 512], BF16, "Wgu%d" % i) for i in range(2)]
            Wd = [C.sb([128, 2, D], BF16, "Wd%d" % i) for i in range(2)]
            Lt = [C.sb([128, NB, 4], F32, "Lt%d" % i) for i in range(3)]
            Li = [C.sb([128, NB, 2], I32, "Lti%d" % i) for i in range(4)]
            Lw = [C.sb([128, NB], F32, "Lw%d" % i) for i in range(4)]
            xg = [[C.sb([128, D], BF16, "xg%d_%d" % (i, j)) for j in range(NB)] for i in range(2)]
            xgT = [C.sb([128, 8, CAP], BF16, "xgT%d" % i) for i in range(2)]
            sg = [C.sb([128, CAP], F32, "e_sg%d" % i) for i in range(2)]
            hb = [C.sb([128, 2, CAP], BF16, "e_hb%d" % i) for i in range(2)]
            yo = [[C.sb([128, D], F32, "yo%d_%d" % (i, j)) for j in range(NB)] for i in range(2)]

            def load(e_):
                b = e_ % 3
                C.dma(lambda e: e.dma_start(out=Wf[b][:, 0:2048].rearrange("p (k f) -> p k f", k=8), in_=moe_g.t[e_ * D:(e_ + 1) * D, :].rearrange("(p k) f -> p k f", k=8)), R=[moe_g], W=[Wf[b]])
                C.dma(lambda e: e.dma_start(out=Wf[b][:, 2048:4096].rearrange("p (k f) -> p k f", k=8), in_=moe_u.t[e_ * D:(e_ + 1) * D, :].rearrange("(p k) f -> p k f", k=8)), R=[moe_u], W=[Wf[b]])
                C.dma(lambda e: e.dma_start(out=Wf[b][:, 4096:6144].rearrange("p (k n) -> p k n", k=2), in_=moe_d.t[e_ * 256:(e_ + 1) * 256, :].rearrange("(k p) n -> p k n", p=128)), R=[moe_d], W=[Wf[b]])
                C.dma(lambda e: e.dma_start(out=Lt[b][:, :, :], in_=self.L_d.t[e_ * CAP:(e_ + 1) * CAP, :].rearrange("(a p) n -> p a n", p=128)), R=[self.L_d], W=[Lt[b]])

            def small(e_):
                b4 = e_ % 4; b3 = e_ % 3
                C.op("dve", lambda e: e.tensor_copy(out=Li[b4][:, :, :], in_=Lt[b3][:, :, 0:2]), R=[Lt[b3]], W=[Li[b4]])
                C.op("dve", lambda e: e.tensor_copy(out=Lw[b4][:, :], in_=Lt[b3][:, :, 2]), R=[Lt[b3]], W=[Lw[b4]])

            def castW(e_):
                b = e_ % 2; b3 = e_ % 3
                C.op("act", lambda e: e.activation(out=Wgu[b][:, :, 0:256], in_=Wf[b3][:, 0:2048].rearrange("p (k f) -> p k f", k=8), func=AF.Copy), R=[Wf[b3]], W=[Wgu[b]])
                C.op("act", lambda e: e.activation(out=Wgu[b][:, :, 256:512], in_=Wf[b3][:, 2048:4096].rearrange("p (k f) -> p k f", k=8), func=AF.Copy), R=[Wf[b3]], W=[Wgu[b]])
                C.op("act", lambda e: e.activation(out=Wd[b][:, :, :], in_=Wf[b3][:, 4096:6144].rearrange("p (k n) -> p k n", k=2), func=AF.Copy), R=[Wf[b3]], W=[Wd[b]])

            def gather(e_):
                b = e_ % 2
                for blk in range(NB):
                    C.dma(lambda e: e.indirect_dma_start(out=xg[b][blk][:, :], out_offset=None, in_=self.u2_d.t[:, :], in_offset=bass.IndirectOffsetOnAxis(ap=Li[e_ % 4][:, blk, 0:1], axis=0),
                                                         bounds_check=self.bnd["u2"], oob_is_err=False), R=[self.u2_d, Li[e_ % 4]], W=[xg[b][blk]], q="pool")

            def transp(e_):
                b = e_ % 2
                for blk in range(NB):
                    pt = self.tbank()
                    for k in range(8):
                        self.tr(pt, pt[:, k * 128:(k + 1) * 128], xg[b][blk][:, k:D:8], self.identb[:, :], R=[xg[b][blk], self.identb])
                    C.op("dve", lambda e: e.tensor_copy(out=xgT[b][:, :, blk * 128:(blk + 1) * 128], in_=pt[:, :].rearrange("p (k n) -> p k n", k=8)), R=[pt], W=[xgT[b]])

            load(0); load(1); load(2); small(0); gather(0); castW(0); transp(0)
            for e_ in range(NEXP):
                b = e_ % 2
                if e_ + 1 < NEXP:
                    small(e_ + 1); gather(e_ + 1)
                pbs = []
                for j in range(4):
                    pb = self.abank()
                    for k in range(8):
                        self.mm(pb, pb[:, 0:CAP], Wgu[b][:, k, j * 128:(j + 1) * 128], xgT[b][:, k, :], R=[Wgu[b], xgT[b]], start=(k == 0), stop=(k == 7))
                    pbs.append(pb)
                for fb in range(2):
                    C.op("act", lambda e: e.activation(out=sg[fb][:, :], in_=pbs[fb][:, 0:CAP], func=AF.Silu), R=[pbs[fb]], W=[sg[fb]])
                    C.op("dve", lambda e: e.tensor_tensor(out=hb[b][:, fb, :], in0=sg[fb][:, :], in1=pbs[2 + fb][:, 0:CAP], op=ALU.mult), R=[sg[fb], pbs[2 + fb]], W=[hb[b]])
                if e_ + 1 < NEXP:
                    castW(e_ + 1)
                    transp(e_ + 1)
                for blk in range(NB):
                    for n in range(2):
                        pb = self.abank()
                        for fb in range(2):
                            self.mm(pb, pb[:, 0:512], hb[b][:, fb, blk * 128:(blk + 1) * 128], Wd[b][:, fb, n * 512:(n + 1) * 512], R=[hb[b], Wd[b]], start=(fb == 0), stop=(fb == 1))
                        C.op("dve", lambda e: e.tensor_scalar(out=yo[b][blk][:, n * 512:(n + 1) * 512], in0=pb[:, 0:512], scalar1=Lw[e_ % 4][:, blk:blk + 1], scalar2=None, op0=ALU.mult), R=[pb, Lw[e_ % 4]], W=[yo[b][blk]])
                    C.dma(lambda e: e.indirect_dma_start(out=self.Y_d.t[:, :], out_offset=bass.IndirectOffsetOnAxis(ap=Li[e_ % 4][:, blk, 1:2], axis=0), in_=yo[b][blk][:, :], in_offset=None,
                                                         bounds_check=self.bnd["Y"], oob_is_err=False), R=[yo[b][blk], Li[e_ % 4], self.Y_d], W=[], q="pool")
                if e_ + 3 < NEXP:
                    load(e_ + 3)

    def final(self, vecs, out):
        C = self.C
        with Scope(C):
            gate = C.sb([128, D], F32, "fgate"); gfin = C.sb([128, D], F32, "gfin")
            self.col_to_bcast(self.gcol, 8, gate)
            C.dma(lambda e: e.dma_start(out=gfin[:, :], in_=vecs.t[256:384, :]), R=[vecs], W=[gfin])
            Yt = [C.sb([128, 8, D], F32, "Yt%d" % i) for i in range(2)]
            x1 = [C.sb([128, D], F32, "fx1%d" % i) for i in range(2)]; sh = [C.sb([128, D], F32, "fsh%d" % i) for i in range(2)]
            acc = C.sb([128, D], F32, "facc"); junk = C.sb([128, D], BF16, "fjunk"); ss = C.sb([128, 1], F32, "fss"); rstd = C.sb([128, 1], F32, "frstd")
            ot = [C.sb([128, D], F32, "fot%d" % i) for i in range(2)]
            for t in range(32):
                r0 = t * 128; b = t % 2
                C.dma(lambda e: e.dma_start(out=Yt[b][:, :, :], in_=self.Y_d.t[r0 * 8:(r0 + 128) * 8, :].rearrange("(p j) n -> p j n", j=8)), R=[self.Y_d], W=[Yt[b]])
                C.dma(lambda e: e.dma_start(out=x1[b][:, :], in_=self.x1_d.t[r0:r0 + 128, :]), R=[self.x1_d], W=[x1[b]])
                C.dma(lambda e: e.dma_start(out=sh[b][:, :], in_=self.sh_d.t[r0:r0 + 128, :]), R=[self.sh_d], W=[sh[b]])
                C.op("dve", lambda e: e.tensor_reduce(out=acc[:, :], in_=Yt[b][:, :, :].rearrange("p j n -> p n j"), axis=AX.X, op=ALU.add), R=[Yt[b]], W=[acc])
                C.op("pool", lambda e: e.tensor_tensor(out=acc[:, :], in0=acc[:, :], in1=sh[b][:, :], op=ALU.add), R=[acc, sh[b]], W=[acc])
                C.op("dve", lambda e: e.tensor_tensor(out=acc[:, :], in0=acc[:, :], in1=gate[:, :], op=ALU.mult), R=[acc, gate], W=[acc])
                C.op("pool", lambda e: e.tensor_tensor(out=acc[:, :], in0=acc[:, :], in1=x1[b][:, :], op=ALU.add), R=[acc, x1[b]], W=[acc])
                self.rms_rstd(acc[:, :], acc, junk, ss, rstd, D)
                C.op("dve", lambda e: e.scalar_tensor_tensor(out=ot[b][:, :], in0=acc[:, :], scalar=rstd[:, 0:1], in1=gfin[:, :], op0=ALU.mult, op1=ALU.mult), R=[acc, rstd, gfin], W=[ot[b]])
                C.dma(lambda e: e.dma_start(out=out.t[r0:r0 + 128, :], in_=ot[b][:, :]), R=[ot[b]], W=[out])


def col_layout(v):
    return np.ascontiguousarray(np.asarray(v).reshape(8, 128).T)


def rep128(v):
    v = np.asarray(v).reshape(1, -1)
    return np.ascontiguousarray(np.broadcast_to(v, (128, v.shape[1])))


def host_layout(inp):
    f32 = np.float32
    x = np.asarray(inp["x"], f32); ctx = np.asarray(inp["ctx"], f32)
    w_in = np.ascontiguousarray(np.asarray(inp["w_in"], f32)[0])
    wsc = w_in[:, 1024:1024 + WS]
    cxw = np.asarray(inp["conv_xbc_w"], f32)[0]; cxb = np.asarray(inp["conv_xbc_b"], f32)[0]
    cqw = np.asarray(inp["conv_qk_w"], f32)[0]; cqb = np.asarray(inp["conv_qk_b"], f32)[0]
    dtb = np.asarray(inp["ssd_dt_bias"], f32)[0]; alog = np.asarray(inp["ssd_a_log"], f32)[0]
    ib = np.asarray(inp["mlstm_i_bias"], f32)[0]; fb = np.asarray(inp["mlstm_f_bias"], f32)[0]

    def taps(w, nt, flip):
        ww = w[::-1] if flip else w
        return np.ascontiguousarray(ww.reshape(5, nt, 128).transpose(2, 1, 0).reshape(128, nt * 5))

    def bias(b, nt):
        return np.ascontiguousarray(b.reshape(nt, 128).T)

    def wl(d):
        w = wsc.copy()
        if d == 1:
            w[:, O_DT:O_DT + 16] = wsc[:, O_DT + 16:O_DT + 32]
            w[:, O_GT:O_GT + 16] = wsc[:, O_GT + 16:O_GT + 32]
        return w

    def smallset(d):
        return rep128(np.concatenate([dtb[d], alog[d], ib[d], fb[d]]))
    wl_d = [wl(0), wl(1)]
    shared = dict(
        ada_w=np.ascontiguousarray(np.asarray(inp["ada_w"], f32)[0]),
        ada_b=np.ascontiguousarray(np.asarray(inp["ada_b"], f32)[0].reshape(48, 128).T),
        gcols=np.concatenate([col_layout(inp["norm_mix_g"][0]), col_layout(inp["norm_ffn_g"][0])], axis=1).astype(f32),
        w_in=w_in,
        ssd_d=rep128(np.asarray(inp["ssd_d"], f32)[0]),
        vecs=np.concatenate([rep128(inp["ssd_norm_g"][0]), rep128(inp["mlstm_norm_g"][0]), rep128(inp["norm_final_g"])], axis=0).astype(f32),
        w_ssd_out=np.ascontiguousarray(np.asarray(inp["w_ssd_out"], f32)[0]),
        w_mlstm_out=np.ascontiguousarray(np.asarray(inp["w_mlstm_out"], f32)[0]),
        w_out=np.ascontiguousarray(np.asarray(inp["w_out"], f32)[0]),
        router_w=np.ascontiguousarray(np.asarray(inp["router_w"], f32)[0]),
        router_b=rep128(np.asarray(inp["router_bias"], f32)[0]),
        moe_w_gate=np.asarray(inp["moe_w_gate"], f32)[0].reshape(NEXP * D, 256),
        moe_w_up=np.asarray(inp["moe_w_up"], f32)[0].reshape(NEXP * D, 256),
        moe_w_down=np.asarray(inp["moe_w_down"], f32)[0].reshape(NEXP * 256, D),
        shared_w_gate=np.ascontiguousarray(np.asarray(inp["shared_w_gate"], f32)[0]),
        shared_w_up=np.ascontiguousarray(np.asarray(inp["shared_w_up"], f32)[0]),
        shared_w_down=np.ascontiguousarray(np.asarray(inp["shared_w_down"], f32)[0]),
    )
    maps = []
    for c in range(NCORE):
        b, s = c // 4, c % 4
        m = dict(shared)
        m["xo"] = np.ascontiguousarray(x[b, s * SEG:(s + 1) * SEG])
        pre = []; dirs = []
        for j in range(3):
            if j < s:
                pre.append(x[b, j * SEG:(j + 1) * SEG]); dirs.append(0)
            else:
                g = 3 - (j - s)
                pre.append(x[b, g * SEG:(g + 1) * SEG][::-1]); dirs.append(1)
        m["xpre"] = np.ascontiguousarray(np.concatenate(pre, axis=0))
        m["ctx2"] = np.ascontiguousarray(np.concatenate([ctx[b], ctx[b][::-1]], axis=0))
        cv = np.zeros((128, 16), f32)
        cv[:, 0::2] = col_layout(inp["c"][b]); cv[:, 1::2] = col_layout(inp["c_ctx"])
        m["cvec"] = cv
        sd = [0, 1] + dirs
        m["wlite"] = np.ascontiguousarray(np.concatenate([wl_d[d] for d in sd], axis=0))
        m["cwx"] = np.concatenate([taps(cxw, 12, d == 1) for d in sd] + [taps(cxw, 12, False)], axis=0)
        m["cbx"] = np.concatenate([bias(cxb, 12)] * 6, axis=0)
        m["cwq"] = np.concatenate([taps(cqw, 8, d == 1) for d in sd] + [taps(cqw, 8, False)], axis=0)
        m["cbq"] = np.concatenate([bias(cqb, 8)] * 6, axis=0)
        m["small"] = np.concatenate([smallset(d) for d in sd] + [smallset(0), smallset(1)], axis=0)
        fl = np.zeros((128, 8), f32)
        for j in range(3):
            fl[:, j] = 1.0 if dirs[j] == 0 else 0.0
            fl[:, 4 + j] = 1.0 - fl[:, j]
        m["flags"] = fl
        maps.append(m)
    return maps


def kernel(**inputs):
    maps = host_layout(inputs)
    prog = Prog()
    nc = prog.build()
    maps = [{k: v for k, v in m.items() if k in prog.ins} for m in maps]
    res = run_bass_kernel_spmd(nc, maps, core_ids=list(range(NCORE)))
    out = np.zeros((2, 4 * SEG, D), np.float32)
    for c in range(NCORE):
        b, s = c // 4, c % 4
        out[b, s * SEG:(s + 1) * SEG] = res.results[c]["out"]
    return out
```

```python
from contextlib import ExitStack
import os
import numpy as np
import concourse.bass as bass
import concourse.mybir as mybir
from concourse.bass_utils import run_bass_kernel_spmd

F32 = mybir.dt.float32
BF16 = mybir.dt.bfloat16
I32 = mybir.dt.int32
ALU = mybir.AluOpType
AF = mybir.ActivationFunctionType
AX = mybir.AxisListType

NCORE = 8
D = 1024
SEG = 4096
NSEGC = 32
CTXL = 256
EPS = 1e-6
BIG = 30000.0
NEXP = 256
CAP = 512
WS = 3648
O_XBC, O_DT, O_QK, O_V, O_GT = 0, 1536, 1568, 2592, 3616


class Buf:
    __slots__ = ("t", "w", "r", "name", "off", "excl")

    def __init__(self, t, name, off=0, excl=False):
        self.t = t
        self.name = name
        self.off = off
        self.excl = excl
        self.w = {}
        self.r = {}

    def __getitem__(self, idx):
        return self.t[idx]


class Ctx:
    ENG = ("pe", "dve", "act", "pool", "sp")
    SAME_WIN = 4

    def __init__(self, nc, es, n_dma_sems=48):
        self.nc = nc
        self.es_stack = [es]
        self.e = {"pe": nc.tensor, "dve": nc.vector, "act": nc.scalar, "pool": nc.gpsimd, "sp": nc.sync}
        self.sem = {k: es.enter_context(nc.semaphore("s_" + k)) for k in self.ENG}
        self.cnt = {k: 0 for k in self.ENG}
        self.seen = {k: {} for k in self.ENG}
        self.dsem = [es.enter_context(nc.semaphore("d%d" % i)) for i in range(n_dma_sems)]
        self.dcnt = [0] * n_dma_sems
        self.dnext = 0
        self.nid = 0
        self.n_inst = 0

    @property
    def es(self):
        return self.es_stack[-1]

    def sb(self, shape, dt=F32, name=None):
        self.nid += 1
        name = "%s_%d" % (name or "sb", self.nid)
        return Buf(self.es.enter_context(self.nc.sbuf_tensor(name, list(shape), dt)), name)

    def ps(self, shape, dt=F32, name=None):
        self.nid += 1
        name = "%s_%d" % (name or "ps", self.nid)
        return Buf(self.es.enter_context(self.nc.psum_tensor(name, list(shape), dt)), name, excl=True)

    def dram(self, shape, dt=F32, name=None):
        self.nid += 1
        name = name or ("dr_%d" % self.nid)
        return Buf(self.nc.dram_tensor(name, list(shape), dt, kind="Internal"), name)

    def _wait(self, eng, key, val):
        if self.seen[eng].get(key, 0) >= val:
            return
        if isinstance(key, int):
            self.e[eng].wait_ge(self.dsem[key], val)
        else:
            if key == eng and (eng == "pe" or val <= self.cnt[eng] - self.SAME_WIN):
                return
            self.e[eng].wait_ge(self.sem[key], val)
        self.seen[eng][key] = val

    def _deps(self, eng, R, W):
        for b in R:
            for k, v in b.w.items():
                self._wait(eng, k, v)
            if b.excl:
                for k, v in b.r.items():
                    if k != eng:
                        self._wait(eng, k, v)
        for b in W:
            for k, v in b.w.items():
                self._wait(eng, k, v)
            for k, v in b.r.items():
                self._wait(eng, k, v)

    def op(self, eng, fn, R=(), W=()):
        self._deps(eng, R, W)
        inst = fn(self.e[eng])
        self.cnt[eng] += 1
        idx = self.cnt[eng]
        inst.then_inc(self.sem[eng], 1)
        self.n_inst += 1
        for b in R:
            b.r[eng] = idx
        for b in W:
            b.w[eng] = idx
        return inst

    def dma(self, fn, R=(), W=(), q="sp"):
        k = self.dnext
        self.dnext = (self.dnext + 1) % len(self.dsem)
        if self.dcnt[k] > 0:
            self._wait(q, k, self.dcnt[k])
        self._deps(q, R, W)
        inst = fn(self.e[q])
        self.dcnt[k] += 16
        inst.then_inc(self.dsem[k], 16)
        self.n_inst += 1
        for b in R:
            b.r[k] = self.dcnt[k]
        for b in W:
            b.w[k] = self.dcnt[k]
        return inst

    def barrier(self):
        for eng in self.ENG:
            for k in self.ENG:
                if k != eng and self.cnt[k] > 0:
                    self._wait(eng, k, self.cnt[k])
            for k in range(len(self.dsem)):
                if self.dcnt[k] > 0:
                    self._wait(eng, k, self.dcnt[k])

    def finish(self, bufs, eng="sp"):
        for b in bufs:
            for k, v in b.w.items():
                self._wait(eng, k, v)


class Scope:
    def __init__(self, C):
        self.C = C

    def __enter__(self):
        self.es = ExitStack()
        self.es.__enter__()
        self.C.es_stack.append(self.es)
        return self

    def __exit__(self, *a):
        self.C.barrier()
        self.C.es_stack.pop()
        return self.es.__exit__(*a)


def bc(buf, col0, n, rep):
    t = buf.t
    Fsz = int(np.prod(t.shape[1:]))
    return bass.AP(t, col0, [[Fsz, t.shape[0]], [1, n], [0, rep]])


def bcp(buf, p0, npart, col0, n, rep):
    t = buf.t
    Fsz = int(np.prod(t.shape[1:]))
    return bass.AP(t, p0 * Fsz + col0, [[Fsz, npart], [1, n], [0, rep]])


def colbc(buf, col, rep):
    t = buf.t
    Fsz = int(np.prod(t.shape[1:]))
    return bass.AP(t, col, [[Fsz, t.shape[0]], [0, rep]])


class Prog:
    def __init__(self, dbg=None, stop_after=None, addD=True):
        self.addD = addD
        self.dbg = dbg or []
        self.stop_after = stop_after
        self.nc = bass.Bass("TRN2", target_bir_lowering=False)
        self.ins = {}
        self.outs = {}

    def inp(self, name, shape, dt=F32):
        b = Buf(self.nc.dram_tensor(name, list(shape), dt, kind="ExternalInput"), name)
        self.ins[name] = b
        return b

    def outp(self, name, shape, dt=F32):
        b = Buf(self.nc.dram_tensor(name, list(shape), dt, kind="ExternalOutput"), name)
        self.outs[name] = b
        return b

    def mm(self, ob, oap, lhsT, rhs, R, start=True, stop=True):
        self.C.op("pe", lambda e: e.matmul(oap, lhsT=lhsT, rhs=rhs, start=start, stop=stop), R=R, W=[ob])

    def tr(self, ob, oap, in_ap, ident_ap, R):
        self.C.op("pe", lambda e: e.transpose(out=oap, in_=in_ap, identity=ident_ap), R=R, W=[ob])

    def gbank(self):
        self._g = (self._g + 1) % len(self.G)
        return self.G[self._g]

    def tbank(self):
        self._t = (self._t + 1) % len(self.T)
        return self.T[self._t]

    def dslot(self):
        self._d = (self._d + 1) % len(self.DS)
        return self.DS[self._d]

    def dump(self, name, buf, ap, shape, dt=F32):
        if name in self.dbg:
            o = self.outp("dbg_" + name, shape, dt)
            self.C.dma(lambda e: e.dma_start(out=o.t.ap() if len(shape) == 0 else o.t[tuple(slice(None) for _ in shape)], in_=ap), R=[buf], W=[o])

    def build(self):
        nc = self.nc
        I = self.inp
        xo = I("xo", [SEG, D]); xpre = I("xpre", [3 * SEG, D]); ctx2 = I("ctx2", [2 * CTXL, D])
        cvec = I("cvec", [128, 16]); ada_w = I("ada_w", [D, 6 * D]); ada_b = I("ada_b", [128, 48])
        gcols = I("gcols", [128, 16])
        wlite = I("wlite", [5 * D, WS]); w_in = I("w_in", [D, 7744])
        cwx = I("cwx", [6 * 128, 60]); cbx = I("cbx", [6 * 128, 12]); cwq = I("cwq", [6 * 128, 40]); cbq = I("cbq", [6 * 128, 8])
        small = I("small", [7 * 128, 48]); flags = I("flags", [128, 8]); ssd_d = I("ssd_d", [128, 16])
        if self.stop_after is None or self.stop_after in ("merge", "route"):
            vecs = I("vecs", [3 * 128, D])
            w_so = I("w_ssd_out", [D, D]); w_mo = I("w_mlstm_out", [D, D]); w_o = I("w_out", [D, D])
            router_w = I("router_w", [D, NEXP]); router_b = I("router_b", [128, NEXP])
            sh_g = I("shared_w_gate", [D, 256]); sh_u = I("shared_w_up", [D, 256]); sh_d = I("shared_w_down", [256, D])
        if self.stop_after is None:
            moe_g = I("moe_w_gate", [NEXP * D, 256]); moe_u = I("moe_w_up", [NEXP * D, 256]); moe_d = I("moe_w_down", [NEXP * 256, D])
        out = self.outp("out", [SEG, D])

        with ExitStack() as es:
            C = self.C = Ctx(nc, es)
            self.bnd = {}
            for nm, val in (("L", NEXP * CAP - 1), ("u2", SEG + 127), ("Y", SEG * 8 - 1)):
                r = es.enter_context(nc.gpsimd.register("bnd_" + nm))
                nc.gpsimd.reg_mov(r, val)
                self.bnd[nm] = r
            self.build_consts()
            self.G = [C.ps([128, 512], F32, "G%d" % i) for i in range(2)]
            self.T = [C.ps([128, 1024], BF16, "T%d" % i) for i in range(2)]
            self.PY = C.ps([128, 512], F32, "PY"); self.PYI = C.ps([128, 512], F32, "PYI")
            self.DS = [C.ps([128, 512], F32, "DS%d" % i) for i in range(2)]
            self._g = self._t = self._d = 0
            self.y_d = [C.dram([SEG, D], F32, "y_d%d" % d) for d in range(2)]
            self.h_d = [C.dram([SEG, D], F32, "h_d%d" % d) for d in range(2)]
            later = self.stop_after is None or self.stop_after in ("merge", "route")
            self.uT_d = C.dram([8 * 128, 4096], BF16, "uT_d") if later else None
            self.x1_d = C.dram([SEG, D], F32, "x1_d")
            self.sh_d = C.dram([SEG, D], F32, "sh_d")
            self.u2_d = C.dram([SEG + 128, D], BF16, "u2_d")
            self.L_d = C.dram([NEXP * CAP, 4], F32, "L_d")
            self.Y_d = C.dram([SEG * 8, D], F32, "Y_d") if self.stop_after is None else None

            self.adaln(cvec, ada_w, ada_b, gcols)
            if self.stop_after == "adaln":
                return self.end()
            self.mixer_scans(xo, xpre, ctx2, wlite, w_in, cwx, cbx, cwq, cbq, small, flags, ssd_d)
            if self.stop_after == "ctx":
                return self.end()
            if self.stop_after == "scans":
                for d in range(2):
                    if "y_d" in self.dbg:
                        self.copy_dram(self.y_d[d], self.outp("dbg_y_d%d" % d, [SEG, D]))
                        self.copy_dram(self.h_d[d], self.outp("dbg_h_d%d" % d, [SEG, D]))
                return self.end()
            self.merge_pass(xo, w_in, vecs, w_so, w_mo, w_o)
            if self.stop_after == "merge":
                self.copy_dram(self.x1_d, self.outp("dbg_x1", [SEG, D]))
                return self.end()
            self.ffn_prep(vecs, router_w, router_b, sh_g, sh_u, sh_d)
            if self.stop_after == "route":
                self.copy_dram(self.x1_d, self.outp("dbg_x1", [SEG, D]))
                self.copy_dram(self.sh_d, self.outp("dbg_sh", [SEG, D]))
                self.copy_dram(self.L_d, self.outp("dbg_L", [NEXP * CAP, 4]), rows=0, flat=(128, NEXP * CAP * 4 // 128))
                return self.end()
            self.experts(moe_g, moe_u, moe_d)
            self.final(vecs, out)
            return self.end()

    def copy_dram(self, src, dst, rows=SEG, flat=None):
        C = self.C
        if flat is not None:
            with Scope(C):
                p, f = flat
                t = C.sb([p, f], F32, "cpf")
                C.dma(lambda e: e.dma_start(out=t[:, :], in_=src.t.ap().rearrange("(p a) n -> p (a n)", p=p)), R=[src], W=[t])
                C.dma(lambda e: e.dma_start(out=dst.t.ap().rearrange("(p a) n -> p (a n)", p=p), in_=t[:, :]), R=[t], W=[dst])
            return
        with Scope(C):
            tl = [C.sb([128, 4, D], F32, "cp") for _ in range(2)]
            for i in range(rows // 512):
                t = tl[i % 2]
                C.dma(lambda e: e.dma_start(out=t[:, :, :], in_=src.t[i * 512:(i + 1) * 512, :].rearrange("(a p) n -> p a n", p=128)), R=[src], W=[t])
                C.dma(lambda e: e.dma_start(out=dst.t[i * 512:(i + 1) * 512, :].rearrange("(a p) n -> p a n", p=128), in_=t[:, :, :]), R=[t], W=[dst])

    def end(self):
        self.C.finish(list(self.outs.values()))
        self.C.barrier()
        return self.nc

    def build_consts(self):
        C = self.C

        def tri(name, cm, pat, op, val=1.0, fill=0.0):
            t = C.sb([128, 128], F32, name)
            C.op("pool", lambda e: e.memset(t[:, :], val), W=[t])
            C.op("pool", lambda e: e.affine_select(out=t[:, :], in_=t[:, :], pattern=[[pat, 128]], compare_op=op, fill=fill, base=0, channel_multiplier=cm), R=[t], W=[t])
            return t

        self.identf = tri("identf", 1, -1, ALU.is_equal)
        self.Uf = tri("Uf", -1, 1, ALU.is_ge)
        self.Lf = tri("Lf", 1, -1, ALU.is_ge)
        self.SUf = tri("SUf", -1, 1, ALU.is_gt)
        self.NEGU = tri("NEGU", 1, -1, ALU.is_gt, val=-BIG)
        self.NEGL = tri("NEGL", -1, 1, ALU.is_gt, val=-BIG)
        self.onesf = C.sb([128, 128], F32, "onesf")
        C.op("pool", lambda e: e.memset(self.onesf[:, :], 1.0), W=[self.onesf])
        self.epsb = C.sb([128, 1], F32, "epsb")
        C.op("pool", lambda e: e.memset(self.epsb[:, :], EPS), W=[self.epsb])

        def tobf(src, name):
            t = C.sb([128, 128], BF16, name)
            C.op("dve", lambda e: e.tensor_copy(out=t[:, :], in_=src[:, :]), R=[src], W=[t])
            return t
        self.identb = tobf(self.identf, "identb")
        self.Ub = tobf(self.Uf, "Ub"); self.Lb = tobf(self.Lf, "Lb"); self.SUb = tobf(self.SUf, "SUb")
        self.onesb = tobf(self.onesf, "onesb")

    def col_to_bcast(self, colbuf, c0, dst):
        C = self.C
        for half in range(2):
            pb = self.gbank()
            for jj in range(4):
                j = half * 4 + jj
                self.mm(pb, pb[:, jj * 128:(jj + 1) * 128], colbc(colbuf, c0 + j, 128), self.identf[:, :], R=[colbuf, self.identf])
            C.op("act", lambda e: e.activation(out=dst[:, half * 512:(half + 1) * 512], in_=pb[:, 0:512], func=AF.Copy), R=[pb], W=[dst])

    def adaln(self, cvec, ada_w, ada_b, gcols):
        C = self.C
        self.mod = C.sb([128, 96], F32, "mod")
        self.gc = C.sb([128, 16], F32, "gc")
        C.dma(lambda e: e.dma_start(out=self.gc[:, :], in_=gcols.t[:, :]), R=[gcols], W=[self.gc])
        with Scope(C):
            cv = C.sb([128, 16], F32, "cv"); sg = C.sb([128, 16], F32, "sg"); sc = C.sb([128, 16], F32, "sc")
            ab = C.sb([128, 48], F32, "ab")
            C.dma(lambda e: e.dma_start(out=cv[:, :], in_=cvec.t[:, :]), R=[cvec], W=[cv])
            C.dma(lambda e: e.dma_start(out=ab[:, :], in_=ada_b.t[:, :]), R=[ada_b], W=[ab])
            C.op("act", lambda e: e.activation(out=sg[:, :], in_=cv[:, :], func=AF.Sigmoid), R=[cv], W=[sg])
            C.op("dve", lambda e: e.tensor_tensor(out=sc[:, :], in0=cv[:, :], in1=sg[:, :], op=ALU.mult), R=[cv, sg], W=[sc])
            wt = [C.sb([128, 6 * D], F32, "adaw%d" % i) for i in range(2)]
            macc = C.sb([128, 96], F32, "macc")
            C.op("dve", lambda e: e.tensor_copy(out=macc[:, :].rearrange("p (j w) -> p j w", w=2), in_=bc(ab, 0, 48, 2)), R=[ab], W=[macc])
            for k in range(8):
                w = wt[k % 2]
                pm = self.gbank()
                for hh in range(2):
                    C.dma(lambda e: e.dma_start(out=w[:, hh * 3072:(hh + 1) * 3072], in_=ada_w.t[k * 128:(k + 1) * 128, hh * 3072:(hh + 1) * 3072]), R=[ada_w], W=[w])
                for j in range(48):
                    self.mm(pm, pm[:, 2 * j:2 * j + 2], w[:, j * 128:(j + 1) * 128], sc[:, 2 * k:2 * k + 2], R=[w, sc], start=True, stop=True)
                C.op("dve", lambda e: e.tensor_tensor(out=macc[:, :], in0=macc[:, :], in1=pm[:, 0:96], op=ALU.add), R=[pm, macc], W=[macc])
            C.op("dve", lambda e: e.tensor_copy(out=self.mod[:, :], in_=macc[:, :]), R=[macc], W=[self.mod])
        if "mod" in self.dbg:
            self.dump_now("mod", self.mod, [128, 96])
        mod3 = self.mod[:, :].rearrange("p (j w) -> p j w", w=2)
        self.cols = C.sb([128, 48], F32, "cols")
        cols = self.cols

        def gs(dst0, scale_j0, which, g0):
            C.op("dve", lambda e: e.scalar_tensor_tensor(out=cols[:, dst0:dst0 + 8], in0=mod3[:, scale_j0:scale_j0 + 8, which], scalar=1.0, in1=self.gc[:, g0:g0 + 8], op0=ALU.add, op1=ALU.mult), R=[self.mod, self.gc], W=[cols])

        def cp(dst0, j0, which):
            C.op("dve", lambda e: e.tensor_copy(out=cols[:, dst0:dst0 + 8], in_=mod3[:, j0:j0 + 8, which]), R=[self.mod], W=[cols])
        gs(0, 8, 0, 0); cp(8, 0, 0); gs(16, 8, 1, 0); cp(24, 0, 1); gs(32, 32, 0, 8); cp(40, 24, 0)
        self.gcol = C.sb([128, 16], F32, "gcol")
        C.op("dve", lambda e: e.tensor_copy(out=self.gcol[:, 0:8], in_=mod3[:, 16:24, 0]), R=[self.mod], W=[self.gcol])
        C.op("dve", lambda e: e.tensor_copy(out=self.gcol[:, 8:16], in_=mod3[:, 40:48, 0]), R=[self.mod], W=[self.gcol])

    def mixer_scans(self, xo, xpre, ctx2, wlite, w_in, cwx, cbx, cwq, cbq, small, flags, ssd_d):
        C = self.C
        with Scope(C):
            K = self.K = type("K", (), {})()
            K.GS = C.sb([128, D], F32, "GS"); K.SH = C.sb([128, D], F32, "SH")
            K.W = C.sb([128, 8, WS], BF16, "Wscan")
            K.cwx = C.sb([128, 60], F32, "cwx"); K.cbx = C.sb([128, 12], F32, "cbx"); K.cwq = C.sb([128, 40], F32, "cwq"); K.cbq = C.sb([128, 8], F32, "cbq")
            K.small = C.sb([128, 48], F32, "small"); K.aneg = C.sb([128, 16], F32, "aneg")
            K.flags = C.sb([128, 8], F32, "flags"); K.Dh = C.sb([128, 16], F32, "Dh")
            C.dma(lambda e: e.dma_start(out=K.flags[:, :], in_=flags.t[:, :]), R=[flags], W=[K.flags])
            C.dma(lambda e: e.dma_start(out=K.Dh[:, :], in_=ssd_d.t[:, :]), R=[ssd_d], W=[K.Dh])
            K.xt = [C.sb([128, D], F32, "xt%d" % i) for i in range(2)]
            K.junk = C.sb([128, D], BF16, "junk"); K.xm = C.sb([128, D], F32, "xm"); K.xn = C.sb([128, D], BF16, "xn")
            K.ss = C.sb([128, 1], F32, "ss"); K.rstd = C.sb([128, 1], F32, "rstd")
            K.uT = C.sb([128, 8, 512], BF16, "uT")
            K.cv = C.sb([128, 12, 512], BF16, "cv"); K.cvq = C.sb([128, 8, 512], BF16, "cvq")
            K.acc = [C.sb([128, 512], F32, "acc%d" % i) for i in range(4)]
            K.raw = [C.sb([128, 512], F32, "raw%d" % i) for i in range(4)]
            K.nconv = 0
            K.Vtok = [C.sb([128, D], BF16, "Vtok%d" % i) for i in range(4)]
            K.SM = [C.sb([128, 48], F32, "SM%d" % i) for i in range(4)]
            K.sm = C.sb([128, 32], F32, "sm"); K.e1 = C.sb([128, 32], F32, "e1")
            K.S = [C.sb([128, 512], F32, "S%d" % g) for g in range(2)]
            K.Sbf = [C.sb([128, 512], BF16, "Sbf%d" % g) for g in range(2)]
            K.Cm = C.sb([128, 4, 128], F32, "Cm"); K.nm = C.sb([128, 4], F32, "nm"); K.mbc = C.sb([128, 8], F32, "mbc")
            K.Cbf = C.sb([128, 4, 128], BF16, "Cbf"); K.nbf = C.sb([128, 4], BF16, "nbf")
            K.sav = []
            for d in range(2):
                K.sav.append(dict(S=[C.sb([128, 512], F32, "SS%d%d" % (d, g)) for g in range(2)], Cm=C.sb([128, 512], F32, "SCm%d" % d),
                                  nm=C.sb([128, 4], F32, "Snm%d" % d), mbc=C.sb([128, 8], F32, "Smbc%d" % d)))
            K.cs = C.sb([128, 48], F32, "cs"); K.Xtok = C.sb([128, D], BF16, "Xtok"); K.BK = C.sb([128, 768], BF16, "BK")
            K.t16 = C.sb([128, 16], F32, "t16"); K.wS = C.sb([128, 16], F32, "wS"); K.expcum = C.sb([128, 16], F32, "expcum"); K.ncum = C.sb([128, 16], F32, "ncum")
            K.exptot = C.sb([128, 16], F32, "exptot")
            K.Xw = C.sb([128, D], BF16, "Xw"); K.Xdt = C.sb([128, D], BF16, "Xdt"); K.XD = C.sb([128, D], BF16, "XD")
            K.CBT = [C.sb([128, 128], BF16, "CBT%d" % g) for g in range(2)]
            K.E = [C.sb([128, 128], BF16, "E%d" % i) for i in range(2)]; K.M = [C.sb([128, 128], BF16, "M%d" % i) for i in range(2)]
            K.yout = [C.sb([128, D], F32, "yout%d" % i) for i in range(2)]; K.hout = [C.sb([128, D], F32, "hout%d" % i) for i in range(2)]
            K.a8 = C.sb([128, 8], F32, "a8"); K.amax = C.sb([8, 1], F32, "amax"); K.dg = C.sb([8, 8], F32, "dg")
            K.Mc = C.sb([128, 8], F32, "Mc"); K.cd = C.sb([128, 8], F32, "cd"); K.w8 = C.sb([128, 8], F32, "w8"); K.w8b = C.sb([128, 8], BF16, "w8b")
            K.t8 = C.sb([128, 8], F32, "t8"); K.fl = C.sb([128, 8], F32, "fl"); K.den = C.sb([128, 8], F32, "den"); K.rc = C.sb([128, 8], F32, "rc")
            K.Vw = C.sb([128, 8, 128], BF16, "Vw"); K.A = [C.sb([128, 128], BF16, "A%d" % h) for h in range(8)]
            K.nchunk = 0

            def P(**kw):
                return type("P", (), kw)()
            passes = [
                P(name="ctxF", src=ctx2, row0=0, n_sc=1, sc_tok=256, rows=256, wsrc=(wlite, 0), slot=0, sset=0, kind="ctx", full=False, rev=False, dd=0, init="zero", save=0, flag=None),
                P(name="ctxB", src=ctx2, row0=CTXL, n_sc=1, sc_tok=256, rows=256, wsrc=(wlite, 1), slot=1, sset=1, kind="ctx", full=False, rev=False, dd=0, init="zero", save=1, flag=None),
            ]
            for j in range(3):
                passes.append(P(name="pre%d" % j, src=xpre, row0=j * SEG, n_sc=8, sc_tok=512, rows=64, wsrc=(wlite, 2 + j), slot=2 + j, sset=2 + j, kind="lat", full=False, rev=False, dd=0, init="blend", save="blend", flag=j))
            passes.append(P(name="ownF", src=xo, row0=0, n_sc=8, sc_tok=512, rows=64, wsrc=(w_in, None), slot=5, sset=5, kind="lat", full=True, rev=False, dd=0, init=0, save=None, flag=None))
            passes.append(P(name="ownB", src=xo, row0=0, n_sc=8, sc_tok=512, rows=64, wsrc=(w_in, None), slot=5, sset=6, kind="lat", full=True, rev=True, dd=1, init=1, save=None, flag=None))
            if self.stop_after == "ctx":
                passes = passes[:2]
            cur_kind = None
            for Pp in passes:
                if Pp.kind != cur_kind:
                    cur_kind = Pp.kind
                    o = 0 if Pp.kind == "lat" else 16
                    self.col_to_bcast(self.cols, o, K.GS)
                    self.col_to_bcast(self.cols, o + 8, K.SH)
                self.run_pass(Pp, cwx, cbx, cwq, cbq, small)
            if "ctxstate" in self.dbg:
                for d in range(2):
                    for g in range(2):
                        self.dump_now("S%d%d" % (d, g), K.sav[d]["S"][g], [128, 512])
                    self.dump_now("Cm%d" % d, K.sav[d]["Cm"], [128, 512])
                    self.dump_now("mbc%d" % d, K.sav[d]["mbc"], [128, 8])
                    self.dump_now("nm%d" % d, K.sav[d]["nm"], [128, 4])

    def dump_now(self, name, buf, shape):
        o = self.outp("dbg_" + name, shape)
        self.C.dma(lambda e: e.dma_start(out=o.t[:, :], in_=buf[:, :]), R=[buf], W=[o])

    def run_pass(self, P, cwx, cbx, cwq, cbq, small):
        C = self.C; K = self.K
        wsrc, wi = P.wsrc
        for k in range(8):
            if wi is None:
                src_ap = wsrc.t[k * 128:(k + 1) * 128, 1024:1024 + WS]
            else:
                src_ap = wsrc.t[wi * D + k * 128: wi * D + (k + 1) * 128, :]
            C.dma(lambda e: e.dma_start(out=K.W[:, k, :], in_=src_ap), R=[wsrc], W=[K.W], q="pool")
        s = P.slot
        C.dma(lambda e: e.dma_start(out=K.cwx[:, :], in_=cwx.t[s * 128:(s + 1) * 128, :]), R=[cwx], W=[K.cwx])
        C.dma(lambda e: e.dma_start(out=K.cbx[:, :], in_=cbx.t[s * 128:(s + 1) * 128, :]), R=[cbx], W=[K.cbx])
        C.dma(lambda e: e.dma_start(out=K.cwq[:, :], in_=cwq.t[s * 128:(s + 1) * 128, :]), R=[cwq], W=[K.cwq])
        C.dma(lambda e: e.dma_start(out=K.cbq[:, :], in_=cbq.t[s * 128:(s + 1) * 128, :]), R=[cbq], W=[K.cbq])
        C.dma(lambda e: e.dma_start(out=K.small[:, :], in_=small.t[P.sset * 128:(P.sset + 1) * 128, :]), R=[small], W=[K.small])
        C.op("act", lambda e: e.activation(out=K.aneg[:, :], in_=K.small[:, 16:32], func=AF.Exp), R=[K.small], W=[K.aneg])
        C.op("dve", lambda e: e.tensor_scalar(out=K.aneg[:, :], in0=K.aneg[:, :], scalar1=-1.0, scalar2=None, op0=ALU.mult), R=[K.aneg], W=[K.aneg])
        st = [(K.S[0], lambda d: K.sav[d]["S"][0], 128), (K.S[1], lambda d: K.sav[d]["S"][1], 128), (K.Cm, lambda d: K.sav[d]["Cm"], 128),
              (K.nm, lambda d: K.sav[d]["nm"], 128), (K.mbc, lambda d: K.sav[d]["mbc"], 128)]

        def flat(b):
            return b[:, :, :].rearrange("p a b -> p (a b)") if len(b.t.shape) == 3 else b[:, :]
        if P.init == "zero":
            for cur, _, _ in st:
                C.op("pool", lambda e: e.memset(flat(cur), 0.0), W=[cur])
        elif P.init == "blend":
            f = K.flags[:, P.flag:P.flag + 1]; nf = K.flags[:, 4 + P.flag:5 + P.flag]
            for cur, sv, _ in st:
                a = sv(0); b = sv(1)
                C.op("dve", lambda e: e.tensor_scalar(out=flat(cur), in0=flat(a), scalar1=f, scalar2=None, op0=ALU.mult), R=[a, K.flags], W=[cur])
                C.op("dve", lambda e: e.scalar_tensor_tensor(out=flat(cur), in0=flat(b), scalar=nf, in1=flat(cur), op0=ALU.mult, op1=ALU.add), R=[b, K.flags, cur], W=[cur])
        else:
            for cur, sv, _ in st:
                a = sv(P.init)
                C.op("dve", lambda e: e.tensor_copy(out=flat(cur), in_=flat(a)), R=[a], W=[cur])
        for g in range(2):
            C.op("act", lambda e: e.activation(out=K.Sbf[g][:, :], in_=K.S[g][:, :], func=AF.Copy), R=[K.S[g]], W=[K.Sbf[g]])
        scs = list(range(P.n_sc))
        if P.rev:
            scs = scs[::-1]
        for sc in scs:
            self.prep_sc(P, sc)
            tiles = list(range(P.sc_tok // 128))
            if P.rev:
                tiles = tiles[::-1]
            for i in tiles:
                self.scan_chunk(P, sc, i)
        if P.save == "blend":
            f = K.flags[:, P.flag:P.flag + 1]; nf = K.flags[:, 4 + P.flag:5 + P.flag]
            for cur, sv, _ in st:
                a = sv(0); b = sv(1)
                C.op("dve", lambda e: e.tensor_scalar(out=flat(a), in0=flat(a), scalar1=nf, scalar2=None, op0=ALU.mult), R=[a, K.flags], W=[a])
                C.op("dve", lambda e: e.scalar_tensor_tensor(out=flat(a), in0=flat(cur), scalar=f, in1=flat(a), op0=ALU.mult, op1=ALU.add), R=[cur, K.flags, a], W=[a])
                C.op("dve", lambda e: e.tensor_scalar(out=flat(b), in0=flat(b), scalar1=f, scalar2=None, op0=ALU.mult), R=[b, K.flags], W=[b])
                C.op("dve", lambda e: e.scalar_tensor_tensor(out=flat(b), in0=flat(cur), scalar=nf, in1=flat(b), op0=ALU.mult, op1=ALU.add), R=[cur, K.flags, b], W=[b])
        elif P.save is not None:
            for cur, sv, _ in st:
                a = sv(P.save)
                C.op("dve", lambda e: e.tensor_copy(out=flat(a), in_=flat(cur)), R=[cur], W=[a])

    def prep_sc(self, P, sc):
        C = self.C; K = self.K
        T = P.sc_tok
        nt = T // 128
        for i in range(nt):
            xt = K.xt[i % 2]
            r0 = P.row0 + sc * T + i * 128
            C.dma(lambda e: e.dma_start(out=xt[:, :], in_=P.src.t[r0:r0 + 128, :]), R=[P.src], W=[xt])
            C.op("pool", lambda e: e.memset(K.ss[:, :], 0.0), W=[K.ss])
            C.op("act", lambda e: e.activation(out=K.junk[:, :], in_=xt[:, :], func=AF.Square, accum_out=K.ss[:, :]), R=[xt, K.ss], W=[K.junk, K.ss])
            C.op("act", lambda e: e.activation(out=K.rstd[:, :], in_=K.ss[:, :], func=AF.Sqrt, scale=1.0 / D, bias=self.epsb[:, :]), R=[K.ss, self.epsb], W=[K.rstd])
            C.op("dve", lambda e: e.reciprocal(out=K.rstd[:, :], in_=K.rstd[:, :]), R=[K.rstd], W=[K.rstd])
            C.op("dve", lambda e: e.scalar_tensor_tensor(out=K.xm[:, :], in0=xt[:, :], scalar=K.rstd[:, 0:1], in1=K.GS[:, :], op0=ALU.mult, op1=ALU.mult), R=[xt, K.rstd, K.GS], W=[K.xm])
            C.op("pool", lambda e: e.tensor_tensor(out=K.xn[:, :], in0=K.xm[:, :], in1=K.SH[:, :], op=ALU.add), R=[K.xm, K.SH], W=[K.xn])
            pt = self.tbank()
            for k in range(8):
                self.tr(pt, pt[:, k * 128:(k + 1) * 128], K.xn[:, k * 128:(k + 1) * 128], self.identb[:, :], R=[K.xn, self.identb])
            C.op("act", lambda e: e.activation(out=K.uT[:, :, i * 128:(i + 1) * 128], in_=pt[:, :].rearrange("p (k n) -> p k n", k=8), func=AF.Copy), R=[pt], W=[K.uT])
        if P.name == "ownB" and self.uT_d is not None:
            C.dma(lambda e: e.dma_start(out=self.uT_d.t[sc * 128:(sc + 1) * 128, :], in_=K.uT[:, :, :].rearrange("p k n -> p (k n)")), R=[K.uT], W=[self.uT_d])
        if "uT" in self.dbg and P.name == "ownF" and sc == 0:
            self.dump_bf("uT", K.uT, K.uT[:, :, :].rearrange("p k n -> p (k n)"), [128, 4096])
        xt_list = list(range(12)) if P.full else list(range(10))
        qt_list = list(range(8)) if P.full else list(range(4, 8))
        jobs = [("x", ct) for ct in xt_list] + [("q", ct) for ct in qt_list]
        for j0 in range(0, len(jobs), 2):
            ctxs = []
            for (kind, ct) in jobs[j0:j0 + 2]:
                off = (O_XBC if kind == "x" else O_QK) + ct * 128
                cw, cb, dst = (K.cwx, K.cbx, K.cv) if kind == "x" else (K.cwq, K.cbq, K.cvq)
                pb = self.gbank()
                for k in range(8):
                    self.mm(pb, pb[:, 0:T], K.W[:, k, off:off + 128], K.uT[:, k, 0:T], R=[K.W, K.uT], start=(k == 0), stop=(k == 7))
                K.nconv += 1
                raw = K.raw[K.nconv % 4]; acc = K.acc[K.nconv % 4]
                C.op("act", lambda e: e.activation(out=raw[:, 0:T], in_=pb[:, 0:T], func=AF.Copy), R=[pb], W=[raw])
                ctxs.append((ct, cw, cb, dst, raw, acc))
            for (ct, cw, cb, dst, raw, acc) in ctxs:
                C.op("dve", lambda e: e.tensor_scalar(out=acc[:, 0:T], in0=raw[:, 0:T], scalar1=cw[:, ct * 5 + 2:ct * 5 + 3], scalar2=cb[:, ct:ct + 1], op0=ALU.mult, op1=ALU.add), R=[raw, cw, cb], W=[acc])
            for j in (0, 1, 3, 4):
                o = j - 2
                lo = max(0, -o); hi = P.rows - max(0, o)
                for (ct, cw, cb, dst, raw, acc) in ctxs:
                    a3 = acc[:, 0:T].rearrange("p (r t) -> p r t", t=P.rows); p3 = raw[:, 0:T].rearrange("p (r t) -> p r t", t=P.rows)
                    C.op("dve", lambda e: e.scalar_tensor_tensor(out=a3[:, :, lo:hi], in0=p3[:, :, lo + o:hi + o], scalar=cw[:, ct * 5 + j:ct * 5 + j + 1], in1=a3[:, :, lo:hi], op0=ALU.mult, op1=ALU.add), R=[raw, cw, acc], W=[acc])
            for (ct, cw, cb, dst, raw, acc) in ctxs:
                C.op("act", lambda e: e.activation(out=dst[:, ct, 0:T], in_=acc[:, 0:T], func=AF.Silu), R=[acc], W=[dst])
        for i in range(nt):
            for n in range(2):
                pb = self.gbank()
                for k in range(8):
                    self.mm(pb, pb[:, 0:512], K.uT[:, k, i * 128:(i + 1) * 128], K.W[:, k, O_V + n * 512:O_V + (n + 1) * 512], R=[K.W, K.uT], start=(k == 0), stop=(k == 7))
                C.op("act", lambda e: e.activation(out=K.Vtok[i][:, n * 512:(n + 1) * 512], in_=pb[:, 0:512], func=AF.Copy), R=[pb], W=[K.Vtok[i]])
            pb = self.gbank()
            dto = O_DT + 16 * P.dd; gto = O_GT + 16 * P.dd
            for k in range(8):
                self.mm(pb, pb[:, 0:16], K.uT[:, k, i * 128:(i + 1) * 128], K.W[:, k, dto:dto + 16], R=[K.W, K.uT], start=(k == 0), stop=(k == 7))
            for k in range(8):
                self.mm(pb, pb[:, 16:32], K.uT[:, k, i * 128:(i + 1) * 128], K.W[:, k, gto:gto + 16], R=[K.W, K.uT], start=(k == 0), stop=(k == 7))
            SM = K.SM[i]
            C.op("dve", lambda e: e.tensor_tensor(out=K.sm[:, 0:16], in0=pb[:, 0:16], in1=K.small[:, 0:16], op=ALU.add), R=[pb, K.small], W=[K.sm])
            C.op("dve", lambda e: e.tensor_tensor(out=K.sm[:, 16:32], in0=pb[:, 16:32], in1=K.small[:, 32:48], op=ALU.add), R=[pb, K.small], W=[K.sm])
            C.op("act", lambda e: e.activation(out=K.e1[:, 0:16], in_=K.sm[:, 0:16], func=AF.Exp), R=[K.sm], W=[K.e1])
            C.op("act", lambda e: e.activation(out=K.e1[:, 16:24], in_=K.sm[:, 24:32], func=AF.Exp, scale=-1.0), R=[K.sm], W=[K.e1])
            C.op("act", lambda e: e.activation(out=SM[:, 24:40], in_=K.e1[:, 0:16], func=AF.Ln, bias=1.0, scale=1.0), R=[K.e1], W=[SM])
            C.op("act", lambda e: e.activation(out=K.e1[:, 24:32], in_=K.e1[:, 16:24], func=AF.Ln, bias=1.0, scale=1.0), R=[K.e1], W=[K.e1])
            C.op("dve", lambda e: e.tensor_scalar(out=SM[:, 16:24], in0=K.e1[:, 24:32], scalar1=-1.0, scalar2=None, op0=ALU.mult), R=[K.e1], W=[SM])
            C.op("dve", lambda e: e.tensor_tensor(out=SM[:, 0:16], in0=SM[:, 24:40], in1=K.aneg[:, :], op=ALU.mult), R=[SM, K.aneg], W=[SM])
            C.op("dve", lambda e: e.tensor_copy(out=SM[:, 40:48], in_=K.sm[:, 16:24]), R=[K.sm], W=[SM])
            if "SM" in self.dbg and P.name == "ownF" and sc == 0 and i == 0:
                self.dump_now("SM", SM, [128, 48])
        if "cv" in self.dbg and P.name == "ownF" and sc == 0:
            self.dump_bf("cv", K.cv, K.cv[:, :, :].rearrange("p k n -> p (k n)"), [128, 12 * 512])
            self.dump_bf("cvq", K.cvq, K.cvq[:, :, :].rearrange("p k n -> p (k n)"), [128, 8 * 512])

    def dump_bf(self, name, buf, ap, shape):
        C = self.C
        o = self.outp("dbg_" + name, shape)
        n = shape[1]
        for c0 in range(0, n, 2048):
            c1 = min(n, c0 + 2048)
            t = C.sb([128, 2048], F32, "dmp")
            C.op("dve", lambda e: e.tensor_copy(out=t[:, 0:c1 - c0], in_=ap[:, c0:c1]), R=[buf], W=[t])
            C.dma(lambda e: e.dma_start(out=o.t[:, c0:c1], in_=t[:, 0:c1 - c0]), R=[t], W=[o])

    def scan_chunk(self, P, sc, i):
        C = self.C; K = self.K
        SKIP = os.environ.get('KSKIP', '').split(',')
        if 'scan' in SKIP:
            return
        tok = slice(i * 128, (i + 1) * 128)
        Uf = self.Lf if P.rev else self.Uf
        Ub = self.Lb if P.rev else self.Ub
        NEGM = self.NEGL if P.rev else self.NEGU
        SM = K.SM[i]
        full = P.full
        K.nchunk += 1
        par = K.nchunk % 2
        sl = self.dslot()
        o = sl.off
        self.mm(sl, sl.t[:, o:o + 24], Uf[:, :], SM[:, 0:24], R=[Uf, SM])
        self.mm(sl, sl.t[:, o + 24:o + 48], self.onesf[:, :], SM[:, 0:24], R=[self.onesf, SM])
        C.op("dve", lambda e: e.tensor_copy(out=K.cs[:, :], in_=sl.t[:, o:o + 48]), R=[sl], W=[K.cs])
        pt = self.tbank()
        for c in range(8):
            self.tr(pt, pt[:, c * 128:(c + 1) * 128], K.cv[:, c, tok], self.identb[:, :], R=[K.cv, self.identb])
        C.op("act", lambda e: e.activation(out=K.Xtok[:, :], in_=pt[:, :], func=AF.Copy), R=[pt], W=[K.Xtok])
        pt = self.tbank()
        for c in range(2):
            self.tr(pt, pt[:, c * 128:(c + 1) * 128], K.cv[:, 8 + c, tok], self.identb[:, :], R=[K.cv, self.identb])
        for c in range(4):
            self.tr(pt, pt[:, 256 + c * 128:256 + (c + 1) * 128], K.cvq[:, 4 + c, tok], self.identb[:, :], R=[K.cvq, self.identb])
        C.op("dve", lambda e: e.tensor_copy(out=K.BK[:, :], in_=pt[:, 0:768]), R=[pt], W=[K.BK])
        C.op("dve", lambda e: e.tensor_tensor(out=K.t16[:, :], in0=K.cs[:, 24:40], in1=K.cs[:, 0:16], op=ALU.subtract), R=[K.cs], W=[K.t16])
        C.op("act", lambda e: e.activation(out=K.t16[:, :], in_=K.t16[:, :], func=AF.Exp), R=[K.t16], W=[K.t16])
        C.op("dve", lambda e: e.tensor_tensor(out=K.wS[:, :], in0=K.t16[:, :], in1=SM[:, 24:40], op=ALU.mult), R=[K.t16, SM], W=[K.wS])
        X3 = K.Xtok[:, :].rearrange("p (h d) -> p h d", h=16)
        C.op("dve", lambda e: e.tensor_tensor(out=K.Xw[:, :].rearrange("p (h d) -> p h d", h=16), in0=X3, in1=bc(K.wS, 0, 16, 64), op=ALU.mult), R=[K.Xtok, K.wS], W=[K.Xw])
        C.op("act", lambda e: e.activation(out=K.exptot[:, :], in_=K.cs[:, 24:40], func=AF.Exp), R=[K.cs], W=[K.exptot])
        if full:
            C.op("act", lambda e: e.activation(out=K.expcum[:, :], in_=K.cs[:, 0:16], func=AF.Exp), R=[K.cs], W=[K.expcum])
            C.op("dve", lambda e: e.tensor_scalar(out=K.ncum[:, :], in0=K.cs[:, 0:16], scalar1=-1.0, scalar2=None, op0=ALU.mult), R=[K.cs], W=[K.ncum])
            C.op("pool", lambda e: e.tensor_tensor(out=K.Xdt[:, :].rearrange("p (h d) -> p h d", h=16), in0=X3, in1=bc(SM, 24, 16, 64), op=ALU.mult), R=[K.Xtok, SM], W=[K.Xdt])
            if P.dd == 0 and self.addD:
                C.op("pool", lambda e: e.tensor_tensor(out=K.XD[:, :].rearrange("p (h d) -> p h d", h=16), in0=X3, in1=bc(K.Dh, 0, 16, 64), op=ALU.mult), R=[K.Xtok, K.Dh], W=[K.XD])
            yout = K.yout[par]
            for g in range(2):
                sl = self.dslot(); o = sl.off
                self.mm(sl, sl.t[:, o:o + 128], K.cv[:, 8 + g, tok], K.cv[:, 10 + g, tok], R=[K.cv])
                C.op("act", lambda e: e.activation(out=K.CBT[g][:, :], in_=sl.t[:, o:o + 128], func=AF.Copy), R=[sl], W=[K.CBT[g]])
                self.mm(self.PYI, self.PYI[:, :], K.cv[:, 10 + g, tok], K.Sbf[g][:, :], R=[K.cv, K.Sbf[g]])
                addD = (P.dd == 0) and self.addD
                if addD:
                    self.mm(self.PY, self.PY[:, :], self.identb[:, :], K.XD[:, g * 512:(g + 1) * 512], R=[self.identb, K.XD], start=True, stop=False)
                pend = None
                for hh in range(8):
                    h = g * 8 + hh
                    sl = self.dslot(); o = sl.off
                    self.mm(sl, sl.t[:, o:o + 128], colbc(SM, h, 128), Uf[:, :], R=[SM, Uf], start=True, stop=False)
                    self.mm(sl, sl.t[:, o:o + 128], self.identf[:, :], NEGM[:, :], R=[self.identf, NEGM], start=False, stop=True)
                    E = K.E[hh % 2]; M = K.M[hh % 2]
                    C.op("act", lambda e: e.activation(out=E[:, :], in_=sl.t[:, o:o + 128], func=AF.Exp, bias=K.ncum[:, h:h + 1], scale=1.0), R=[sl, K.ncum], W=[E])
                    C.op("dve", lambda e: e.tensor_tensor(out=M[:, :], in0=E[:, :], in1=K.CBT[g][:, :], op=ALU.mult), R=[E, K.CBT[g]], W=[M])
                    if pend is not None:
                        pM, phh, ph = pend
                        self.mm(self.PY, self.PY[:, phh * 64:(phh + 1) * 64], pM[:, :], K.Xdt[:, ph * 64:(ph + 1) * 64], R=[pM, K.Xdt], start=(not addD), stop=True)
                    pend = (M, hh, h)
                pM, phh, ph = pend
                self.mm(self.PY, self.PY[:, phh * 64:(phh + 1) * 64], pM[:, :], K.Xdt[:, ph * 64:(ph + 1) * 64], R=[pM, K.Xdt], start=(not addD), stop=True)
                yg = yout[:, g * 512:(g + 1) * 512]
                C.op("dve", lambda e: e.tensor_tensor(out=yg.rearrange("p (h d) -> p h d", h=8), in0=self.PYI[:, :].rearrange("p (h d) -> p h d", h=8), in1=bc(K.expcum, g * 8, 8, 64), op=ALU.mult), R=[self.PYI, K.expcum], W=[yout])
                C.op("dve", lambda e: e.tensor_tensor(out=yg, in0=yg, in1=self.PY[:, :], op=ALU.add), R=[self.PY, yout], W=[yout])
            r0 = sc * P.sc_tok + i * 128
            yd = self.y_d[P.dd]
            C.dma(lambda e: e.dma_start(out=yd.t[r0:r0 + 128, :], in_=yout[:, :]), R=[yout], W=[yd])
        for g in range(2):
            pb = self.gbank()
            self.mm(pb, pb[:, 0:512], K.BK[:, g * 128:(g + 1) * 128], K.Xw[:, g * 512:(g + 1) * 512], R=[K.BK, K.Xw])
            S3 = K.S[g][:, :].rearrange("p (h d) -> p h d", h=8)
            C.op("dve", lambda e: e.tensor_tensor(out=S3, in0=S3, in1=bc(K.exptot, g * 8, 8, 64), op=ALU.mult), R=[K.S[g], K.exptot], W=[K.S[g]])
            C.op("dve", lambda e: e.tensor_tensor(out=K.S[g][:, :], in0=K.S[g][:, :], in1=pb[:, 0:512], op=ALU.add), R=[K.S[g], pb], W=[K.S[g]])
            C.op("act", lambda e: e.activation(out=K.Sbf[g][:, :], in_=K.S[g][:, :], func=AF.Copy), R=[K.S[g]], W=[K.Sbf[g]])
        if 'mlstm' in SKIP:
            return
        C.op("dve", lambda e: e.tensor_tensor(out=K.a8[:, :], in0=SM[:, 40:48], in1=K.cs[:, 16:24], op=ALU.subtract), R=[SM, K.cs], W=[K.a8])
        sl = self.dslot(); o = sl.off
        self.tr(sl, sl.t[0:8, o:o + 128], K.a8[:, 0:8], self.identf[:, :], R=[K.a8, self.identf])
        C.op("dve", lambda e: e.reduce_max(out=K.amax[:, :], in_=sl.t[0:8, o:o + 128], axis=AX.X), R=[sl], W=[K.amax])
        C.op("dve", lambda e: e.tensor_scalar(out=K.dg[:, :], in0=self.identf[0:8, 0:8], scalar1=K.amax[:, 0:1], scalar2=None, op0=ALU.mult), R=[self.identf, K.amax], W=[K.dg])
        sl = self.dslot(); o = sl.off
        self.mm(sl, sl.t[:, o:o + 8], self.onesf[0:8, :], K.dg[:, :], R=[self.onesf, K.dg])
        C.op("dve", lambda e: e.tensor_tensor(out=K.Mc[:, :], in0=K.mbc[:, :], in1=sl.t[:, o:o + 8], op=ALU.max), R=[K.mbc, sl], W=[K.Mc])
        C.op("dve", lambda e: e.tensor_tensor(out=K.cd[:, :], in0=K.mbc[:, :], in1=K.Mc[:, :], op=ALU.subtract), R=[K.mbc, K.Mc], W=[K.cd])
        C.op("act", lambda e: e.activation(out=K.cd[:, :], in_=K.cd[:, :], func=AF.Exp), R=[K.cd], W=[K.cd])
        C.op("dve", lambda e: e.tensor_tensor(out=K.mbc[:, :], in0=K.cs[:, 40:48], in1=K.Mc[:, :], op=ALU.add), R=[K.cs, K.Mc], W=[K.mbc])
        C.op("dve", lambda e: e.tensor_tensor(out=K.w8[:, :], in0=K.a8[:, :], in1=K.Mc[:, :], op=ALU.subtract), R=[K.a8, K.Mc], W=[K.w8])
        C.op("act", lambda e: e.activation(out=K.w8[:, :], in_=K.w8[:, :], func=AF.Exp), R=[K.w8], W=[K.w8])
        C.op("dve", lambda e: e.tensor_copy(out=K.w8b[:, :], in_=K.w8[:, :]), R=[K.w8], W=[K.w8b])
        C.op("pool", lambda e: e.tensor_tensor(out=K.Vw[:, :, :], in0=K.Vtok[i][:, :].rearrange("p (h d) -> p h d", h=8), in1=bc(K.w8, 0, 8, 128), op=ALU.mult), R=[K.Vtok[i], K.w8], W=[K.Vw])
        for hh in range(2):
            ps_ = slice(hh * 64, (hh + 1) * 64)
            cdv = bass.AP(K.cd.t, hh * 64 * 8 + hh, [[8, 64], [2, 4], [0, 128]])
            C.op("dve", lambda e: e.tensor_tensor(out=K.Cm[ps_, :, :], in0=K.Cm[ps_, :, :], in1=cdv, op=ALU.mult), R=[K.Cm, K.cd], W=[K.Cm])
            cdn = bass.AP(K.cd.t, hh * 64 * 8 + hh, [[8, 64], [2, 4]])
            C.op("dve", lambda e: e.tensor_tensor(out=K.nm[ps_, :], in0=K.nm[ps_, :], in1=cdn, op=ALU.mult), R=[K.nm, K.cd], W=[K.nm])
        if full:
            C.op("act", lambda e: e.activation(out=K.Cbf[:, :, :], in_=K.Cm[:, :, :], func=AF.Copy, scale=0.125), R=[K.Cm], W=[K.Cbf])
            C.op("act", lambda e: e.activation(out=K.nbf[:, :], in_=K.nm[:, :], func=AF.Copy, scale=0.125), R=[K.nm], W=[K.nbf])
            C.op("dve", lambda e: e.tensor_tensor(out=K.t8[:, :], in0=K.cs[:, 16:24], in1=K.Mc[:, :], op=ALU.add), R=[K.cs, K.Mc], W=[K.t8])
            C.op("act", lambda e: e.activation(out=K.fl[:, :], in_=K.t8[:, :], func=AF.Exp, scale=-1.0), R=[K.t8], W=[K.fl])
            sden = self.PYI; od = 0
            for h in range(8):
                c = h // 2; pr = (h % 2) * 64
                KT = K.cvq[pr:pr + 64, 4 + c, tok]; QT = K.cvq[pr:pr + 64, c, tok]
                sl = self.dslot(); o = sl.off
                self.mm(sl, sl.t[:, o:o + 128], KT, QT, R=[K.cvq])
                A = K.A[h]
                C.op("dve", lambda e: e.scalar_tensor_tensor(out=A[:, :], in0=sl.t[:, o:o + 128], scalar=0.125, in1=Ub[:, :], op0=ALU.mult, op1=ALU.mult), R=[sl, Ub], W=[A])
                self.mm(sden, sden.t[:, od + h:od + h + 1], A[:, :], K.w8b[:, h:h + 1], R=[A, K.w8b], start=True, stop=False)
                self.mm(sden, sden.t[:, od + h:od + h + 1], QT, K.nbf[pr:pr + 64, c:c + 1], R=[K.cvq, K.nbf], start=False, stop=True)
            C.op("dve", lambda e: e.tensor_copy(out=K.den[:, :], in_=sden.t[:, od:od + 8]), R=[sden], W=[K.den])
            C.op("dve", lambda e: e.scalar_tensor_tensor(out=K.den[:, :], in0=K.den[:, :], scalar=-1.0, in1=K.den[:, :], op0=ALU.mult, op1=ALU.max), R=[K.den], W=[K.den])
            C.op("dve", lambda e: e.tensor_tensor(out=K.den[:, :], in0=K.den[:, :], in1=K.fl[:, :], op=ALU.max), R=[K.den, K.fl], W=[K.den])
            C.op("dve", lambda e: e.reciprocal(out=K.rc[:, :], in_=K.den[:, :]), R=[K.den], W=[K.rc])
            hout = K.hout[par]
            for h in range(8):
                c = h // 2; pr = (h % 2) * 64
                QT = K.cvq[pr:pr + 64, c, tok]
                sl = self.dslot(); o = sl.off
                self.mm(sl, sl.t[:, o:o + 128], K.A[h][:, :], K.Vw[:, h, :], R=[K.A[h], K.Vw], start=True, stop=False)
                self.mm(sl, sl.t[:, o:o + 128], QT, K.Cbf[pr:pr + 64, c, :], R=[K.cvq, K.Cbf], start=False, stop=True)
                C.op("act", lambda e: e.activation(out=hout[:, h * 128:(h + 1) * 128], in_=sl.t[:, o:o + 128], func=AF.Copy, scale=K.rc[:, h:h + 1]), R=[sl, K.rc], W=[hout])
            r0 = sc * P.sc_tok + i * 128
            hd = self.h_d[P.dd]
            C.dma(lambda e: e.dma_start(out=hd.t[r0:r0 + 128, :], in_=hout[:, :]), R=[hout], W=[hd])
        if 'mupd' in SKIP:
            return
        sn2 = self.PY; on = 0
        for h in range(8):
            c = h // 2; hh = h % 2; ps_ = slice(hh * 64, (hh + 1) * 64)
            Kp = K.BK[:, 256 + c * 128:256 + (c + 1) * 128]
            sl = self.dslot(); o = sl.off
            self.mm(sl, sl.t[:, o:o + 128], Kp, K.Vw[:, h, :], R=[K.BK, K.Vw])
            C.op("dve", lambda e: e.tensor_tensor(out=K.Cm[ps_, c, :], in0=K.Cm[ps_, c, :], in1=sl.t[ps_, o:o + 128], op=ALU.add), R=[K.Cm, sl], W=[K.Cm])
            self.mm(sn2, sn2.t[:, on + h:on + h + 1], Kp, K.w8b[:, h:h + 1], R=[K.BK, K.w8b])
        for hh in range(2):
            ps_ = slice(hh * 64, (hh + 1) * 64)
            src = bass.AP(sn2.t, hh * 64 * 512 + on + hh, [[512, 64], [2, 4]])
            C.op("dve", lambda e: e.tensor_tensor(out=K.nm[ps_, :], in0=K.nm[ps_, :], in1=src, op=ALU.add), R=[K.nm, sn2], W=[K.nm])


    def load_w_bf(self, dst, dram, row0, nk, c0, c1, dcol0=0):
        C = self.C
        for k in range(nk):
            C.dma(lambda e: e.dma_start(out=dst[:, k, dcol0:dcol0 + (c1 - c0)], in_=dram.t[row0 + k * 128:row0 + (k + 1) * 128, c0:c1]), R=[dram], W=[dst], q="pool")

    def abank(self):
        self._a = (self._a + 1) % len(self.Gall)
        return self.Gall[self._a]

    def rms_rstd(self, src_ap, srcbuf, junk, ss, rstd, n):
        C = self.C
        C.op("pool", lambda e: e.memset(ss[:, :], 0.0), W=[ss])
        C.op("act", lambda e: e.activation(out=junk[:, :], in_=src_ap, func=AF.Square, accum_out=ss[:, :]), R=[srcbuf, ss], W=[junk, ss])
        C.op("act", lambda e: e.activation(out=rstd[:, :], in_=ss[:, :], func=AF.Sqrt, scale=1.0 / n, bias=self.epsb[:, :]), R=[ss, self.epsb], W=[rstd])
        C.op("dve", lambda e: e.reciprocal(out=rstd[:, :], in_=rstd[:, :]), R=[rstd], W=[rstd])

    def proj(self, lhsT_buf, W, wc0, n512, evac):
        for n in range(n512):
            pb = self.abank()
            for k in range(8):
                self.mm(pb, pb[:, 0:512], lhsT_buf[:, k, :], W[:, k, wc0 + n * 512:wc0 + (n + 1) * 512], R=[lhsT_buf, W], start=(k == 0), stop=(k == 7))
            evac(n, pb)

    def transp8(self, src, dstT):
        C = self.C
        pt = self.tbank()
        for k in range(8):
            self.tr(pt, pt[:, k * 128:(k + 1) * 128], src[:, k * 128:(k + 1) * 128], self.identb[:, :], R=[src, self.identb])
        C.op("act", lambda e: e.activation(out=dstT[:, :, :], in_=pt[:, :].rearrange("p (k n) -> p k n", k=8), func=AF.Copy), R=[pt], W=[dstT])

    def merge_pass(self, xo, w_in, vecs, w_so, w_mo, w_o):
        C = self.C
        self.Gall = self.G + [self.PY, self.PYI] + self.DS
        self._a = 0
        with Scope(C):
            Wz = C.sb([128, 8, 4096], BF16, "Wz")
            self.load_w_bf(Wz, w_in, 0, 8, 0, 1024, 0)
            self.load_w_bf(Wz, w_in, 0, 8, 4672, 7744, 1024)
            Wso = C.sb([128, 8, D], BF16, "Wso"); Wmo = C.sb([128, 8, D], BF16, "Wmo"); Wo = C.sb([128, 8, D], BF16, "Wo")
            self.load_w_bf(Wso, w_so, 0, 8, 0, D); self.load_w_bf(Wmo, w_mo, 0, 8, 0, D); self.load_w_bf(Wo, w_o, 0, 8, 0, D)
            gssd = C.sb([128, D], F32, "gssd"); gml = C.sb([128, D], F32, "gml"); gate = C.sb([128, D], F32, "gate")
            C.dma(lambda e: e.dma_start(out=gssd[:, :], in_=vecs.t[0:128, :]), R=[vecs], W=[gssd])
            C.dma(lambda e: e.dma_start(out=gml[:, :], in_=vecs.t[128:256, :]), R=[vecs], W=[gml])
            self.col_to_bcast(self.gcol, 0, gate)
            xt = C.sb([128, D], F32, "m_xt"); uT = C.sb([128, 8, 128], BF16, "m_uT")
            A1 = C.sb([128, D], F32, "A1"); A2 = C.sb([128, D], F32, "A2"); A3 = C.sb([128, D], F32, "A3"); A4 = C.sb([128, D], F32, "A4")
            Zz = C.sb([128, D], F32, "Zz"); Zo = C.sb([128, D], F32, "Zo"); Zg1 = C.sb([128, D], F32, "Zg1"); Zg2 = C.sb([128, D], F32, "Zg2"); junk = C.sb([128, D], BF16, "m_junk")
            yn = C.sb([128, D], BF16, "yn"); hn = C.sb([128, D], BF16, "hn"); nT = C.sb([128, 8, 128], BF16, "nT"); nT2 = C.sb([128, 8, 128], BF16, "nT2"); nT3 = C.sb([128, 8, 128], BF16, "nT3")
            mg = C.sb([128, D], BF16, "mg"); x1 = C.sb([128, D], F32, "x1t")
            ss = C.sb([128, 1], F32, "m_ss"); rstd = C.sb([128, 1], F32, "m_rstd"); ss8 = C.sb([128, 8], F32, "ss8")
            for t in range(32):
                r0 = t * 128
                sc, i = t // 4, t % 4
                C.dma(lambda e: e.dma_start(out=xt[:, :], in_=xo.t[r0:r0 + 128, :]), R=[xo], W=[xt])
                C.dma(lambda e: e.dma_start(out=uT[:, :, :], in_=self.uT_d.t[sc * 128:(sc + 1) * 128, :].rearrange("p (k n) -> p k n", k=8)[:, :, i * 128:(i + 1) * 128]), R=[self.uT_d], W=[uT])
                C.dma(lambda e: e.dma_start(out=A1[:, :], in_=self.y_d[0].t[r0:r0 + 128, :]), R=[self.y_d[0]], W=[A1])
                C.dma(lambda e: e.dma_start(out=A2[:, :], in_=self.y_d[1].t[r0:r0 + 128, :]), R=[self.y_d[1]], W=[A2])
                C.dma(lambda e: e.dma_start(out=A3[:, :], in_=self.h_d[0].t[r0:r0 + 128, :]), R=[self.h_d[0]], W=[A3])
                C.dma(lambda e: e.dma_start(out=A4[:, :], in_=self.h_d[1].t[r0:r0 + 128, :]), R=[self.h_d[1]], W=[A4])
                def actev(dst, func):
                    return lambda n, pb: C.op("act", lambda e: e.activation(out=dst[:, n * 512:(n + 1) * 512], in_=pb[:, 0:512], func=func), R=[pb], W=[dst])
                self.proj(uT, Wz, 0, 2, actev(Zz, AF.Silu))
                self.proj(uT, Wz, 1024, 2, actev(Zo, AF.Sigmoid))
                self.proj(uT, Wz, 2048, 2, actev(Zg1, AF.Sigmoid))
                self.proj(uT, Wz, 3072, 2, actev(Zg2, AF.Sigmoid))
                C.op("dve", lambda e: e.tensor_tensor(out=A1[:, :], in0=A1[:, :], in1=A2[:, :], op=ALU.add), R=[A1, A2], W=[A1])
                C.op("dve", lambda e: e.tensor_tensor(out=A1[:, :], in0=A1[:, :], in1=Zz[:, :], op=ALU.mult), R=[A1, Zz], W=[A1])
                self.rms_rstd(A1[:, :], A1, junk, ss, rstd, D)
                C.op("dve", lambda e: e.scalar_tensor_tensor(out=yn[:, :], in0=A1[:, :], scalar=rstd[:, 0:1], in1=gssd[:, :], op0=ALU.mult, op1=ALU.mult), R=[A1, rstd, gssd], W=[yn])
                self.transp8(yn, nT)
                C.op("dve", lambda e: e.tensor_tensor(out=A3[:, :], in0=A3[:, :], in1=A4[:, :], op=ALU.add), R=[A3, A4], W=[A3])
                C.op("pool", lambda e: e.tensor_tensor(out=A4[:, :], in0=A3[:, :], in1=A3[:, :], op=ALU.mult), R=[A3], W=[A4])
                C.op("dve", lambda e: e.tensor_reduce(out=ss8[:, :], in_=A4[:, :].rearrange("p (h d) -> p h d", h=8), axis=AX.X, op=ALU.add), R=[A4], W=[ss8])
                C.op("act", lambda e: e.activation(out=ss8[:, :], in_=ss8[:, :], func=AF.Sqrt, scale=1.0 / 128, bias=self.epsb[:, :]), R=[ss8, self.epsb], W=[ss8])
                C.op("dve", lambda e: e.reciprocal(out=ss8[:, :], in_=ss8[:, :]), R=[ss8], W=[ss8])
                C.op("dve", lambda e: e.tensor_tensor(out=A3[:, :].rearrange("p (h d) -> p h d", h=8), in0=A3[:, :].rearrange("p (h d) -> p h d", h=8), in1=bc(ss8, 0, 8, 128), op=ALU.mult), R=[A3, ss8], W=[A3])
                C.op("pool", lambda e: e.tensor_tensor(out=A3[:, :], in0=A3[:, :], in1=gml[:, :], op=ALU.mult), R=[A3, gml], W=[A3])
                C.op("dve", lambda e: e.tensor_tensor(out=hn[:, :], in0=A3[:, :], in1=Zo[:, :], op=ALU.mult), R=[A3, Zo], W=[hn])
                self.transp8(hn, nT2)
                self.proj(nT, Wso, 0, 2, lambda n, pb: C.op("dve", lambda e: e.tensor_tensor(out=A1[:, n * 512:(n + 1) * 512], in0=pb[:, 0:512], in1=Zg1[:, n * 512:(n + 1) * 512], op=ALU.mult), R=[pb, Zg1], W=[A1]))
                self.proj(nT2, Wmo, 0, 2, lambda n, pb: C.op("dve", lambda e: e.tensor_tensor(out=A2[:, n * 512:(n + 1) * 512], in0=pb[:, 0:512], in1=Zg2[:, n * 512:(n + 1) * 512], op=ALU.mult), R=[pb, Zg2], W=[A2]))
                C.op("dve", lambda e: e.tensor_tensor(out=mg[:, :], in0=A1[:, :], in1=A2[:, :], op=ALU.add), R=[A1, A2], W=[mg])
                self.transp8(mg, nT3)
                self.proj(nT3, Wo, 0, 2, lambda n, pb: C.op("dve", lambda e: e.tensor_tensor(out=x1[:, n * 512:(n + 1) * 512], in0=pb[:, 0:512], in1=gate[:, n * 512:(n + 1) * 512], op=ALU.mult), R=[pb, gate], W=[x1]))
                C.op("pool", lambda e: e.tensor_tensor(out=x1[:, :], in0=x1[:, :], in1=xt[:, :], op=ALU.add), R=[x1, xt], W=[x1])
                C.dma(lambda e: e.dma_start(out=self.x1_d.t[r0:r0 + 128, :], in_=x1[:, :]), R=[x1], W=[self.x1_d])

    def ffn_prep(self, vecs, router_w, router_b, sh_g, sh_u, sh_d):
        C = self.C
        with Scope(C):
            if self.Y_d is not None:
                zt = C.sb([128, 2048], F32, "zt")
                C.op("pool", lambda e: e.memset(zt[:, :], 0.0), W=[zt])
                for i in range(SEG * 8 // 256):
                    C.dma(lambda e: e.dma_start(out=self.Y_d.t[i * 256:(i + 1) * 256, :].rearrange("(p a) n -> p (a n)", p=128), in_=zt[:, :]), R=[zt], W=[])
            GS2 = C.sb([128, D], F32, "GS2"); SH2 = C.sb([128, D], F32, "SH2")
            self.col_to_bcast(self.cols, 32, GS2); self.col_to_bcast(self.cols, 40, SH2)
            rw = C.sb([128, 8, NEXP], F32, "rw"); rb = C.sb([128, NEXP], F32, "rb")
            C.dma(lambda e: e.dma_start(out=rw[:, :, :], in_=router_w.t.ap().rearrange("(k p) n -> p k n", p=128)), R=[router_w], W=[rw])
            C.dma(lambda e: e.dma_start(out=rb[:, :], in_=router_b.t[:, :]), R=[router_b], W=[rb])
            Wsgu = C.sb([128, 8, 512], BF16, "Wsgu"); Wsd = C.sb([128, 2, D], BF16, "Wsd")
            self.load_w_bf(Wsgu, sh_g, 0, 8, 0, 256, 0); self.load_w_bf(Wsgu, sh_u, 0, 8, 0, 256, 256); self.load_w_bf(Wsd, sh_d, 0, 2, 0, D)
            iotaE = C.sb([128, NEXP], F32, "iotaE")
            C.op("pool", lambda e: e.iota(iotaE[:, :], pattern=[[1, NEXP]], base=0, channel_multiplier=0, allow_small_or_imprecise_dtypes=True), W=[iotaE])
            C.op("dve", lambda e: e.tensor_scalar(out=iotaE[:, :], in0=iotaE[:, :], scalar1=float(CAP), scalar2=None, op0=ALU.mult), R=[iotaE], W=[iotaE])
            cnt = C.sb([128, NEXP], F32, "cnt")
            C.op("pool", lambda e: e.memset(cnt[:, :], 0.0), W=[cnt])
            SUF = C.sb([128, 2, NEXP], BF16, "SUF")
            C.op("pool", lambda e: e.memset(SUF[:, :, :], 0.0), W=[SUF])
            C.op("dve", lambda e: e.tensor_copy(out=SUF[:, 0, 0:128], in_=self.SUb[:, :]), R=[self.SUb, SUF], W=[SUF])
            C.op("dve", lambda e: e.tensor_copy(out=SUF[:, 0, 128:256], in_=self.onesb[:, :]), R=[self.onesb, SUF], W=[SUF])
            C.op("dve", lambda e: e.tensor_copy(out=SUF[:, 1, 128:256], in_=self.SUb[:, :]), R=[self.SUb, SUF], W=[SUF])
            jv = C.sb([128, 8, NEXP], F32, "jv")
            C.op("pool", lambda e: e.iota(jv[:, :, :], pattern=[[1, 8], [0, NEXP]], base=0, channel_multiplier=0, allow_small_or_imprecise_dtypes=True), W=[jv])
            emT = C.sb([128, 2, 128], BF16, "emT"); jr = C.sb([128, NEXP], F32, "jr")
            Li = C.sb([128, NEXP * CAP // 128, 4], F32, "Li")
            C.op("pool", lambda e: e.memset(Li[:, :, :], 0.0), W=[Li])
            C.op("pool", lambda e: e.memset(Li[:, :, 0:1], float(SEG)), R=[Li], W=[Li])
            C.op("pool", lambda e: e.memset(Li[:, :, 1:2], 1.0e9), R=[Li], W=[Li])
            C.dma(lambda e: e.dma_start(out=self.L_d.t.ap().rearrange("(p a) n -> p a n", p=128), in_=Li[:, :, :]), R=[Li], W=[self.L_d])
            zb = C.sb([128, D], BF16, "zb")
            C.op("pool", lambda e: e.memset(zb[:, :], 0.0), W=[zb])
            C.dma(lambda e: e.dma_start(out=self.u2_d.t[SEG:SEG + 128, :], in_=zb[:, :]), R=[zb], W=[self.u2_d])
            x1 = C.sb([128, D], F32, "f_x1"); junk = C.sb([128, D], BF16, "f_junk"); ss = C.sb([128, 1], F32, "f_ss"); rstd = C.sb([128, 1], F32, "f_rstd")
            u2 = C.sb([128, D], F32, "u2"); u2b = C.sb([128, D], BF16, "u2b")
            u2Tf = C.sb([128, 8, 128], F32, "u2Tf"); u2Tb = C.sb([128, 8, 128], BF16, "u2Tb")
            sc_ = C.sb([128, NEXP], F32, "scores"); gr = C.sb([128, NEXP], F32, "grouped"); sel = C.sb([128, NEXP], F32, "sel")
            m8g = C.sb([128, 8, 8], F32, "m8g"); gs = C.sb([128, 8], F32, "gs"); g8 = C.sb([128, 8], F32, "g8"); pen = C.sb([128, 8], F32, "pen")
            e8 = C.sb([128, 8], F32, "e8"); em = C.sb([128, NEXP], F32, "em"); emb = C.sb([128, NEXP], BF16, "emb")
            Wc = C.sb([128, NEXP], F32, "Wc"); wsum = C.sb([128, 1], F32, "wsum")
            pos = C.sb([128, NEXP], F32, "pos"); dst = C.sb([128, NEXP], F32, "dst"); ov = C.sb([128, NEXP], F32, "ov")
            oh = C.sb([128, 8, NEXP], F32, "oh"); tmp = C.sb([128, 8, NEXP], F32, "ohtmp")
            dj = C.sb([128, 8], F32, "dj"); dji = C.sb([128, 8], I32, "dji"); rowd = C.sb([128, 8, 4], F32, "rowd")
            hs = C.sb([128, 2, 128], BF16, "hs"); sg = C.sb([128, 256], F32, "sgs"); sho = C.sb([128, D], F32, "sho")
            for t in range(32):
                r0 = t * 128
                C.dma(lambda e: e.dma_start(out=x1[:, :], in_=self.x1_d.t[r0:r0 + 128, :]), R=[self.x1_d], W=[x1])
                self.rms_rstd(x1[:, :], x1, junk, ss, rstd, D)
                C.op("dve", lambda e: e.scalar_tensor_tensor(out=u2[:, :], in0=x1[:, :], scalar=rstd[:, 0:1], in1=GS2[:, :], op0=ALU.mult, op1=ALU.mult), R=[x1, rstd, GS2], W=[u2])
                C.op("pool", lambda e: e.tensor_tensor(out=u2[:, :], in0=u2[:, :], in1=SH2[:, :], op=ALU.add), R=[u2, SH2], W=[u2])
                C.op("act", lambda e: e.activation(out=u2b[:, :], in_=u2[:, :], func=AF.Copy), R=[u2], W=[u2b])
                C.dma(lambda e: e.dma_start(out=self.u2_d.t[r0:r0 + 128, :], in_=u2b[:, :]), R=[u2b], W=[self.u2_d])
                for half in range(2):
                    pb = self.abank()
                    for kk in range(4):
                        k = half * 4 + kk
                        self.tr(pb, pb[:, kk * 128:(kk + 1) * 128], u2[:, k * 128:(k + 1) * 128], self.identf[:, :], R=[u2, self.identf])
                    C.op("act", lambda e: e.activation(out=u2Tf[:, half * 4:(half + 1) * 4, :], in_=pb[:, 0:512].rearrange("p (k n) -> p k n", k=4), func=AF.Copy), R=[pb], W=[u2Tf])
                C.op("dve", lambda e: e.tensor_copy(out=u2Tb[:, :, :], in_=u2Tf[:, :, :]), R=[u2Tf], W=[u2Tb])
                pb = self.abank()
                for k in range(8):
                    self.mm(pb, pb[:, 0:NEXP], u2Tf[:, k, :], rw[:, k, :], R=[u2Tf, rw], start=(k == 0), stop=(k == 7))
                C.op("act", lambda e: e.activation(out=sc_[:, :], in_=pb[:, 0:NEXP], func=AF.Sigmoid), R=[pb], W=[sc_])
                C.op("dve", lambda e: e.tensor_tensor(out=gr[:, :], in0=sc_[:, :], in1=rb[:, :], op=ALU.add), R=[sc_, rb], W=[gr])
                for g in range(8):
                    C.op("dve", lambda e: e.max(out=m8g[:, g, :], in_=gr[:, g * 32:(g + 1) * 32]), R=[gr], W=[m8g])
                C.op("dve", lambda e: e.tensor_tensor(out=gs[:, :], in0=m8g[:, :, 0], in1=m8g[:, :, 1], op=ALU.add), R=[m8g], W=[gs])
                C.op("dve", lambda e: e.max(out=g8[:, :], in_=gs[:, :]), R=[gs], W=[g8])
                C.op("dve", lambda e: e.tensor_scalar(out=pen[:, :], in0=gs[:, :], scalar1=g8[:, 3:4], scalar2=None, op0=ALU.is_ge), R=[gs, g8], W=[pen])
                C.op("dve", lambda e: e.tensor_scalar(out=pen[:, :], in0=pen[:, :], scalar1=-1.0, scalar2=1.0e4, op0=ALU.add, op1=ALU.mult), R=[pen], W=[pen])
                C.op("dve", lambda e: e.tensor_tensor(out=sel[:, :].rearrange("p (g e) -> p g e", g=8), in0=gr[:, :].rearrange("p (g e) -> p g e", g=8), in1=bc(pen, 0, 8, 32), op=ALU.add), R=[gr, pen], W=[sel])
                C.op("dve", lambda e: e.max(out=e8[:, :], in_=sel[:, :]), R=[sel], W=[e8])
                C.op("dve", lambda e: e.tensor_scalar(out=em[:, :], in0=sel[:, :], scalar1=e8[:, 7:8], scalar2=None, op0=ALU.is_ge), R=[sel, e8], W=[em])
                C.op("act", lambda e: e.activation(out=emb[:, :], in_=em[:, :], func=AF.Copy), R=[em], W=[emb])
                C.op("dve", lambda e: e.tensor_tensor(out=Wc[:, :], in0=sc_[:, :], in1=em[:, :], op=ALU.mult), R=[sc_, em], W=[Wc])
                C.op("dve", lambda e: e.reduce_sum(out=wsum[:, :], in_=Wc[:, :], axis=AX.X), R=[Wc], W=[wsum])
                C.op("dve", lambda e: e.reciprocal(out=wsum[:, :], in_=wsum[:, :]), R=[wsum], W=[wsum])
                C.op("dve", lambda e: e.tensor_scalar(out=Wc[:, :], in0=Wc[:, :], scalar1=wsum[:, 0:1], scalar2=2.5, op0=ALU.mult, op1=ALU.mult), R=[Wc, wsum], W=[Wc])
                pb = self.abank()
                self.mm(pb, pb[:, 0:NEXP], self.SUb[:, :], emb[:, :], R=[self.SUb, emb])
                C.op("dve", lambda e: e.tensor_tensor(out=pos[:, :], in0=pb[:, 0:NEXP], in1=cnt[:, :], op=ALU.add), R=[pb, cnt], W=[pos])
                pb = self.abank()
                self.mm(pb, pb[:, 0:NEXP], self.onesb[:, :], emb[:, :], R=[self.onesb, emb])
                C.op("dve", lambda e: e.tensor_tensor(out=cnt[:, :], in0=cnt[:, :], in1=pb[:, 0:NEXP], op=ALU.add), R=[pb, cnt], W=[cnt])
                C.op("dve", lambda e: e.tensor_scalar(out=ov[:, :], in0=pos[:, :], scalar1=float(CAP), scalar2=1.0e7, op0=ALU.is_ge, op1=ALU.mult), R=[pos], W=[ov])
                C.op("dve", lambda e: e.tensor_tensor(out=dst[:, :], in0=pos[:, :], in1=iotaE[:, :], op=ALU.add), R=[pos, iotaE], W=[dst])
                C.op("dve", lambda e: e.tensor_tensor(out=dst[:, :], in0=dst[:, :], in1=ov[:, :], op=ALU.add), R=[dst, ov], W=[dst])
                sel_b = bass.AP(sel.t, 0, [[NEXP, 128], [0, 8], [1, NEXP]])
                dst_b = bass.AP(dst.t, 0, [[NEXP, 128], [0, 8], [1, NEXP]])
                wc_b = bass.AP(Wc.t, 0, [[NEXP, 128], [0, 8], [1, NEXP]])
                pt = self.tbank()
                for kc in range(2):
                    self.tr(pt, pt[:, kc * 128:(kc + 1) * 128], emb[:, kc * 128:(kc + 1) * 128], self.identb[:, :], R=[emb, self.identb])
                C.op("act", lambda e: e.activation(out=emT[:, :, :].rearrange("p a b -> p (a b)"), in_=pt[:, 0:256], func=AF.Copy), R=[pt], W=[emT])
                pb = self.abank()
                for kc in range(2):
                    self.mm(pb, pb[:, 0:NEXP], emT[:, kc, :], SUF[:, kc, :], R=[emT, SUF], start=(kc == 0), stop=(kc == 1))
                C.op("act", lambda e: e.activation(out=jr[:, :], in_=pb[:, 0:NEXP], func=AF.Copy), R=[pb], W=[jr])
                C.op("dve", lambda e: e.tensor_tensor(out=dst[:, :], in0=dst[:, :], in1=em[:, :], op=ALU.mult), R=[dst, em], W=[dst])
                jr_b = bass.AP(jr.t, 0, [[NEXP, 128], [0, 8], [1, NEXP]])
                C.op("dve", lambda e: e.tensor_tensor(out=oh[:, :, :], in0=jr_b, in1=jv[:, :, :], op=ALU.is_equal), R=[jr, jv], W=[oh])
                C.op("pool", lambda e: e.tensor_tensor(out=tmp[:, :, :], in0=oh[:, :, :], in1=dst_b, op=ALU.mult), R=[oh, dst], W=[tmp])
                C.op("dve", lambda e: e.tensor_reduce(out=dj[:, :], in_=tmp[:, :, :], axis=AX.X, op=ALU.add), R=[tmp], W=[dj])
                C.op("dve", lambda e: e.tensor_copy(out=dji[:, :], in_=dj[:, :]), R=[dj], W=[dji])
                C.op("pool", lambda e: e.tensor_tensor(out=tmp[:, :, :], in0=oh[:, :, :], in1=wc_b, op=ALU.mult), R=[oh, Wc, tmp], W=[tmp])
                C.op("dve", lambda e: e.tensor_reduce(out=rowd[:, :, 2], in_=tmp[:, :, :], axis=AX.X, op=ALU.add), R=[tmp], W=[rowd])
                C.op("pool", lambda e: e.iota(rowd[:, :, 0], pattern=[[0, 8]], base=r0, channel_multiplier=1, allow_small_or_imprecise_dtypes=True), R=[rowd], W=[rowd])
                C.op("pool", lambda e: e.iota(rowd[:, :, 1], pattern=[[1, 8]], base=r0 * 8, channel_multiplier=8, allow_small_or_imprecise_dtypes=True), R=[rowd], W=[rowd])
                C.op("pool", lambda e: e.memset(rowd[:, :, 3], 0.0), R=[rowd], W=[rowd])
                for j in range(8):
                    C.dma(lambda e: e.indirect_dma_start(out=self.L_d.t[:, :], out_offset=bass.IndirectOffsetOnAxis(ap=dji[:, j:j + 1], axis=0), in_=rowd[:, j, :], in_offset=None,
                                                         bounds_check=self.bnd["L"], oob_is_err=False), R=[rowd, dji, self.L_d], W=[], q="pool")
                pb = self.abank()
                for blk in range(4):
                    for k in range(8):
                        self.mm(pb, pb[:, blk * 128:(blk + 1) * 128], Wsgu[:, k, blk * 128:(blk + 1) * 128], u2Tb[:, k, :], R=[Wsgu, u2Tb], start=(k == 0), stop=(k == 7))
                C.op("act", lambda e: e.activation(out=sg[:, :], in_=pb[:, 0:256], func=AF.Silu), R=[pb], W=[sg])
                C.op("dve", lambda e: e.tensor_tensor(out=hs[:, :, :].rearrange("p a b -> p (a b)"), in0=sg[:, :], in1=pb[:, 256:512], op=ALU.mult), R=[sg, pb], W=[hs])
                for n in range(2):
                    pb = self.abank()
                    for fb in range(2):
                        self.mm(pb, pb[:, 0:512], hs[:, fb, :], Wsd[:, fb, n * 512:(n + 1) * 512], R=[hs, Wsd], start=(fb == 0), stop=(fb == 1))
                    C.op("act", lambda e: e.activation(out=sho[:, n * 512:(n + 1) * 512], in_=pb[:, 0:512], func=AF.Copy), R=[pb], W=[sho])
                C.dma(lambda e: e.dma_start(out=self.sh_d.t[r0:r0 + 128, :], in_=sho[:, :]), R=[sho], W=[self.sh_d])

    def experts(self, moe_g, moe_u, moe_d):
        C = self.C
        NB = CAP // 128
        with Scope(C):
            Wf = [C.sb([128, 6144], F32, "Wf%d" % i) for i in range(3)]
            Wgu = [C.sb([128, 8, 512], BF16, "Wgu%d" % i) for i in range(2)]
            Wd = [C.sb([128, 2, D], BF16, "Wd%d" % i) for i in range(2)]
            Lt = [C.sb([128, NB, 4], F32, "Lt%d" % i) for i in range(3)]
            Li = [C.sb([128, NB, 2], I32, "Lti%d" % i) for i in range(4)]
            Lw = [C.sb([128, NB], F32, "Lw%d" % i) for i in range(4)]
            xg = [[C.sb([128, D], BF16, "xg%d_%d" % (i, j)) for j in range(NB)] for i in range(2)]
            xgT = [C.sb([128, 8, CAP], BF16, "xgT%d" % i) for i in range(2)]
            sg = [C.sb([128, CAP], F32, "e_sg%d" % i) for i in range(2)]
            hb = [C.sb([128, 2, CAP], BF16, "e_hb%d" % i) for i in range(2)]
            yo = [[C.sb([128, D], F32, "yo%d_%d" % (i, j)) for j in range(NB)] for i in range(2)]

            def load(e_):
                b = e_ % 3
                C.dma(lambda e: e.dma_start(out=Wf[b][:, 0:2048].rearrange("p (k f) -> p k f", k=8), in_=moe_g.t[e_ * D:(e_ + 1) * D, :].rearrange("(p k) f -> p k f", k=8)), R=[moe_g], W=[Wf[b]])
                C.dma(lambda e: e.dma_start(out=Wf[b][:, 2048:4096].rearrange("p (k f) -> p k f", k=8), in_=moe_u.t[e_ * D:(e_ + 1) * D, :].rearrange("(p k) f -> p k f", k=8)), R=[moe_u], W=[Wf[b]])
                C.dma(lambda e: e.dma_start(out=Wf[b][:, 4096:6144].rearrange("p (k n) -> p k n", k=2), in_=moe_d.t[e_ * 256:(e_ + 1) * 256, :].rearrange("(k p) n -> p k n", p=128)), R=[moe_d], W=[Wf[b]])
                C.dma(lambda e: e.dma_start(out=Lt[b][:, :, :], in_=self.L_d.t[e_ * CAP:(e_ + 1) * CAP, :].rearrange("(a p) n -> p a n", p=128)), R=[self.L_d], W=[Lt[b]])

            def small(e_):
                b4 = e_ % 4; b3 = e_ % 3
                C.op("dve", lambda e: e.tensor_copy(out=Li[b4][:, :, :], in_=Lt[b3][:, :, 0:2]), R=[Lt[b3]], W=[Li[b4]])
                C.op("dve", lambda e: e.tensor_copy(out=Lw[b4][:, :], in_=Lt[b3][:, :, 2]), R=[Lt[b3]], W=[Lw[b4]])

            def castW(e_):
                b = e_ % 2; b3 = e_ % 3
                C.op("act", lambda e: e.activation(out=Wgu[b][:, :, 0:256], in_=Wf[b3][:, 0:2048].rearrange("p (k f) -> p k f", k=8), func=AF.Copy), R=[Wf[b3]], W=[Wgu[b]])
                C.op("act", lambda e: e.activation(out=Wgu[b][:, :, 256:512], in_=Wf[b3][:, 2048:4096].rearrange("p (k f) -> p k f", k=8), func=AF.Copy), R=[Wf[b3]], W=[Wgu[b]])
                C.op("act", lambda e: e.activation(out=Wd[b][:, :, :], in_=Wf[b3][:, 4096:6144].rearrange("p (k n) -> p k n", k=2), func=AF.Copy), R=[Wf[b3]], W=[Wd[b]])

            def gather(e_):
                b = e_ % 2
                for blk in range(NB):
                    C.dma(lambda e: e.indirect_dma_start(out=xg[b][blk][:, :], out_offset=None, in_=self.u2_d.t[:, :], in_offset=bass.IndirectOffsetOnAxis(ap=Li[e_ % 4][:, blk, 0:1], axis=0),
                                                         bounds_check=self.bnd["u2"], oob_is_err=False), R=[self.u2_d, Li[e_ % 4]], W=[xg[b][blk]], q="pool")

            def transp(e_):
                b = e_ % 2
                for blk in range(NB):
                    pt = self.tbank()
                    for k in range(8):
                        self.tr(pt, pt[:, k * 128:(k + 1) * 128], xg[b][blk][:, k:D:8], self.identb[:, :], R=[xg[b][blk], self.identb])
                    C.op("dve", lambda e: e.tensor_copy(out=xgT[b][:, :, blk * 128:(blk + 1) * 128], in_=pt[:, :].rearrange("p (k n) -> p k n", k=8)), R=[pt], W=[xgT[b]])

            load(0); load(1); load(2); small(0); gather(0); castW(0); transp(0)
            for e_ in range(NEXP):
                b = e_ % 2
                if e_ + 1 < NEXP:
                    small(e_ + 1); gather(e_ + 1)
                pbs = []
                for j in range(4):
                    pb = self.abank()
                    for k in range(8):
                        self.mm(pb, pb[:, 0:CAP], Wgu[b][:, k, j * 128:(j + 1) * 128], xgT[b][:, k, :], R=[Wgu[b], xgT[b]], start=(k == 0), stop=(k == 7))
                    pbs.append(pb)
                for fb in range(2):
                    C.op("act", lambda e: e.activation(out=sg[fb][:, :], in_=pbs[fb][:, 0:CAP], func=AF.Silu), R=[pbs[fb]], W=[sg[fb]])
                    C.op("dve", lambda e: e.tensor_tensor(out=hb[b][:, fb, :], in0=sg[fb][:, :], in1=pbs[2 + fb][:, 0:CAP], op=ALU.mult), R=[sg[fb], pbs[2 + fb]], W=[hb[b]])
                if e_ + 1 < NEXP:
                    castW(e_ + 1)
                    transp(e_ + 1)
                for blk in range(NB):
                    for n in range(2):
                        pb = self.abank()
                        for fb in range(2):
                            self.mm(pb, pb[:, 0:512], hb[b][:, fb, blk * 128:(blk + 1) * 128], Wd[b][:, fb, n * 512:(n + 1) * 512], R=[hb[b], Wd[b]], start=(fb == 0), stop=(fb == 1))
                        C.op("dve", lambda e: e.tensor_scalar(out=yo[b][blk][:, n * 512:(n + 1) * 512], in0=pb[:, 0:512], scalar1=Lw[e_ % 4][:, blk:blk + 1], scalar2=None, op0=ALU.mult), R=[pb, Lw[e_ % 4]], W=[yo[b][blk]])
                    C.dma(lambda e: e.indirect_dma_start(out=self.Y_d.t[:, :], out_offset=bass.IndirectOffsetOnAxis(ap=Li[e_ % 4][:, blk, 1:2], axis=0), in_=yo[b][blk][:, :], in_offset=None,
                                                         bounds_check=self.bnd["Y"], oob_is_err=False), R=[yo[b][blk], Li[e_ % 4], self.Y_d], W=[], q="pool")
                if e_ + 3 < NEXP:
                    load(e_ + 3)

    def final(self, vecs, out):
        C = self.C
        with Scope(C):
            gate = C.sb([128, D], F32, "fgate"); gfin = C.sb([128, D], F32, "gfin")
            self.col_to_bcast(self.gcol, 8, gate)
            C.dma(lambda e: e.dma_start(out=gfin[:, :], in_=vecs.t[256:384, :]), R=[vecs], W=[gfin])
            Yt = [C.sb([128, 8, D], F32, "Yt%d" % i) for i in range(2)]
            x1 = [C.sb([128, D], F32, "fx1%d" % i) for i in range(2)]; sh = [C.sb([128, D], F32, "fsh%d" % i) for i in range(2)]
            acc = C.sb([128, D], F32, "facc"); junk = C.sb([128, D], BF16, "fjunk"); ss = C.sb([128, 1], F32, "fss"); rstd = C.sb([128, 1], F32, "frstd")
            ot = [C.sb([128, D], F32, "fot%d" % i) for i in range(2)]
            for t in range(32):
                r0 = t * 128; b = t % 2
                C.dma(lambda e: e.dma_start(out=Yt[b][:, :, :], in_=self.Y_d.t[r0 * 8:(r0 + 128) * 8, :].rearrange("(p j) n -> p j n", j=8)), R=[self.Y_d], W=[Yt[b]])
                C.dma(lambda e: e.dma_start(out=x1[b][:, :], in_=self.x1_d.t[r0:r0 + 128, :]), R=[self.x1_d], W=[x1[b]])
                C.dma(lambda e: e.dma_start(out=sh[b][:, :], in_=self.sh_d.t[r0:r0 + 128, :]), R=[self.sh_d], W=[sh[b]])
                C.op("dve", lambda e: e.tensor_reduce(out=acc[:, :], in_=Yt[b][:, :, :].rearrange("p j n -> p n j"), axis=AX.X, op=ALU.add), R=[Yt[b]], W=[acc])
                C.op("pool", lambda e: e.tensor_tensor(out=acc[:, :], in0=acc[:, :], in1=sh[b][:, :], op=ALU.add), R=[acc, sh[b]], W=[acc])
                C.op("dve", lambda e: e.tensor_tensor(out=acc[:, :], in0=acc[:, :], in1=gate[:, :], op=ALU.mult), R=[acc, gate], W=[acc])
                C.op("pool", lambda e: e.tensor_tensor(out=acc[:, :], in0=acc[:, :], in1=x1[b][:, :], op=ALU.add), R=[acc, x1[b]], W=[acc])
                self.rms_rstd(acc[:, :], acc, junk, ss, rstd, D)
                C.op("dve", lambda e: e.scalar_tensor_tensor(out=ot[b][:, :], in0=acc[:, :], scalar=rstd[:, 0:1], in1=gfin[:, :], op0=ALU.mult, op1=ALU.mult), R=[acc, rstd, gfin], W=[ot[b]])
                C.dma(lambda e: e.dma_start(out=out.t[r0:r0 + 128, :], in_=ot[b][:, :]), R=[ot[b]], W=[out])


def col_layout(v):
    return np.ascontiguousarray(np.asarray(v).reshape(8, 128).T)


def rep128(v):
    v = np.asarray(v).reshape(1, -1)
    return np.ascontiguousarray(np.broadcast_to(v, (128, v.shape[1])))


def host_layout(inp):
    f32 = np.float32
    x = np.asarray(inp["x"], f32); ctx = np.asarray(inp["ctx"], f32)
    w_in = np.ascontiguousarray(np.asarray(inp["w_in"], f32)[0])
    wsc = w_in[:, 1024:1024 + WS]
    cxw = np.asarray(inp["conv_xbc_w"], f32)[0]; cxb = np.asarray(inp["conv_xbc_b"], f32)[0]
    cqw = np.asarray(inp["conv_qk_w"], f32)[0]; cqb = np.asarray(inp["conv_qk_b"], f32)[0]
    dtb = np.asarray(inp["ssd_dt_bias"], f32)[0]; alog = np.asarray(inp["ssd_a_log"], f32)[0]
    ib = np.asarray(inp["mlstm_i_bias"], f32)[0]; fb = np.asarray(inp["mlstm_f_bias"], f32)[0]

    def taps(w, nt, flip):
        ww = w[::-1] if flip else w
        return np.ascontiguousarray(ww.reshape(5, nt, 128).transpose(2, 1, 0).reshape(128, nt * 5))

    def bias(b, nt):
        return np.ascontiguousarray(b.reshape(nt, 128).T)

    def wl(d):
        w = wsc.copy()
        if d == 1:
            w[:, O_DT:O_DT + 16] = wsc[:, O_DT + 16:O_DT + 32]
            w[:, O_GT:O_GT + 16] = wsc[:, O_GT + 16:O_GT + 32]
        return w

    def smallset(d):
        return rep128(np.concatenate([dtb[d], alog[d], ib[d], fb[d]]))
    wl_d = [wl(0), wl(1)]
    shared = dict(
        ada_w=np.ascontiguousarray(np.asarray(inp["ada_w"], f32)[0]),
        ada_b=np.ascontiguousarray(np.asarray(inp["ada_b"], f32)[0].reshape(48, 128).T),
        gcols=np.concatenate([col_layout(inp["norm_mix_g"][0]), col_layout(inp["norm_ffn_g"][0])], axis=1).astype(f32),
        w_in=w_in,
        ssd_d=rep128(np.asarray(inp["ssd_d"], f32)[0]),
        vecs=np.concatenate([rep128(inp["ssd_norm_g"][0]), rep128(inp["mlstm_norm_g"][0]), rep128(inp["norm_final_g"])], axis=0).astype(f32),
        w_ssd_out=np.ascontiguousarray(np.asarray(inp["w_ssd_out"], f32)[0]),
        w_mlstm_out=np.ascontiguousarray(np.asarray(inp["w_mlstm_out"], f32)[0]),
        w_out=np.ascontiguousarray(np.asarray(inp["w_out"], f32)[0]),
        router_w=np.ascontiguousarray(np.asarray(inp["router_w"], f32)[0]),
        router_b=rep128(np.asarray(inp["router_bias"], f32)[0]),
        moe_w_gate=np.asarray(inp["moe_w_gate"], f32)[0].reshape(NEXP * D, 256),
        moe_w_up=np.asarray(inp["moe_w_up"], f32)[0].reshape(NEXP * D, 256),
        moe_w_down=np.asarray(inp["moe_w_down"], f32)[0].reshape(NEXP * 256, D),
        shared_w_gate=np.ascontiguousarray(np.asarray(inp["shared_w_gate"], f32)[0]),
        shared_w_up=np.ascontiguousarray(np.asarray(inp["shared_w_up"], f32)[0]),
        shared_w_down=np.ascontiguousarray(np.asarray(inp["shared_w_down"], f32)[0]),
    )
    maps = []
    for c in range(NCORE):
        b, s = c // 4, c % 4
        m = dict(shared)
        m["xo"] = np.ascontiguousarray(x[b, s * SEG:(s + 1) * SEG])
        pre = []; dirs = []
        for j in range(3):
            if j < s:
                pre.append(x[b, j * SEG:(j + 1) * SEG]); dirs.append(0)
            else:
                g = 3 - (j - s)
                pre.append(x[b, g * SEG:(g + 1) * SEG][::-1]); dirs.append(1)
        m["xpre"] = np.ascontiguousarray(np.concatenate(pre, axis=0))
        m["ctx2"] = np.ascontiguousarray(np.concatenate([ctx[b], ctx[b][::-1]], axis=0))
        cv = np.zeros((128, 16), f32)
        cv[:, 0::2] = col_layout(inp["c"][b]); cv[:, 1::2] = col_layout(inp["c_ctx"])
        m["cvec"] = cv
        sd = [0, 1] + dirs
        m["wlite"] = np.ascontiguousarray(np.concatenate([wl_d[d] for d in sd], axis=0))
        m["cwx"] = np.concatenate([taps(cxw, 12, d == 1) for d in sd] + [taps(cxw, 12, False)], axis=0)
        m["cbx"] = np.concatenate([bias(cxb, 12)] * 6, axis=0)
        m["cwq"] = np.concatenate([taps(cqw, 8, d == 1) for d in sd] + [taps(cqw, 8, False)], axis=0)
        m["cbq"] = np.concatenate([bias(cqb, 8)] * 6, axis=0)
        m["small"] = np.concatenate([smallset(d) for d in sd] + [smallset(0), smallset(1)], axis=0)
        fl = np.zeros((128, 8), f32)
        for j in range(3):
            fl[:, j] = 1.0 if dirs[j] == 0 else 0.0
            fl[:, 4 + j] = 1.0 - fl[:, j]
        m["flags"] = fl
        maps.append(m)
    return maps


def kernel(**inputs):
    maps = host_layout(inputs)
    prog = Prog()
    nc = prog.build()
    maps = [{k: v for k, v in m.items() if k in prog.ins} for m in maps]
    res = run_bass_kernel_spmd(nc, maps, core_ids=list(range(NCORE)))
    out = np.zeros((2, 4 * SEG, D), np.float32)
    for c in range(NCORE):
        b, s = c // 4, c % 4
        out[b, s * SEG:(s + 1) * SEG] = res.results[c]["out"]
    return out
```

```python
from contextlib import ExitStack
import os
import numpy as np
import concourse.bass as bass
import concourse.mybir as mybir
from concourse.bass_utils import run_bass_kernel_spmd

F32 = mybir.dt.float32
BF16 = mybir.dt.bfloat16
I32 = mybir.dt.int32
ALU = mybir.AluOpType
AF = mybir.ActivationFunctionType
AX = mybir.AxisListType

NCORE = 8
D = 1024
SEG = 4096
NSEGC = 32
CTXL = 256
EPS = 1e-6
BIG = 30000.0
NEXP = 256
CAP = 512
WS = 3648
O_XBC, O_DT, O_QK, O_V, O_GT = 0, 1536, 1568, 2592, 3616


class Buf:
    __slots__ = ("t", "w", "r", "name", "off", "excl")

    def __init__(self, t, name, off=0, excl=False):
        self.t = t
        self.name = name
        self.off = off
        self.excl = excl
        self.w = {}
        self.r = {}

    def __getitem__(self, idx):
        return self.t[idx]


class Ctx:
    ENG = ("pe", "dve", "act", "pool", "sp")
    SAME_WIN = 6

    def __init__(self, nc, es, n_dma_sems=48):
        self.nc = nc
        self.es_stack = [es]
        self.e = {"pe": nc.tensor, "dve": nc.vector, "act": nc.scalar, "pool": nc.gpsimd, "sp": nc.sync}
        self.sem = {k: es.enter_context(nc.semaphore("s_" + k)) for k in self.ENG}
        self.cnt = {k: 0 for k in self.ENG}
        self.seen = {k: {} for k in self.ENG}
        self.dsem = [es.enter_context(nc.semaphore("d%d" % i)) for i in range(n_dma_sems)]
        self.dcnt = [0] * n_dma_sems
        self.dnext = 0
        self.nid = 0
        self.n_inst = 0

    @property
    def es(self):
        return self.es_stack[-1]

    def sb(self, shape, dt=F32, name=None):
        self.nid += 1
        name = "%s_%d" % (name or "sb", self.nid)
        return Buf(self.es.enter_context(self.nc.sbuf_tensor(name, list(shape), dt)), name)

    def ps(self, shape, dt=F32, name=None):
        self.nid += 1
        name = "%s_%d" % (name or "ps", self.nid)
        return Buf(self.es.enter_context(self.nc.psum_tensor(name, list(shape), dt)), name, excl=True)

    def dram(self, shape, dt=F32, name=None):
        self.nid += 1
        name = name or ("dr_%d" % self.nid)
        return Buf(self.nc.dram_tensor(name, list(shape), dt, kind="Internal"), name)

    def _wait(self, eng, key, val):
        if self.seen[eng].get(key, 0) >= val:
            return
        if isinstance(key, int):
            self.e[eng].wait_ge(self.dsem[key], val)
        else:
            if key == eng and (eng == "pe" or val <= self.cnt[eng] - self.SAME_WIN):
                return
            self.e[eng].wait_ge(self.sem[key], val)
        self.seen[eng][key] = val

    def _deps(self, eng, R, W):
        for b in R:
            for k, v in b.w.items():
                self._wait(eng, k, v)
            if b.excl:
                for k, v in b.r.items():
                    if k != eng:
                        self._wait(eng, k, v)
        for b in W:
            for k, v in b.w.items():
                self._wait(eng, k, v)
            for k, v in b.r.items():
                self._wait(eng, k, v)

    def op(self, eng, fn, R=(), W=()):
        self._deps(eng, R, W)
        inst = fn(self.e[eng])
        self.cnt[eng] += 1
        idx = self.cnt[eng]
        inst.then_inc(self.sem[eng], 1)
        self.n_inst += 1
        for b in R:
            b.r[eng] = idx
        for b in W:
            b.w[eng] = idx
        return inst

    def dma(self, fn, R=(), W=(), q="sp"):
        k = self.dnext
        self.dnext = (self.dnext + 1) % len(self.dsem)
        if self.dcnt[k] > 0:
            self._wait(q, k, self.dcnt[k])
        self._deps(q, R, W)
        inst = fn(self.e[q])
        self.dcnt[k] += 16
        inst.then_inc(self.dsem[k], 16)
        self.n_inst += 1
        for b in R:
            b.r[k] = self.dcnt[k]
        for b in W:
            b.w[k] = self.dcnt[k]
        return inst

    def barrier(self):
        for eng in self.ENG:
            for k in self.ENG:
                if k != eng and self.cnt[k] > 0:
                    self._wait(eng, k, self.cnt[k])
            for k in range(len(self.dsem)):
                if self.dcnt[k] > 0:
                    self._wait(eng, k, self.dcnt[k])

    def finish(self, bufs, eng="sp"):
        for b in bufs:
            for k, v in b.w.items():
                self._wait(eng, k, v)


class Scope:
    def __init__(self, C):
        self.C = C

    def __enter__(self):
        self.es = ExitStack()
        self.es.__enter__()
        self.C.es_stack.append(self.es)
        return self

    def __exit__(self, *a):
        self.C.barrier()
        self.C.es_stack.pop()
        return self.es.__exit__(*a)


def bc(buf, col0, n, rep):
    t = buf.t
    Fsz = int(np.prod(t.shape[1:]))
    return bass.AP(t, col0, [[Fsz, t.shape[0]], [1, n], [0, rep]])


def bcp(buf, p0, npart, col0, n, rep):
    t = buf.t
    Fsz = int(np.prod(t.shape[1:]))
    return bass.AP(t, p0 * Fsz + col0, [[Fsz, npart], [1, n], [0, rep]])


def colbc(buf, col, rep):
    t = buf.t
    Fsz = int(np.prod(t.shape[1:]))
    return bass.AP(t, col, [[Fsz, t.shape[0]], [0, rep]])


class Prog:
    def __init__(self, dbg=None, stop_after=None, addD=True):
        self.addD = addD
        self.dbg = dbg or []
        self.stop_after = stop_after
        self.nc = bass.Bass("TRN2", target_bir_lowering=False)
        self.ins = {}
        self.outs = {}

    def inp(self, name, shape, dt=F32):
        b = Buf(self.nc.dram_tensor(name, list(shape), dt, kind="ExternalInput"), name)
        self.ins[name] = b
        return b

    def outp(self, name, shape, dt=F32):
        b = Buf(self.nc.dram_tensor(name, list(shape), dt, kind="ExternalOutput"), name)
        self.outs[name] = b
        return b

    def mm(self, ob, oap, lhsT, rhs, R, start=True, stop=True):
        self.C.op("pe", lambda e: e.matmul(oap, lhsT=lhsT, rhs=rhs, start=start, stop=stop), R=R, W=[ob])

    def tr(self, ob, oap, in_ap, ident_ap, R):
        self.C.op("pe", lambda e: e.transpose(out=oap, in_=in_ap, identity=ident_ap), R=R, W=[ob])

    def gbank(self):
        self._g = (self._g + 1) % len(self.G)
        return self.G[self._g]

    def tbank(self):
        self._t = (self._t + 1) % len(self.T)
        return self.T[self._t]

    def dslot(self):
        self._d = (self._d + 1) % len(self.DS)
        return self.DS[self._d]

    def dump(self, name, buf, ap, shape, dt=F32):
        if name in self.dbg:
            o = self.outp("dbg_" + name, shape, dt)
            self.C.dma(lambda e: e.dma_start(out=o.t.ap() if len(shape) == 0 else o.t[tuple(slice(None) for _ in shape)], in_=ap), R=[buf], W=[o])

    def build(self):
        nc = self.nc
        I = self.inp
        xo = I("xo", [SEG, D]); xpre = I("xpre", [3 * SEG, D]); ctx2 = I("ctx2", [2 * CTXL, D])
        cvec = I("cvec", [128, 16]); ada_w = I("ada_w", [D, 6 * D]); ada_b = I("ada_b", [128, 48])
        gcols = I("gcols", [128, 16])
        wlite = I("wlite", [5 * D, WS]); w_in = I("w_in", [D, 7744])
        cwx = I("cwx", [6 * 128, 60]); cbx = I("cbx", [6 * 128, 12]); cwq = I("cwq", [6 * 128, 40]); cbq = I("cbq", [6 * 128, 8])
        small = I("small", [7 * 128, 48]); flags = I("flags", [128, 8]); ssd_d = I("ssd_d", [128, 16])
        if self.stop_after is None or self.stop_after in ("merge", "route"):
            vecs = I("vecs", [3 * 128, D])
            w_so = I("w_ssd_out", [D, D]); w_mo = I("w_mlstm_out", [D, D]); w_o = I("w_out", [D, D])
            router_w = I("router_w", [D, NEXP]); router_b = I("router_b", [128, NEXP])
            sh_g = I("shared_w_gate", [D, 256]); sh_u = I("shared_w_up", [D, 256]); sh_d = I("shared_w_down", [256, D])
        if self.stop_after is None:
            moe_g = I("moe_w_gate", [NEXP * D, 256]); moe_u = I("moe_w_up", [NEXP * D, 256]); moe_d = I("moe_w_down", [NEXP * 256, D])
        out = self.outp("out", [SEG, D])

        with ExitStack() as es:
            C = self.C = Ctx(nc, es)
            self.bnd = {}
            for nm, val in (("L", NEXP * CAP - 1), ("u2", SEG + 127), ("Y", SEG * 8 - 1)):
                r = es.enter_context(nc.gpsimd.register("bnd_" + nm))
                nc.gpsimd.reg_mov(r, val)
                self.bnd[nm] = r
            self.build_consts()
            self.G = [C.ps([128, 512], F32, "G%d" % i) for i in range(2)]
            self.T = [C.ps([128, 1024], BF16, "T%d" % i) for i in range(2)]
            self.PY = C.ps([128, 512], F32, "PY"); self.PYI = C.ps([128, 512], F32, "PYI")
            self.DS = [C.ps([128, 512], F32, "DS%d" % i) for i in range(2)]
            self._g = self._t = self._d = 0
            self.y_d = [C.dram([SEG, D], F32, "y_d%d" % d) for d in range(2)]
            self.h_d = [C.dram([SEG, D], F32, "h_d%d" % d) for d in range(2)]
            later = self.stop_after is None or self.stop_after in ("merge", "route")
            self.uT_d = C.dram([8 * 128, 4096], BF16, "uT_d") if later else None
            self.x1_d = C.dram([SEG, D], F32, "x1_d")
            self.sh_d = C.dram([SEG, D], F32, "sh_d")
            self.u2_d = C.dram([SEG + 128, D], BF16, "u2_d")
            self.L_d = C.dram([NEXP * CAP, 4], F32, "L_d")
            self.Y_d = C.dram([SEG * 8, D], F32, "Y_d") if self.stop_after is None else None

            self.adaln(cvec, ada_w, ada_b, gcols)
            if self.stop_after == "adaln":
                return self.end()
            self.mixer_scans(xo, xpre, ctx2, wlite, w_in, cwx, cbx, cwq, cbq, small, flags, ssd_d)
            if self.stop_after == "ctx":
                return self.end()
            if self.stop_after == "scans":
                for d in range(2):
                    if "y_d" in self.dbg:
                        self.copy_dram(self.y_d[d], self.outp("dbg_y_d%d" % d, [SEG, D]))
                        self.copy_dram(self.h_d[d], self.outp("dbg_h_d%d" % d, [SEG, D]))
                return self.end()
            self.merge_pass(xo, w_in, vecs, w_so, w_mo, w_o)
            if self.stop_after == "merge":
                self.copy_dram(self.x1_d, self.outp("dbg_x1", [SEG, D]))
                return self.end()
            self.ffn_prep(vecs, router_w, router_b, sh_g, sh_u, sh_d)
            if self.stop_after == "route":
                self.copy_dram(self.x1_d, self.outp("dbg_x1", [SEG, D]))
                self.copy_dram(self.sh_d, self.outp("dbg_sh", [SEG, D]))
                self.copy_dram(self.L_d, self.outp("dbg_L", [NEXP * CAP, 4]), rows=0, flat=(128, NEXP * CAP * 4 // 128))
                return self.end()
            self.experts(moe_g, moe_u, moe_d)
            self.final(vecs, out)
            return self.end()

    def copy_dram(self, src, dst, rows=SEG, flat=None):
        C = self.C
        if flat is not None:
            with Scope(C):
                p, f = flat
                t = C.sb([p, f], F32, "cpf")
                C.dma(lambda e: e.dma_start(out=t[:, :], in_=src.t.ap().rearrange("(p a) n -> p (a n)", p=p)), R=[src], W=[t])
                C.dma(lambda e: e.dma_start(out=dst.t.ap().rearrange("(p a) n -> p (a n)", p=p), in_=t[:, :]), R=[t], W=[dst])
            return
        with Scope(C):
            tl = [C.sb([128, 4, D], F32, "cp") for _ in range(2)]
            for i in range(rows // 512):
                t = tl[i % 2]
                C.dma(lambda e: e.dma_start(out=t[:, :, :], in_=src.t[i * 512:(i + 1) * 512, :].rearrange("(a p) n -> p a n", p=128)), R=[src], W=[t])
                C.dma(lambda e: e.dma_start(out=dst.t[i * 512:(i + 1) * 512, :].rearrange("(a p) n -> p a n", p=128), in_=t[:, :, :]), R=[t], W=[dst])

    def end(self):
        self.C.finish(list(self.outs.values()))
        self.C.barrier()
        return self.nc

    def build_consts(self):
        C = self.C

        def tri(name, cm, pat, op, val=1.0, fill=0.0):
            t = C.sb([128, 128], F32, name)
            C.op("pool", lambda e: e.memset(t[:, :], val), W=[t])
            C.op("pool", lambda e: e.affine_select(out=t[:, :], in_=t[:, :], pattern=[[pat, 128]], compare_op=op, fill=fill, base=0, channel_multiplier=cm), R=[t], W=[t])
            return t

        self.identf = tri("identf", 1, -1, ALU.is_equal)
        self.Uf = tri("Uf", -1, 1, ALU.is_ge)
        self.Lf = tri("Lf", 1, -1, ALU.is_ge)
        self.SUf = tri("SUf", -1, 1, ALU.is_gt)
        self.NEGU = tri("NEGU", 1, -1, ALU.is_gt, val=-BIG)
        self.NEGL = tri("NEGL", -1, 1, ALU.is_gt, val=-BIG)
        self.onesf = C.sb([128, 128], F32, "onesf")
        C.op("pool", lambda e: e.memset(self.onesf[:, :], 1.0), W=[self.onesf])
        self.epsb = C.sb([128, 1], F32, "epsb")
        C.op("pool", lambda e: e.memset(self.epsb[:, :], EPS), W=[self.epsb])

        def tobf(src, name):
            t = C.sb([128, 128], BF16, name)
            C.op("dve", lambda e: e.tensor_copy(out=t[:, :], in_=src[:, :]), R=[src], W=[t])
            return t
        self.identb = tobf(self.identf, "identb")
        self.Ub = tobf(self.Uf, "Ub"); self.Lb = tobf(self.Lf, "Lb"); self.SUb = tobf(self.SUf, "SUb")
        self.onesb = tobf(self.onesf, "onesb")

    def col_to_bcast(self, colbuf, c0, dst):
        C = self.C
        for half in range(2):
            pb = self.gbank()
            for jj in range(4):
                j = half * 4 + jj
                self.mm(pb, pb[:, jj * 128:(jj + 1) * 128], colbc(colbuf, c0 + j, 128), self.identf[:, :], R=[colbuf, self.identf])
            C.op("act", lambda e: e.activation(out=dst[:, half * 512:(half + 1) * 512], in_=pb[:, 0:512], func=AF.Copy), R=[pb], W=[dst])

    def adaln(self, cvec, ada_w, ada_b, gcols):
        C = self.C
        self.mod = C.sb([128, 96], F32, "mod")
        self.gc = C.sb([128, 16], F32, "gc")
        C.dma(lambda e: e.dma_start(out=self.gc[:, :], in_=gcols.t[:, :]), R=[gcols], W=[self.gc])
        with Scope(C):
            cv = C.sb([128, 16], F32, "cv"); sg = C.sb([128, 16], F32, "sg"); sc = C.sb([128, 16], F32, "sc")
            ab = C.sb([128, 48], F32, "ab")
            C.dma(lambda e: e.dma_start(out=cv[:, :], in_=cvec.t[:, :]), R=[cvec], W=[cv])
            C.dma(lambda e: e.dma_start(out=ab[:, :], in_=ada_b.t[:, :]), R=[ada_b], W=[ab])
            C.op("act", lambda e: e.activation(out=sg[:, :], in_=cv[:, :], func=AF.Sigmoid), R=[cv], W=[sg])
            C.op("dve", lambda e: e.tensor_tensor(out=sc[:, :], in0=cv[:, :], in1=sg[:, :], op=ALU.mult), R=[cv, sg], W=[sc])
            wt = [C.sb([128, 6 * D], F32, "adaw%d" % i) for i in range(2)]
            macc = C.sb([128, 96], F32, "macc")
            C.op("dve", lambda e: e.tensor_copy(out=macc[:, :].rearrange("p (j w) -> p j w", w=2), in_=bc(ab, 0, 48, 2)), R=[ab], W=[macc])
            for k in range(8):
                w = wt[k % 2]
                pm = self.gbank()
                for hh in range(2):
                    C.dma(lambda e: e.dma_start(out=w[:, hh * 3072:(hh + 1) * 3072], in_=ada_w.t[k * 128:(k + 1) * 128, hh * 3072:(hh + 1) * 3072]), R=[ada_w], W=[w])
                for j in range(48):
                    self.mm(pm, pm[:, 2 * j:2 * j + 2], w[:, j * 128:(j + 1) * 128], sc[:, 2 * k:2 * k + 2], R=[w, sc], start=True, stop=True)
                C.op("dve", lambda e: e.tensor_tensor(out=macc[:, :], in0=macc[:, :], in1=pm[:, 0:96], op=ALU.add), R=[pm, macc], W=[macc])
            C.op("dve", lambda e: e.tensor_copy(out=self.mod[:, :], in_=macc[:, :]), R=[macc], W=[self.mod])
        if "mod" in self.dbg:
            self.dump_now("mod", self.mod, [128, 96])
        mod3 = self.mod[:, :].rearrange("p (j w) -> p j w", w=2)
        self.cols = C.sb([128, 48], F32, "cols")
        cols = self.cols

        def gs(dst0, scale_j0, which, g0):
            C.op("dve", lambda e: e.scalar_tensor_tensor(out=cols[:, dst0:dst0 + 8], in0=mod3[:, scale_j0:scale_j0 + 8, which], scalar=1.0, in1=self.gc[:, g0:g0 + 8], op0=ALU.add, op1=ALU.mult), R=[self.mod, self.gc], W=[cols])

        def cp(dst0, j0, which):
            C.op("dve", lambda e: e.tensor_copy(out=cols[:, dst0:dst0 + 8], in_=mod3[:, j0:j0 + 8, which]), R=[self.mod], W=[cols])
        gs(0, 8, 0, 0); cp(8, 0, 0); gs(16, 8, 1, 0); cp(24, 0, 1); gs(32, 32, 0, 8); cp(40, 24, 0)
        self.gcol = C.sb([128, 16], F32, "gcol")
        C.op("dve", lambda e: e.tensor_copy(out=self.gcol[:, 0:8], in_=mod3[:, 16:24, 0]), R=[self.mod], W=[self.gcol])
        C.op("dve", lambda e: e.tensor_copy(out=self.gcol[:, 8:16], in_=mod3[:, 40:48, 0]), R=[self.mod], W=[self.gcol])

    def mixer_scans(self, xo, xpre, ctx2, wlite, w_in, cwx, cbx, cwq, cbq, small, flags, ssd_d):
        C = self.C
        with Scope(C):
            K = self.K = type("K", (), {})()
            K.GS = C.sb([128, D], F32, "GS"); K.SH = C.sb([128, D], F32, "SH")
            K.W = C.sb([128, 8, WS], BF16, "Wscan")
            K.cwx = C.sb([128, 60], F32, "cwx"); K.cbx = C.sb([128, 12], F32, "cbx"); K.cwq = C.sb([128, 40], F32, "cwq"); K.cbq = C.sb([128, 8], F32, "cbq")
            K.small = C.sb([128, 48], F32, "small"); K.aneg = C.sb([128, 16], F32, "aneg")
            K.flags = C.sb([128, 8], F32, "flags"); K.Dh = C.sb([128, 16], F32, "Dh")
            C.dma(lambda e: e.dma_start(out=K.flags[:, :], in_=flags.t[:, :]), R=[flags], W=[K.flags])
            C.dma(lambda e: e.dma_start(out=K.Dh[:, :], in_=ssd_d.t[:, :]), R=[ssd_d], W=[K.Dh])
            K.xt = [C.sb([128, D], F32, "xt%d" % i) for i in range(2)]
            K.junk = C.sb([128, D], BF16, "junk"); K.xm = C.sb([128, D], F32, "xm"); K.xn = C.sb([128, D], BF16, "xn")
            K.ss = C.sb([128, 1], F32, "ss"); K.rstd = C.sb([128, 1], F32, "rstd")
            K.uT = C.sb([128, 8, 512], BF16, "uT")
            K.cv = C.sb([128, 12, 512], BF16, "cv"); K.cvq = C.sb([128, 8, 512], BF16, "cvq")
            K.acc = [C.sb([128, 512], F32, "acc%d" % i) for i in range(4)]
            K.raw = [C.sb([128, 512], F32, "raw%d" % i) for i in range(4)]
            K.nconv = 0
            K.Vtok = [C.sb([128, D], BF16, "Vtok%d" % i) for i in range(4)]
            K.SM = [C.sb([128, 48], F32, "SM%d" % i) for i in range(4)]
            K.sm = C.sb([128, 32], F32, "sm"); K.e1 = C.sb([128, 32], F32, "e1")
            K.S = [C.sb([128, 512], F32, "S%d" % g) for g in range(2)]
            K.Sbf = [C.sb([128, 512], BF16, "Sbf%d" % g) for g in range(2)]
            K.Cm = C.sb([128, 4, 128], F32, "Cm"); K.nm = C.sb([128, 4], F32, "nm"); K.mbc = C.sb([128, 8], F32, "mbc")
            K.Cbf = C.sb([128, 4, 128], BF16, "Cbf"); K.nbf = C.sb([128, 4], BF16, "nbf")
            K.sav = []
            for d in range(2):
                K.sav.append(dict(S=[C.sb([128, 512], F32, "SS%d%d" % (d, g)) for g in range(2)], Cm=C.sb([128, 512], F32, "SCm%d" % d),
                                  nm=C.sb([128, 4], F32, "Snm%d" % d), mbc=C.sb([128, 8], F32, "Smbc%d" % d)))
            K.cs = C.sb([128, 48], F32, "cs"); K.Xtok = C.sb([128, D], BF16, "Xtok"); K.BK = C.sb([128, 768], BF16, "BK")
            K.t16 = C.sb([128, 16], F32, "t16"); K.wS = C.sb([128, 16], F32, "wS"); K.expcum = C.sb([128, 16], F32, "expcum"); K.ncum = C.sb([128, 16], F32, "ncum")
            K.exptot = C.sb([128, 16], F32, "exptot")
            K.Xw = C.sb([128, D], BF16, "Xw"); K.Xdt = C.sb([128, D], BF16, "Xdt"); K.XD = C.sb([128, D], BF16, "XD")
            K.CBT = [C.sb([128, 128], BF16, "CBT%d" % g) for g in range(2)]
            K.E = [C.sb([128, 128], BF16, "E%d" % i) for i in range(2)]; K.M = [C.sb([128, 128], BF16, "M%d" % i) for i in range(2)]
            K.yout = [C.sb([128, D], F32, "yout%d" % i) for i in range(2)]; K.hout = [C.sb([128, D], F32, "hout%d" % i) for i in range(2)]
            K.a8 = C.sb([128, 8], F32, "a8"); K.amax = C.sb([8, 1], F32, "amax"); K.dg = C.sb([8, 8], F32, "dg")
            K.Mc = C.sb([128, 8], F32, "Mc"); K.cd = C.sb([128, 8], F32, "cd"); K.w8 = C.sb([128, 8], F32, "w8"); K.w8b = C.sb([128, 8], BF16, "w8b")
            K.t8 = C.sb([128, 8], F32, "t8"); K.fl = C.sb([128, 8], F32, "fl"); K.den = C.sb([128, 8], F32, "den"); K.rc = C.sb([128, 8], F32, "rc")
            K.Vw = C.sb([128, 8, 128], BF16, "Vw"); K.A = [C.sb([128, 128], BF16, "A%d" % h) for h in range(8)]
            K.nchunk = 0

            def P(**kw):
                return type("P", (), kw)()
            passes = [
                P(name="ctxF", src=ctx2, row0=0, n_sc=1, sc_tok=256, rows=256, wsrc=(wlite, 0), slot=0, sset=0, kind="ctx", full=False, rev=False, dd=0, init="zero", save=0, flag=None),
                P(name="ctxB", src=ctx2, row0=CTXL, n_sc=1, sc_tok=256, rows=256, wsrc=(wlite, 1), slot=1, sset=1, kind="ctx", full=False, rev=False, dd=0, init="zero", save=1, flag=None),
            ]
            for j in range(3):
                passes.append(P(name="pre%d" % j, src=xpre, row0=j * SEG, n_sc=8, sc_tok=512, rows=64, wsrc=(wlite, 2 + j), slot=2 + j, sset=2 + j, kind="lat", full=False, rev=False, dd=0, init="blend", save="blend", flag=j))
            passes.append(P(name="ownF", src=xo, row0=0, n_sc=8, sc_tok=512, rows=64, wsrc=(w_in, None), slot=5, sset=5, kind="lat", full=True, rev=False, dd=0, init=0, save=None, flag=None))
            passes.append(P(name="ownB", src=xo, row0=0, n_sc=8, sc_tok=512, rows=64, wsrc=(w_in, None), slot=5, sset=6, kind="lat", full=True, rev=True, dd=1, init=1, save=None, flag=None))
            if self.stop_after == "ctx":
                passes = passes[:2]
            cur_kind = None
            for Pp in passes:
                if Pp.kind != cur_kind:
                    cur_kind = Pp.kind
                    o = 0 if Pp.kind == "lat" else 16
                    self.col_to_bcast(self.cols, o, K.GS)
                    self.col_to_bcast(self.cols, o + 8, K.SH)
                self.run_pass(Pp, cwx, cbx, cwq, cbq, small)
            if "ctxstate" in self.dbg:
                for d in range(2):
                    for g in range(2):
                        self.dump_now("S%d%d" % (d, g), K.sav[d]["S"][g], [128, 512])
                    self.dump_now("Cm%d" % d, K.sav[d]["Cm"], [128, 512])
                    self.dump_now("mbc%d" % d, K.sav[d]["mbc"], [128, 8])
                    self.dump_now("nm%d" % d, K.sav[d]["nm"], [128, 4])

    def dump_now(self, name, buf, shape):
        o = self.outp("dbg_" + name, shape)
        self.C.dma(lambda e: e.dma_start(out=o.t[:, :], in_=buf[:, :]), R=[buf], W=[o])

    def run_pass(self, P, cwx, cbx, cwq, cbq, small):
        C = self.C; K = self.K
        wsrc, wi = P.wsrc
        for k in range(8):
            if wi is None:
                src_ap = wsrc.t[k * 128:(k + 1) * 128, 1024:1024 + WS]
            else:
                src_ap = wsrc.t[wi * D + k * 128: wi * D + (k + 1) * 128, :]
            C.dma(lambda e: e.dma_start(out=K.W[:, k, :], in_=src_ap), R=[wsrc], W=[K.W], q="pool")
        s = P.slot
        C.dma(lambda e: e.dma_start(out=K.cwx[:, :], in_=cwx.t[s * 128:(s + 1) * 128, :]), R=[cwx], W=[K.cwx])
        C.dma(lambda e: e.dma_start(out=K.cbx[:, :], in_=cbx.t[s * 128:(s + 1) * 128, :]), R=[cbx], W=[K.cbx])
        C.dma(lambda e: e.dma_start(out=K.cwq[:, :], in_=cwq.t[s * 128:(s + 1) * 128, :]), R=[cwq], W=[K.cwq])
        C.dma(lambda e: e.dma_start(out=K.cbq[:, :], in_=cbq.t[s * 128:(s + 1) * 128, :]), R=[cbq], W=[K.cbq])
        C.dma(lambda e: e.dma_start(out=K.small[:, :], in_=small.t[P.sset * 128:(P.sset + 1) * 128, :]), R=[small], W=[K.small])
        C.op("act", lambda e: e.activation(out=K.aneg[:, :], in_=K.small[:, 16:32], func=AF.Exp), R=[K.small], W=[K.aneg])
        C.op("dve", lambda e: e.tensor_scalar(out=K.aneg[:, :], in0=K.aneg[:, :], scalar1=-1.0, scalar2=None, op0=ALU.mult), R=[K.aneg], W=[K.aneg])
        st = [(K.S[0], lambda d: K.sav[d]["S"][0], 128), (K.S[1], lambda d: K.sav[d]["S"][1], 128), (K.Cm, lambda d: K.sav[d]["Cm"], 128),
              (K.nm, lambda d: K.sav[d]["nm"], 128), (K.mbc, lambda d: K.sav[d]["mbc"], 128)]

        def flat(b):
            return b[:, :, :].rearrange("p a b -> p (a b)") if len(b.t.shape) == 3 else b[:, :]
        if P.init == "zero":
            for cur, _, _ in st:
                C.op("pool", lambda e: e.memset(flat(cur), 0.0), W=[cur])
        elif P.init == "blend":
            f = K.flags[:, P.flag:P.flag + 1]; nf = K.flags[:, 4 + P.flag:5 + P.flag]
            for cur, sv, _ in st:
                a = sv(0); b = sv(1)
                C.op("dve", lambda e: e.tensor_scalar(out=flat(cur), in0=flat(a), scalar1=f, scalar2=None, op0=ALU.mult), R=[a, K.flags], W=[cur])
                C.op("dve", lambda e: e.scalar_tensor_tensor(out=flat(cur), in0=flat(b), scalar=nf, in1=flat(cur), op0=ALU.mult, op1=ALU.add), R=[b, K.flags, cur], W=[cur])
        else:
            for cur, sv, _ in st:
                a = sv(P.init)
                C.op("dve", lambda e: e.tensor_copy(out=flat(cur), in_=flat(a)), R=[a], W=[cur])
        for g in range(2):
            C.op("act", lambda e: e.activation(out=K.Sbf[g][:, :], in_=K.S[g][:, :], func=AF.Copy), R=[K.S[g]], W=[K.Sbf[g]])
        scs = list(range(P.n_sc))
        if P.rev:
            scs = scs[::-1]
        for sc in scs:
            self.prep_sc(P, sc)
            tiles = list(range(P.sc_tok // 128))
            if P.rev:
                tiles = tiles[::-1]
            for i in tiles:
                self.scan_chunk(P, sc, i)
        if P.save == "blend":
            f = K.flags[:, P.flag:P.flag + 1]; nf = K.flags[:, 4 + P.flag:5 + P.flag]
            for cur, sv, _ in st:
                a = sv(0); b = sv(1)
                C.op("dve", lambda e: e.tensor_scalar(out=flat(a), in0=flat(a), scalar1=nf, scalar2=None, op0=ALU.mult), R=[a, K.flags], W=[a])
                C.op("dve", lambda e: e.scalar_tensor_tensor(out=flat(a), in0=flat(cur), scalar=f, in1=flat(a), op0=ALU.mult, op1=ALU.add), R=[cur, K.flags, a], W=[a])
                C.op("dve", lambda e: e.tensor_scalar(out=flat(b), in0=flat(b), scalar1=f, scalar2=None, op0=ALU.mult), R=[b, K.flags], W=[b])
                C.op("dve", lambda e: e.scalar_tensor_tensor(out=flat(b), in0=flat(cur), scalar=nf, in1=flat(b), op0=ALU.mult, op1=ALU.add), R=[cur, K.flags, b], W=[b])
        elif P.save is not None:
            for cur, sv, _ in st:
                a = sv(P.save)
                C.op("dve", lambda e: e.tensor_copy(out=flat(a), in_=flat(cur)), R=[cur], W=[a])

    def prep_sc(self, P, sc):
        C = self.C; K = self.K
        T = P.sc_tok
        nt = T // 128
        for i in range(nt):
            xt = K.xt[i % 2]
            r0 = P.row0 + sc * T + i * 128
            C.dma(lambda e: e.dma_start(out=xt[:, :], in_=P.src.t[r0:r0 + 128, :]), R=[P.src], W=[xt])
            C.op("pool", lambda e: e.memset(K.ss[:, :], 0.0), W=[K.ss])
            C.op("act", lambda e: e.activation(out=K.junk[:, :], in_=xt[:, :], func=AF.Square, accum_out=K.ss[:, :]), R=[xt, K.ss], W=[K.junk, K.ss])
            C.op("act", lambda e: e.activation(out=K.rstd[:, :], in_=K.ss[:, :], func=AF.Sqrt, scale=1.0 / D, bias=self.epsb[:, :]), R=[K.ss, self.epsb], W=[K.rstd])
            C.op("dve", lambda e: e.reciprocal(out=K.rstd[:, :], in_=K.rstd[:, :]), R=[K.rstd], W=[K.rstd])
            C.op("dve", lambda e: e.scalar_tensor_tensor(out=K.xm[:, :], in0=xt[:, :], scalar=K.rstd[:, 0:1], in1=K.GS[:, :], op0=ALU.mult, op1=ALU.mult), R=[xt, K.rstd, K.GS], W=[K.xm])
            C.op("pool", lambda e: e.tensor_tensor(out=K.xn[:, :], in0=K.xm[:, :], in1=K.SH[:, :], op=ALU.add), R=[K.xm, K.SH], W=[K.xn])
            pt = self.tbank()
            for k in range(8):
                self.tr(pt, pt[:, k * 128:(k + 1) * 128], K.xn[:, k * 128:(k + 1) * 128], self.identb[:, :], R=[K.xn, self.identb])
            C.op("act", lambda e: e.activation(out=K.uT[:, :, i * 128:(i + 1) * 128], in_=pt[:, :].rearrange("p (k n) -> p k n", k=8), func=AF.Copy), R=[pt], W=[K.uT])
        if P.name == "ownB" and self.uT_d is not None:
            C.dma(lambda e: e.dma_start(out=self.uT_d.t[sc * 128:(sc + 1) * 128, :], in_=K.uT[:, :, :].rearrange("p k n -> p (k n)")), R=[K.uT], W=[self.uT_d])
        if "uT" in self.dbg and P.name == "ownF" and sc == 0:
            self.dump_bf("uT", K.uT, K.uT[:, :, :].rearrange("p k n -> p (k n)"), [128, 4096])
        xt_list = list(range(12)) if P.full else list(range(10))
        qt_list = list(range(8)) if P.full else list(range(4, 8))
        jobs = [("x", ct) for ct in xt_list] + [("q", ct) for ct in qt_list]
        for j0 in range(0, len(jobs), 2):
            ctxs = []
            for (kind, ct) in jobs[j0:j0 + 2]:
                off = (O_XBC if kind == "x" else O_QK) + ct * 128
                cw, cb, dst = (K.cwx, K.cbx, K.cv) if kind == "x" else (K.cwq, K.cbq, K.cvq)
                pb = self.gbank()
                for k in range(8):
                    self.mm(pb, pb[:, 0:T], K.W[:, k, off:off + 128], K.uT[:, k, 0:T], R=[K.W, K.uT], start=(k == 0), stop=(k == 7))
                K.nconv += 1
                raw = K.raw[K.nconv % 4]; acc = K.acc[K.nconv % 4]
                C.op("act", lambda e: e.activation(out=raw[:, 0:T], in_=pb[:, 0:T], func=AF.Copy), R=[pb], W=[raw])
                ctxs.append((ct, cw, cb, dst, raw, acc))
            for (ct, cw, cb, dst, raw, acc) in ctxs:
                C.op("dve", lambda e: e.tensor_scalar(out=acc[:, 0:T], in0=raw[:, 0:T], scalar1=cw[:, ct * 5 + 2:ct * 5 + 3], scalar2=cb[:, ct:ct + 1], op0=ALU.mult, op1=ALU.add), R=[raw, cw, cb], W=[acc])
            for j in (0, 1, 3, 4):
                o = j - 2
                lo = max(0, -o); hi = P.rows - max(0, o)
                for (ct, cw, cb, dst, raw, acc) in ctxs:
                    a3 = acc[:, 0:T].rearrange("p (r t) -> p r t", t=P.rows); p3 = raw[:, 0:T].rearrange("p (r t) -> p r t", t=P.rows)
                    C.op("dve", lambda e: e.scalar_tensor_tensor(out=a3[:, :, lo:hi], in0=p3[:, :, lo + o:hi + o], scalar=cw[:, ct * 5 + j:ct * 5 + j + 1], in1=a3[:, :, lo:hi], op0=ALU.mult, op1=ALU.add), R=[raw, cw, acc], W=[acc])
            for (ct, cw, cb, dst, raw, acc) in ctxs:
                C.op("act", lambda e: e.activation(out=dst[:, ct, 0:T], in_=acc[:, 0:T], func=AF.Silu), R=[acc], W=[dst])
        for i in range(nt):
            for n in range(2):
                pb = self.gbank()
                for k in range(8):
                    self.mm(pb, pb[:, 0:512], K.uT[:, k, i * 128:(i + 1) * 128], K.W[:, k, O_V + n * 512:O_V + (n + 1) * 512], R=[K.W, K.uT], start=(k == 0), stop=(k == 7))
                C.op("act", lambda e: e.activation(out=K.Vtok[i][:, n * 512:(n + 1) * 512], in_=pb[:, 0:512], func=AF.Copy), R=[pb], W=[K.Vtok[i]])
            pb = self.gbank()
            dto = O_DT + 16 * P.dd; gto = O_GT + 16 * P.dd
            for k in range(8):
                self.mm(pb, pb[:, 0:16], K.uT[:, k, i * 128:(i + 1) * 128], K.W[:, k, dto:dto + 16], R=[K.W, K.uT], start=(k == 0), stop=(k == 7))
            for k in range(8):
                self.mm(pb, pb[:, 16:32], K.uT[:, k, i * 128:(i + 1) * 128], K.W[:, k, gto:gto + 16], R=[K.W, K.uT], start=(k == 0), stop=(k == 7))
            SM = K.SM[i]
            C.op("dve", lambda e: e.tensor_tensor(out=K.sm[:, 0:16], in0=pb[:, 0:16], in1=K.small[:, 0:16], op=ALU.add), R=[pb, K.small], W=[K.sm])
            C.op("dve", lambda e: e.tensor_tensor(out=K.sm[:, 16:32], in0=pb[:, 16:32], in1=K.small[:, 32:48], op=ALU.add), R=[pb, K.small], W=[K.sm])
            C.op("act", lambda e: e.activation(out=K.e1[:, 0:16], in_=K.sm[:, 0:16], func=AF.Exp), R=[K.sm], W=[K.e1])
            C.op("act", lambda e: e.activation(out=K.e1[:, 16:24], in_=K.sm[:, 24:32], func=AF.Exp, scale=-1.0), R=[K.sm], W=[K.e1])
            C.op("act", lambda e: e.activation(out=SM[:, 24:40], in_=K.e1[:, 0:16], func=AF.Ln, bias=1.0, scale=1.0), R=[K.e1], W=[SM])
            C.op("act", lambda e: e.activation(out=K.e1[:, 24:32], in_=K.e1[:, 16:24], func=AF.Ln, bias=1.0, scale=1.0), R=[K.e1], W=[K.e1])
            C.op("dve", lambda e: e.tensor_scalar(out=SM[:, 16:24], in0=K.e1[:, 24:32], scalar1=-1.0, scalar2=None, op0=ALU.mult), R=[K.e1], W=[SM])
            C.op("dve", lambda e: e.tensor_tensor(out=SM[:, 0:16], in0=SM[:, 24:40], in1=K.aneg[:, :], op=ALU.mult), R=[SM, K.aneg], W=[SM])
            C.op("dve", lambda e: e.tensor_copy(out=SM[:, 40:48], in_=K.sm[:, 16:24]), R=[K.sm], W=[SM])
            if "SM" in self.dbg and P.name == "ownF" and sc == 0 and i == 0:
                self.dump_now("SM", SM, [128, 48])
        if "cv" in self.dbg and P.name == "ownF" and sc == 0:
            self.dump_bf("cv", K.cv, K.cv[:, :, :].rearrange("p k n -> p (k n)"), [128, 12 * 512])
            self.dump_bf("cvq", K.cvq, K.cvq[:, :, :].rearrange("p k n -> p (k n)"), [128, 8 * 512])

    def dump_bf(self, name, buf, ap, shape):
        C = self.C
        o = self.outp("dbg_" + name, shape)
        n = shape[1]
        for c0 in range(0, n, 2048):
            c1 = min(n, c0 + 2048)
            t = C.sb([128, 2048], F32, "dmp")
            C.op("dve", lambda e: e.tensor_copy(out=t[:, 0:c1 - c0], in_=ap[:, c0:c1]), R=[buf], W=[t])
            C.dma(lambda e: e.dma_start(out=o.t[:, c0:c1], in_=t[:, 0:c1 - c0]), R=[t], W=[o])

    def scan_chunk(self, P, sc, i):
        C = self.C; K = self.K
        SKIP = os.environ.get('KSKIP', '').split(',')
        if 'scan' in SKIP:
            return
        tok = slice(i * 128, (i + 1) * 128)
        Uf = self.Lf if P.rev else self.Uf
        Ub = self.Lb if P.rev else self.Ub
        NEGM = self.NEGL if P.rev else self.NEGU
        SM = K.SM[i]
        full = P.full
        K.nchunk += 1
        par = K.nchunk % 2
        sl = self.dslot()
        o = sl.off
        self.mm(sl, sl.t[:, o:o + 24], Uf[:, :], SM[:, 0:24], R=[Uf, SM])
        self.mm(sl, sl.t[:, o + 24:o + 48], self.onesf[:, :], SM[:, 0:24], R=[self.onesf, SM])
        C.op("dve", lambda e: e.tensor_copy(out=K.cs[:, :], in_=sl.t[:, o:o + 48]), R=[sl], W=[K.cs])
        pt = self.tbank()
        for c in range(8):
            self.tr(pt, pt[:, c * 128:(c + 1) * 128], K.cv[:, c, tok], self.identb[:, :], R=[K.cv, self.identb])
        C.op("act", lambda e: e.activation(out=K.Xtok[:, :], in_=pt[:, :], func=AF.Copy), R=[pt], W=[K.Xtok])
        pt = self.tbank()
        for c in range(2):
            self.tr(pt, pt[:, c * 128:(c + 1) * 128], K.cv[:, 8 + c, tok], self.identb[:, :], R=[K.cv, self.identb])
        for c in range(4):
            self.tr(pt, pt[:, 256 + c * 128:256 + (c + 1) * 128], K.cvq[:, 4 + c, tok], self.identb[:, :], R=[K.cvq, self.identb])
        C.op("dve", lambda e: e.tensor_copy(out=K.BK[:, :], in_=pt[:, 0:768]), R=[pt], W=[K.BK])
        C.op("dve", lambda e: e.tensor_tensor(out=K.t16[:, :], in0=K.cs[:, 24:40], in1=K.cs[:, 0:16], op=ALU.subtract), R=[K.cs], W=[K.t16])
        C.op("act", lambda e: e.activation(out=K.t16[:, :], in_=K.t16[:, :], func=AF.Exp), R=[K.t16], W=[K.t16])
        C.op("dve", lambda e: e.tensor_tensor(out=K.wS[:, :], in0=K.t16[:, :], in1=SM[:, 24:40], op=ALU.mult), R=[K.t16, SM], W=[K.wS])
        X3 = K.Xtok[:, :].rearrange("p (h d) -> p h d", h=16)
        C.op("dve", lambda e: e.tensor_tensor(out=K.Xw[:, :].rearrange("p (h d) -> p h d", h=16), in0=X3, in1=bc(K.wS, 0, 16, 64), op=ALU.mult), R=[K.Xtok, K.wS], W=[K.Xw])
        C.op("act", lambda e: e.activation(out=K.exptot[:, :], in_=K.cs[:, 24:40], func=AF.Exp), R=[K.cs], W=[K.exptot])
        if full:
            C.op("act", lambda e: e.activation(out=K.expcum[:, :], in_=K.cs[:, 0:16], func=AF.Exp), R=[K.cs], W=[K.expcum])
            C.op("dve", lambda e: e.tensor_scalar(out=K.ncum[:, :], in0=K.cs[:, 0:16], scalar1=-1.0, scalar2=None, op0=ALU.mult), R=[K.cs], W=[K.ncum])
            C.op("pool", lambda e: e.tensor_tensor(out=K.Xdt[:, :].rearrange("p (h d) -> p h d", h=16), in0=X3, in1=bc(SM, 24, 16, 64), op=ALU.mult), R=[K.Xtok, SM], W=[K.Xdt])
            if P.dd == 0 and self.addD:
                C.op("pool", lambda e: e.tensor_tensor(out=K.XD[:, :].rearrange("p (h d) -> p h d", h=16), in0=X3, in1=bc(K.Dh, 0, 16, 64), op=ALU.mult), R=[K.Xtok, K.Dh], W=[K.XD])
            yout = K.yout[par]
            for g in range(2):
                sl = self.dslot(); o = sl.off
                self.mm(sl, sl.t[:, o:o + 128], K.cv[:, 8 + g, tok], K.cv[:, 10 + g, tok], R=[K.cv])
                C.op("act", lambda e: e.activation(out=K.CBT[g][:, :], in_=sl.t[:, o:o + 128], func=AF.Copy), R=[sl], W=[K.CBT[g]])
                self.mm(self.PYI, self.PYI[:, :], K.cv[:, 10 + g, tok], K.Sbf[g][:, :], R=[K.cv, K.Sbf[g]])
                addD = (P.dd == 0) and self.addD
                if addD:
                    self.mm(self.PY, self.PY[:, :], self.identb[:, :], K.XD[:, g * 512:(g + 1) * 512], R=[self.identb, K.XD], start=True, stop=False)
                pend = None
                for hh in range(8):
                    h = g * 8 + hh
                    sl = self.dslot(); o = sl.off
                    self.mm(sl, sl.t[:, o:o + 128], colbc(SM, h, 128), Uf[:, :], R=[SM, Uf], start=True, stop=False)
                    self.mm(sl, sl.t[:, o:o + 128], self.identf[:, :], NEGM[:, :], R=[self.identf, NEGM], start=False, stop=True)
                    E = K.E[hh % 2]; M = K.M[hh % 2]
                    C.op("act", lambda e: e.activation(out=E[:, :], in_=sl.t[:, o:o + 128], func=AF.Exp, bias=K.ncum[:, h:h + 1], scale=1.0), R=[sl, K.ncum], W=[E])
                    C.op("dve", lambda e: e.tensor_tensor(out=M[:, :], in0=E[:, :], in1=K.CBT[g][:, :], op=ALU.mult), R=[E, K.CBT[g]], W=[M])
                    if pend is not None:
                        pM, phh, ph = pend
                        self.mm(self.PY, self.PY[:, phh * 64:(phh + 1) * 64], pM[:, :], K.Xdt[:, ph * 64:(ph + 1) * 64], R=[pM, K.Xdt], start=(not addD), stop=True)
                    pend = (M, hh, h)
                pM, phh, ph = pend
                self.mm(self.PY, self.PY[:, phh * 64:(phh + 1) * 64], pM[:, :], K.Xdt[:, ph * 64:(ph + 1) * 64], R=[pM, K.Xdt], start=(not addD), stop=True)
                yg = yout[:, g * 512:(g + 1) * 512]
                C.op("dve", lambda e: e.tensor_tensor(out=yg.rearrange("p (h d) -> p h d", h=8), in0=self.PYI[:, :].rearrange("p (h d) -> p h d", h=8), in1=bc(K.expcum, g * 8, 8, 64), op=ALU.mult), R=[self.PYI, K.expcum], W=[yout])
                C.op("dve", lambda e: e.tensor_tensor(out=yg, in0=yg, in1=self.PY[:, :], op=ALU.add), R=[self.PY, yout], W=[yout])
            r0 = sc * P.sc_tok + i * 128
            yd = self.y_d[P.dd]
            C.dma(lambda e: e.dma_start(out=yd.t[r0:r0 + 128, :], in_=yout[:, :]), R=[yout], W=[yd])
        for g in range(2):
            pb = self.gbank()
            self.mm(pb, pb[:, 0:512], K.BK[:, g * 128:(g + 1) * 128], K.Xw[:, g * 512:(g + 1) * 512], R=[K.BK, K.Xw])
            S3 = K.S[g][:, :].rearrange("p (h d) -> p h d", h=8)
            C.op("dve", lambda e: e.tensor_tensor(out=S3, in0=S3, in1=bc(K.exptot, g * 8, 8, 64), op=ALU.mult), R=[K.S[g], K.exptot], W=[K.S[g]])
            C.op("dve", lambda e: e.tensor_tensor(out=K.S[g][:, :], in0=K.S[g][:, :], in1=pb[:, 0:512], op=ALU.add), R=[K.S[g], pb], W=[K.S[g]])
            C.op("act", lambda e: e.activation(out=K.Sbf[g][:, :], in_=K.S[g][:, :], func=AF.Copy), R=[K.S[g]], W=[K.Sbf[g]])
        if 'mlstm' in SKIP:
            return
        C.op("dve", lambda e: e.tensor_tensor(out=K.a8[:, :], in0=SM[:, 40:48], in1=K.cs[:, 16:24], op=ALU.subtract), R=[SM, K.cs], W=[K.a8])
        sl = self.dslot(); o = sl.off
        self.tr(sl, sl.t[0:8, o:o + 128], K.a8[:, 0:8], self.identf[:, :], R=[K.a8, self.identf])
        C.op("dve", lambda e: e.reduce_max(out=K.amax[:, :], in_=sl.t[0:8, o:o + 128], axis=AX.X), R=[sl], W=[K.amax])
        C.op("dve", lambda e: e.tensor_scalar(out=K.dg[:, :], in0=self.identf[0:8, 0:8], scalar1=K.amax[:, 0:1], scalar2=None, op0=ALU.mult), R=[self.identf, K.amax], W=[K.dg])
        sl = self.dslot(); o = sl.off
        self.mm(sl, sl.t[:, o:o + 8], self.onesf[0:8, :], K.dg[:, :], R=[self.onesf, K.dg])
        C.op("dve", lambda e: e.tensor_tensor(out=K.Mc[:, :], in0=K.mbc[:, :], in1=sl.t[:, o:o + 8], op=ALU.max), R=[K.mbc, sl], W=[K.Mc])
        C.op("dve", lambda e: e.tensor_tensor(out=K.cd[:, :], in0=K.mbc[:, :], in1=K.Mc[:, :], op=ALU.subtract), R=[K.mbc, K.Mc], W=[K.cd])
        C.op("act", lambda e: e.activation(out=K.cd[:, :], in_=K.cd[:, :], func=AF.Exp), R=[K.cd], W=[K.cd])
        C.op("dve", lambda e: e.tensor_tensor(out=K.mbc[:, :], in0=K.cs[:, 40:48], in1=K.Mc[:, :], op=ALU.add), R=[K.cs, K.Mc], W=[K.mbc])
        C.op("dve", lambda e: e.tensor_tensor(out=K.w8[:, :], in0=K.a8[:, :], in1=K.Mc[:, :], op=ALU.subtract), R=[K.a8, K.Mc], W=[K.w8])
        C.op("act", lambda e: e.activation(out=K.w8[:, :], in_=K.w8[:, :], func=AF.Exp), R=[K.w8], W=[K.w8])
        C.op("dve", lambda e: e.tensor_copy(out=K.w8b[:, :], in_=K.w8[:, :]), R=[K.w8], W=[K.w8b])
        C.op("pool", lambda e: e.tensor_tensor(out=K.Vw[:, :, :], in0=K.Vtok[i][:, :].rearrange("p (h d) -> p h d", h=8), in1=bc(K.w8, 0, 8, 128), op=ALU.mult), R=[K.Vtok[i], K.w8], W=[K.Vw])
        for hh in range(2):
            ps_ = slice(hh * 64, (hh + 1) * 64)
            cdv = bass.AP(K.cd.t, hh * 64 * 8 + hh, [[8, 64], [2, 4], [0, 128]])
            C.op("dve", lambda e: e.tensor_tensor(out=K.Cm[ps_, :, :], in0=K.Cm[ps_, :, :], in1=cdv, op=ALU.mult), R=[K.Cm, K.cd], W=[K.Cm])
            cdn = bass.AP(K.cd.t, hh * 64 * 8 + hh, [[8, 64], [2, 4]])
            C.op("dve", lambda e: e.tensor_tensor(out=K.nm[ps_, :], in0=K.nm[ps_, :], in1=cdn, op=ALU.mult), R=[K.nm, K.cd], W=[K.nm])
        if full:
            C.op("act", lambda e: e.activation(out=K.Cbf[:, :, :], in_=K.Cm[:, :, :], func=AF.Copy, scale=0.125), R=[K.Cm], W=[K.Cbf])
            C.op("act", lambda e: e.activation(out=K.nbf[:, :], in_=K.nm[:, :], func=AF.Copy, scale=0.125), R=[K.nm], W=[K.nbf])
            C.op("dve", lambda e: e.tensor_tensor(out=K.t8[:, :], in0=K.cs[:, 16:24], in1=K.Mc[:, :], op=ALU.add), R=[K.cs, K.Mc], W=[K.t8])
            C.op("act", lambda e: e.activation(out=K.fl[:, :], in_=K.t8[:, :], func=AF.Exp, scale=-1.0), R=[K.t8], W=[K.fl])
            sden = self.PYI; od = 0
            for h in range(8):
                c = h // 2; pr = (h % 2) * 64
                KT = K.cvq[pr:pr + 64, 4 + c, tok]; QT = K.cvq[pr:pr + 64, c, tok]
                sl = self.dslot(); o = sl.off
                self.mm(sl, sl.t[:, o:o + 128], KT, QT, R=[K.cvq])
                A = K.A[h]
                C.op("dve", lambda e: e.scalar_tensor_tensor(out=A[:, :], in0=sl.t[:, o:o + 128], scalar=0.125, in1=Ub[:, :], op0=ALU.mult, op1=ALU.mult), R=[sl, Ub], W=[A])
                self.mm(sden, sden.t[:, od + h:od + h + 1], A[:, :], K.w8b[:, h:h + 1], R=[A, K.w8b], start=True, stop=False)
                self.mm(sden, sden.t[:, od + h:od + h + 1], QT, K.nbf[pr:pr + 64, c:c + 1], R=[K.cvq, K.nbf], start=False, stop=True)
            C.op("dve", lambda e: e.tensor_copy(out=K.den[:, :], in_=sden.t[:, od:od + 8]), R=[sden], W=[K.den])
            C.op("dve", lambda e: e.scalar_tensor_tensor(out=K.den[:, :], in0=K.den[:, :], scalar=-1.0, in1=K.den[:, :], op0=ALU.mult, op1=ALU.max), R=[K.den], W=[K.den])
            C.op("dve", lambda e: e.tensor_tensor(out=K.den[:, :], in0=K.den[:, :], in1=K.fl[:, :], op=ALU.max), R=[K.den, K.fl], W=[K.den])
            C.op("dve", lambda e: e.reciprocal(out=K.rc[:, :], in_=K.den[:, :]), R=[K.den], W=[K.rc])
            hout = K.hout[par]
            for h in range(8):
                c = h // 2; pr = (h % 2) * 64
                QT = K.cvq[pr:pr + 64, c, tok]
                sl = self.dslot(); o = sl.off
                self.mm(sl, sl.t[:, o:o + 128], K.A[h][:, :], K.Vw[:, h, :], R=[K.A[h], K.Vw], start=True, stop=False)
                self.mm(sl, sl.t[:, o:o + 128], QT, K.Cbf[pr:pr + 64, c, :], R=[K.cvq, K.Cbf], start=False, stop=True)
                C.op("act", lambda e: e.activation(out=hout[:, h * 128:(h + 1) * 128], in_=sl.t[:, o:o + 128], func=AF.Copy, scale=K.rc[:, h:h + 1]), R=[sl, K.rc], W=[hout])
            r0 = sc * P.sc_tok + i * 128
            hd = self.h_d[P.dd]
            C.dma(lambda e: e.dma_start(out=hd.t[r0:r0 + 128, :], in_=hout[:, :]), R=[hout], W=[hd])
        if 'mupd' in SKIP:
            return
        sn2 = self.PY; on = 0
        for h in range(8):
            c = h // 2; hh = h % 2; ps_ = slice(hh * 64, (hh + 1) * 64)
            Kp = K.BK[:, 256 + c * 128:256 + (c + 1) * 128]
            sl = self.dslot(); o = sl.off
            self.mm(sl, sl.t[:, o:o + 128], Kp, K.Vw[:, h, :], R=[K.BK, K.Vw])
            C.op("dve", lambda e: e.tensor_tensor(out=K.Cm[ps_, c, :], in0=K.Cm[ps_, c, :], in1=sl.t[ps_, o:o + 128], op=ALU.add), R=[K.Cm, sl], W=[K.Cm])
            self.mm(sn2, sn2.t[:, on + h:on + h + 1], Kp, K.w8b[:, h:h + 1], R=[K.BK, K.w8b])
        for hh in range(2):
            ps_ = slice(hh * 64, (hh + 1) * 64)
            src = bass.AP(sn2.t, hh * 64 * 512 + on + hh, [[512, 64], [2, 4]])
            C.op("dve", lambda e: e.tensor_tensor(out=K.nm[ps_, :], in0=K.nm[ps_, :], in1=src, op=ALU.add), R=[K.nm, sn2], W=[K.nm])


    def load_w_bf(self, dst, dram, row0, nk, c0, c1, dcol0=0):
        C = self.C
        for k in range(nk):
            C.dma(lambda e: e.dma_start(out=dst[:, k, dcol0:dcol0 + (c1 - c0)], in_=dram.t[row0 + k * 128:row0 + (k + 1) * 128, c0:c1]), R=[dram], W=[dst], q="pool")

    def abank(self):
        self._a = (self._a + 1) % len(self.Gall)
        return self.Gall[self._a]

    def rms_rstd(self, src_ap, srcbuf, junk, ss, rstd, n):
        C = self.C
        C.op("pool", lambda e: e.memset(ss[:, :], 0.0), W=[ss])
        C.op("act", lambda e: e.activation(out=junk[:, :], in_=src_ap, func=AF.Square, accum_out=ss[:, :]), R=[srcbuf, ss], W=[junk, ss])
        C.op("act", lambda e: e.activation(out=rstd[:, :], in_=ss[:, :], func=AF.Sqrt, scale=1.0 / n, bias=self.epsb[:, :]), R=[ss, self.epsb], W=[rstd])
        C.op("dve", lambda e: e.reciprocal(out=rstd[:, :], in_=rstd[:, :]), R=[rstd], W=[rstd])

    def proj(self, lhsT_buf, W, wc0, n512, evac):
        for n in range(n512):
            pb = self.abank()
            for k in range(8):
                self.mm(pb, pb[:, 0:512], lhsT_buf[:, k, :], W[:, k, wc0 + n * 512:wc0 + (n + 1) * 512], R=[lhsT_buf, W], start=(k == 0), stop=(k == 7))
            evac(n, pb)

    def transp8(self, src, dstT):
        C = self.C
        pt = self.tbank()
        for k in range(8):
            self.tr(pt, pt[:, k * 128:(k + 1) * 128], src[:, k * 128:(k + 1) * 128], self.identb[:, :], R=[src, self.identb])
        C.op("act", lambda e: e.activation(out=dstT[:, :, :], in_=pt[:, :].rearrange("p (k n) -> p k n", k=8), func=AF.Copy), R=[pt], W=[dstT])

    def merge_pass(self, xo, w_in, vecs, w_so, w_mo, w_o):
        C = self.C
        self.Gall = self.G + [self.PY, self.PYI] + self.DS
        self._a = 0
        with Scope(C):
            Wz = C.sb([128, 8, 4096], BF16, "Wz")
            self.load_w_bf(Wz, w_in, 0, 8, 0, 1024, 0)
            self.load_w_bf(Wz, w_in, 0, 8, 4672, 7744, 1024)
            Wso = C.sb([128, 8, D], BF16, "Wso"); Wmo = C.sb([128, 8, D], BF16, "Wmo"); Wo = C.sb([128, 8, D], BF16, "Wo")
            self.load_w_bf(Wso, w_so, 0, 8, 0, D); self.load_w_bf(Wmo, w_mo, 0, 8, 0, D); self.load_w_bf(Wo, w_o, 0, 8, 0, D)
            gssd = C.sb([128, D], F32, "gssd"); gml = C.sb([128, D], F32, "gml"); gate = C.sb([128, D], F32, "gate")
            C.dma(lambda e: e.dma_start(out=gssd[:, :], in_=vecs.t[0:128, :]), R=[vecs], W=[gssd])
            C.dma(lambda e: e.dma_start(out=gml[:, :], in_=vecs.t[128:256, :]), R=[vecs], W=[gml])
            self.col_to_bcast(self.gcol, 0, gate)
            xt = C.sb([128, D], F32, "m_xt"); uT = C.sb([128, 8, 128], BF16, "m_uT")
            A1 = C.sb([128, D], F32, "A1"); A2 = C.sb([128, D], F32, "A2"); A3 = C.sb([128, D], F32, "A3"); A4 = C.sb([128, D], F32, "A4")
            Z = C.sb([128, D], F32, "Z"); junk = C.sb([128, D], BF16, "m_junk")
            yn = C.sb([128, D], BF16, "yn"); hn = C.sb([128, D], BF16, "hn"); nT = C.sb([128, 8, 128], BF16, "nT")
            mg = C.sb([128, D], BF16, "mg"); x1 = C.sb([128, D], F32, "x1t")
            ss = C.sb([128, 1], F32, "m_ss"); rstd = C.sb([128, 1], F32, "m_rstd"); ss8 = C.sb([128, 8], F32, "ss8")
            for t in range(32):
                r0 = t * 128
                sc, i = t // 4, t % 4
                C.dma(lambda e: e.dma_start(out=xt[:, :], in_=xo.t[r0:r0 + 128, :]), R=[xo], W=[xt])
                C.dma(lambda e: e.dma_start(out=uT[:, :, :], in_=self.uT_d.t[sc * 128:(sc + 1) * 128, :].rearrange("p (k n) -> p k n", k=8)[:, :, i * 128:(i + 1) * 128]), R=[self.uT_d], W=[uT])
                C.dma(lambda e: e.dma_start(out=A1[:, :], in_=self.y_d[0].t[r0:r0 + 128, :]), R=[self.y_d[0]], W=[A1])
                C.dma(lambda e: e.dma_start(out=A2[:, :], in_=self.y_d[1].t[r0:r0 + 128, :]), R=[self.y_d[1]], W=[A2])
                C.dma(lambda e: e.dma_start(out=A3[:, :], in_=self.h_d[0].t[r0:r0 + 128, :]), R=[self.h_d[0]], W=[A3])
                C.dma(lambda e: e.dma_start(out=A4[:, :], in_=self.h_d[1].t[r0:r0 + 128, :]), R=[self.h_d[1]], W=[A4])
                self.proj(uT, Wz, 0, 2, lambda n, pb: C.op("act", lambda e: e.activation(out=Z[:, n * 512:(n + 1) * 512], in_=pb[:, 0:512], func=AF.Silu), R=[pb], W=[Z]))
                C.op("dve", lambda e: e.tensor_tensor(out=A1[:, :], in0=A1[:, :], in1=A2[:, :], op=ALU.add), R=[A1, A2], W=[A1])
                C.op("dve", lambda e: e.tensor_tensor(out=A1[:, :], in0=A1[:, :], in1=Z[:, :], op=ALU.mult), R=[A1, Z], W=[A1])
                self.rms_rstd(A1[:, :], A1, junk, ss, rstd, D)
                C.op("dve", lambda e: e.scalar_tensor_tensor(out=yn[:, :], in0=A1[:, :], scalar=rstd[:, 0:1], in1=gssd[:, :], op0=ALU.mult, op1=ALU.mult), R=[A1, rstd, gssd], W=[yn])
                self.proj(uT, Wz, 1024, 2, lambda n, pb: C.op("act", lambda e: e.activation(out=Z[:, n * 512:(n + 1) * 512], in_=pb[:, 0:512], func=AF.Sigmoid), R=[pb], W=[Z]))
                C.op("dve", lambda e: e.tensor_tensor(out=A3[:, :], in0=A3[:, :], in1=A4[:, :], op=ALU.add), R=[A3, A4], W=[A3])
                C.op("pool", lambda e: e.tensor_tensor(out=A4[:, :], in0=A3[:, :], in1=A3[:, :], op=ALU.mult), R=[A3], W=[A4])
                C.op("dve", lambda e: e.tensor_reduce(out=ss8[:, :], in_=A4[:, :].rearrange("p (h d) -> p h d", h=8), axis=AX.X, op=ALU.add), R=[A4], W=[ss8])
                C.op("act", lambda e: e.activation(out=ss8[:, :], in_=ss8[:, :], func=AF.Sqrt, scale=1.0 / 128, bias=self.epsb[:, :]), R=[ss8, self.epsb], W=[ss8])
                C.op("dve", lambda e: e.reciprocal(out=ss8[:, :], in_=ss8[:, :]), R=[ss8], W=[ss8])
                C.op("dve", lambda e: e.tensor_tensor(out=A3[:, :].rearrange("p (h d) -> p h d", h=8), in0=A3[:, :].rearrange("p (h d) -> p h d", h=8), in1=bc(ss8, 0, 8, 128), op=ALU.mult), R=[A3, ss8], W=[A3])
                C.op("pool", lambda e: e.tensor_tensor(out=A3[:, :], in0=A3[:, :], in1=gml[:, :], op=ALU.mult), R=[A3, gml], W=[A3])
                C.op("dve", lambda e: e.tensor_tensor(out=hn[:, :], in0=A3[:, :], in1=Z[:, :], op=ALU.mult), R=[A3, Z], W=[hn])
                self.proj(uT, Wz, 2048, 2, lambda n, pb: C.op("act", lambda e: e.activation(out=Z[:, n * 512:(n + 1) * 512], in_=pb[:, 0:512], func=AF.Sigmoid), R=[pb], W=[Z]))
                self.transp8(yn, nT)
                self.proj(nT, Wso, 0, 2, lambda n, pb: C.op("dve", lambda e: e.tensor_tensor(out=A1[:, n * 512:(n + 1) * 512], in0=pb[:, 0:512], in1=Z[:, n * 512:(n + 1) * 512], op=ALU.mult), R=[pb, Z], W=[A1]))
                self.proj(uT, Wz, 3072, 2, lambda n, pb: C.op("act", lambda e: e.activation(out=Z[:, n * 512:(n + 1) * 512], in_=pb[:, 0:512], func=AF.Sigmoid), R=[pb], W=[Z]))
                self.transp8(hn, nT)
                self.proj(nT, Wmo, 0, 2, lambda n, pb: C.op("dve", lambda e: e.tensor_tensor(out=A2[:, n * 512:(n + 1) * 512], in0=pb[:, 0:512], in1=Z[:, n * 512:(n + 1) * 512], op=ALU.mult), R=[pb, Z], W=[A2]))
                C.op("dve", lambda e: e.tensor_tensor(out=mg[:, :], in0=A1[:, :], in1=A2[:, :], op=ALU.add), R=[A1, A2], W=[mg])
                self.transp8(mg, nT)
                self.proj(nT, Wo, 0, 2, lambda n, pb: C.op("dve", lambda e: e.tensor_tensor(out=x1[:, n * 512:(n + 1) * 512], in0=pb[:, 0:512], in1=gate[:, n * 512:(n + 1) * 512], op=ALU.mult), R=[pb, gate], W=[x1]))
                C.op("pool", lambda e: e.tensor_tensor(out=x1[:, :], in0=x1[:, :], in1=xt[:, :], op=ALU.add), R=[x1, xt], W=[x1])
                C.dma(lambda e: e.dma_start(out=self.x1_d.t[r0:r0 + 128, :], in_=x1[:, :]), R=[x1], W=[self.x1_d])

    def ffn_prep(self, vecs, router_w, router_b, sh_g, sh_u, sh_d):
        C = self.C
        with Scope(C):
            if self.Y_d is not None:
                zt = C.sb([128, 2048], F32, "zt")
                C.op("pool", lambda e: e.memset(zt[:, :], 0.0), W=[zt])
                for i in range(SEG * 8 // 256):
                    C.dma(lambda e: e.dma_start(out=self.Y_d.t[i * 256:(i + 1) * 256, :].rearrange("(p a) n -> p (a n)", p=128), in_=zt[:, :]), R=[zt], W=[])
            GS2 = C.sb([128, D], F32, "GS2"); SH2 = C.sb([128, D], F32, "SH2")
            self.col_to_bcast(self.cols, 32, GS2); self.col_to_bcast(self.cols, 40, SH2)
            rw = C.sb([128, 8, NEXP], F32, "rw"); rb = C.sb([128, NEXP], F32, "rb")
            C.dma(lambda e: e.dma_start(out=rw[:, :, :], in_=router_w.t.ap().rearrange("(k p) n -> p k n", p=128)), R=[router_w], W=[rw])
            C.dma(lambda e: e.dma_start(out=rb[:, :], in_=router_b.t[:, :]), R=[router_b], W=[rb])
            Wsgu = C.sb([128, 8, 512], BF16, "Wsgu"); Wsd = C.sb([128, 2, D], BF16, "Wsd")
            self.load_w_bf(Wsgu, sh_g, 0, 8, 0, 256, 0); self.load_w_bf(Wsgu, sh_u, 0, 8, 0, 256, 256); self.load_w_bf(Wsd, sh_d, 0, 2, 0, D)
            iotaE = C.sb([128, NEXP], F32, "iotaE")
            C.op("pool", lambda e: e.iota(iotaE[:, :], pattern=[[1, NEXP]], base=0, channel_multiplier=0, allow_small_or_imprecise_dtypes=True), W=[iotaE])
            C.op("dve", lambda e: e.tensor_scalar(out=iotaE[:, :], in0=iotaE[:, :], scalar1=float(CAP), scalar2=None, op0=ALU.mult), R=[iotaE], W=[iotaE])
            cnt = C.sb([128, NEXP], F32, "cnt")
            C.op("pool", lambda e: e.memset(cnt[:, :], 0.0), W=[cnt])
            SUF = C.sb([128, 2, NEXP], BF16, "SUF")
            C.op("pool", lambda e: e.memset(SUF[:, :, :], 0.0), W=[SUF])
            C.op("dve", lambda e: e.tensor_copy(out=SUF[:, 0, 0:128], in_=self.SUb[:, :]), R=[self.SUb, SUF], W=[SUF])
            C.op("dve", lambda e: e.tensor_copy(out=SUF[:, 0, 128:256], in_=self.onesb[:, :]), R=[self.onesb, SUF], W=[SUF])
            C.op("dve", lambda e: e.tensor_copy(out=SUF[:, 1, 128:256], in_=self.SUb[:, :]), R=[self.SUb, SUF], W=[SUF])
            jv = C.sb([128, 8, NEXP], F32, "jv")
            C.op("pool", lambda e: e.iota(jv[:, :, :], pattern=[[1, 8], [0, NEXP]], base=0, channel_multiplier=0, allow_small_or_imprecise_dtypes=True), W=[jv])
            emT = C.sb([128, 2, 128], BF16, "emT"); jr = C.sb([128, NEXP], F32, "jr")
            Li = C.sb([128, NEXP * CAP // 128, 4], F32, "Li")
            C.op("pool", lambda e: e.memset(Li[:, :, :], 0.0), W=[Li])
            C.op("pool", lambda e: e.memset(Li[:, :, 0:1], 1.0e9), R=[Li], W=[Li])
            C.op("pool", lambda e: e.memset(Li[:, :, 1:2], 1.0e9), R=[Li], W=[Li])
            C.dma(lambda e: e.dma_start(out=self.L_d.t.ap().rearrange("(p a) n -> p a n", p=128), in_=Li[:, :, :]), R=[Li], W=[self.L_d])
            zb = C.sb([128, D], BF16, "zb")
            C.op("pool", lambda e: e.memset(zb[:, :], 0.0), W=[zb])
            C.dma(lambda e: e.dma_start(out=self.u2_d.t[SEG:SEG + 128, :], in_=zb[:, :]), R=[zb], W=[self.u2_d])
            x1 = C.sb([128, D], F32, "f_x1"); junk = C.sb([128, D], BF16, "f_junk"); ss = C.sb([128, 1], F32, "f_ss"); rstd = C.sb([128, 1], F32, "f_rstd")
            u2 = C.sb([128, D], F32, "u2"); u2b = C.sb([128, D], BF16, "u2b")
            u2Tf = C.sb([128, 8, 128], F32, "u2Tf"); u2Tb = C.sb([128, 8, 128], BF16, "u2Tb")
            sc_ = C.sb([128, NEXP], F32, "scores"); gr = C.sb([128, NEXP], F32, "grouped"); sel = C.sb([128, NEXP], F32, "sel")
            m8g = C.sb([128, 8, 8], F32, "m8g"); gs = C.sb([128, 8], F32, "gs"); g8 = C.sb([128, 8], F32, "g8"); pen = C.sb([128, 8], F32, "pen")
            e8 = C.sb([128, 8], F32, "e8"); em = C.sb([128, NEXP], F32, "em"); emb = C.sb([128, NEXP], BF16, "emb")
            Wc = C.sb([128, NEXP], F32, "Wc"); wsum = C.sb([128, 1], F32, "wsum")
            pos = C.sb([128, NEXP], F32, "pos"); dst = C.sb([128, NEXP], F32, "dst"); ov = C.sb([128, NEXP], F32, "ov")
            oh = C.sb([128, 8, NEXP], F32, "oh"); tmp = C.sb([128, 8, NEXP], F32, "ohtmp")
            dj = C.sb([128, 8], F32, "dj"); dji = C.sb([128, 8], I32, "dji"); rowd = C.sb([128, 8, 4], F32, "rowd")
            hs = C.sb([128, 2, 128], BF16, "hs"); sg = C.sb([128, 256], F32, "sgs"); sho = C.sb([128, D], F32, "sho")
            for t in range(32):
                r0 = t * 128
                C.dma(lambda e: e.dma_start(out=x1[:, :], in_=self.x1_d.t[r0:r0 + 128, :]), R=[self.x1_d], W=[x1])
                self.rms_rstd(x1[:, :], x1, junk, ss, rstd, D)
                C.op("dve", lambda e: e.scalar_tensor_tensor(out=u2[:, :], in0=x1[:, :], scalar=rstd[:, 0:1], in1=GS2[:, :], op0=ALU.mult, op1=ALU.mult), R=[x1, rstd, GS2], W=[u2])
                C.op("pool", lambda e: e.tensor_tensor(out=u2[:, :], in0=u2[:, :], in1=SH2[:, :], op=ALU.add), R=[u2, SH2], W=[u2])
                C.op("act", lambda e: e.activation(out=u2b[:, :], in_=u2[:, :], func=AF.Copy), R=[u2], W=[u2b])
                C.dma(lambda e: e.dma_start(out=self.u2_d.t[r0:r0 + 128, :], in_=u2b[:, :]), R=[u2b], W=[self.u2_d])
                for half in range(2):
                    pb = self.abank()
                    for kk in range(4):
                        k = half * 4 + kk
                        self.tr(pb, pb[:, kk * 128:(kk + 1) * 128], u2[:, k * 128:(k + 1) * 128], self.identf[:, :], R=[u2, self.identf])
                    C.op("act", lambda e: e.activation(out=u2Tf[:, half * 4:(half + 1) * 4, :], in_=pb[:, 0:512].rearrange("p (k n) -> p k n", k=4), func=AF.Copy), R=[pb], W=[u2Tf])
                C.op("dve", lambda e: e.tensor_copy(out=u2Tb[:, :, :], in_=u2Tf[:, :, :]), R=[u2Tf], W=[u2Tb])
                pb = self.abank()
                for k in range(8):
                    self.mm(pb, pb[:, 0:NEXP], u2Tf[:, k, :], rw[:, k, :], R=[u2Tf, rw], start=(k == 0), stop=(k == 7))
                C.op("act", lambda e: e.activation(out=sc_[:, :], in_=pb[:, 0:NEXP], func=AF.Sigmoid), R=[pb], W=[sc_])
                C.op("dve", lambda e: e.tensor_tensor(out=gr[:, :], in0=sc_[:, :], in1=rb[:, :], op=ALU.add), R=[sc_, rb], W=[gr])
                for g in range(8):
                    C.op("dve", lambda e: e.max(out=m8g[:, g, :], in_=gr[:, g * 32:(g + 1) * 32]), R=[gr], W=[m8g])
                C.op("dve", lambda e: e.tensor_tensor(out=gs[:, :], in0=m8g[:, :, 0], in1=m8g[:, :, 1], op=ALU.add), R=[m8g], W=[gs])
                C.op("dve", lambda e: e.max(out=g8[:, :], in_=gs[:, :]), R=[gs], W=[g8])
                C.op("dve", lambda e: e.tensor_scalar(out=pen[:, :], in0=gs[:, :], scalar1=g8[:, 3:4], scalar2=None, op0=ALU.is_ge), R=[gs, g8], W=[pen])
                C.op("dve", lambda e: e.tensor_scalar(out=pen[:, :], in0=pen[:, :], scalar1=-1.0, scalar2=1.0e4, op0=ALU.add, op1=ALU.mult), R=[pen], W=[pen])
                C.op("dve", lambda e: e.tensor_tensor(out=sel[:, :].rearrange("p (g e) -> p g e", g=8), in0=gr[:, :].rearrange("p (g e) -> p g e", g=8), in1=bc(pen, 0, 8, 32), op=ALU.add), R=[gr, pen], W=[sel])
                C.op("dve", lambda e: e.max(out=e8[:, :], in_=sel[:, :]), R=[sel], W=[e8])
                C.op("dve", lambda e: e.tensor_scalar(out=em[:, :], in0=sel[:, :], scalar1=e8[:, 7:8], scalar2=None, op0=ALU.is_ge), R=[sel, e8], W=[em])
                C.op("act", lambda e: e.activation(out=emb[:, :], in_=em[:, :], func=AF.Copy), R=[em], W=[emb])
                C.op("dve", lambda e: e.tensor_tensor(out=Wc[:, :], in0=sc_[:, :], in1=em[:, :], op=ALU.mult), R=[sc_, em], W=[Wc])
                C.op("dve", lambda e: e.reduce_sum(out=wsum[:, :], in_=Wc[:, :], axis=AX.X), R=[Wc], W=[wsum])
                C.op("dve", lambda e: e.reciprocal(out=wsum[:, :], in_=wsum[:, :]), R=[wsum], W=[wsum])
                C.op("dve", lambda e: e.tensor_scalar(out=Wc[:, :], in0=Wc[:, :], scalar1=wsum[:, 0:1], scalar2=2.5, op0=ALU.mult, op1=ALU.mult), R=[Wc, wsum], W=[Wc])
                pb = self.abank()
                self.mm(pb, pb[:, 0:NEXP], self.SUb[:, :], emb[:, :], R=[self.SUb, emb])
                C.op("dve", lambda e: e.tensor_tensor(out=pos[:, :], in0=pb[:, 0:NEXP], in1=cnt[:, :], op=ALU.add), R=[pb, cnt], W=[pos])
                pb = self.abank()
                self.mm(pb, pb[:, 0:NEXP], self.onesb[:, :], emb[:, :], R=[self.onesb, emb])
                C.op("dve", lambda e: e.tensor_tensor(out=cnt[:, :], in0=cnt[:, :], in1=pb[:, 0:NEXP], op=ALU.add), R=[pb, cnt], W=[cnt])
                C.op("dve", lambda e: e.tensor_scalar(out=ov[:, :], in0=pos[:, :], scalar1=float(CAP), scalar2=1.0e7, op0=ALU.is_ge, op1=ALU.mult), R=[pos], W=[ov])
                C.op("dve", lambda e: e.tensor_tensor(out=dst[:, :], in0=pos[:, :], in1=iotaE[:, :], op=ALU.add), R=[pos, iotaE], W=[dst])
                C.op("dve", lambda e: e.tensor_tensor(out=dst[:, :], in0=dst[:, :], in1=ov[:, :], op=ALU.add), R=[dst, ov], W=[dst])
                sel_b = bass.AP(sel.t, 0, [[NEXP, 128], [0, 8], [1, NEXP]])
                dst_b = bass.AP(dst.t, 0, [[NEXP, 128], [0, 8], [1, NEXP]])
                wc_b = bass.AP(Wc.t, 0, [[NEXP, 128], [0, 8], [1, NEXP]])
                pt = self.tbank()
                for kc in range(2):
                    self.tr(pt, pt[:, kc * 128:(kc + 1) * 128], emb[:, kc * 128:(kc + 1) * 128], self.identb[:, :], R=[emb, self.identb])
                C.op("act", lambda e: e.activation(out=emT[:, :, :].rearrange("p a b -> p (a b)"), in_=pt[:, 0:256], func=AF.Copy), R=[pt], W=[emT])
                pb = self.abank()
                for kc in range(2):
                    self.mm(pb, pb[:, 0:NEXP], emT[:, kc, :], SUF[:, kc, :], R=[emT, SUF], start=(kc == 0), stop=(kc == 1))
                C.op("act", lambda e: e.activation(out=jr[:, :], in_=pb[:, 0:NEXP], func=AF.Copy), R=[pb], W=[jr])
                C.op("dve", lambda e: e.tensor_tensor(out=dst[:, :], in0=dst[:, :], in1=em[:, :], op=ALU.mult), R=[dst, em], W=[dst])
                jr_b = bass.AP(jr.t, 0, [[NEXP, 128], [0, 8], [1, NEXP]])
                C.op("dve", lambda e: e.tensor_tensor(out=oh[:, :, :], in0=jr_b, in1=jv[:, :, :], op=ALU.is_equal), R=[jr, jv], W=[oh])
                C.op("pool", lambda e: e.tensor_tensor(out=tmp[:, :, :], in0=oh[:, :, :], in1=dst_b, op=ALU.mult), R=[oh, dst], W=[tmp])
                C.op("dve", lambda e: e.tensor_reduce(out=dj[:, :], in_=tmp[:, :, :], axis=AX.X, op=ALU.add), R=[tmp], W=[dj])
                C.op("dve", lambda e: e.tensor_copy(out=dji[:, :], in_=dj[:, :]), R=[dj], W=[dji])
                C.op("pool", lambda e: e.tensor_tensor(out=tmp[:, :, :], in0=oh[:, :, :], in1=wc_b, op=ALU.mult), R=[oh, Wc, tmp], W=[tmp])
                C.op("dve", lambda e: e.tensor_reduce(out=rowd[:, :, 2], in_=tmp[:, :, :], axis=AX.X, op=ALU.add), R=[tmp], W=[rowd])
                C.op("pool", lambda e: e.iota(rowd[:, :, 0], pattern=[[0, 8]], base=r0, channel_multiplier=1, allow_small_or_imprecise_dtypes=True), R=[rowd], W=[rowd])
                C.op("pool", lambda e: e.iota(rowd[:, :, 1], pattern=[[1, 8]], base=r0 * 8, channel_multiplier=8, allow_small_or_imprecise_dtypes=True), R=[rowd], W=[rowd])
                C.op("pool", lambda e: e.memset(rowd[:, :, 3], 0.0), R=[rowd], W=[rowd])
                for j in range(8):
                    C.dma(lambda e: e.indirect_dma_start(out=self.L_d.t[:, :], out_offset=bass.IndirectOffsetOnAxis(ap=dji[:, j:j + 1], axis=0), in_=rowd[:, j, :], in_offset=None,
                                                         bounds_check=self.bnd["L"], oob_is_err=False), R=[rowd, dji, self.L_d], W=[], q="pool")
                pb = self.abank()
                for blk in range(4):
                    for k in range(8):
                        self.mm(pb, pb[:, blk * 128:(blk + 1) * 128], Wsgu[:, k, blk * 128:(blk + 1) * 128], u2Tb[:, k, :], R=[Wsgu, u2Tb], start=(k == 0), stop=(k == 7))
                C.op("act", lambda e: e.activation(out=sg[:, :], in_=pb[:, 0:256], func=AF.Silu), R=[pb], W=[sg])
                C.op("dve", lambda e: e.tensor_tensor(out=hs[:, :, :].rearrange("p a b -> p (a b)"), in0=sg[:, :], in1=pb[:, 256:512], op=ALU.mult), R=[sg, pb], W=[hs])
                for n in range(2):
                    pb = self.abank()
                    for fb in range(2):
                        self.mm(pb, pb[:, 0:512], hs[:, fb, :], Wsd[:, fb, n * 512:(n + 1) * 512], R=[hs, Wsd], start=(fb == 0), stop=(fb == 1))
                    C.op("act", lambda e: e.activation(out=sho[:, n * 512:(n + 1) * 512], in_=pb[:, 0:512], func=AF.Copy), R=[pb], W=[sho])
                C.dma(lambda e: e.dma_start(out=self.sh_d.t[r0:r0 + 128, :], in_=sho[:, :]), R=[sho], W=[self.sh_d])

    def experts(self, moe_g, moe_u, moe_d):
        C = self.C
        NB = CAP // 128
        with Scope(C):
            Wf = [C.sb([128, 6144], F32, "Wf%d" % i) for i in range(3)]
            Wgu = [C.sb([128, 8, 512], BF16, "Wgu%d" % i) for i in range(2)]
            Wd = [C.sb([128, 2, D], BF16, "Wd%d" % i) for i in range(2)]
            Lt = [C.sb([128, NB, 4], F32, "Lt%d" % i) for i in range(3)]
            Li = [C.sb([128, NB, 2], I32, "Lti%d" % i) for i in range(4)]
            Lw = [C.sb([128, NB], F32, "Lw%d" % i) for i in range(4)]
            xg = [[C.sb([128, D], BF16, "xg%d_%d" % (i, j)) for j in range(NB)] for i in range(2)]
            xgT = [C.sb([128, 8, CAP], BF16, "xgT%d" % i) for i in range(2)]
            sg = [C.sb([128, CAP], F32, "e_sg%d" % i) for i in range(2)]
            hb = [C.sb([128, 2, CAP], BF16, "e_hb%d" % i) for i in range(2)]
            yo = [[C.sb([128, D], F32, "yo%d_%d" % (i, j)) for j in range(NB)] for i in range(2)]

            def load(e_):
                b = e_ % 3
                C.dma(lambda e: e.dma_start(out=Wf[b][:, 0:2048].rearrange("p (k f) -> p k f", k=8), in_=moe_g.t[e_ * D:(e_ + 1) * D, :].rearrange("(p k) f -> p k f", k=8)), R=[moe_g], W=[Wf[b]])
                C.dma(lambda e: e.dma_start(out=Wf[b][:, 2048:4096].rearrange("p (k f) -> p k f", k=8), in_=moe_u.t[e_ * D:(e_ + 1) * D, :].rearrange("(p k) f -> p k f", k=8)), R=[moe_u], W=[Wf[b]])
                C.dma(lambda e: e.dma_start(out=Wf[b][:, 4096:6144].rearrange("p (k n) -> p k n", k=2), in_=moe_d.t[e_ * 256:(e_ + 1) * 256, :].rearrange("(k p) n -> p k n", p=128)), R=[moe_d], W=[Wf[b]])
                C.dma(lambda e: e.dma_start(out=Lt[b][:, :, :], in_=self.L_d.t[e_ * CAP:(e_ + 1) * CAP, :].rearrange("(a p) n -> p a n", p=128)), R=[self.L_d], W=[Lt[b]])

            def small(e_):
                b4 = e_ % 4; b3 = e_ % 3
                C.op("dve", lambda e: e.tensor_copy(out=Li[b4][:, :, :], in_=Lt[b3][:, :, 0:2]), R=[Lt[b3]], W=[Li[b4]])
                C.op("dve", lambda e: e.tensor_copy(out=Lw[b4][:, :], in_=Lt[b3][:, :, 2]), R=[Lt[b3]], W=[Lw[b4]])

            def castW(e_):
                b = e_ % 2; b3 = e_ % 3
                C.op("act", lambda e: e.activation(out=Wgu[b][:, :, 0:256], in_=Wf[b3][:, 0:2048].rearrange("p (k f) -> p k f", k=8), func=AF.Copy), R=[Wf[b3]], W=[Wgu[b]])
                C.op("act", lambda e: e.activation(out=Wgu[b][:, :, 256:512], in_=Wf[b3][:, 2048:4096].rearrange("p (k f) -> p k f", k=8), func=AF.Copy), R=[Wf[b3]], W=[Wgu[b]])
                C.op("act", lambda e: e.activation(out=Wd[b][:, :, :], in_=Wf[b3][:, 4096:6144].rearrange("p (k n) -> p k n", k=2), func=AF.Copy), R=[Wf[b3]], W=[Wd[b]])

            def gather(e_):
                b = e_ % 2
                for blk in range(NB):
                    C.dma(lambda e: e.indirect_dma_start(out=xg[b][blk][:, :], out_offset=None, in_=self.u2_d.t[:, :], in_offset=bass.IndirectOffsetOnAxis(ap=Li[e_ % 4][:, blk, 0:1], axis=0),
                                                         bounds_check=self.bnd["u2"], oob_is_err=False), R=[self.u2_d, Li[e_ % 4]], W=[xg[b][blk]], q="pool")

            def transp(e_):
                b = e_ % 2
                for blk in range(NB):
                    pt = self.tbank()
                    for k in range(8):
                        self.tr(pt, pt[:, k * 128:(k + 1) * 128], xg[b][blk][:, k:D:8], self.identb[:, :], R=[xg[b][blk], self.identb])
                    C.op("dve", lambda e: e.tensor_copy(out=xgT[b][:, :, blk * 128:(blk + 1) * 128], in_=pt[:, :].rearrange("p (k n) -> p k n", k=8)), R=[pt], W=[xgT[b]])

            load(0); load(1); load(2); small(0); gather(0); castW(0); transp(0)
            for e_ in range(NEXP):
                b = e_ % 2
                if e_ + 1 < NEXP:
                    small(e_ + 1); gather(e_ + 1)
                pbs = []
                for j in range(4):
                    pb = self.abank()
                    for k in range(8):
                        self.mm(pb, pb[:, 0:CAP], Wgu[b][:, k, j * 128:(j + 1) * 128], xgT[b][:, k, :], R=[Wgu[b], xgT[b]], start=(k == 0), stop=(k == 7))
                    pbs.append(pb)
                for fb in range(2):
                    C.op("act", lambda e: e.activation(out=sg[fb][:, :], in_=pbs[fb][:, 0:CAP], func=AF.Silu), R=[pbs[fb]], W=[sg[fb]])
                    C.op("dve", lambda e: e.tensor_tensor(out=hb[b][:, fb, :], in0=sg[fb][:, :], in1=pbs[2 + fb][:, 0:CAP], op=ALU.mult), R=[sg[fb], pbs[2 + fb]], W=[hb[b]])
                if e_ + 1 < NEXP:
                    castW(e_ + 1)
                    transp(e_ + 1)
                for blk in range(NB):
                    for n in range(2):
                        pb = self.abank()
                        for fb in range(2):
                            self.mm(pb, pb[:, 0:512], hb[b][:, fb, blk * 128:(blk + 1) * 128], Wd[b][:, fb, n * 512:(n + 1) * 512], R=[hb[b], Wd[b]], start=(fb == 0), stop=(fb == 1))
                        C.op("dve", lambda e: e.tensor_scalar(out=yo[b][blk][:, n * 512:(n + 1) * 512], in0=pb[:, 0:512], scalar1=Lw[e_ % 4][:, blk:blk + 1], scalar2=None, op0=ALU.mult), R=[pb, Lw[e_ % 4]], W=[yo[b][blk]])
                    C.dma(lambda e: e.indirect_dma_start(out=self.Y_d.t[:, :], out_offset=bass.IndirectOffsetOnAxis(ap=Li[e_ % 4][:, blk, 1:2], axis=0), in_=yo[b][blk][:, :], in_offset=None,
                                                         bounds_check=self.bnd["Y"], oob_is_err=False), R=[yo[b][blk], Li[e_ % 4], self.Y_d], W=[], q="pool")
                if e_ + 3 < NEXP:
                    load(e_ + 3)

    def final(self, vecs, out):
        C = self.C
        with Scope(C):
            gate = C.sb([128, D], F32, "fgate"); gfin = C.sb([128, D], F32, "gfin")
            self.col_to_bcast(self.gcol, 8, gate)
            C.dma(lambda e: e.dma_start(out=gfin[:, :], in_=vecs.t[256:384, :]), R=[vecs], W=[gfin])
            Yt = [C.sb([128, 8, D], F32, "Yt%d" % i) for i in range(2)]
            x1 = [C.sb([128, D], F32, "fx1%d" % i) for i in range(2)]; sh = [C.sb([128, D], F32, "fsh%d" % i) for i in range(2)]
            acc = C.sb([128, D], F32, "facc"); junk = C.sb([128, D], BF16, "fjunk"); ss = C.sb([128, 1], F32, "fss"); rstd = C.sb([128, 1], F32, "frstd")
            ot = [C.sb([128, D], F32, "fot%d" % i) for i in range(2)]
            for t in range(32):
                r0 = t * 128; b = t % 2
                C.dma(lambda e: e.dma_start(out=Yt[b][:, :, :], in_=self.Y_d.t[r0 * 8:(r0 + 128) * 8, :].rearrange("(p j) n -> p j n", j=8)), R=[self.Y_d], W=[Yt[b]])
                C.dma(lambda e: e.dma_start(out=x1[b][:, :], in_=self.x1_d.t[r0:r0 + 128, :]), R=[self.x1_d], W=[x1[b]])
                C.dma(lambda e: e.dma_start(out=sh[b][:, :], in_=self.sh_d.t[r0:r0 + 128, :]), R=[self.sh_d], W=[sh[b]])
                C.op("dve", lambda e: e.tensor_reduce(out=acc[:, :], in_=Yt[b][:, :, :].rearrange("p j n -> p n j"), axis=AX.X, op=ALU.add), R=[Yt[b]], W=[acc])
                C.op("pool", lambda e: e.tensor_tensor(out=acc[:, :], in0=acc[:, :], in1=sh[b][:, :], op=ALU.add), R=[acc, sh[b]], W=[acc])
                C.op("dve", lambda e: e.tensor_tensor(out=acc[:, :], in0=acc[:, :], in1=gate[:, :], op=ALU.mult), R=[acc, gate], W=[acc])
                C.op("pool", lambda e: e.tensor_tensor(out=acc[:, :], in0=acc[:, :], in1=x1[b][:, :], op=ALU.add), R=[acc, x1[b]], W=[acc])
                self.rms_rstd(acc[:, :], acc, junk, ss, rstd, D)
                C.op("dve", lambda e: e.scalar_tensor_tensor(out=ot[b][:, :], in0=acc[:, :], scalar=rstd[:, 0:1], in1=gfin[:, :], op0=ALU.mult, op1=ALU.mult), R=[acc, rstd, gfin], W=[ot[b]])
                C.dma(lambda e: e.dma_start(out=out.t[r0:r0 + 128, :], in_=ot[b][:, :]), R=[ot[b]], W=[out])


def col_layout(v):
    return np.ascontiguousarray(np.asarray(v).reshape(8, 128).T)


def rep128(v):
    v = np.asarray(v).reshape(1, -1)
    return np.ascontiguousarray(np.broadcast_to(v, (128, v.shape[1])))


def host_layout(inp):
    f32 = np.float32
    x = np.asarray(inp["x"], f32); ctx = np.asarray(inp["ctx"], f32)
    w_in = np.ascontiguousarray(np.asarray(inp["w_in"], f32)[0])
    wsc = w_in[:, 1024:1024 + WS]
    cxw = np.asarray(inp["conv_xbc_w"], f32)[0]; cxb = np.asarray(inp["conv_xbc_b"], f32)[0]
    cqw = np.asarray(inp["conv_qk_w"], f32)[0]; cqb = np.asarray(inp["conv_qk_b"], f32)[0]
    dtb = np.asarray(inp["ssd_dt_bias"], f32)[0]; alog = np.asarray(inp["ssd_a_log"], f32)[0]
    ib = np.asarray(inp["mlstm_i_bias"], f32)[0]; fb = np.asarray(inp["mlstm_f_bias"], f32)[0]

    def taps(w, nt, flip):
        ww = w[::-1] if flip else w
        return np.ascontiguousarray(ww.reshape(5, nt, 128).transpose(2, 1, 0).reshape(128, nt * 5))

    def bias(b, nt):
        return np.ascontiguousarray(b.reshape(nt, 128).T)

    def wl(d):
        w = wsc.copy()
        if d == 1:
            w[:, O_DT:O_DT + 16] = wsc[:, O_DT + 16:O_DT + 32]
            w[:, O_GT:O_GT + 16] = wsc[:, O_GT + 16:O_GT + 32]
        return w

    def smallset(d):
        return rep128(np.concatenate([dtb[d], alog[d], ib[d], fb[d]]))
    wl_d = [wl(0), wl(1)]
    shared = dict(
        ada_w=np.ascontiguousarray(np.asarray(inp["ada_w"], f32)[0]),
        ada_b=np.ascontiguousarray(np.asarray(inp["ada_b"], f32)[0].reshape(48, 128).T),
        gcols=np.concatenate([col_layout(inp["norm_mix_g"][0]), col_layout(inp["norm_ffn_g"][0])], axis=1).astype(f32),
        w_in=w_in,
        ssd_d=rep128(np.asarray(inp["ssd_d"], f32)[0]),
        vecs=np.concatenate([rep128(inp["ssd_norm_g"][0]), rep128(inp["mlstm_norm_g"][0]), rep128(inp["norm_final_g"])], axis=0).astype(f32),
        w_ssd_out=np.ascontiguousarray(np.asarray(inp["w_ssd_out"], f32)[0]),
        w_mlstm_out=np.ascontiguousarray(np.asarray(inp["w_mlstm_out"], f32)[0]),
        w_out=np.ascontiguousarray(np.asarray(inp["w_out"], f32)[0]),
        router_w=np.ascontiguousarray(np.asarray(inp["router_w"], f32)[0]),
        router_b=rep128(np.asarray(inp["router_bias"], f32)[0]),
        moe_w_gate=np.asarray(inp["moe_w_gate"], f32)[0].reshape(NEXP * D, 256),
        moe_w_up=np.asarray(inp["moe_w_up"], f32)[0].reshape(NEXP * D, 256),
        moe_w_down=np.asarray(inp["moe_w_down"], f32)[0].reshape(NEXP * 256, D),
        shared_w_gate=np.ascontiguousarray(np.asarray(inp["shared_w_gate"], f32)[0]),
        shared_w_up=np.ascontiguousarray(np.asarray(inp["shared_w_up"], f32)[0]),
        shared_w_down=np.ascontiguousarray(np.asarray(inp["shared_w_down"], f32)[0]),
    )
    maps = []
    for c in range(NCORE):
        b, s = c // 4, c % 4
        m = dict(shared)
        m["xo"] = np.ascontiguousarray(x[b, s * SEG:(s + 1) * SEG])
        pre = []; dirs = []
        for j in range(3):
            if j < s:
                pre.append(x[b, j * SEG:(j + 1) * SEG]); dirs.append(0)
            else:
                g = 3 - (j - s)
                pre.append(x[b, g * SEG:(g + 1) * SEG][::-1]); dirs.append(1)
        m["xpre"] = np.ascontiguousarray(np.concatenate(pre, axis=0))
        m["ctx2"] = np.ascontiguousarray(np.concatenate([ctx[b], ctx[b][::-1]], axis=0))
        cv = np.zeros((128, 16), f32)
        cv[:, 0::2] = col_layout(inp["c"][b]); cv[:, 1::2] = col_layout(inp["c_ctx"])
        m["cvec"] = cv
        sd = [0, 1] + dirs
        m["wlite"] = np.ascontiguousarray(np.concatenate([wl_d[d] for d in sd], axis=0))
        m["cwx"] = np.concatenate([taps(cxw, 12, d == 1) for d in sd] + [taps(cxw, 12, False)], axis=0)
        m["cbx"] = np.concatenate([bias(cxb, 12)] * 6, axis=0)
        m["cwq"] = np.concatenate([taps(cqw, 8, d == 1) for d in sd] + [taps(cqw, 8, False)], axis=0)
        m["cbq"] = np.concatenate([bias(cqb, 8)] * 6, axis=0)
        m["small"] = np.concatenate([smallset(d) for d in sd] + [smallset(0), smallset(1)], axis=0)
        fl = np.zeros((128, 8), f32)
        for j in range(3):
            fl[:, j] = 1.0 if dirs[j] == 0 else 0.0
            fl[:, 4 + j] = 1.0 - fl[:, j]
        m["flags"] = fl
        maps.append(m)
    return maps


def kernel(**inputs):
    maps = host_layout(inputs)
    prog = Prog()
    nc = prog.build()
    maps = [{k: v for k, v in m.items() if k in prog.ins} for m in maps]
    res = run_bass_kernel_spmd(nc, maps, core_ids=list(range(NCORE)))
    out = np.zeros((2, 4 * SEG, D), np.float32)
    for c in range(NCORE):
        b, s = c // 4, c % 4
        out[b, s * SEG:(s + 1) * SEG] = res.results[c]["out"]
    return out
```

```python
from contextlib import ExitStack
import os
import numpy as np
import concourse.bass as bass
import concourse.mybir as mybir
from concourse.bass_utils import run_bass_kernel_spmd

F32 = mybir.dt.float32
BF16 = mybir.dt.bfloat16
I32 = mybir.dt.int32
ALU = mybir.AluOpType
AF = mybir.ActivationFunctionType
AX = mybir.AxisListType

NCORE = 8
D = 1024
SEG = 4096
NSEGC = 32
CTXL = 256
EPS = 1e-6
BIG = 30000.0
NEXP = 256
CAP = 512
WS = 3648
O_XBC, O_DT, O_QK, O_V, O_GT = 0, 1536, 1568, 2592, 3616


class Buf:
    __slots__ = ("t", "w", "r", "name", "off", "excl")

    def __init__(self, t, name, off=0, excl=False):
        self.t = t
        self.name = name
        self.off = off
        self.excl = excl
        self.w = {}
        self.r = {}

    def __getitem__(self, idx):
        return self.t[idx]


class Ctx:
    ENG = ("pe", "dve", "act", "pool", "sp")
    SAME_WIN = 4

    def __init__(self, nc, es, n_dma_sems=48):
        self.nc = nc
        self.es_stack = [es]
        self.e = {"pe": nc.tensor, "dve": nc.vector, "act": nc.scalar, "pool": nc.gpsimd, "sp": nc.sync}
        self.sem = {k: es.enter_context(nc.semaphore("s_" + k)) for k in self.ENG}
        self.cnt = {k: 0 for k in self.ENG}
        self.seen = {k: {} for k in self.ENG}
        self.dsem = [es.enter_context(nc.semaphore("d%d" % i)) for i in range(n_dma_sems)]
        self.dcnt = [0] * n_dma_sems
        self.dnext = 0
        self.nid = 0
        self.n_inst = 0

    @property
    def es(self):
        return self.es_stack[-1]

    def sb(self, shape, dt=F32, name=None):
        self.nid += 1
        name = "%s_%d" % (name or "sb", self.nid)
        return Buf(self.es.enter_context(self.nc.sbuf_tensor(name, list(shape), dt)), name)

    def ps(self, shape, dt=F32, name=None):
        self.nid += 1
        name = "%s_%d" % (name or "ps", self.nid)
        return Buf(self.es.enter_context(self.nc.psum_tensor(name, list(shape), dt)), name, excl=True)

    def dram(self, shape, dt=F32, name=None):
        self.nid += 1
        name = name or ("dr_%d" % self.nid)
        return Buf(self.nc.dram_tensor(name, list(shape), dt, kind="Internal"), name)

    def _wait(self, eng, key, val):
        if self.seen[eng].get(key, 0) >= val:
            return
        if isinstance(key, int):
            self.e[eng].wait_ge(self.dsem[key], val)
        else:
            if key == eng and (eng == "pe" or val <= self.cnt[eng] - self.SAME_WIN):
                return
            self.e[eng].wait_ge(self.sem[key], val)
        self.seen[eng][key] = val

    def _deps(self, eng, R, W):
        for b in R:
            for k, v in b.w.items():
                self._wait(eng, k, v)
            if b.excl:
                for k, v in b.r.items():
                    if k != eng:
                        self._wait(eng, k, v)
        for b in W:
            for k, v in b.w.items():
                self._wait(eng, k, v)
            for k, v in b.r.items():
                self._wait(eng, k, v)

    def op(self, eng, fn, R=(), W=()):
        self._deps(eng, R, W)
        inst = fn(self.e[eng])
        self.cnt[eng] += 1
        idx = self.cnt[eng]
        inst.then_inc(self.sem[eng], 1)
        self.n_inst += 1
        for b in R:
            b.r[eng] = idx
        for b in W:
            b.w[eng] = idx
        return inst

    def dma(self, fn, R=(), W=(), q="sp"):
        k = self.dnext
        self.dnext = (self.dnext + 1) % len(self.dsem)
        if self.dcnt[k] > 0:
            self._wait(q, k, self.dcnt[k])
        self._deps(q, R, W)
        inst = fn(self.e[q])
        self.dcnt[k] += 16
        inst.then_inc(self.dsem[k], 16)
        self.n_inst += 1
        for b in R:
            b.r[k] = self.dcnt[k]
        for b in W:
            b.w[k] = self.dcnt[k]
        return inst

    def barrier(self):
        for eng in self.ENG:
            for k in self.ENG:
                if k != eng and self.cnt[k] > 0:
                    self._wait(eng, k, self.cnt[k])
            for k in range(len(self.dsem)):
                if self.dcnt[k] > 0:
                    self._wait(eng, k, self.dcnt[k])

    def finish(self, bufs, eng="sp"):
        for b in bufs:
            for k, v in b.w.items():
                self._wait(eng, k, v)


class Scope:
    def __init__(self, C):
        self.C = C

    def __enter__(self):
        self.es = ExitStack()
        self.es.__enter__()
        self.C.es_stack.append(self.es)
        return self

    def __exit__(self, *a):
        self.C.barrier()
        self.C.es_stack.pop()
        return self.es.__exit__(*a)


def bc(buf, col0, n, rep):
    t = buf.t
    Fsz = int(np.prod(t.shape[1:]))
    return bass.AP(t, col0, [[Fsz, t.shape[0]], [1, n], [0, rep]])


def bcp(buf, p0, npart, col0, n, rep):
    t = buf.t
    Fsz = int(np.prod(t.shape[1:]))
    return bass.AP(t, p0 * Fsz + col0, [[Fsz, npart], [1, n], [0, rep]])


def colbc(buf, col, rep):
    t = buf.t
    Fsz = int(np.prod(t.shape[1:]))
    return bass.AP(t, col, [[Fsz, t.shape[0]], [0, rep]])


class Prog:
    def __init__(self, dbg=None, stop_after=None, addD=True):
        self.addD = addD
        self.dbg = dbg or []
        self.stop_after = stop_after
        self.nc = bass.Bass("TRN2", target_bir_lowering=False)
        self.ins = {}
        self.outs = {}

    def inp(self, name, shape, dt=F32):
        b = Buf(self.nc.dram_tensor(name, list(shape), dt, kind="ExternalInput"), name)
        self.ins[name] = b
        return b

    def outp(self, name, shape, dt=F32):
        b = Buf(self.nc.dram_tensor(name, list(shape), dt, kind="ExternalOutput"), name)
        self.outs[name] = b
        return b

    def mm(self, ob, oap, lhsT, rhs, R, start=True, stop=True):
        self.C.op("pe", lambda e: e.matmul(oap, lhsT=lhsT, rhs=rhs, start=start, stop=stop), R=R, W=[ob])

    def tr(self, ob, oap, in_ap, ident_ap, R):
        self.C.op("pe", lambda e: e.transpose(out=oap, in_=in_ap, identity=ident_ap), R=R, W=[ob])

    def gbank(self):
        self._g = (self._g + 1) % len(self.G)
        return self.G[self._g]

    def tbank(self):
        self._t = (self._t + 1) % len(self.T)
        return self.T[self._t]

    def dslot(self):
        self._d = (self._d + 1) % len(self.DS)
        return self.DS[self._d]

    def dump(self, name, buf, ap, shape, dt=F32):
        if name in self.dbg:
            o = self.outp("dbg_" + name, shape, dt)
            self.C.dma(lambda e: e.dma_start(out=o.t.ap() if len(shape) == 0 else o.t[tuple(slice(None) for _ in shape)], in_=ap), R=[buf], W=[o])

    def build(self):
        nc = self.nc
        I = self.inp
        xo = I("xo", [SEG, D]); xpre = I("xpre", [3 * SEG, D]); ctx2 = I("ctx2", [2 * CTXL, D])
        cvec = I("cvec", [128, 16]); ada_w = I("ada_w", [D, 6 * D]); ada_b = I("ada_b", [128, 48])
        gcols = I("gcols", [128, 16])
        wlite = I("wlite", [5 * D, WS]); w_in = I("w_in", [D, 7744])
        cwx = I("cwx", [6 * 128, 60]); cbx = I("cbx", [6 * 128, 12]); cwq = I("cwq", [6 * 128, 40]); cbq = I("cbq", [6 * 128, 8])
        small = I("small", [7 * 128, 48]); flags = I("flags", [128, 8]); ssd_d = I("ssd_d", [128, 16])
        if self.stop_after is None or self.stop_after in ("merge", "route"):
            vecs = I("vecs", [3 * 128, D])
            w_so = I("w_ssd_out", [D, D]); w_mo = I("w_mlstm_out", [D, D]); w_o = I("w_out", [D, D])
            router_w = I("router_w", [D, NEXP]); router_b = I("router_b", [128, NEXP])
            sh_g = I("shared_w_gate", [D, 256]); sh_u = I("shared_w_up", [D, 256]); sh_d = I("shared_w_down", [256, D])
        if self.stop_after is None:
            moe_g = I("moe_w_gate", [NEXP * D, 256]); moe_u = I("moe_w_up", [NEXP * D, 256]); moe_d = I("moe_w_down", [NEXP * 256, D])
        out = self.outp("out", [SEG, D])

        with ExitStack() as es:
            C = self.C = Ctx(nc, es)
            self.bnd = {}
            for nm, val in (("L", NEXP * CAP - 1), ("u2", SEG + 127), ("Y", SEG * 8 - 1)):
                r = es.enter_context(nc.gpsimd.register("bnd_" + nm))
                nc.gpsimd.reg_mov(r, val)
                self.bnd[nm] = r
            self.build_consts()
            self.G = [C.ps([128, 512], F32, "G%d" % i) for i in range(2)]
            self.T = [C.ps([128, 1024], BF16, "T%d" % i) for i in range(2)]
            self.PY = C.ps([128, 512], F32, "PY"); self.PYI = C.ps([128, 512], F32, "PYI")
            self.DS = [C.ps([128, 512], F32, "DS%d" % i) for i in range(2)]
            self._g = self._t = self._d = 0
            self.y_d = [C.dram([SEG, D], F32, "y_d%d" % d) for d in range(2)]
            self.h_d = [C.dram([SEG, D], F32, "h_d%d" % d) for d in range(2)]
            later = self.stop_after is None or self.stop_after in ("merge", "route")
            self.uT_d = C.dram([8 * 128, 4096], BF16, "uT_d") if later else None
            self.x1_d = C.dram([SEG, D], F32, "x1_d")
            self.sh_d = C.dram([SEG, D], F32, "sh_d")
            self.u2_d = C.dram([SEG + 128, D], BF16, "u2_d")
            self.L_d = C.dram([NEXP * CAP, 4], F32, "L_d")
            self.Y_d = C.dram([SEG * 8, D], F32, "Y_d") if self.stop_after is None else None

            self.adaln(cvec, ada_w, ada_b, gcols)
            if self.stop_after == "adaln":
                return self.end()
            self.mixer_scans(xo, xpre, ctx2, wlite, w_in, cwx, cbx, cwq, cbq, small, flags, ssd_d)
            if self.stop_after == "ctx":
                return self.end()
            if self.stop_after == "scans":
                for d in range(2):
                    if "y_d" in self.dbg:
                        self.copy_dram(self.y_d[d], self.outp("dbg_y_d%d" % d, [SEG, D]))
                        self.copy_dram(self.h_d[d], self.outp("dbg_h_d%d" % d, [SEG, D]))
                return self.end()
            self.merge_pass(xo, w_in, vecs, w_so, w_mo, w_o)
            if self.stop_after == "merge":
                self.copy_dram(self.x1_d, self.outp("dbg_x1", [SEG, D]))
                return self.end()
            self.ffn_prep(vecs, router_w, router_b, sh_g, sh_u, sh_d)
            if self.stop_after == "route":
                self.copy_dram(self.x1_d, self.outp("dbg_x1", [SEG, D]))
                self.copy_dram(self.sh_d, self.outp("dbg_sh", [SEG, D]))
                self.copy_dram(self.L_d, self.outp("dbg_L", [NEXP * CAP, 4]), rows=0, flat=(128, NEXP * CAP * 4 // 128))
                return self.end()
            self.experts(moe_g, moe_u, moe_d)
            self.final(vecs, out)
            return self.end()

    def copy_dram(self, src, dst, rows=SEG, flat=None):
        C = self.C
        if flat is not None:
            with Scope(C):
                p, f = flat
                t = C.sb([p, f], F32, "cpf")
                C.dma(lambda e: e.dma_start(out=t[:, :], in_=src.t.ap().rearrange("(p a) n -> p (a n)", p=p)), R=[src], W=[t])
                C.dma(lambda e: e.dma_start(out=dst.t.ap().rearrange("(p a) n -> p (a n)", p=p), in_=t[:, :]), R=[t], W=[dst])
            return
        with Scope(C):
            tl = [C.sb([128, 4, D], F32, "cp") for _ in range(2)]
            for i in range(rows // 512):
                t = tl[i % 2]
                C.dma(lambda e: e.dma_start(out=t[:, :, :], in_=src.t[i * 512:(i + 1) * 512, :].rearrange("(a p) n -> p a n", p=128)), R=[src], W=[t])
                C.dma(lambda e: e.dma_start(out=dst.t[i * 512:(i + 1) * 512, :].rearrange("(a p) n -> p a n", p=128), in_=t[:, :, :]), R=[t], W=[dst])

    def end(self):
        self.C.finish(list(self.outs.values()))
        self.C.barrier()
        return self.nc

    def build_consts(self):
        C = self.C

        def tri(name, cm, pat, op, val=1.0, fill=0.0):
            t = C.sb([128, 128], F32, name)
            C.op("pool", lambda e: e.memset(t[:, :], val), W=[t])
            C.op("pool", lambda e: e.affine_select(out=t[:, :], in_=t[:, :], pattern=[[pat, 128]], compare_op=op, fill=fill, base=0, channel_multiplier=cm), R=[t], W=[t])
            return t

        self.identf = tri("identf", 1, -1, ALU.is_equal)
        self.Uf = tri("Uf", -1, 1, ALU.is_ge)
        self.Lf = tri("Lf", 1, -1, ALU.is_ge)
        self.SUf = tri("SUf", -1, 1, ALU.is_gt)
        self.NEGU = tri("NEGU", 1, -1, ALU.is_gt, val=-BIG)
        self.NEGL = tri("NEGL", -1, 1, ALU.is_gt, val=-BIG)
        self.onesf = C.sb([128, 128], F32, "onesf")
        C.op("pool", lambda e: e.memset(self.onesf[:, :], 1.0), W=[self.onesf])
        self.epsb = C.sb([128, 1], F32, "epsb")
        C.op("pool", lambda e: e.memset(self.epsb[:, :], EPS), W=[self.epsb])

        def tobf(src, name):
            t = C.sb([128, 128], BF16, name)
            C.op("dve", lambda e: e.tensor_copy(out=t[:, :], in_=src[:, :]), R=[src], W=[t])
            return t
        self.identb = tobf(self.identf, "identb")
        self.Ub = tobf(self.Uf, "Ub"); self.Lb = tobf(self.Lf, "Lb"); self.SUb = tobf(self.SUf, "SUb")
        self.onesb = tobf(self.onesf, "onesb")

    def col_to_bcast(self, colbuf, c0, dst):
        C = self.C
        for half in range(2):
            pb = self.gbank()
            for jj in range(4):
                j = half * 4 + jj
                self.mm(pb, pb[:, jj * 128:(jj + 1) * 128], colbc(colbuf, c0 + j, 128), self.identf[:, :], R=[colbuf, self.identf])
            C.op("act", lambda e: e.activation(out=dst[:, half * 512:(half + 1) * 512], in_=pb[:, 0:512], func=AF.Copy), R=[pb], W=[dst])

    def adaln(self, cvec, ada_w, ada_b, gcols):
        C = self.C
        self.mod = C.sb([128, 96], F32, "mod")
        self.gc = C.sb([128, 16], F32, "gc")
        C.dma(lambda e: e.dma_start(out=self.gc[:, :], in_=gcols.t[:, :]), R=[gcols], W=[self.gc])
        with Scope(C):
            cv = C.sb([128, 16], F32, "cv"); sg = C.sb([128, 16], F32, "sg"); sc = C.sb([128, 16], F32, "sc")
            ab = C.sb([128, 48], F32, "ab")
            C.dma(lambda e: e.dma_start(out=cv[:, :], in_=cvec.t[:, :]), R=[cvec], W=[cv])
            C.dma(lambda e: e.dma_start(out=ab[:, :], in_=ada_b.t[:, :]), R=[ada_b], W=[ab])
            C.op("act", lambda e: e.activation(out=sg[:, :], in_=cv[:, :], func=AF.Sigmoid), R=[cv], W=[sg])
            C.op("dve", lambda e: e.tensor_tensor(out=sc[:, :], in0=cv[:, :], in1=sg[:, :], op=ALU.mult), R=[cv, sg], W=[sc])
            wt = [C.sb([128, 6 * D], F32, "adaw%d" % i) for i in range(2)]
            macc = C.sb([128, 96], F32, "macc")
            C.op("dve", lambda e: e.tensor_copy(out=macc[:, :].rearrange("p (j w) -> p j w", w=2), in_=bc(ab, 0, 48, 2)), R=[ab], W=[macc])
            for k in range(8):
                w = wt[k % 2]
                pm = self.gbank()
                for hh in range(2):
                    C.dma(lambda e: e.dma_start(out=w[:, hh * 3072:(hh + 1) * 3072], in_=ada_w.t[k * 128:(k + 1) * 128, hh * 3072:(hh + 1) * 3072]), R=[ada_w], W=[w])
                for j in range(48):
                    self.mm(pm, pm[:, 2 * j:2 * j + 2], w[:, j * 128:(j + 1) * 128], sc[:, 2 * k:2 * k + 2], R=[w, sc], start=True, stop=True)
                C.op("dve", lambda e: e.tensor_tensor(out=macc[:, :], in0=macc[:, :], in1=pm[:, 0:96], op=ALU.add), R=[pm, macc], W=[macc])
            C.op("dve", lambda e: e.tensor_copy(out=self.mod[:, :], in_=macc[:, :]), R=[macc], W=[self.mod])
        if "mod" in self.dbg:
            self.dump_now("mod", self.mod, [128, 96])
        mod3 = self.mod[:, :].rearrange("p (j w) -> p j w", w=2)
        self.cols = C.sb([128, 48], F32, "cols")
        cols = self.cols

        def gs(dst0, scale_j0, which, g0):
            C.op("dve", lambda e: e.scalar_tensor_tensor(out=cols[:, dst0:dst0 + 8], in0=mod3[:, scale_j0:scale_j0 + 8, which], scalar=1.0, in1=self.gc[:, g0:g0 + 8], op0=ALU.add, op1=ALU.mult), R=[self.mod, self.gc], W=[cols])

        def cp(dst0, j0, which):
            C.op("dve", lambda e: e.tensor_copy(out=cols[:, dst0:dst0 + 8], in_=mod3[:, j0:j0 + 8, which]), R=[self.mod], W=[cols])
        gs(0, 8, 0, 0); cp(8, 0, 0); gs(16, 8, 1, 0); cp(24, 0, 1); gs(32, 32, 0, 8); cp(40, 24, 0)
        self.gcol = C.sb([128, 16], F32, "gcol")
        C.op("dve", lambda e: e.tensor_copy(out=self.gcol[:, 0:8], in_=mod3[:, 16:24, 0]), R=[self.mod], W=[self.gcol])
        C.op("dve", lambda e: e.tensor_copy(out=self.gcol[:, 8:16], in_=mod3[:, 40:48, 0]), R=[self.mod], W=[self.gcol])

    def mixer_scans(self, xo, xpre, ctx2, wlite, w_in, cwx, cbx, cwq, cbq, small, flags, ssd_d):
        C = self.C
        with Scope(C):
            K = self.K = type("K", (), {})()
            K.GS = C.sb([128, D], F32, "GS"); K.SH = C.sb([128, D], F32, "SH")
            K.W = C.sb([128, 8, WS], BF16, "Wscan")
            K.cwx = C.sb([128, 60], F32, "cwx"); K.cbx = C.sb([128, 12], F32, "cbx"); K.cwq = C.sb([128, 40], F32, "cwq"); K.cbq = C.sb([128, 8], F32, "cbq")
            K.small = C.sb([128, 48], F32, "small"); K.aneg = C.sb([128, 16], F32, "aneg")
            K.flags = C.sb([128, 8], F32, "flags"); K.Dh = C.sb([128, 16], F32, "Dh")
            C.dma(lambda e: e.dma_start(out=K.flags[:, :], in_=flags.t[:, :]), R=[flags], W=[K.flags])
            C.dma(lambda e: e.dma_start(out=K.Dh[:, :], in_=ssd_d.t[:, :]), R=[ssd_d], W=[K.Dh])
            K.xt = [C.sb([128, D], F32, "xt%d" % i) for i in range(2)]
            K.junk = C.sb([128, D], BF16, "junk"); K.xm = C.sb([128, D], F32, "xm"); K.xn = C.sb([128, D], BF16, "xn")
            K.ss = C.sb([128, 1], F32, "ss"); K.rstd = C.sb([128, 1], F32, "rstd")
            K.uT = C.sb([128, 8, 512], BF16, "uT")
            K.cv = C.sb([128, 12, 512], BF16, "cv"); K.cvq = C.sb([128, 8, 512], BF16, "cvq")
            K.acc = [C.sb([128, 512], F32, "acc%d" % i) for i in range(4)]
            K.raw = [C.sb([128, 512], F32, "raw%d" % i) for i in range(4)]
            K.nconv = 0
            K.Vtok = [C.sb([128, D], BF16, "Vtok%d" % i) for i in range(4)]
            K.SM = [C.sb([128, 48], F32, "SM%d" % i) for i in range(4)]
            K.sm = C.sb([128, 32], F32, "sm"); K.e1 = C.sb([128, 32], F32, "e1")
            K.S = [C.sb([128, 512], F32, "S%d" % g) for g in range(2)]
            K.Sbf = [C.sb([128, 512], BF16, "Sbf%d" % g) for g in range(2)]
            K.Cm = C.sb([128, 4, 128], F32, "Cm"); K.nm = C.sb([128, 4], F32, "nm"); K.mbc = C.sb([128, 8], F32, "mbc")
            K.Cbf = C.sb([128, 4, 128], BF16, "Cbf"); K.nbf = C.sb([128, 4], BF16, "nbf")
            K.sav = []
            for d in range(2):
                K.sav.append(dict(S=[C.sb([128, 512], F32, "SS%d%d" % (d, g)) for g in range(2)], Cm=C.sb([128, 512], F32, "SCm%d" % d),
                                  nm=C.sb([128, 4], F32, "Snm%d" % d), mbc=C.sb([128, 8], F32, "Smbc%d" % d)))
            K.cs = C.sb([128, 48], F32, "cs"); K.Xtok = C.sb([128, D], BF16, "Xtok"); K.BK = C.sb([128, 768], BF16, "BK")
            K.t16 = C.sb([128, 16], F32, "t16"); K.wS = C.sb([128, 16], F32, "wS"); K.expcum = C.sb([128, 16], F32, "expcum"); K.ncum = C.sb([128, 16], F32, "ncum")
            K.exptot = C.sb([128, 16], F32, "exptot")
            K.Xw = C.sb([128, D], BF16, "Xw"); K.Xdt = C.sb([128, D], BF16, "Xdt"); K.XD = C.sb([128, D], BF16, "XD")
            K.CBT = [C.sb([128, 128], BF16, "CBT%d" % g) for g in range(2)]
            K.E = [C.sb([128, 128], BF16, "E%d" % i) for i in range(2)]; K.M = [C.sb([128, 128], BF16, "M%d" % i) for i in range(2)]
            K.yout = [C.sb([128, D], F32, "yout%d" % i) for i in range(2)]; K.hout = [C.sb([128, D], F32, "hout%d" % i) for i in range(2)]
            K.a8 = C.sb([128, 8], F32, "a8"); K.amax = C.sb([8, 1], F32, "amax"); K.dg = C.sb([8, 8], F32, "dg")
            K.Mc = C.sb([128, 8], F32, "Mc"); K.cd = C.sb([128, 8], F32, "cd"); K.w8 = C.sb([128, 8], F32, "w8"); K.w8b = C.sb([128, 8], BF16, "w8b")
            K.t8 = C.sb([128, 8], F32, "t8"); K.fl = C.sb([128, 8], F32, "fl"); K.den = C.sb([128, 8], F32, "den"); K.rc = C.sb([128, 8], F32, "rc")
            K.Vw = C.sb([128, 8, 128], BF16, "Vw"); K.A = [C.sb([128, 128], BF16, "A%d" % h) for h in range(8)]
            K.nchunk = 0

            def P(**kw):
                return type("P", (), kw)()
            passes = [
                P(name="ctxF", src=ctx2, row0=0, n_sc=1, sc_tok=256, rows=256, wsrc=(wlite, 0), slot=0, sset=0, kind="ctx", full=False, rev=False, dd=0, init="zero", save=0, flag=None),
                P(name="ctxB", src=ctx2, row0=CTXL, n_sc=1, sc_tok=256, rows=256, wsrc=(wlite, 1), slot=1, sset=1, kind="ctx", full=False, rev=False, dd=0, init="zero", save=1, flag=None),
            ]
            for j in range(3):
                passes.append(P(name="pre%d" % j, src=xpre, row0=j * SEG, n_sc=8, sc_tok=512, rows=64, wsrc=(wlite, 2 + j), slot=2 + j, sset=2 + j, kind="lat", full=False, rev=False, dd=0, init="blend", save="blend", flag=j))
            passes.append(P(name="ownF", src=xo, row0=0, n_sc=8, sc_tok=512, rows=64, wsrc=(w_in, None), slot=5, sset=5, kind="lat", full=True, rev=False, dd=0, init=0, save=None, flag=None))
            passes.append(P(name="ownB", src=xo, row0=0, n_sc=8, sc_tok=512, rows=64, wsrc=(w_in, None), slot=5, sset=6, kind="lat", full=True, rev=True, dd=1, init=1, save=None, flag=None))
            if self.stop_after == "ctx":
                passes = passes[:2]
            cur_kind = None
            for Pp in passes:
                if Pp.kind != cur_kind:
                    cur_kind = Pp.kind
                    o = 0 if Pp.kind == "lat" else 16
                    self.col_to_bcast(self.cols, o, K.GS)
                    self.col_to_bcast(self.cols, o + 8, K.SH)
                self.run_pass(Pp, cwx, cbx, cwq, cbq, small)
            if "ctxstate" in self.dbg:
                for d in range(2):
                    for g in range(2):
                        self.dump_now("S%d%d" % (d, g), K.sav[d]["S"][g], [128, 512])
                    self.dump_now("Cm%d" % d, K.sav[d]["Cm"], [128, 512])
                    self.dump_now("mbc%d" % d, K.sav[d]["mbc"], [128, 8])
                    self.dump_now("nm%d" % d, K.sav[d]["nm"], [128, 4])

    def dump_now(self, name, buf, shape):
        o = self.outp("dbg_" + name, shape)
        self.C.dma(lambda e: e.dma_start(out=o.t[:, :], in_=buf[:, :]), R=[buf], W=[o])

    def run_pass(self, P, cwx, cbx, cwq, cbq, small):
        C = self.C; K = self.K
        wsrc, wi = P.wsrc
        for k in range(8):
            if wi is None:
                src_ap = wsrc.t[k * 128:(k + 1) * 128, 1024:1024 + WS]
            else:
                src_ap = wsrc.t[wi * D + k * 128: wi * D + (k + 1) * 128, :]
            C.dma(lambda e: e.dma_start(out=K.W[:, k, :], in_=src_ap), R=[wsrc], W=[K.W], q="pool")
        s = P.slot
        C.dma(lambda e: e.dma_start(out=K.cwx[:, :], in_=cwx.t[s * 128:(s + 1) * 128, :]), R=[cwx], W=[K.cwx])
        C.dma(lambda e: e.dma_start(out=K.cbx[:, :], in_=cbx.t[s * 128:(s + 1) * 128, :]), R=[cbx], W=[K.cbx])
        C.dma(lambda e: e.dma_start(out=K.cwq[:, :], in_=cwq.t[s * 128:(s + 1) * 128, :]), R=[cwq], W=[K.cwq])
        C.dma(lambda e: e.dma_start(out=K.cbq[:, :], in_=cbq.t[s * 128:(s + 1) * 128, :]), R=[cbq], W=[K.cbq])
        C.dma(lambda e: e.dma_start(out=K.small[:, :], in_=small.t[P.sset * 128:(P.sset + 1) * 128, :]), R=[small], W=[K.small])
        C.op("act", lambda e: e.activation(out=K.aneg[:, :], in_=K.small[:, 16:32], func=AF.Exp), R=[K.small], W=[K.aneg])
        C.op("dve", lambda e: e.tensor_scalar(out=K.aneg[:, :], in0=K.aneg[:, :], scalar1=-1.0, scalar2=None, op0=ALU.mult), R=[K.aneg], W=[K.aneg])
        st = [(K.S[0], lambda d: K.sav[d]["S"][0], 128), (K.S[1], lambda d: K.sav[d]["S"][1], 128), (K.Cm, lambda d: K.sav[d]["Cm"], 128),
              (K.nm, lambda d: K.sav[d]["nm"], 128), (K.mbc, lambda d: K.sav[d]["mbc"], 128)]

        def flat(b):
            return b[:, :, :].rearrange("p a b -> p (a b)") if len(b.t.shape) == 3 else b[:, :]
        if P.init == "zero":
            for cur, _, _ in st:
                C.op("pool", lambda e: e.memset(flat(cur), 0.0), W=[cur])
        elif P.init == "blend":
            f = K.flags[:, P.flag:P.flag + 1]; nf = K.flags[:, 4 + P.flag:5 + P.flag]
            for cur, sv, _ in st:
                a = sv(0); b = sv(1)
                C.op("dve", lambda e: e.tensor_scalar(out=flat(cur), in0=flat(a), scalar1=f, scalar2=None, op0=ALU.mult), R=[a, K.flags], W=[cur])
                C.op("dve", lambda e: e.scalar_tensor_tensor(out=flat(cur), in0=flat(b), scalar=nf, in1=flat(cur), op0=ALU.mult, op1=ALU.add), R=[b, K.flags, cur], W=[cur])
        else:
            for cur, sv, _ in st:
                a = sv(P.init)
                C.op("dve", lambda e: e.tensor_copy(out=flat(cur), in_=flat(a)), R=[a], W=[cur])
        for g in range(2):
            C.op("act", lambda e: e.activation(out=K.Sbf[g][:, :], in_=K.S[g][:, :], func=AF.Copy), R=[K.S[g]], W=[K.Sbf[g]])
        scs = list(range(P.n_sc))
        if P.rev:
            scs = scs[::-1]
        for sc in scs:
            self.prep_sc(P, sc)
            tiles = list(range(P.sc_tok // 128))
            if P.rev:
                tiles = tiles[::-1]
            for i in tiles:
                self.scan_chunk(P, sc, i)
        if P.save == "blend":
            f = K.flags[:, P.flag:P.flag + 1]; nf = K.flags[:, 4 + P.flag:5 + P.flag]
            for cur, sv, _ in st:
                a = sv(0); b = sv(1)
                C.op("dve", lambda e: e.tensor_scalar(out=flat(a), in0=flat(a), scalar1=nf, scalar2=None, op0=ALU.mult), R=[a, K.flags], W=[a])
                C.op("dve", lambda e: e.scalar_tensor_tensor(out=flat(a), in0=flat(cur), scalar=f, in1=flat(a), op0=ALU.mult, op1=ALU.add), R=[cur, K.flags, a], W=[a])
                C.op("dve", lambda e: e.tensor_scalar(out=flat(b), in0=flat(b), scalar1=f, scalar2=None, op0=ALU.mult), R=[b, K.flags], W=[b])
                C.op("dve", lambda e: e.scalar_tensor_tensor(out=flat(b), in0=flat(cur), scalar=nf, in1=flat(b), op0=ALU.mult, op1=ALU.add), R=[cur, K.flags, b], W=[b])
        elif P.save is not None:
            for cur, sv, _ in st:
                a = sv(P.save)
                C.op("dve", lambda e: e.tensor_copy(out=flat(a), in_=flat(cur)), R=[cur], W=[a])

    def prep_sc(self, P, sc):
        C = self.C; K = self.K
        T = P.sc_tok
        nt = T // 128
        for i in range(nt):
            xt = K.xt[i % 2]
            r0 = P.row0 + sc * T + i * 128
            C.dma(lambda e: e.dma_start(out=xt[:, :], in_=P.src.t[r0:r0 + 128, :]), R=[P.src], W=[xt])
            C.op("pool", lambda e: e.memset(K.ss[:, :], 0.0), W=[K.ss])
            C.op("act", lambda e: e.activation(out=K.junk[:, :], in_=xt[:, :], func=AF.Square, accum_out=K.ss[:, :]), R=[xt, K.ss], W=[K.junk, K.ss])
            C.op("act", lambda e: e.activation(out=K.rstd[:, :], in_=K.ss[:, :], func=AF.Sqrt, scale=1.0 / D, bias=self.epsb[:, :]), R=[K.ss, self.epsb], W=[K.rstd])
            C.op("dve", lambda e: e.reciprocal(out=K.rstd[:, :], in_=K.rstd[:, :]), R=[K.rstd], W=[K.rstd])
            C.op("dve", lambda e: e.scalar_tensor_tensor(out=K.xm[:, :], in0=xt[:, :], scalar=K.rstd[:, 0:1], in1=K.GS[:, :], op0=ALU.mult, op1=ALU.mult), R=[xt, K.rstd, K.GS], W=[K.xm])
            C.op("pool", lambda e: e.tensor_tensor(out=K.xn[:, :], in0=K.xm[:, :], in1=K.SH[:, :], op=ALU.add), R=[K.xm, K.SH], W=[K.xn])
            pt = self.tbank()
            for k in range(8):
                self.tr(pt, pt[:, k * 128:(k + 1) * 128], K.xn[:, k * 128:(k + 1) * 128], self.identb[:, :], R=[K.xn, self.identb])
            C.op("act", lambda e: e.activation(out=K.uT[:, :, i * 128:(i + 1) * 128], in_=pt[:, :].rearrange("p (k n) -> p k n", k=8), func=AF.Copy), R=[pt], W=[K.uT])
        if P.name == "ownB" and self.uT_d is not None:
            C.dma(lambda e: e.dma_start(out=self.uT_d.t[sc * 128:(sc + 1) * 128, :], in_=K.uT[:, :, :].rearrange("p k n -> p (k n)")), R=[K.uT], W=[self.uT_d])
        if "uT" in self.dbg and P.name == "ownF" and sc == 0:
            self.dump_bf("uT", K.uT, K.uT[:, :, :].rearrange("p k n -> p (k n)"), [128, 4096])
        xt_list = list(range(12)) if P.full else list(range(10))
        qt_list = list(range(8)) if P.full else list(range(4, 8))
        jobs = [("x", ct) for ct in xt_list] + [("q", ct) for ct in qt_list]
        for j0 in range(0, len(jobs), 2):
            ctxs = []
            for (kind, ct) in jobs[j0:j0 + 2]:
                off = (O_XBC if kind == "x" else O_QK) + ct * 128
                cw, cb, dst = (K.cwx, K.cbx, K.cv) if kind == "x" else (K.cwq, K.cbq, K.cvq)
                pb = self.gbank()
                for k in range(8):
                    self.mm(pb, pb[:, 0:T], K.W[:, k, off:off + 128], K.uT[:, k, 0:T], R=[K.W, K.uT], start=(k == 0), stop=(k == 7))
                K.nconv += 1
                raw = K.raw[K.nconv % 4]; acc = K.acc[K.nconv % 4]
                C.op("act", lambda e: e.activation(out=raw[:, 0:T], in_=pb[:, 0:T], func=AF.Copy), R=[pb], W=[raw])
                ctxs.append((ct, cw, cb, dst, raw, acc))
            for (ct, cw, cb, dst, raw, acc) in ctxs:
                C.op("dve", lambda e: e.tensor_scalar(out=acc[:, 0:T], in0=raw[:, 0:T], scalar1=cw[:, ct * 5 + 2:ct * 5 + 3], scalar2=cb[:, ct:ct + 1], op0=ALU.mult, op1=ALU.add), R=[raw, cw, cb], W=[acc])
            for j in (0, 1, 3, 4):
                o = j - 2
                lo = max(0, -o); hi = P.rows - max(0, o)
                for (ct, cw, cb, dst, raw, acc) in ctxs:
                    a3 = acc[:, 0:T].rearrange("p (r t) -> p r t", t=P.rows); p3 = raw[:, 0:T].rearrange("p (r t) -> p r t", t=P.rows)
                    C.op("dve", lambda e: e.scalar_tensor_tensor(out=a3[:, :, lo:hi], in0=p3[:, :, lo + o:hi + o], scalar=cw[:, ct * 5 + j:ct * 5 + j + 1], in1=a3[:, :, lo:hi], op0=ALU.mult, op1=ALU.add), R=[raw, cw, acc], W=[acc])
            for (ct, cw, cb, dst, raw, acc) in ctxs:
                C.op("act", lambda e: e.activation(out=dst[:, ct, 0:T], in_=acc[:, 0:T], func=AF.Silu), R=[acc], W=[dst])
        for i in range(nt):
            for n in range(2):
                pb = self.gbank()
                for k in range(8):
                    self.mm(pb, pb[:, 0:512], K.uT[:, k, i * 128:(i + 1) * 128], K.W[:, k, O_V + n * 512:O_V + (n + 1) * 512], R=[K.W, K.uT], start=(k == 0), stop=(k == 7))
                C.op("act", lambda e: e.activation(out=K.Vtok[i][:, n * 512:(n + 1) * 512], in_=pb[:, 0:512], func=AF.Copy), R=[pb], W=[K.Vtok[i]])
            pb = self.gbank()
            dto = O_DT + 16 * P.dd; gto = O_GT + 16 * P.dd
            for k in range(8):
                self.mm(pb, pb[:, 0:16], K.uT[:, k, i * 128:(i + 1) * 128], K.W[:, k, dto:dto + 16], R=[K.W, K.uT], start=(k == 0), stop=(k == 7))
            for k in range(8):
                self.mm(pb, pb[:, 16:32], K.uT[:, k, i * 128:(i + 1) * 128], K.W[:, k, gto:gto + 16], R=[K.W, K.uT], start=(k == 0), stop=(k == 7))
            SM = K.SM[i]
            C.op("dve", lambda e: e.tensor_tensor(out=K.sm[:, 0:16], in0=pb[:, 0:16], in1=K.small[:, 0:16], op=ALU.add), R=[pb, K.small], W=[K.sm])
            C.op("dve", lambda e: e.tensor_tensor(out=K.sm[:, 16:32], in0=pb[:, 16:32], in1=K.small[:, 32:48], op=ALU.add), R=[pb, K.small], W=[K.sm])
            C.op("act", lambda e: e.activation(out=K.e1[:, 0:16], in_=K.sm[:, 0:16], func=AF.Exp), R=[K.sm], W=[K.e1])
            C.op("act", lambda e: e.activation(out=K.e1[:, 16:24], in_=K.sm[:, 24:32], func=AF.Exp, scale=-1.0), R=[K.sm], W=[K.e1])
            C.op("act", lambda e: e.activation(out=SM[:, 24:40], in_=K.e1[:, 0:16], func=AF.Ln, bias=1.0, scale=1.0), R=[K.e1], W=[SM])
            C.op("act", lambda e: e.activation(out=K.e1[:, 24:32], in_=K.e1[:, 16:24], func=AF.Ln, bias=1.0, scale=1.0), R=[K.e1], W=[K.e1])
            C.op("dve", lambda e: e.tensor_scalar(out=SM[:, 16:24], in0=K.e1[:, 24:32], scalar1=-1.0, scalar2=None, op0=ALU.mult), R=[K.e1], W=[SM])
            C.op("dve", lambda e: e.tensor_tensor(out=SM[:, 0:16], in0=SM[:, 24:40], in1=K.aneg[:, :], op=ALU.mult), R=[SM, K.aneg], W=[SM])
            C.op("dve", lambda e: e.tensor_copy(out=SM[:, 40:48], in_=K.sm[:, 16:24]), R=[K.sm], W=[SM])
            if "SM" in self.dbg and P.name == "ownF" and sc == 0 and i == 0:
                self.dump_now("SM", SM, [128, 48])
        if "cv" in self.dbg and P.name == "ownF" and sc == 0:
            self.dump_bf("cv", K.cv, K.cv[:, :, :].rearrange("p k n -> p (k n)"), [128, 12 * 512])
            self.dump_bf("cvq", K.cvq, K.cvq[:, :, :].rearrange("p k n -> p (k n)"), [128, 8 * 512])

    def dump_bf(self, name, buf, ap, shape):
        C = self.C
        o = self.outp("dbg_" + name, shape)
        n = shape[1]
        for c0 in range(0, n, 2048):
            c1 = min(n, c0 + 2048)
            t = C.sb([128, 2048], F32, "dmp")
            C.op("dve", lambda e: e.tensor_copy(out=t[:, 0:c1 - c0], in_=ap[:, c0:c1]), R=[buf], W=[t])
            C.dma(lambda e: e.dma_start(out=o.t[:, c0:c1], in_=t[:, 0:c1 - c0]), R=[t], W=[o])

    def scan_chunk(self, P, sc, i):
        C = self.C; K = self.K
        SKIP = os.environ.get('KSKIP', '').split(',')
        if 'scan' in SKIP:
            return
        tok = slice(i * 128, (i + 1) * 128)
        Uf = self.Lf if P.rev else self.Uf
        Ub = self.Lb if P.rev else self.Ub
        NEGM = self.NEGL if P.rev else self.NEGU
        SM = K.SM[i]
        full = P.full
        K.nchunk += 1
        par = K.nchunk % 2
        sl = self.dslot()
        o = sl.off
        self.mm(sl, sl.t[:, o:o + 24], Uf[:, :], SM[:, 0:24], R=[Uf, SM])
        self.mm(sl, sl.t[:, o + 24:o + 48], self.onesf[:, :], SM[:, 0:24], R=[self.onesf, SM])
        C.op("dve", lambda e: e.tensor_copy(out=K.cs[:, :], in_=sl.t[:, o:o + 48]), R=[sl], W=[K.cs])
        pt = self.tbank()
        for c in range(8):
            self.tr(pt, pt[:, c * 128:(c + 1) * 128], K.cv[:, c, tok], self.identb[:, :], R=[K.cv, self.identb])
        C.op("act", lambda e: e.activation(out=K.Xtok[:, :], in_=pt[:, :], func=AF.Copy), R=[pt], W=[K.Xtok])
        pt = self.tbank()
        for c in range(2):
            self.tr(pt, pt[:, c * 128:(c + 1) * 128], K.cv[:, 8 + c, tok], self.identb[:, :], R=[K.cv, self.identb])
        for c in range(4):
            self.tr(pt, pt[:, 256 + c * 128:256 + (c + 1) * 128], K.cvq[:, 4 + c, tok], self.identb[:, :], R=[K.cvq, self.identb])
        C.op("dve", lambda e: e.tensor_copy(out=K.BK[:, :], in_=pt[:, 0:768]), R=[pt], W=[K.BK])
        C.op("dve", lambda e: e.tensor_tensor(out=K.t16[:, :], in0=K.cs[:, 24:40], in1=K.cs[:, 0:16], op=ALU.subtract), R=[K.cs], W=[K.t16])
        C.op("act", lambda e: e.activation(out=K.t16[:, :], in_=K.t16[:, :], func=AF.Exp), R=[K.t16], W=[K.t16])
        C.op("dve", lambda e: e.tensor_tensor(out=K.wS[:, :], in0=K.t16[:, :], in1=SM[:, 24:40], op=ALU.mult), R=[K.t16, SM], W=[K.wS])
        X3 = K.Xtok[:, :].rearrange("p (h d) -> p h d", h=16)
        C.op("dve", lambda e: e.tensor_tensor(out=K.Xw[:, :].rearrange("p (h d) -> p h d", h=16), in0=X3, in1=bc(K.wS, 0, 16, 64), op=ALU.mult), R=[K.Xtok, K.wS], W=[K.Xw])
        C.op("act", lambda e: e.activation(out=K.exptot[:, :], in_=K.cs[:, 24:40], func=AF.Exp), R=[K.cs], W=[K.exptot])
        if full:
            C.op("act", lambda e: e.activation(out=K.expcum[:, :], in_=K.cs[:, 0:16], func=AF.Exp), R=[K.cs], W=[K.expcum])
            C.op("dve", lambda e: e.tensor_scalar(out=K.ncum[:, :], in0=K.cs[:, 0:16], scalar1=-1.0, scalar2=None, op0=ALU.mult), R=[K.cs], W=[K.ncum])
            C.op("pool", lambda e: e.tensor_tensor(out=K.Xdt[:, :].rearrange("p (h d) -> p h d", h=16), in0=X3, in1=bc(SM, 24, 16, 64), op=ALU.mult), R=[K.Xtok, SM], W=[K.Xdt])
            if P.dd == 0 and self.addD:
                C.op("pool", lambda e: e.tensor_tensor(out=K.XD[:, :].rearrange("p (h d) -> p h d", h=16), in0=X3, in1=bc(K.Dh, 0, 16, 64), op=ALU.mult), R=[K.Xtok, K.Dh], W=[K.XD])
            yout = K.yout[par]
            for g in range(2):
                sl = self.dslot(); o = sl.off
                self.mm(sl, sl.t[:, o:o + 128], K.cv[:, 8 + g, tok], K.cv[:, 10 + g, tok], R=[K.cv])
                C.op("act", lambda e: e.activation(out=K.CBT[g][:, :], in_=sl.t[:, o:o + 128], func=AF.Copy), R=[sl], W=[K.CBT[g]])
                self.mm(self.PYI, self.PYI[:, :], K.cv[:, 10 + g, tok], K.Sbf[g][:, :], R=[K.cv, K.Sbf[g]])
                addD = (P.dd == 0) and self.addD
                if addD:
                    self.mm(self.PY, self.PY[:, :], self.identb[:, :], K.XD[:, g * 512:(g + 1) * 512], R=[self.identb, K.XD], start=True, stop=False)
                pend = None
                for hh in range(8):
                    h = g * 8 + hh
                    sl = self.dslot(); o = sl.off
                    self.mm(sl, sl.t[:, o:o + 128], colbc(SM, h, 128), Uf[:, :], R=[SM, Uf], start=True, stop=False)
                    self.mm(sl, sl.t[:, o:o + 128], self.identf[:, :], NEGM[:, :], R=[self.identf, NEGM], start=False, stop=True)
                    E = K.E[hh % 2]; M = K.M[hh % 2]
                    C.op("act", lambda e: e.activation(out=E[:, :], in_=sl.t[:, o:o + 128], func=AF.Exp, bias=K.ncum[:, h:h + 1], scale=1.0), R=[sl, K.ncum], W=[E])
                    C.op("dve", lambda e: e.tensor_tensor(out=M[:, :], in0=E[:, :], in1=K.CBT[g][:, :], op=ALU.mult), R=[E, K.CBT[g]], W=[M])
                    if pend is not None:
                        pM, phh, ph = pend
                        self.mm(self.PY, self.PY[:, phh * 64:(phh + 1) * 64], pM[:, :], K.Xdt[:, ph * 64:(ph + 1) * 64], R=[pM, K.Xdt], start=(not addD), stop=True)
                    pend = (M, hh, h)
                pM, phh, ph = pend
                self.mm(self.PY, self.PY[:, phh * 64:(phh + 1) * 64], pM[:, :], K.Xdt[:, ph * 64:(ph + 1) * 64], R=[pM, K.Xdt], start=(not addD), stop=True)
                yg = yout[:, g * 512:(g + 1) * 512]
                C.op("dve", lambda e: e.tensor_tensor(out=yg.rearrange("p (h d) -> p h d", h=8), in0=self.PYI[:, :].rearrange("p (h d) -> p h d", h=8), in1=bc(K.expcum, g * 8, 8, 64), op=ALU.mult), R=[self.PYI, K.expcum], W=[yout])
                C.op("dve", lambda e: e.tensor_tensor(out=yg, in0=yg, in1=self.PY[:, :], op=ALU.add), R=[self.PY, yout], W=[yout])
            r0 = sc * P.sc_tok + i * 128
            yd = self.y_d[P.dd]
            C.dma(lambda e: e.dma_start(out=yd.t[r0:r0 + 128, :], in_=yout[:, :]), R=[yout], W=[yd])
        for g in range(2):
            pb = self.gbank()
            self.mm(pb, pb[:, 0:512], K.BK[:, g * 128:(g + 1) * 128], K.Xw[:, g * 512:(g + 1) * 512], R=[K.BK, K.Xw])
            S3 = K.S[g][:, :].rearrange("p (h d) -> p h d", h=8)
            C.op("dve", lambda e: e.tensor_tensor(out=S3, in0=S3, in1=bc(K.exptot, g * 8, 8, 64), op=ALU.mult), R=[K.S[g], K.exptot], W=[K.S[g]])
            C.op("dve", lambda e: e.tensor_tensor(out=K.S[g][:, :], in0=K.S[g][:, :], in1=pb[:, 0:512], op=ALU.add), R=[K.S[g], pb], W=[K.S[g]])
            C.op("act", lambda e: e.activation(out=K.Sbf[g][:, :], in_=K.S[g][:, :], func=AF.Copy), R=[K.S[g]], W=[K.Sbf[g]])
        if 'mlstm' in SKIP:
            return
        C.op("dve", lambda e: e.tensor_tensor(out=K.a8[:, :], in0=SM[:, 40:48], in1=K.cs[:, 16:24], op=ALU.subtract), R=[SM, K.cs], W=[K.a8])
        sl = self.dslot(); o = sl.off
        self.tr(sl, sl.t[0:8, o:o + 128], K.a8[:, 0:8], self.identf[:, :], R=[K.a8, self.identf])
        C.op("dve", lambda e: e.reduce_max(out=K.amax[:, :], in_=sl.t[0:8, o:o + 128], axis=AX.X), R=[sl], W=[K.amax])
        C.op("dve", lambda e: e.tensor_scalar(out=K.dg[:, :], in0=self.identf[0:8, 0:8], scalar1=K.amax[:, 0:1], scalar2=None, op0=ALU.mult), R=[self.identf, K.amax], W=[K.dg])
        sl = self.dslot(); o = sl.off
        self.mm(sl, sl.t[:, o:o + 8], self.onesf[0:8, :], K.dg[:, :], R=[self.onesf, K.dg])
        C.op("dve", lambda e: e.tensor_tensor(out=K.Mc[:, :], in0=K.mbc[:, :], in1=sl.t[:, o:o + 8], op=ALU.max), R=[K.mbc, sl], W=[K.Mc])
        C.op("dve", lambda e: e.tensor_tensor(out=K.cd[:, :], in0=K.mbc[:, :], in1=K.Mc[:, :], op=ALU.subtract), R=[K.mbc, K.Mc], W=[K.cd])
        C.op("act", lambda e: e.activation(out=K.cd[:, :], in_=K.cd[:, :], func=AF.Exp), R=[K.cd], W=[K.cd])
        C.op("dve", lambda e: e.tensor_tensor(out=K.mbc[:, :], in0=K.cs[:, 40:48], in1=K.Mc[:, :], op=ALU.add), R=[K.cs, K.Mc], W=[K.mbc])
        C.op("dve", lambda e: e.tensor_tensor(out=K.w8[:, :], in0=K.a8[:, :], in1=K.Mc[:, :], op=ALU.subtract), R=[K.a8, K.Mc], W=[K.w8])
        C.op("act", lambda e: e.activation(out=K.w8[:, :], in_=K.w8[:, :], func=AF.Exp), R=[K.w8], W=[K.w8])
        C.op("dve", lambda e: e.tensor_copy(out=K.w8b[:, :], in_=K.w8[:, :]), R=[K.w8], W=[K.w8b])
        C.op("pool", lambda e: e.tensor_tensor(out=K.Vw[:, :, :], in0=K.Vtok[i][:, :].rearrange("p (h d) -> p h d", h=8), in1=bc(K.w8, 0, 8, 128), op=ALU.mult), R=[K.Vtok[i], K.w8], W=[K.Vw])
        for hh in range(2):
            ps_ = slice(hh * 64, (hh + 1) * 64)
            cdv = bass.AP(K.cd.t, hh * 64 * 8 + hh, [[8, 64], [2, 4], [0, 128]])
            C.op("dve", lambda e: e.tensor_tensor(out=K.Cm[ps_, :, :], in0=K.Cm[ps_, :, :], in1=cdv, op=ALU.mult), R=[K.Cm, K.cd], W=[K.Cm])
            cdn = bass.AP(K.cd.t, hh * 64 * 8 + hh, [[8, 64], [2, 4]])
            C.op("dve", lambda e: e.tensor_tensor(out=K.nm[ps_, :], in0=K.nm[ps_, :], in1=cdn, op=ALU.mult), R=[K.nm, K.cd], W=[K.nm])
        if full:
            C.op("act", lambda e: e.activation(out=K.Cbf[:, :, :], in_=K.Cm[:, :, :], func=AF.Copy, scale=0.125), R=[K.Cm], W=[K.Cbf])
            C.op("act", lambda e: e.activation(out=K.nbf[:, :], in_=K.nm[:, :], func=AF.Copy, scale=0.125), R=[K.nm], W=[K.nbf])
            C.op("dve", lambda e: e.tensor_tensor(out=K.t8[:, :], in0=K.cs[:, 16:24], in1=K.Mc[:, :], op=ALU.add), R=[K.cs, K.Mc], W=[K.t8])
            C.op("act", lambda e: e.activation(out=K.fl[:, :], in_=K.t8[:, :], func=AF.Exp, scale=-1.0), R=[K.t8], W=[K.fl])
            sden = self.PYI; od = 0
            for h in range(8):
                c = h // 2; pr = (h % 2) * 64
                KT = K.cvq[pr:pr + 64, 4 + c, tok]; QT = K.cvq[pr:pr + 64, c, tok]
                sl = self.dslot(); o = sl.off
                self.mm(sl, sl.t[:, o:o + 128], KT, QT, R=[K.cvq])
                A = K.A[h]
                C.op("dve", lambda e: e.scalar_tensor_tensor(out=A[:, :], in0=sl.t[:, o:o + 128], scalar=0.125, in1=Ub[:, :], op0=ALU.mult, op1=ALU.mult), R=[sl, Ub], W=[A])
                self.mm(sden, sden.t[:, od + h:od + h + 1], A[:, :], K.w8b[:, h:h + 1], R=[A, K.w8b], start=True, stop=False)
                self.mm(sden, sden.t[:, od + h:od + h + 1], QT, K.nbf[pr:pr + 64, c:c + 1], R=[K.cvq, K.nbf], start=False, stop=True)
            C.op("dve", lambda e: e.tensor_copy(out=K.den[:, :], in_=sden.t[:, od:od + 8]), R=[sden], W=[K.den])
            C.op("dve", lambda e: e.scalar_tensor_tensor(out=K.den[:, :], in0=K.den[:, :], scalar=-1.0, in1=K.den[:, :], op0=ALU.mult, op1=ALU.max), R=[K.den], W=[K.den])
            C.op("dve", lambda e: e.tensor_tensor(out=K.den[:, :], in0=K.den[:, :], in1=K.fl[:, :], op=ALU.max), R=[K.den, K.fl], W=[K.den])
            C.op("dve", lambda e: e.reciprocal(out=K.rc[:, :], in_=K.den[:, :]), R=[K.den], W=[K.rc])
            hout = K.hout[par]
            for h in range(8):
                c = h // 2; pr = (h % 2) * 64
                QT = K.cvq[pr:pr + 64, c, tok]
                sl = self.dslot(); o = sl.off
                self.mm(sl, sl.t[:, o:o + 128], K.A[h][:, :], K.Vw[:, h, :], R=[K.A[h], K.Vw], start=True, stop=False)
                self.mm(sl, sl.t[:, o:o + 128], QT, K.Cbf[pr:pr + 64, c, :], R=[K.cvq, K.Cbf], start=False, stop=True)
                C.op("act", lambda e: e.activation(out=hout[:, h * 128:(h + 1) * 128], in_=sl.t[:, o:o + 128], func=AF.Copy, scale=K.rc[:, h:h + 1]), R=[sl, K.rc], W=[hout])
            r0 = sc * P.sc_tok + i * 128
            hd = self.h_d[P.dd]
            C.dma(lambda e: e.dma_start(out=hd.t[r0:r0 + 128, :], in_=hout[:, :]), R=[hout], W=[hd])
        if 'mupd' in SKIP:
            return
        sn2 = self.PY; on = 0
        for h in range(8):
            c = h // 2; hh = h % 2; ps_ = slice(hh * 64, (hh + 1) * 64)
            Kp = K.BK[:, 256 + c * 128:256 + (c + 1) * 128]
            sl = self.dslot(); o = sl.off
            self.mm(sl, sl.t[:, o:o + 128], Kp, K.Vw[:, h, :], R=[K.BK, K.Vw])
            C.op("dve", lambda e: e.tensor_tensor(out=K.Cm[ps_, c, :], in0=K.Cm[ps_, c, :], in1=sl.t[ps_, o:o + 128], op=ALU.add), R=[K.Cm, sl], W=[K.Cm])
            self.mm(sn2, sn2.t[:, on + h:on + h + 1], Kp, K.w8b[:, h:h + 1], R=[K.BK, K.w8b])
        for hh in range(2):
            ps_ = slice(hh * 64, (hh + 1) * 64)
            src = bass.AP(sn2.t, hh * 64 * 512 + on + hh, [[512, 64], [2, 4]])
            C.op("dve", lambda e: e.tensor_tensor(out=K.nm[ps_, :], in0=K.nm[ps_, :], in1=src, op=ALU.add), R=[K.nm, sn2], W=[K.nm])


    def load_w_bf(self, dst, dram, row0, nk, c0, c1, dcol0=0):
        C = self.C
        for k in range(nk):
            C.dma(lambda e: e.dma_start(out=dst[:, k, dcol0:dcol0 + (c1 - c0)], in_=dram.t[row0 + k * 128:row0 + (k + 1) * 128, c0:c1]), R=[dram], W=[dst], q="pool")

    def abank(self):
        self._a = (self._a + 1) % len(self.Gall)
        return self.Gall[self._a]

    def rms_rstd(self, src_ap, srcbuf, junk, ss, rstd, n):
        C = self.C
        C.op("pool", lambda e: e.memset(ss[:, :], 0.0), W=[ss])
        C.op("act", lambda e: e.activation(out=junk[:, :], in_=src_ap, func=AF.Square, accum_out=ss[:, :]), R=[srcbuf, ss], W=[junk, ss])
        C.op("act", lambda e: e.activation(out=rstd[:, :], in_=ss[:, :], func=AF.Sqrt, scale=1.0 / n, bias=self.epsb[:, :]), R=[ss, self.epsb], W=[rstd])
        C.op("dve", lambda e: e.reciprocal(out=rstd[:, :], in_=rstd[:, :]), R=[rstd], W=[rstd])

    def proj(self, lhsT_buf, W, wc0, n512, evac):
        for n in range(n512):
            pb = self.abank()
            for k in range(8):
                self.mm(pb, pb[:, 0:512], lhsT_buf[:, k, :], W[:, k, wc0 + n * 512:wc0 + (n + 1) * 512], R=[lhsT_buf, W], start=(k == 0), stop=(k == 7))
            evac(n, pb)

    def transp8(self, src, dstT):
        C = self.C
        pt = self.tbank()
        for k in range(8):
            self.tr(pt, pt[:, k * 128:(k + 1) * 128], src[:, k * 128:(k + 1) * 128], self.identb[:, :], R=[src, self.identb])
        C.op("act", lambda e: e.activation(out=dstT[:, :, :], in_=pt[:, :].rearrange("p (k n) -> p k n", k=8), func=AF.Copy), R=[pt], W=[dstT])

    def merge_pass(self, xo, w_in, vecs, w_so, w_mo, w_o):
        C = self.C
        self.Gall = self.G + [self.PY, self.PYI] + self.DS
        self._a = 0
        with Scope(C):
            Wz = C.sb([128, 8, 4096], BF16, "Wz")
            self.load_w_bf(Wz, w_in, 0, 8, 0, 1024, 0)
            self.load_w_bf(Wz, w_in, 0, 8, 4672, 7744, 1024)
            Wso = C.sb([128, 8, D], BF16, "Wso"); Wmo = C.sb([128, 8, D], BF16, "Wmo"); Wo = C.sb([128, 8, D], BF16, "Wo")
            self.load_w_bf(Wso, w_so, 0, 8, 0, D); self.load_w_bf(Wmo, w_mo, 0, 8, 0, D); self.load_w_bf(Wo, w_o, 0, 8, 0, D)
            gssd = C.sb([128, D], F32, "gssd"); gml = C.sb([128, D], F32, "gml"); gate = C.sb([128, D], F32, "gate")
            C.dma(lambda e: e.dma_start(out=gssd[:, :], in_=vecs.t[0:128, :]), R=[vecs], W=[gssd])
            C.dma(lambda e: e.dma_start(out=gml[:, :], in_=vecs.t[128:256, :]), R=[vecs], W=[gml])
            self.col_to_bcast(self.gcol, 0, gate)
            xt = C.sb([128, D], F32, "m_xt"); uT = C.sb([128, 8, 128], BF16, "m_uT")
            A1 = C.sb([128, D], F32, "A1"); A2 = C.sb([128, D], F32, "A2"); A3 = C.sb([128, D], F32, "A3"); A4 = C.sb([128, D], F32, "A4")
            Zz = C.sb([128, D], F32, "Zz"); Zo = C.sb([128, D], F32, "Zo"); Zg1 = C.sb([128, D], F32, "Zg1"); Zg2 = C.sb([128, D], F32, "Zg2"); junk = C.sb([128, D], BF16, "m_junk")
            yn = C.sb([128, D], BF16, "yn"); hn = C.sb([128, D], BF16, "hn"); nT = C.sb([128, 8, 128], BF16, "nT"); nT2 = C.sb([128, 8, 128], BF16, "nT2"); nT3 = C.sb([128, 8, 128], BF16, "nT3")
            mg = C.sb([128, D], BF16, "mg"); x1 = C.sb([128, D], F32, "x1t")
            ss = C.sb([128, 1], F32, "m_ss"); rstd = C.sb([128, 1], F32, "m_rstd"); ss8 = C.sb([128, 8], F32, "ss8")
            for t in range(32):
                r0 = t * 128
                sc, i = t // 4, t % 4
                C.dma(lambda e: e.dma_start(out=xt[:, :], in_=xo.t[r0:r0 + 128, :]), R=[xo], W=[xt])
                C.dma(lambda e: e.dma_start(out=uT[:, :, :], in_=self.uT_d.t[sc * 128:(sc + 1) * 128, :].rearrange("p (k n) -> p k n", k=8)[:, :, i * 128:(i + 1) * 128]), R=[self.uT_d], W=[uT])
                C.dma(lambda e: e.dma_start(out=A1[:, :], in_=self.y_d[0].t[r0:r0 + 128, :]), R=[self.y_d[0]], W=[A1])
                C.dma(lambda e: e.dma_start(out=A2[:, :], in_=self.y_d[1].t[r0:r0 + 128, :]), R=[self.y_d[1]], W=[A2])
                C.dma(lambda e: e.dma_start(out=A3[:, :], in_=self.h_d[0].t[r0:r0 + 128, :]), R=[self.h_d[0]], W=[A3])
                C.dma(lambda e: e.dma_start(out=A4[:, :], in_=self.h_d[1].t[r0:r0 + 128, :]), R=[self.h_d[1]], W=[A4])
                def actev(dst, func):
                    return lambda n, pb: C.op("act", lambda e: e.activation(out=dst[:, n * 512:(n + 1) * 512], in_=pb[:, 0:512], func=func), R=[pb], W=[dst])
                self.proj(uT, Wz, 0, 2, actev(Zz, AF.Silu))
                self.proj(uT, Wz, 1024, 2, actev(Zo, AF.Sigmoid))
                self.proj(uT, Wz, 2048, 2, actev(Zg1, AF.Sigmoid))
                self.proj(uT, Wz, 3072, 2, actev(Zg2, AF.Sigmoid))
                C.op("dve", lambda e: e.tensor_tensor(out=A1[:, :], in0=A1[:, :], in1=A2[:, :], op=ALU.add), R=[A1, A2], W=[A1])
                C.op("dve", lambda e: e.tensor_tensor(out=A1[:, :], in0=A1[:, :], in1=Zz[:, :], op=ALU.mult), R=[A1, Zz], W=[A1])
                self.rms_rstd(A1[:, :], A1, junk, ss, rstd, D)
                C.op("dve", lambda e: e.scalar_tensor_tensor(out=yn[:, :], in0=A1[:, :], scalar=rstd[:, 0:1], in1=gssd[:, :], op0=ALU.mult, op1=ALU.mult), R=[A1, rstd, gssd], W=[yn])
                self.transp8(yn, nT)
                C.op("dve", lambda e: e.tensor_tensor(out=A3[:, :], in0=A3[:, :], in1=A4[:, :], op=ALU.add), R=[A3, A4], W=[A3])
                C.op("pool", lambda e: e.tensor_tensor(out=A4[:, :], in0=A3[:, :], in1=A3[:, :], op=ALU.mult), R=[A3], W=[A4])
                C.op("dve", lambda e: e.tensor_reduce(out=ss8[:, :], in_=A4[:, :].rearrange("p (h d) -> p h d", h=8), axis=AX.X, op=ALU.add), R=[A4], W=[ss8])
                C.op("act", lambda e: e.activation(out=ss8[:, :], in_=ss8[:, :], func=AF.Sqrt, scale=1.0 / 128, bias=self.epsb[:, :]), R=[ss8, self.epsb], W=[ss8])
                C.op("dve", lambda e: e.reciprocal(out=ss8[:, :], in_=ss8[:, :]), R=[ss8], W=[ss8])
                C.op("dve", lambda e: e.tensor_tensor(out=A3[:, :].rearrange("p (h d) -> p h d", h=8), in0=A3[:, :].rearrange("p (h d) -> p h d", h=8), in1=bc(ss8, 0, 8, 128), op=ALU.mult), R=[A3, ss8], W=[A3])
                C.op("pool", lambda e: e.tensor_tensor(out=A3[:, :], in0=A3[:, :], in1=gml[:, :], op=ALU.mult), R=[A3, gml], W=[A3])
                C.op("dve", lambda e: e.tensor_tensor(out=hn[:, :], in0=A3[:, :], in1=Zo[:, :], op=ALU.mult), R=[A3, Zo], W=[hn])
                self.transp8(hn, nT2)
                self.proj(nT, Wso, 0, 2, lambda n, pb: C.op("dve", lambda e: e.tensor_tensor(out=A1[:, n * 512:(n + 1) * 512], in0=pb[:, 0:512], in1=Zg1[:, n * 512:(n + 1) * 512], op=ALU.mult), R=[pb, Zg1], W=[A1]))
                self.proj(nT2, Wmo, 0, 2, lambda n, pb: C.op("dve", lambda e: e.tensor_tensor(out=A2[:, n * 512:(n + 1) * 512], in0=pb[:, 0:512], in1=Zg2[:, n * 512:(n + 1) * 512], op=ALU.mult), R=[pb, Zg2], W=[A2]))
                C.op("dve", lambda e: e.tensor_tensor(out=mg[:, :], in0=A1[:, :], in1=A2[:, :], op=ALU.add), R=[A1, A2], W=[mg])
                self.transp8(mg, nT3)
                self.proj(nT3, Wo, 0, 2, lambda n, pb: C.op("dve", lambda e: e.tensor_tensor(out=x1[:, n * 512:(n + 1) * 512], in0=pb[:, 0:512], in1=gate[:, n * 512:(n + 1) * 512], op=ALU.mult), R=[pb, gate], W=[x1]))
                C.op("pool", lambda e: e.tensor_tensor(out=x1[:, :], in0=x1[:, :], in1=xt[:, :], op=ALU.add), R=[x1, xt], W=[x1])
                C.dma(lambda e: e.dma_start(out=self.x1_d.t[r0:r0 + 128, :], in_=x1[:, :]), R=[x1], W=[self.x1_d])

    def ffn_prep(self, vecs, router_w, router_b, sh_g, sh_u, sh_d):
        C = self.C
        with Scope(C):
            if self.Y_d is not None:
                zt = C.sb([128, 2048], F32, "zt")
                C.op("pool", lambda e: e.memset(zt[:, :], 0.0), W=[zt])
                for i in range(SEG * 8 // 256):
                    C.dma(lambda e: e.dma_start(out=self.Y_d.t[i * 256:(i + 1) * 256, :].rearrange("(p a) n -> p (a n)", p=128), in_=zt[:, :]), R=[zt], W=[])
            GS2 = C.sb([128, D], F32, "GS2"); SH2 = C.sb([128, D], F32, "SH2")
            self.col_to_bcast(self.cols, 32, GS2); self.col_to_bcast(self.cols, 40, SH2)
            rw = C.sb([128, 8, NEXP], F32, "rw"); rb = C.sb([128, NEXP], F32, "rb")
            C.dma(lambda e: e.dma_start(out=rw[:, :, :], in_=router_w.t.ap().rearrange("(k p) n -> p k n", p=128)), R=[router_w], W=[rw])
            C.dma(lambda e: e.dma_start(out=rb[:, :], in_=router_b.t[:, :]), R=[router_b], W=[rb])
            Wsgu = C.sb([128, 8, 512], BF16, "Wsgu"); Wsd = C.sb([128, 2, D], BF16, "Wsd")
            self.load_w_bf(Wsgu, sh_g, 0, 8, 0, 256, 0); self.load_w_bf(Wsgu, sh_u, 0, 8, 0, 256, 256); self.load_w_bf(Wsd, sh_d, 0, 2, 0, D)
            iotaE = C.sb([128, NEXP], F32, "iotaE")
            C.op("pool", lambda e: e.iota(iotaE[:, :], pattern=[[1, NEXP]], base=0, channel_multiplier=0, allow_small_or_imprecise_dtypes=True), W=[iotaE])
            C.op("dve", lambda e: e.tensor_scalar(out=iotaE[:, :], in0=iotaE[:, :], scalar1=float(CAP), scalar2=None, op0=ALU.mult), R=[iotaE], W=[iotaE])
            cnt = C.sb([128, NEXP], F32, "cnt")
            C.op("pool", lambda e: e.memset(cnt[:, :], 0.0), W=[cnt])
            SUF = C.sb([128, 2, NEXP], BF16, "SUF")
            C.op("pool", lambda e: e.memset(SUF[:, :, :], 0.0), W=[SUF])
            C.op("dve", lambda e: e.tensor_copy(out=SUF[:, 0, 0:128], in_=self.SUb[:, :]), R=[self.SUb, SUF], W=[SUF])
            C.op("dve", lambda e: e.tensor_copy(out=SUF[:, 0, 128:256], in_=self.onesb[:, :]), R=[self.onesb, SUF], W=[SUF])
            C.op("dve", lambda e: e.tensor_copy(out=SUF[:, 1, 128:256], in_=self.SUb[:, :]), R=[self.SUb, SUF], W=[SUF])
            jv = C.sb([128, 8, NEXP], F32, "jv")
            C.op("pool", lambda e: e.iota(jv[:, :, :], pattern=[[1, 8], [0, NEXP]], base=0, channel_multiplier=0, allow_small_or_imprecise_dtypes=True), W=[jv])
            emT = C.sb([128, 2, 128], BF16, "emT"); jr = C.sb([128, NEXP], F32, "jr")
            Li = C.sb([128, NEXP * CAP // 128, 4], F32, "Li")
            C.op("pool", lambda e: e.memset(Li[:, :, :], 0.0), W=[Li])
            C.op("pool", lambda e: e.memset(Li[:, :, 0:1], 1.0e9), R=[Li], W=[Li])
            C.op("pool", lambda e: e.memset(Li[:, :, 1:2], 1.0e9), R=[Li], W=[Li])
            C.dma(lambda e: e.dma_start(out=self.L_d.t.ap().rearrange("(p a) n -> p a n", p=128), in_=Li[:, :, :]), R=[Li], W=[self.L_d])
            zb = C.sb([128, D], BF16, "zb")
            C.op("pool", lambda e: e.memset(zb[:, :], 0.0), W=[zb])
            C.dma(lambda e: e.dma_start(out=self.u2_d.t[SEG:SEG + 128, :], in_=zb[:, :]), R=[zb], W=[self.u2_d])
            x1 = C.sb([128, D], F32, "f_x1"); junk = C.sb([128, D], BF16, "f_junk"); ss = C.sb([128, 1], F32, "f_ss"); rstd = C.sb([128, 1], F32, "f_rstd")
            u2 = C.sb([128, D], F32, "u2"); u2b = C.sb([128, D], BF16, "u2b")
            u2Tf = C.sb([128, 8, 128], F32, "u2Tf"); u2Tb = C.sb([128, 8, 128], BF16, "u2Tb")
            sc_ = C.sb([128, NEXP], F32, "scores"); gr = C.sb([128, NEXP], F32, "grouped"); sel = C.sb([128, NEXP], F32, "sel")
            m8g = C.sb([128, 8, 8], F32, "m8g"); gs = C.sb([128, 8], F32, "gs"); g8 = C.sb([128, 8], F32, "g8"); pen = C.sb([128, 8], F32, "pen")
            e8 = C.sb([128, 8], F32, "e8"); em = C.sb([128, NEXP], F32, "em"); emb = C.sb([128, NEXP], BF16, "emb")
            Wc = C.sb([128, NEXP], F32, "Wc"); wsum = C.sb([128, 1], F32, "wsum")
            pos = C.sb([128, NEXP], F32, "pos"); dst = C.sb([128, NEXP], F32, "dst"); ov = C.sb([128, NEXP], F32, "ov")
            oh = C.sb([128, 8, NEXP], F32, "oh"); tmp = C.sb([128, 8, NEXP], F32, "ohtmp")
            dj = C.sb([128, 8], F32, "dj"); dji = C.sb([128, 8], I32, "dji"); rowd = C.sb([128, 8, 4], F32, "rowd")
            hs = C.sb([128, 2, 128], BF16, "hs"); sg = C.sb([128, 256], F32, "sgs"); sho = C.sb([128, D], F32, "sho")
            for t in range(32):
                r0 = t * 128
                C.dma(lambda e: e.dma_start(out=x1[:, :], in_=self.x1_d.t[r0:r0 + 128, :]), R=[self.x1_d], W=[x1])
                self.rms_rstd(x1[:, :], x1, junk, ss, rstd, D)
                C.op("dve", lambda e: e.scalar_tensor_tensor(out=u2[:, :], in0=x1[:, :], scalar=rstd[:, 0:1], in1=GS2[:, :], op0=ALU.mult, op1=ALU.mult), R=[x1, rstd, GS2], W=[u2])
                C.op("pool", lambda e: e.tensor_tensor(out=u2[:, :], in0=u2[:, :], in1=SH2[:, :], op=ALU.add), R=[u2, SH2], W=[u2])
                C.op("act", lambda e: e.activation(out=u2b[:, :], in_=u2[:, :], func=AF.Copy), R=[u2], W=[u2b])
                C.dma(lambda e: e.dma_start(out=self.u2_d.t[r0:r0 + 128, :], in_=u2b[:, :]), R=[u2b], W=[self.u2_d])
                for half in range(2):
                    pb = self.abank()
                    for kk in range(4):
                        k = half * 4 + kk
                        self.tr(pb, pb[:, kk * 128:(kk + 1) * 128], u2[:, k * 128:(k + 1) * 128], self.identf[:, :], R=[u2, self.identf])
                    C.op("act", lambda e: e.activation(out=u2Tf[:, half * 4:(half + 1) * 4, :], in_=pb[:, 0:512].rearrange("p (k n) -> p k n", k=4), func=AF.Copy), R=[pb], W=[u2Tf])
                C.op("dve", lambda e: e.tensor_copy(out=u2Tb[:, :, :], in_=u2Tf[:, :, :]), R=[u2Tf], W=[u2Tb])
                pb = self.abank()
                for k in range(8):
                    self.mm(pb, pb[:, 0:NEXP], u2Tf[:, k, :], rw[:, k, :], R=[u2Tf, rw], start=(k == 0), stop=(k == 7))
                C.op("act", lambda e: e.activation(out=sc_[:, :], in_=pb[:, 0:NEXP], func=AF.Sigmoid), R=[pb], W=[sc_])
                C.op("dve", lambda e: e.tensor_tensor(out=gr[:, :], in0=sc_[:, :], in1=rb[:, :], op=ALU.add), R=[sc_, rb], W=[gr])
                for g in range(8):
                    C.op("dve", lambda e: e.max(out=m8g[:, g, :], in_=gr[:, g * 32:(g + 1) * 32]), R=[gr], W=[m8g])
                C.op("dve", lambda e: e.tensor_tensor(out=gs[:, :], in0=m8g[:, :, 0], in1=m8g[:, :, 1], op=ALU.add), R=[m8g], W=[gs])
                C.op("dve", lambda e: e.max(out=g8[:, :], in_=gs[:, :]), R=[gs], W=[g8])
                C.op("dve", lambda e: e.tensor_scalar(out=pen[:, :], in0=gs[:, :], scalar1=g8[:, 3:4], scalar2=None, op0=ALU.is_ge), R=[gs, g8], W=[pen])
                C.op("dve", lambda e: e.tensor_scalar(out=pen[:, :], in0=pen[:, :], scalar1=-1.0, scalar2=1.0e4, op0=ALU.add, op1=ALU.mult), R=[pen], W=[pen])
                C.op("dve", lambda e: e.tensor_tensor(out=sel[:, :].rearrange("p (g e) -> p g e", g=8), in0=gr[:, :].rearrange("p (g e) -> p g e", g=8), in1=bc(pen, 0, 8, 32), op=ALU.add), R=[gr, pen], W=[sel])
                C.op("dve", lambda e: e.max(out=e8[:, :], in_=sel[:, :]), R=[sel], W=[e8])
                C.op("dve", lambda e: e.tensor_scalar(out=em[:, :], in0=sel[:, :], scalar1=e8[:, 7:8], scalar2=None, op0=ALU.is_ge), R=[sel, e8], W=[em])
                C.op("act", lambda e: e.activation(out=emb[:, :], in_=em[:, :], func=AF.Copy), R=[em], W=[emb])
                C.op("dve", lambda e: e.tensor_tensor(out=Wc[:, :], in0=sc_[:, :], in1=em[:, :], op=ALU.mult), R=[sc_, em], W=[Wc])
                C.op("dve", lambda e: e.reduce_sum(out=wsum[:, :], in_=Wc[:, :], axis=AX.X), R=[Wc], W=[wsum])
                C.op("dve", lambda e: e.reciprocal(out=wsum[:, :], in_=wsum[:, :]), R=[wsum], W=[wsum])
                C.op("dve", lambda e: e.tensor_scalar(out=Wc[:, :], in0=Wc[:, :], scalar1=wsum[:, 0:1], scalar2=2.5, op0=ALU.mult, op1=ALU.mult), R=[Wc, wsum], W=[Wc])
                pb = self.abank()
                self.mm(pb, pb[:, 0:NEXP], self.SUb[:, :], emb[:, :], R=[self.SUb, emb])
                C.op("dve", lambda e: e.tensor_tensor(out=pos[:, :], in0=pb[:, 0:NEXP], in1=cnt[:, :], op=ALU.add), R=[pb, cnt], W=[pos])
                pb = self.abank()
                self.mm(pb, pb[:, 0:NEXP], self.onesb[:, :], emb[:, :], R=[self.onesb, emb])
                C.op("dve", lambda e: e.tensor_tensor(out=cnt[:, :], in0=cnt[:, :], in1=pb[:, 0:NEXP], op=ALU.add), R=[pb, cnt], W=[cnt])
                C.op("dve", lambda e: e.tensor_scalar(out=ov[:, :], in0=pos[:, :], scalar1=float(CAP), scalar2=1.0e7, op0=ALU.is_ge, op1=ALU.mult), R=[pos], W=[ov])
                C.op("dve", lambda e: e.tensor_tensor(out=dst[:, :], in0=pos[:, :], in1=iotaE[:, :], op=ALU.add), R=[pos, iotaE], W=[dst])
                C.op("dve", lambda e: e.tensor_tensor(out=dst[:, :], in0=dst[:, :], in1=ov[:, :], op=ALU.add), R=[dst, ov], W=[dst])
                sel_b = bass.AP(sel.t, 0, [[NEXP, 128], [0, 8], [1, NEXP]])
                dst_b = bass.AP(dst.t, 0, [[NEXP, 128], [0, 8], [1, NEXP]])
                wc_b = bass.AP(Wc.t, 0, [[NEXP, 128], [0, 8], [1, NEXP]])
                pt = self.tbank()
                for kc in range(2):
                    self.tr(pt, pt[:, kc * 128:(kc + 1) * 128], emb[:, kc * 128:(kc + 1) * 128], self.identb[:, :], R=[emb, self.identb])
                C.op("act", lambda e: e.activation(out=emT[:, :, :].rearrange("p a b -> p (a b)"), in_=pt[:, 0:256], func=AF.Copy), R=[pt], W=[emT])
                pb = self.abank()
                for kc in range(2):
                    self.mm(pb, pb[:, 0:NEXP], emT[:, kc, :], SUF[:, kc, :], R=[emT, SUF], start=(kc == 0), stop=(kc == 1))
                C.op("act", lambda e: e.activation(out=jr[:, :], in_=pb[:, 0:NEXP], func=AF.Copy), R=[pb], W=[jr])
                C.op("dve", lambda e: e.tensor_tensor(out=dst[:, :], in0=dst[:, :], in1=em[:, :], op=ALU.mult), R=[dst, em], W=[dst])
                jr_b = bass.AP(jr.t, 0, [[NEXP, 128], [0, 8], [1, NEXP]])
                C.op("dve", lambda e: e.tensor_tensor(out=oh[:, :, :], in0=jr_b, in1=jv[:, :, :], op=ALU.is_equal), R=[jr, jv], W=[oh])
                C.op("pool", lambda e: e.tensor_tensor(out=tmp[:, :, :], in0=oh[:, :, :], in1=dst_b, op=ALU.mult), R=[oh, dst], W=[tmp])
                C.op("dve", lambda e: e.tensor_reduce(out=dj[:, :], in_=tmp[:, :, :], axis=AX.X, op=ALU.add), R=[tmp], W=[dj])
                C.op("dve", lambda e: e.tensor_copy(out=dji[:, :], in_=dj[:, :]), R=[dj], W=[dji])
                C.op("pool", lambda e: e.tensor_tensor(out=tmp[:, :, :], in0=oh[:, :, :], in1=wc_b, op=ALU.mult), R=[oh, Wc, tmp], W=[tmp])
                C.op("dve", lambda e: e.tensor_reduce(out=rowd[:, :, 2], in_=tmp[:, :, :], axis=AX.X, op=ALU.add), R=[tmp], W=[rowd])
                C.op("pool", lambda e: e.iota(rowd[:, :, 0], pattern=[[0, 8]], base=r0, channel_multiplier=1, allow_small_or_imprecise_dtypes=True), R=[rowd], W=[rowd])
                C.op("pool", lambda e: e.iota(rowd[:, :, 1], pattern=[[1, 8]], base=r0 * 8, channel_multiplier=8, allow_small_or_imprecise_dtypes=True), R=[rowd], W=[rowd])
                C.op("pool", lambda e: e.memset(rowd[:, :, 3], 0.0), R=[rowd], W=[rowd])
                for j in range(8):
                    C.dma(lambda e: e.indirect_dma_start(out=self.L_d.t[:, :], out_offset=bass.IndirectOffsetOnAxis(ap=dji[:, j:j + 1], axis=0), in_=rowd[:, j, :], in_offset=None,
                                                         bounds_check=self.bnd["L"], oob_is_err=False), R=[rowd, dji, self.L_d], W=[], q="pool")
                pb = self.abank()
                for blk in range(4):
                    for k in range(8):
                        self.mm(pb, pb[:, blk * 128:(blk + 1) * 128], Wsgu[:, k, blk * 128:(blk + 1) * 128], u2Tb[:, k, :], R=[Wsgu, u2Tb], start=(k == 0), stop=(k == 7))
                C.op("act", lambda e: e.activation(out=sg[:, :], in_=pb[:, 0:256], func=AF.Silu), R=[pb], W=[sg])
                C.op("dve", lambda e: e.tensor_tensor(out=hs[:, :, :].rearrange("p a b -> p (a b)"), in0=sg[:, :], in1=pb[:, 256:512], op=ALU.mult), R=[sg, pb], W=[hs])
                for n in range(2):
                    pb = self.abank()
                    for fb in range(2):
                        self.mm(pb, pb[:, 0:512], hs[:, fb, :], Wsd[:, fb, n * 512:(n + 1) * 512], R=[hs, Wsd], start=(fb == 0), stop=(fb == 1))
                    C.op("act", lambda e: e.activation(out=sho[:, n * 512:(n + 1) * 512], in_=pb[:, 0:512], func=AF.Copy), R=[pb], W=[sho])
                C.dma(lambda e: e.dma_start(out=self.sh_d.t[r0:r0 + 128, :], in_=sho[:, :]), R=[sho], W=[self.sh_d])

    def experts(self, moe_g, moe_u, moe_d):
        C = self.C
        NB = CAP // 128
        with Scope(C):
            Wf = [C.sb([128, 6144], F32, "Wf%d" % i) for i in range(3)]
            Wgu = [C.sb([128, 8, 512], BF16, "Wgu%d" % i) for i in range(2)]
            Wd = [C.sb([128, 2, D], BF16, "Wd%d" % i) for i in range(2)]
            Lt = [C.sb([128, NB, 4], F32, "Lt%d" % i) for i in range(3)]
            Li = [C.sb([128, NB, 2], I32, "Lti%d" % i) for i in range(4)]
            Lw = [C.sb([128, NB], F32, "Lw%d" % i) for i in range(4)]
            xg = [[C.sb([128, D], BF16, "xg%d_%d" % (i, j)) for j in range(NB)] for i in range(2)]
            xgT = [C.sb([128, 8, CAP], BF16, "xgT%d" % i) for i in range(2)]
            sg = [C.sb([128, CAP], F32, "e_sg%d" % i) for i in range(2)]
            hb = [C.sb([128, 2, CAP], BF16, "e_hb%d" % i) for i in range(2)]
            yo = [[C.sb([128, D], F32, "yo%d_%d" % (i, j)) for j in range(NB)] for i in range(2)]

            def load(e_):
                b = e_ % 3
                C.dma(lambda e: e.dma_start(out=Wf[b][:, 0:2048].rearrange("p (k f) -> p k f", k=8), in_=moe_g.t[e_ * D:(e_ + 1) * D, :].rearrange("(p k) f -> p k f", k=8)), R=[moe_g], W=[Wf[b]])
                C.dma(lambda e: e.dma_start(out=Wf[b][:, 2048:4096].rearrange("p (k f) -> p k f", k=8), in_=moe_u.t[e_ * D:(e_ + 1) * D, :].rearrange("(p k) f -> p k f", k=8)), R=[moe_u], W=[Wf[b]])
                C.dma(lambda e: e.dma_start(out=Wf[b][:, 4096:6144].rearrange("p (k n) -> p k n", k=2), in_=moe_d.t[e_ * 256:(e_ + 1) * 256, :].rearrange("(k p) n -> p k n", p=128)), R=[moe_d], W=[Wf[b]])
                C.dma(lambda e: e.dma_start(out=Lt[b][:, :, :], in_=self.L_d.t[e_ * CAP:(e_ + 1) * CAP, :].rearrange("(a p) n -> p a n", p=128)), R=[self.L_d], W=[Lt[b]])

            def small(e_):
                b4 = e_ % 4; b3 = e_ % 3
                C.op("dve", lambda e: e.tensor_copy(out=Li[b4][:, :, :], in_=Lt[b3][:, :, 0:2]), R=[Lt[b3]], W=[Li[b4]])
                C.op("dve", lambda e: e.tensor_copy(out=Lw[b4][:, :], in_=Lt[b3][:, :, 2]), R=[Lt[b3]], W=[Lw[b4]])

            def castW(e_):
                b = e_ % 2; b3 = e_ % 3
                C.op("act", lambda e: e.activation(out=Wgu[b][:, :, 0:256], in_=Wf[b3][:, 0:2048].rearrange("p (k f) -> p k f", k=8), func=AF.Copy), R=[Wf[b3]], W=[Wgu[b]])
                C.op("act", lambda e: e.activation(out=Wgu[b][:, :, 256:512], in_=Wf[b3][:, 2048:4096].rearrange("p (k f) -> p k f", k=8), func=AF.Copy), R=[Wf[b3]], W=[Wgu[b]])
                C.op("act", lambda e: e.activation(out=Wd[b][:, :, :], in_=Wf[b3][:, 4096:6144].rearrange("p (k n) -> p k n", k=2), func=AF.Copy), R=[Wf[b3]], W=[Wd[b]])

            def gather(e_):
                b = e_ % 2
                for blk in range(NB):
                    C.dma(lambda e: e.indirect_dma_start(out=xg[b][blk][:, :], out_offset=None, in_=self.u2_d.t[:, :], in_offset=bass.IndirectOffsetOnAxis(ap=Li[e_ % 4][:, blk, 0:1], axis=0),
                                                         bounds_check=self.bnd["u2"], oob_is_err=False), R=[self.u2_d, Li[e_ % 4]], W=[xg[b][blk]], q="pool")

            def transp(e_):
                b = e_ % 2
                for blk in range(NB):
                    pt = self.tbank()
                    for k in range(8):
                        self.tr(pt, pt[:, k * 128:(k + 1) * 128], xg[b][blk][:, k:D:8], self.identb[:, :], R=[xg[b][blk], self.identb])
                    C.op("dve", lambda e: e.tensor_copy(out=xgT[b][:, :, blk * 128:(blk + 1) * 128], in_=pt[:, :].rearrange("p (k n) -> p k n", k=8)), R=[pt], W=[xgT[b]])

            load(0); load(1); load(2); small(0); gather(0); castW(0); transp(0)
            for e_ in range(NEXP):
                b = e_ % 2
                if e_ + 1 < NEXP:
                    small(e_ + 1); gather(e_ + 1)
                pbs = []
                for j in range(4):
                    pb = self.abank()
                    for k in range(8):
                        self.mm(pb, pb[:, 0:CAP], Wgu[b][:, k, j * 128:(j + 1) * 128], xgT[b][:, k, :], R=[Wgu[b], xgT[b]], start=(k == 0), stop=(k == 7))
                    pbs.append(pb)
                for fb in range(2):
                    C.op("act", lambda e: e.activation(out=sg[fb][:, :], in_=pbs[fb][:, 0:CAP], func=AF.Silu), R=[pbs[fb]], W=[sg[fb]])
                    C.op("dve", lambda e: e.tensor_tensor(out=hb[b][:, fb, :], in0=sg[fb][:, :], in1=pbs[2 + fb][:, 0:CAP], op=ALU.mult), R=[sg[fb], pbs[2 + fb]], W=[hb[b]])
                if e_ + 1 < NEXP:
                    castW(e_ + 1)
                    transp(e_ + 1)
                for blk in range(NB):
                    for n in range(2):
                        pb = self.abank()
                        for fb in range(2):
                            self.mm(pb, pb[:, 0:512], hb[b][:, fb, blk * 128:(blk + 1) * 128], Wd[b][:, fb, n * 512:(n + 1) * 512], R=[hb[b], Wd[b]], start=(fb == 0), stop=(fb == 1))
                        C.op("dve", lambda e: e.tensor_scalar(out=yo[b][blk][:, n * 512:(n + 1) * 512], in0=pb[:, 0:512], scalar1=Lw[e_ % 4][:, blk:blk + 1], scalar2=None, op0=ALU.mult), R=[pb, Lw[e_ % 4]], W=[yo[b][blk]])
                    C.dma(lambda e: e.indirect_dma_start(out=self.Y_d.t[:, :], out_offset=bass.IndirectOffsetOnAxis(ap=Li[e_ % 4][:, blk, 1:2], axis=0), in_=yo[b][blk][:, :], in_offset=None,
                                                         bounds_check=self.bnd["Y"], oob_is_err=False), R=[yo[b][blk], Li[e_ % 4], self.Y_d], W=[], q="pool")
                if e_ + 3 < NEXP:
                    load(e_ + 3)

    def final(self, vecs, out):
        C = self.C
        with Scope(C):
            gate = C.sb([128, D], F32, "fgate"); gfin = C.sb([128, D], F32, "gfin")
            self.col_to_bcast(self.gcol, 8, gate)
            C.dma(lambda e: e.dma_start(out=gfin[:, :], in_=vecs.t[256:384, :]), R=[vecs], W=[gfin])
            Yt = [C.sb([128, 8, D], F32, "Yt%d" % i) for i in range(2)]
            x1 = [C.sb([128, D], F32, "fx1%d" % i) for i in range(2)]; sh = [C.sb([128, D], F32, "fsh%d" % i) for i in range(2)]
            accs = [C.sb([128, D], F32, "facc%d" % i) for i in range(2)]; junks = [C.sb([128, D], BF16, "fjunk%d" % i) for i in range(2)]; sss = [C.sb([128, 1], F32, "fss%d" % i) for i in range(2)]; rstds = [C.sb([128, 1], F32, "frstd%d" % i) for i in range(2)]
            ot = [C.sb([128, D], F32, "fot%d" % i) for i in range(2)]
            for t in range(32):
                r0 = t * 128; b = t % 2
                acc = accs[b]; junk = junks[b]; ss = sss[b]; rstd = rstds[b]
                C.dma(lambda e: e.dma_start(out=Yt[b][:, :, :], in_=self.Y_d.t[r0 * 8:(r0 + 128) * 8, :].rearrange("(p j) n -> p j n", j=8)), R=[self.Y_d], W=[Yt[b]])
                C.dma(lambda e: e.dma_start(out=x1[b][:, :], in_=self.x1_d.t[r0:r0 + 128, :]), R=[self.x1_d], W=[x1[b]])
                C.dma(lambda e: e.dma_start(out=sh[b][:, :], in_=self.sh_d.t[r0:r0 + 128, :]), R=[self.sh_d], W=[sh[b]])
                C.op("dve", lambda e: e.tensor_reduce(out=acc[:, :], in_=Yt[b][:, :, :].rearrange("p j n -> p n j"), axis=AX.X, op=ALU.add), R=[Yt[b]], W=[acc])
                C.op("pool", lambda e: e.tensor_tensor(out=acc[:, :], in0=acc[:, :], in1=sh[b][:, :], op=ALU.add), R=[acc, sh[b]], W=[acc])
                C.op("dve", lambda e: e.tensor_tensor(out=acc[:, :], in0=acc[:, :], in1=gate[:, :], op=ALU.mult), R=[acc, gate], W=[acc])
                C.op("pool", lambda e: e.tensor_tensor(out=acc[:, :], in0=acc[:, :], in1=x1[b][:, :], op=ALU.add), R=[acc, x1[b]], W=[acc])
                self.rms_rstd(acc[:, :], acc, junk, ss, rstd, D)
                C.op("dve", lambda e: e.scalar_tensor_tensor(out=ot[b][:, :], in0=acc[:, :], scalar=rstd[:, 0:1], in1=gfin[:, :], op0=ALU.mult, op1=ALU.mult), R=[acc, rstd, gfin], W=[ot[b]])
                C.dma(lambda e: e.dma_start(out=out.t[r0:r0 + 128, :], in_=ot[b][:, :]), R=[ot[b]], W=[out])


def col_layout(v):
    return np.ascontiguousarray(np.asarray(v).reshape(8, 128).T)


def rep128(v):
    v = np.asarray(v).reshape(1, -1)
    return np.ascontiguousarray(np.broadcast_to(v, (128, v.shape[1])))


def host_layout(inp):
    f32 = np.float32
    x = np.asarray(inp["x"], f32); ctx = np.asarray(inp["ctx"], f32)
    w_in = np.ascontiguousarray(np.asarray(inp["w_in"], f32)[0])
    wsc = w_in[:, 1024:1024 + WS]
    cxw = np.asarray(inp["conv_xbc_w"], f32)[0]; cxb = np.asarray(inp["conv_xbc_b"], f32)[0]
    cqw = np.asarray(inp["conv_qk_w"], f32)[0]; cqb = np.asarray(inp["conv_qk_b"], f32)[0]
    dtb = np.asarray(inp["ssd_dt_bias"], f32)[0]; alog = np.asarray(inp["ssd_a_log"], f32)[0]
    ib = np.asarray(inp["mlstm_i_bias"], f32)[0]; fb = np.asarray(inp["mlstm_f_bias"], f32)[0]

    def taps(w, nt, flip):
        ww = w[::-1] if flip else w
        return np.ascontiguousarray(ww.reshape(5, nt, 128).transpose(2, 1, 0).reshape(128, nt * 5))

    def bias(b, nt):
        return np.ascontiguousarray(b.reshape(nt, 128).T)

    def wl(d):
        w = wsc.copy()
        if d == 1:
            w[:, O_DT:O_DT + 16] = wsc[:, O_DT + 16:O_DT + 32]
            w[:, O_GT:O_GT + 16] = wsc[:, O_GT + 16:O_GT + 32]
        return w

    def smallset(d):
        return rep128(np.concatenate([dtb[d], alog[d], ib[d], fb[d]]))
    wl_d = [wl(0), wl(1)]
    shared = dict(
        ada_w=np.ascontiguousarray(np.asarray(inp["ada_w"], f32)[0]),
        ada_b=np.ascontiguousarray(np.asarray(inp["ada_b"], f32)[0].reshape(48, 128).T),
        gcols=np.concatenate([col_layout(inp["norm_mix_g"][0]), col_layout(inp["norm_ffn_g"][0])], axis=1).astype(f32),
        w_in=w_in,
        ssd_d=rep128(np.asarray(inp["ssd_d"], f32)[0]),
        vecs=np.concatenate([rep128(inp["ssd_norm_g"][0]), rep128(inp["mlstm_norm_g"][0]), rep128(inp["norm_final_g"])], axis=0).astype(f32),
        w_ssd_out=np.ascontiguousarray(np.asarray(inp["w_ssd_out"], f32)[0]),
        w_mlstm_out=np.ascontiguousarray(np.asarray(inp["w_mlstm_out"], f32)[0]),
        w_out=np.ascontiguousarray(np.asarray(inp["w_out"], f32)[0]),
        router_w=np.ascontiguousarray(np.asarray(inp["router_w"], f32)[0]),
        router_b=rep128(np.asarray(inp["router_bias"], f32)[0]),
        moe_w_gate=np.asarray(inp["moe_w_gate"], f32)[0].reshape(NEXP * D, 256),
        moe_w_up=np.asarray(inp["moe_w_up"], f32)[0].reshape(NEXP * D, 256),
        moe_w_down=np.asarray(inp["moe_w_down"], f32)[0].reshape(NEXP * 256, D),
        shared_w_gate=np.ascontiguousarray(np.asarray(inp["shared_w_gate"], f32)[0]),
        shared_w_up=np.ascontiguousarray(np.asarray(inp["shared_w_up"], f32)[0]),
        shared_w_down=np.ascontiguousarray(np.asarray(inp["shared_w_down"], f32)[0]),
    )
    maps = []
    for c in range(NCORE):
        b, s = c // 4, c % 4
        m = dict(shared)
        m["xo"] = np.ascontiguousarray(x[b, s * SEG:(s + 1) * SEG])
        pre = []; dirs = []
        for j in range(3):
            if j < s:
                pre.append(x[b, j * SEG:(j + 1) * SEG]); dirs.append(0)
            else:
                g = 3 - (j - s)
                pre.append(x[b, g * SEG:(g + 1) * SEG][::-1]); dirs.append(1)
        m["xpre"] = np.ascontiguousarray(np.concatenate(pre, axis=0))
        m["ctx2"] = np.ascontiguousarray(np.concatenate([ctx[b], ctx[b][::-1]], axis=0))
        cv = np.zeros((128, 16), f32)
        cv[:, 0::2] = col_layout(inp["c"][b]); cv[:, 1::2] = col_layout(inp["c_ctx"])
        m["cvec"] = cv
        sd = [0, 1] + dirs
        m["wlite"] = np.ascontiguousarray(np.concatenate([wl_d[d] for d in sd], axis=0))
        m["cwx"] = np.concatenate([taps(cxw, 12, d == 1) for d in sd] + [taps(cxw, 12, False)], axis=0)
        m["cbx"] = np.concatenate([bias(cxb, 12)] * 6, axis=0)
        m["cwq"] = np.concatenate([taps(cqw, 8, d == 1) for d in sd] + [taps(cqw, 8, False)], axis=0)
        m["cbq"] = np.concatenate([bias(cqb, 8)] * 6, axis=0)
        m["small"] = np.concatenate([smallset(d) for d in sd] + [smallset(0), smallset(1)], axis=0)
        fl = np.zeros((128, 8), f32)
        for j in range(3):
            fl[:, j] = 1.0 if dirs[j] == 0 else 0.0
            fl[:, 4 + j] = 1.0 - fl[:, j]
        m["flags"] = fl
        maps.append(m)
    return maps


def kernel(**inputs):
    maps = host_layout(inputs)
    prog = Prog()
    nc = prog.build()
    maps = [{k: v for k, v in m.items() if k in prog.ins} for m in maps]
    res = run_bass_kernel_spmd(nc, maps, core_ids=list(range(NCORE)))
    out = np.zeros((2, 4 * SEG, D), np.float32)
    for c in range(NCORE):
        b, s = c // 4, c % 4
        out[b, s * SEG:(s + 1) * SEG] = res.results[c]["out"]
    return out
```

```python
from contextlib import ExitStack
import os
import numpy as np
import concourse.bass as bass
import concourse.mybir as mybir
from concourse.bass_utils import run_bass_kernel_spmd

F32 = mybir.dt.float32
BF16 = mybir.dt.bfloat16
I32 = mybir.dt.int32
ALU = mybir.AluOpType
AF = mybir.ActivationFunctionType
AX = mybir.AxisListType

NCORE = 8
D = 1024
SEG = 4096
NSEGC = 32
CTXL = 256
EPS = 1e-6
BIG = 30000.0
NEXP = 256
CAP = 512
WS = 3648
O_XBC, O_DT, O_QK, O_V, O_GT = 0, 1536, 1568, 2592, 3616


class Buf:
    __slots__ = ("t", "w", "r", "name", "off", "excl")

    def __init__(self, t, name, off=0, excl=False):
        self.t = t
        self.name = name
        self.off = off
        self.excl = excl
        self.w = {}
        self.r = {}

    def __getitem__(self, idx):
        return self.t[idx]


class Ctx:
    ENG = ("pe", "dve", "act", "pool", "sp")
    SAME_WIN = 4

    def __init__(self, nc, es, n_dma_sems=48):
        self.nc = nc
        self.es_stack = [es]
        self.e = {"pe": nc.tensor, "dve": nc.vector, "act": nc.scalar, "pool": nc.gpsimd, "sp": nc.sync}
        self.sem = {k: es.enter_context(nc.semaphore("s_" + k)) for k in self.ENG}
        self.cnt = {k: 0 for k in self.ENG}
        self.seen = {k: {} for k in self.ENG}
        self.dsem = [es.enter_context(nc.semaphore("d%d" % i)) for i in range(n_dma_sems)]
        self.dcnt = [0] * n_dma_sems
        self.dnext = 0
        self.nid = 0
        self.n_inst = 0

    @property
    def es(self):
        return self.es_stack[-1]

    def sb(self, shape, dt=F32, name=None):
        self.nid += 1
        name = "%s_%d" % (name or "sb", self.nid)
        return Buf(self.es.enter_context(self.nc.sbuf_tensor(name, list(shape), dt)), name)

    def ps(self, shape, dt=F32, name=None):
        self.nid += 1
        name = "%s_%d" % (name or "ps", self.nid)
        return Buf(self.es.enter_context(self.nc.psum_tensor(name, list(shape), dt)), name, excl=True)

    def dram(self, shape, dt=F32, name=None):
        self.nid += 1
        name = name or ("dr_%d" % self.nid)
        return Buf(self.nc.dram_tensor(name, list(shape), dt, kind="Internal"), name)

    def _wait(self, eng, key, val):
        if self.seen[eng].get(key, 0) >= val:
            return
        if isinstance(key, int):
            self.e[eng].wait_ge(self.dsem[key], val)
        else:
            if key == eng and (eng == "pe" or val <= self.cnt[eng] - self.SAME_WIN):
                return
            self.e[eng].wait_ge(self.sem[key], val)
        self.seen[eng][key] = val

    def _deps(self, eng, R, W):
        for b in R:
            for k, v in b.w.items():
                self._wait(eng, k, v)
            if b.excl:
                for k, v in b.r.items():
                    if k != eng:
                        self._wait(eng, k, v)
        for b in W:
            for k, v in b.w.items():
                self._wait(eng, k, v)
            for k, v in b.r.items():
                self._wait(eng, k, v)

    def op(self, eng, fn, R=(), W=()):
        self._deps(eng, R, W)
        inst = fn(self.e[eng])
        self.cnt[eng] += 1
        idx = self.cnt[eng]
        inst.then_inc(self.sem[eng], 1)
        self.n_inst += 1
        for b in R:
            b.r[eng] = idx
        for b in W:
            b.w[eng] = idx
        return inst

    def dma(self, fn, R=(), W=(), q="sp"):
        k = self.dnext
        self.dnext = (self.dnext + 1) % len(self.dsem)
        if self.dcnt[k] > 0:
            self._wait(q, k, self.dcnt[k])
        self._deps(q, R, W)
        inst = fn(self.e[q])
        self.dcnt[k] += 16
        inst.then_inc(self.dsem[k], 16)
        self.n_inst += 1
        for b in R:
            b.r[k] = self.dcnt[k]
        for b in W:
            b.w[k] = self.dcnt[k]
        return inst

    def barrier(self):
        for eng in self.ENG:
            for k in self.ENG:
                if k != eng and self.cnt[k] > 0:
                    self._wait(eng, k, self.cnt[k])
            for k in range(len(self.dsem)):
                if self.dcnt[k] > 0:
                    self._wait(eng, k, self.dcnt[k])

    def finish(self, bufs, eng="sp"):
        for b in bufs:
            for k, v in b.w.items():
                self._wait(eng, k, v)


class Scope:
    def __init__(self, C):
        self.C = C

    def __enter__(self):
        self.es = ExitStack()
        self.es.__enter__()
        self.C.es_stack.append(self.es)
        return self

    def __exit__(self, *a):
        self.C.barrier()
        self.C.es_stack.pop()
        return self.es.__exit__(*a)


def bc(buf, col0, n, rep):
    t = buf.t
    Fsz = int(np.prod(t.shape[1:]))
    return bass.AP(t, col0, [[Fsz, t.shape[0]], [1, n], [0, rep]])


def bcp(buf, p0, npart, col0, n, rep):
    t = buf.t
    Fsz = int(np.prod(t.shape[1:]))
    return bass.AP(t, p0 * Fsz + col0, [[Fsz, npart], [1, n], [0, rep]])


def colbc(buf, col, rep):
    t = buf.t
    Fsz = int(np.prod(t.shape[1:]))
    return bass.AP(t, col, [[Fsz, t.shape[0]], [0, rep]])


class Prog:
    def __init__(self, dbg=None, stop_after=None, addD=True):
        self.addD = addD
        self.dbg = dbg or []
        self.stop_after = stop_after
        self.nc = bass.Bass("TRN2", target_bir_lowering=False)
        self.ins = {}
        self.outs = {}

    def inp(self, name, shape, dt=F32):
        b = Buf(self.nc.dram_tensor(name, list(shape), dt, kind="ExternalInput"), name)
        self.ins[name] = b
        return b

    def outp(self, name, shape, dt=F32):
        b = Buf(self.nc.dram_tensor(name, list(shape), dt, kind="ExternalOutput"), name)
        self.outs[name] = b
        return b

    def mm(self, ob, oap, lhsT, rhs, R, start=True, stop=True):
        self.C.op("pe", lambda e: e.matmul(oap, lhsT=lhsT, rhs=rhs, start=start, stop=stop), R=R, W=[ob])

    def tr(self, ob, oap, in_ap, ident_ap, R):
        self.C.op("pe", lambda e: e.transpose(out=oap, in_=in_ap, identity=ident_ap), R=R, W=[ob])

    def gbank(self):
        self._g = (self._g + 1) % len(self.G)
        return self.G[self._g]

    def tbank(self):
        self._t = (self._t + 1) % len(self.T)
        return self.T[self._t]

    def dslot(self):
        self._d = (self._d + 1) % len(self.DS)
        return self.DS[self._d]

    def dump(self, name, buf, ap, shape, dt=F32):
        if name in self.dbg:
            o = self.outp("dbg_" + name, shape, dt)
            self.C.dma(lambda e: e.dma_start(out=o.t.ap() if len(shape) == 0 else o.t[tuple(slice(None) for _ in shape)], in_=ap), R=[buf], W=[o])

    def build(self):
        nc = self.nc
        I = self.inp
        xo = I("xo", [SEG, D]); xpre = I("xpre", [3 * SEG, D]); ctx2 = I("ctx2", [2 * CTXL, D])
        cvec = I("cvec", [128, 16]); ada_w = I("ada_w", [D, 6 * D]); ada_b = I("ada_b", [128, 48])
        gcols = I("gcols", [128, 16])
        wlite = I("wlite", [5 * D, WS]); w_in = I("w_in", [D, 7744])
        cwx = I("cwx", [6 * 128, 60]); cbx = I("cbx", [6 * 128, 12]); cwq = I("cwq", [6 * 128, 40]); cbq = I("cbq", [6 * 128, 8])
        small = I("small", [7 * 128, 48]); flags = I("flags", [128, 8]); ssd_d = I("ssd_d", [128, 16])
        if self.stop_after is None or self.stop_after in ("merge", "route"):
            vecs = I("vecs", [3 * 128, D])
            w_so = I("w_ssd_out", [D, D]); w_mo = I("w_mlstm_out", [D, D]); w_o = I("w_out", [D, D])
            router_w = I("router_w", [D, NEXP]); router_b = I("router_b", [128, NEXP])
            sh_g = I("shared_w_gate", [D, 256]); sh_u = I("shared_w_up", [D, 256]); sh_d = I("shared_w_down", [256, D])
        if self.stop_after is None:
            moe_g = I("moe_w_gate", [NEXP * D, 256]); moe_u = I("moe_w_up", [NEXP * D, 256]); moe_d = I("moe_w_down", [NEXP * 256, D])
        out = self.outp("out", [SEG, D])

        with ExitStack() as es:
            C = self.C = Ctx(nc, es)
            self.bnd = {}
            for nm, val in (("L", NEXP * CAP - 1), ("u2", SEG + 127), ("Y", SEG * 8 - 1)):
                r = es.enter_context(nc.gpsimd.register("bnd_" + nm))
                nc.gpsimd.reg_mov(r, val)
                self.bnd[nm] = r
            self.build_consts()
            self.G = [C.ps([128, 512], F32, "G%d" % i) for i in range(2)]
            self.T = [C.ps([128, 1024], BF16, "T%d" % i) for i in range(2)]
            self.PY = C.ps([128, 512], F32, "PY"); self.PYI = C.ps([128, 512], F32, "PYI")
            self.DS = [C.ps([128, 512], F32, "DS%d" % i) for i in range(2)]
            self._g = self._t = self._d = 0
            self.Gall = self.G + [self.PY, self.PYI] + self.DS
            self._a = 0
            self.y_d = [C.dram([SEG, D], F32, "y_d%d" % d) for d in range(2)]
            self.h_d = [C.dram([SEG, D], F32, "h_d%d" % d) for d in range(2)]
            later = self.stop_after is None or self.stop_after in ("merge", "route")
            self.uT_d = C.dram([8 * 128, 4096], BF16, "uT_d") if later else None
            self.x1_d = C.dram([SEG, D], F32, "x1_d")
            self.sh_d = C.dram([SEG, D], F32, "sh_d")
            self.u2_d = C.dram([SEG + 128, D], BF16, "u2_d")
            self.L_d = C.dram([NEXP * CAP, 4], F32, "L_d")
            self.Y_d = C.dram([SEG * 8, D], F32, "Y_d") if self.stop_after is None else None

            self.adaln(cvec, ada_w, ada_b, gcols)
            if self.stop_after == "adaln":
                return self.end()
            self.mixer_scans(xo, xpre, ctx2, wlite, w_in, cwx, cbx, cwq, cbq, small, flags, ssd_d)
            if self.stop_after == "ctx":
                return self.end()
            if self.stop_after == "scans":
                for d in range(2):
                    if "y_d" in self.dbg:
                        self.copy_dram(self.y_d[d], self.outp("dbg_y_d%d" % d, [SEG, D]))
                        self.copy_dram(self.h_d[d], self.outp("dbg_h_d%d" % d, [SEG, D]))
                return self.end()
            self.merge_pass(xo, w_in, vecs, w_so, w_mo, w_o)
            if self.stop_after == "merge":
                self.copy_dram(self.x1_d, self.outp("dbg_x1", [SEG, D]))
                return self.end()
            self.ffn_prep(vecs, router_w, router_b, sh_g, sh_u, sh_d)
            if self.stop_after == "route":
                self.copy_dram(self.x1_d, self.outp("dbg_x1", [SEG, D]))
                self.copy_dram(self.sh_d, self.outp("dbg_sh", [SEG, D]))
                self.copy_dram(self.L_d, self.outp("dbg_L", [NEXP * CAP, 4]), rows=0, flat=(128, NEXP * CAP * 4 // 128))
                return self.end()
            self.experts(moe_g, moe_u, moe_d)
            self.final(vecs, out)
            return self.end()

    def copy_dram(self, src, dst, rows=SEG, flat=None):
        C = self.C
        if flat is not None:
            with Scope(C):
                p, f = flat
                t = C.sb([p, f], F32, "cpf")
                C.dma(lambda e: e.dma_start(out=t[:, :], in_=src.t.ap().rearrange("(p a) n -> p (a n)", p=p)), R=[src], W=[t])
                C.dma(lambda e: e.dma_start(out=dst.t.ap().rearrange("(p a) n -> p (a n)", p=p), in_=t[:, :]), R=[t], W=[dst])
            return
        with Scope(C):
            tl = [C.sb([128, 4, D], F32, "cp") for _ in range(2)]
            for i in range(rows // 512):
                t = tl[i % 2]
                C.dma(lambda e: e.dma_start(out=t[:, :, :], in_=src.t[i * 512:(i + 1) * 512, :].rearrange("(a p) n -> p a n", p=128)), R=[src], W=[t])
                C.dma(lambda e: e.dma_start(out=dst.t[i * 512:(i + 1) * 512, :].rearrange("(a p) n -> p a n", p=128), in_=t[:, :, :]), R=[t], W=[dst])

    def end(self):
        self.C.finish(list(self.outs.values()))
        self.C.barrier()
        return self.nc

    def build_consts(self):
        C = self.C

        def tri(name, cm, pat, op, val=1.0, fill=0.0):
            t = C.sb([128, 128], F32, name)
            C.op("pool", lambda e: e.memset(t[:, :], val), W=[t])
            C.op("pool", lambda e: e.affine_select(out=t[:, :], in_=t[:, :], pattern=[[pat, 128]], compare_op=op, fill=fill, base=0, channel_multiplier=cm), R=[t], W=[t])
            return t

        self.identf = tri("identf", 1, -1, ALU.is_equal)
        self.Uf = tri("Uf", -1, 1, ALU.is_ge)
        self.Lf = tri("Lf", 1, -1, ALU.is_ge)
        self.SUf = tri("SUf", -1, 1, ALU.is_gt)
        self.NEGU = tri("NEGU", 1, -1, ALU.is_gt, val=-BIG)
        self.NEGL = tri("NEGL", -1, 1, ALU.is_gt, val=-BIG)
        self.onesf = C.sb([128, 128], F32, "onesf")
        C.op("pool", lambda e: e.memset(self.onesf[:, :], 1.0), W=[self.onesf])
        self.epsb = C.sb([128, 1], F32, "epsb")
        C.op("pool", lambda e: e.memset(self.epsb[:, :], EPS), W=[self.epsb])

        def tobf(src, name):
            t = C.sb([128, 128], BF16, name)
            C.op("dve", lambda e: e.tensor_copy(out=t[:, :], in_=src[:, :]), R=[src], W=[t])
            return t
        self.identb = tobf(self.identf, "identb")
        self.Ub = tobf(self.Uf, "Ub"); self.Lb = tobf(self.Lf, "Lb"); self.SUb = tobf(self.SUf, "SUb")
        self.onesb = tobf(self.onesf, "onesb")

    def col_to_bcast(self, colbuf, c0, dst):
        C = self.C
        for half in range(2):
            pb = self.gbank()
            for jj in range(4):
                j = half * 4 + jj
                self.mm(pb, pb[:, jj * 128:(jj + 1) * 128], colbc(colbuf, c0 + j, 128), self.identf[:, :], R=[colbuf, self.identf])
            C.op("act", lambda e: e.activation(out=dst[:, half * 512:(half + 1) * 512], in_=pb[:, 0:512], func=AF.Copy), R=[pb], W=[dst])

    def adaln(self, cvec, ada_w, ada_b, gcols):
        C = self.C
        self.mod = C.sb([128, 96], F32, "mod")
        self.gc = C.sb([128, 16], F32, "gc")
        C.dma(lambda e: e.dma_start(out=self.gc[:, :], in_=gcols.t[:, :]), R=[gcols], W=[self.gc])
        with Scope(C):
            cv = C.sb([128, 16], F32, "cv"); sg = C.sb([128, 16], F32, "sg"); sc = C.sb([128, 16], F32, "sc")
            ab = C.sb([128, 48], F32, "ab")
            C.dma(lambda e: e.dma_start(out=cv[:, :], in_=cvec.t[:, :]), R=[cvec], W=[cv])
            C.dma(lambda e: e.dma_start(out=ab[:, :], in_=ada_b.t[:, :]), R=[ada_b], W=[ab])
            C.op("act", lambda e: e.activation(out=sg[:, :], in_=cv[:, :], func=AF.Sigmoid), R=[cv], W=[sg])
            C.op("dve", lambda e: e.tensor_tensor(out=sc[:, :], in0=cv[:, :], in1=sg[:, :], op=ALU.mult), R=[cv, sg], W=[sc])
            wt = [C.sb([128, 6 * D], F32, "adaw%d" % i) for i in range(2)]
            macc = C.sb([128, 96], F32, "macc")
            C.op("dve", lambda e: e.tensor_copy(out=macc[:, :].rearrange("p (j w) -> p j w", w=2), in_=bc(ab, 0, 48, 2)), R=[ab], W=[macc])
            for k in range(8):
                w = wt[k % 2]
                pm = self.gbank()
                for hh in range(2):
                    C.dma(lambda e: e.dma_start(out=w[:, hh * 3072:(hh + 1) * 3072], in_=ada_w.t[k * 128:(k + 1) * 128, hh * 3072:(hh + 1) * 3072]), R=[ada_w], W=[w])
                for j in range(48):
                    self.mm(pm, pm[:, 2 * j:2 * j + 2], w[:, j * 128:(j + 1) * 128], sc[:, 2 * k:2 * k + 2], R=[w, sc], start=True, stop=True)
                C.op("dve", lambda e: e.tensor_tensor(out=macc[:, :], in0=macc[:, :], in1=pm[:, 0:96], op=ALU.add), R=[pm, macc], W=[macc])
            C.op("dve", lambda e: e.tensor_copy(out=self.mod[:, :], in_=macc[:, :]), R=[macc], W=[self.mod])
        if "mod" in self.dbg:
            self.dump_now("mod", self.mod, [128, 96])
        mod3 = self.mod[:, :].rearrange("p (j w) -> p j w", w=2)
        self.cols = C.sb([128, 48], F32, "cols")
        cols = self.cols

        def gs(dst0, scale_j0, which, g0):
            C.op("dve", lambda e: e.scalar_tensor_tensor(out=cols[:, dst0:dst0 + 8], in0=mod3[:, scale_j0:scale_j0 + 8, which], scalar=1.0, in1=self.gc[:, g0:g0 + 8], op0=ALU.add, op1=ALU.mult), R=[self.mod, self.gc], W=[cols])

        def cp(dst0, j0, which):
            C.op("dve", lambda e: e.tensor_copy(out=cols[:, dst0:dst0 + 8], in_=mod3[:, j0:j0 + 8, which]), R=[self.mod], W=[cols])
        gs(0, 8, 0, 0); cp(8, 0, 0); gs(16, 8, 1, 0); cp(24, 0, 1); gs(32, 32, 0, 8); cp(40, 24, 0)
        self.gcol = C.sb([128, 16], F32, "gcol")
        C.op("dve", lambda e: e.tensor_copy(out=self.gcol[:, 0:8], in_=mod3[:, 16:24, 0]), R=[self.mod], W=[self.gcol])
        C.op("dve", lambda e: e.tensor_copy(out=self.gcol[:, 8:16], in_=mod3[:, 40:48, 0]), R=[self.mod], W=[self.gcol])

    def mixer_scans(self, xo, xpre, ctx2, wlite, w_in, cwx, cbx, cwq, cbq, small, flags, ssd_d):
        C = self.C
        with Scope(C):
            K = self.K = type("K", (), {})()
            K.GS = C.sb([128, D], F32, "GS"); K.SH = C.sb([128, D], F32, "SH")
            K.W = C.sb([128, 8, WS], BF16, "Wscan")
            K.cwx = C.sb([128, 60], F32, "cwx"); K.cbx = C.sb([128, 12], F32, "cbx"); K.cwq = C.sb([128, 40], F32, "cwq"); K.cbq = C.sb([128, 8], F32, "cbq")
            K.small = C.sb([128, 48], F32, "small"); K.aneg = C.sb([128, 16], F32, "aneg")
            K.flags = C.sb([128, 8], F32, "flags"); K.Dh = C.sb([128, 16], F32, "Dh")
            C.dma(lambda e: e.dma_start(out=K.flags[:, :], in_=flags.t[:, :]), R=[flags], W=[K.flags])
            C.dma(lambda e: e.dma_start(out=K.Dh[:, :], in_=ssd_d.t[:, :]), R=[ssd_d], W=[K.Dh])
            K.xt = [C.sb([128, D], F32, "xt%d" % i) for i in range(2)]
            K.junk = C.sb([128, D], BF16, "junk"); K.xm = C.sb([128, D], F32, "xm"); K.xn = C.sb([128, D], BF16, "xn")
            K.ss = C.sb([128, 1], F32, "ss"); K.rstd = C.sb([128, 1], F32, "rstd")
            K.uT = C.sb([128, 8, 512], BF16, "uT")
            K.cv = C.sb([128, 12, 512], BF16, "cv"); K.cvq = C.sb([128, 8, 512], BF16, "cvq")
            K.acc = [C.sb([128, 512], F32, "acc%d" % i) for i in range(4)]
            K.raw = [C.sb([128, 512], F32, "raw%d" % i) for i in range(4)]
            K.nconv = 0
            K.Vtok = [C.sb([128, D], BF16, "Vtok%d" % i) for i in range(4)]
            K.SM = [C.sb([128, 48], F32, "SM%d" % i) for i in range(4)]
            K.sm = C.sb([128, 32], F32, "sm"); K.e1 = C.sb([128, 32], F32, "e1")
            K.S = [C.sb([128, 512], F32, "S%d" % g) for g in range(2)]
            K.Sbf = [C.sb([128, 512], BF16, "Sbf%d" % g) for g in range(2)]
            K.Cm = C.sb([128, 4, 128], F32, "Cm"); K.nm = C.sb([128, 4], F32, "nm"); K.mbc = C.sb([128, 8], F32, "mbc")
            K.Cbf = C.sb([128, 4, 128], BF16, "Cbf"); K.nbf = C.sb([128, 4], BF16, "nbf")
            K.sav = []
            for d in range(2):
                K.sav.append(dict(S=[C.sb([128, 512], F32, "SS%d%d" % (d, g)) for g in range(2)], Cm=C.sb([128, 512], F32, "SCm%d" % d),
                                  nm=C.sb([128, 4], F32, "Snm%d" % d), mbc=C.sb([128, 8], F32, "Smbc%d" % d)))
            K.cs = C.sb([128, 48], F32, "cs"); K.Xtok = C.sb([128, D], BF16, "Xtok"); K.BK = C.sb([128, 768], BF16, "BK")
            K.t16 = C.sb([128, 16], F32, "t16"); K.wS = C.sb([128, 16], F32, "wS"); K.expcum = C.sb([128, 16], F32, "expcum"); K.ncum = C.sb([128, 16], F32, "ncum")
            K.exptot = C.sb([128, 16], F32, "exptot")
            K.Xw = C.sb([128, D], BF16, "Xw"); K.Xdt = C.sb([128, D], BF16, "Xdt"); K.XD = C.sb([128, D], BF16, "XD")
            K.CBT = [C.sb([128, 128], BF16, "CBT%d" % g) for g in range(2)]
            K.E = [C.sb([128, 128], BF16, "E%d" % i) for i in range(2)]; K.M = [C.sb([128, 128], BF16, "M%d" % i) for i in range(2)]
            K.yout = [C.sb([128, D], F32, "yout%d" % i) for i in range(2)]; K.hout = [C.sb([128, D], F32, "hout%d" % i) for i in range(2)]
            K.a8 = C.sb([128, 8], F32, "a8"); K.amax = C.sb([8, 1], F32, "amax"); K.dg = C.sb([8, 8], F32, "dg")
            K.Mc = C.sb([128, 8], F32, "Mc"); K.cd = C.sb([128, 8], F32, "cd"); K.w8 = C.sb([128, 8], F32, "w8"); K.w8b = C.sb([128, 8], BF16, "w8b")
            K.t8 = C.sb([128, 8], F32, "t8"); K.fl = C.sb([128, 8], F32, "fl"); K.den = C.sb([128, 8], F32, "den"); K.rc = C.sb([128, 8], F32, "rc")
            K.Vw = C.sb([128, 8, 128], BF16, "Vw"); K.A = [C.sb([128, 128], BF16, "A%d" % h) for h in range(8)]
            K.nchunk = 0

            def P(**kw):
                return type("P", (), kw)()
            passes = [
                P(name="ctxF", src=ctx2, row0=0, n_sc=1, sc_tok=256, rows=256, wsrc=(wlite, 0), slot=0, sset=0, kind="ctx", full=False, rev=False, dd=0, init="zero", save=0, flag=None),
                P(name="ctxB", src=ctx2, row0=CTXL, n_sc=1, sc_tok=256, rows=256, wsrc=(wlite, 1), slot=1, sset=1, kind="ctx", full=False, rev=False, dd=0, init="zero", save=1, flag=None),
            ]
            for j in range(3):
                passes.append(P(name="pre%d" % j, src=xpre, row0=j * SEG, n_sc=8, sc_tok=512, rows=64, wsrc=(wlite, 2 + j), slot=2 + j, sset=2 + j, kind="lat", full=False, rev=False, dd=0, init="blend", save="blend", flag=j))
            passes.append(P(name="ownF", src=xo, row0=0, n_sc=8, sc_tok=512, rows=64, wsrc=(w_in, None), slot=5, sset=5, kind="lat", full=True, rev=False, dd=0, init=0, save=None, flag=None))
            passes.append(P(name="ownB", src=xo, row0=0, n_sc=8, sc_tok=512, rows=64, wsrc=(w_in, None), slot=5, sset=6, kind="lat", full=True, rev=True, dd=1, init=1, save=None, flag=None))
            if self.stop_after == "ctx":
                passes = passes[:2]
            cur_kind = None
            for Pp in passes:
                if Pp.kind != cur_kind:
                    cur_kind = Pp.kind
                    o = 0 if Pp.kind == "lat" else 16
                    self.col_to_bcast(self.cols, o, K.GS)
                    self.col_to_bcast(self.cols, o + 8, K.SH)
                self.run_pass(Pp, cwx, cbx, cwq, cbq, small)
            if "ctxstate" in self.dbg:
                for d in range(2):
                    for g in range(2):
                        self.dump_now("S%d%d" % (d, g), K.sav[d]["S"][g], [128, 512])
                    self.dump_now("Cm%d" % d, K.sav[d]["Cm"], [128, 512])
                    self.dump_now("mbc%d" % d, K.sav[d]["mbc"], [128, 8])
                    self.dump_now("nm%d" % d, K.sav[d]["nm"], [128, 4])

    def dump_now(self, name, buf, shape):
        o = self.outp("dbg_" + name, shape)
        self.C.dma(lambda e: e.dma_start(out=o.t[:, :], in_=buf[:, :]), R=[buf], W=[o])

    def run_pass(self, P, cwx, cbx, cwq, cbq, small):
        C = self.C; K = self.K
        wsrc, wi = P.wsrc
        for k in range(8):
            if wi is None:
                src_ap = wsrc.t[k * 128:(k + 1) * 128, 1024:1024 + WS]
            else:
                src_ap = wsrc.t[wi * D + k * 128: wi * D + (k + 1) * 128, :]
            C.dma(lambda e: e.dma_start(out=K.W[:, k, :], in_=src_ap), R=[wsrc], W=[K.W], q="pool")
        s = P.slot
        C.dma(lambda e: e.dma_start(out=K.cwx[:, :], in_=cwx.t[s * 128:(s + 1) * 128, :]), R=[cwx], W=[K.cwx])
        C.dma(lambda e: e.dma_start(out=K.cbx[:, :], in_=cbx.t[s * 128:(s + 1) * 128, :]), R=[cbx], W=[K.cbx])
        C.dma(lambda e: e.dma_start(out=K.cwq[:, :], in_=cwq.t[s * 128:(s + 1) * 128, :]), R=[cwq], W=[K.cwq])
        C.dma(lambda e: e.dma_start(out=K.cbq[:, :], in_=cbq.t[s * 128:(s + 1) * 128, :]), R=[cbq], W=[K.cbq])
        C.dma(lambda e: e.dma_start(out=K.small[:, :], in_=small.t[P.sset * 128:(P.sset + 1) * 128, :]), R=[small], W=[K.small])
        C.op("act", lambda e: e.activation(out=K.aneg[:, :], in_=K.small[:, 16:32], func=AF.Exp), R=[K.small], W=[K.aneg])
        C.op("dve", lambda e: e.tensor_scalar(out=K.aneg[:, :], in0=K.aneg[:, :], scalar1=-1.0, scalar2=None, op0=ALU.mult), R=[K.aneg], W=[K.aneg])
        st = [(K.S[0], lambda d: K.sav[d]["S"][0], 128), (K.S[1], lambda d: K.sav[d]["S"][1], 128), (K.Cm, lambda d: K.sav[d]["Cm"], 128),
              (K.nm, lambda d: K.sav[d]["nm"], 128), (K.mbc, lambda d: K.sav[d]["mbc"], 128)]

        def flat(b):
            return b[:, :, :].rearrange("p a b -> p (a b)") if len(b.t.shape) == 3 else b[:, :]
        if P.init == "zero":
            for cur, _, _ in st:
                C.op("pool", lambda e: e.memset(flat(cur), 0.0), W=[cur])
        elif P.init == "blend":
            f = K.flags[:, P.flag:P.flag + 1]; nf = K.flags[:, 4 + P.flag:5 + P.flag]
            for cur, sv, _ in st:
                a = sv(0); b = sv(1)
                C.op("dve", lambda e: e.tensor_scalar(out=flat(cur), in0=flat(a), scalar1=f, scalar2=None, op0=ALU.mult), R=[a, K.flags], W=[cur])
                C.op("dve", lambda e: e.scalar_tensor_tensor(out=flat(cur), in0=flat(b), scalar=nf, in1=flat(cur), op0=ALU.mult, op1=ALU.add), R=[b, K.flags, cur], W=[cur])
        else:
            for cur, sv, _ in st:
                a = sv(P.init)
                C.op("dve", lambda e: e.tensor_copy(out=flat(cur), in_=flat(a)), R=[a], W=[cur])
        for g in range(2):
            C.op("act", lambda e: e.activation(out=K.Sbf[g][:, :], in_=K.S[g][:, :], func=AF.Copy), R=[K.S[g]], W=[K.Sbf[g]])
        scs = list(range(P.n_sc))
        if P.rev:
            scs = scs[::-1]
        for sc in scs:
            self.prep_sc(P, sc)
            tiles = list(range(P.sc_tok // 128))
            if P.rev:
                tiles = tiles[::-1]
            for i in tiles:
                self.scan_chunk(P, sc, i)
        if P.save == "blend":
            f = K.flags[:, P.flag:P.flag + 1]; nf = K.flags[:, 4 + P.flag:5 + P.flag]
            for cur, sv, _ in st:
                a = sv(0); b = sv(1)
                C.op("dve", lambda e: e.tensor_scalar(out=flat(a), in0=flat(a), scalar1=nf, scalar2=None, op0=ALU.mult), R=[a, K.flags], W=[a])
                C.op("dve", lambda e: e.scalar_tensor_tensor(out=flat(a), in0=flat(cur), scalar=f, in1=flat(a), op0=ALU.mult, op1=ALU.add), R=[cur, K.flags, a], W=[a])
                C.op("dve", lambda e: e.tensor_scalar(out=flat(b), in0=flat(b), scalar1=f, scalar2=None, op0=ALU.mult), R=[b, K.flags], W=[b])
                C.op("dve", lambda e: e.scalar_tensor_tensor(out=flat(b), in0=flat(cur), scalar=nf, in1=flat(b), op0=ALU.mult, op1=ALU.add), R=[cur, K.flags, b], W=[b])
        elif P.save is not None:
            for cur, sv, _ in st:
                a = sv(P.save)
                C.op("dve", lambda e: e.tensor_copy(out=flat(a), in_=flat(cur)), R=[cur], W=[a])

    def prep_sc(self, P, sc):
        C = self.C; K = self.K
        T = P.sc_tok
        nt = T // 128
        for i in range(nt):
            xt = K.xt[i % 2]
            r0 = P.row0 + sc * T + i * 128
            C.dma(lambda e: e.dma_start(out=xt[:, :], in_=P.src.t[r0:r0 + 128, :]), R=[P.src], W=[xt])
            C.op("pool", lambda e: e.memset(K.ss[:, :], 0.0), W=[K.ss])
            C.op("act", lambda e: e.activation(out=K.junk[:, :], in_=xt[:, :], func=AF.Square, accum_out=K.ss[:, :]), R=[xt, K.ss], W=[K.junk, K.ss])
            C.op("act", lambda e: e.activation(out=K.rstd[:, :], in_=K.ss[:, :], func=AF.Sqrt, scale=1.0 / D, bias=self.epsb[:, :]), R=[K.ss, self.epsb], W=[K.rstd])
            C.op("dve", lambda e: e.reciprocal(out=K.rstd[:, :], in_=K.rstd[:, :]), R=[K.rstd], W=[K.rstd])
            C.op("dve", lambda e: e.scalar_tensor_tensor(out=K.xm[:, :], in0=xt[:, :], scalar=K.rstd[:, 0:1], in1=K.GS[:, :], op0=ALU.mult, op1=ALU.mult), R=[xt, K.rstd, K.GS], W=[K.xm])
            C.op("pool", lambda e: e.tensor_tensor(out=K.xn[:, :], in0=K.xm[:, :], in1=K.SH[:, :], op=ALU.add), R=[K.xm, K.SH], W=[K.xn])
            pt = self.tbank()
            for k in range(8):
                self.tr(pt, pt[:, k * 128:(k + 1) * 128], K.xn[:, k * 128:(k + 1) * 128], self.identb[:, :], R=[K.xn, self.identb])
            C.op("act", lambda e: e.activation(out=K.uT[:, :, i * 128:(i + 1) * 128], in_=pt[:, :].rearrange("p (k n) -> p k n", k=8), func=AF.Copy), R=[pt], W=[K.uT])
        if P.name == "ownB" and self.uT_d is not None:
            C.dma(lambda e: e.dma_start(out=self.uT_d.t[sc * 128:(sc + 1) * 128, :], in_=K.uT[:, :, :].rearrange("p k n -> p (k n)")), R=[K.uT], W=[self.uT_d])
        if "uT" in self.dbg and P.name == "ownF" and sc == 0:
            self.dump_bf("uT", K.uT, K.uT[:, :, :].rearrange("p k n -> p (k n)"), [128, 4096])
        xt_list = list(range(12)) if P.full else list(range(10))
        qt_list = list(range(8)) if P.full else list(range(4, 8))
        jobs = [("x", ct) for ct in xt_list] + [("q", ct) for ct in qt_list]
        for j0 in range(0, len(jobs), 2):
            ctxs = []
            for (kind, ct) in jobs[j0:j0 + 2]:
                off = (O_XBC if kind == "x" else O_QK) + ct * 128
                cw, cb, dst = (K.cwx, K.cbx, K.cv) if kind == "x" else (K.cwq, K.cbq, K.cvq)
                pb = self.abank()
                for k in range(8):
                    self.mm(pb, pb[:, 0:T], K.W[:, k, off:off + 128], K.uT[:, k, 0:T], R=[K.W, K.uT], start=(k == 0), stop=(k == 7))
                K.nconv += 1
                raw = K.raw[K.nconv % 4]; acc = K.acc[K.nconv % 4]
                C.op("act", lambda e: e.activation(out=raw[:, 0:T], in_=pb[:, 0:T], func=AF.Copy), R=[pb], W=[raw])
                ctxs.append((ct, cw, cb, dst, raw, acc))
            for (ct, cw, cb, dst, raw, acc) in ctxs:
                C.op("dve", lambda e: e.tensor_scalar(out=acc[:, 0:T], in0=raw[:, 0:T], scalar1=cw[:, ct * 5 + 2:ct * 5 + 3], scalar2=cb[:, ct:ct + 1], op0=ALU.mult, op1=ALU.add), R=[raw, cw, cb], W=[acc])
            for j in (0, 1, 3, 4):
                o = j - 2
                lo = max(0, -o); hi = P.rows - max(0, o)
                for (ct, cw, cb, dst, raw, acc) in ctxs:
                    a3 = acc[:, 0:T].rearrange("p (r t) -> p r t", t=P.rows); p3 = raw[:, 0:T].rearrange("p (r t) -> p r t", t=P.rows)
                    C.op("dve", lambda e: e.scalar_tensor_tensor(out=a3[:, :, lo:hi], in0=p3[:, :, lo + o:hi + o], scalar=cw[:, ct * 5 + j:ct * 5 + j + 1], in1=a3[:, :, lo:hi], op0=ALU.mult, op1=ALU.add), R=[raw, cw, acc], W=[acc])
            for (ct, cw, cb, dst, raw, acc) in ctxs:
                C.op("act", lambda e: e.activation(out=dst[:, ct, 0:T], in_=acc[:, 0:T], func=AF.Silu), R=[acc], W=[dst])
        for i in range(nt):
            for n in range(2):
                pb = self.abank()
                for k in range(8):
                    self.mm(pb, pb[:, 0:512], K.uT[:, k, i * 128:(i + 1) * 128], K.W[:, k, O_V + n * 512:O_V + (n + 1) * 512], R=[K.W, K.uT], start=(k == 0), stop=(k == 7))
                C.op("act", lambda e: e.activation(out=K.Vtok[i][:, n * 512:(n + 1) * 512], in_=pb[:, 0:512], func=AF.Copy), R=[pb], W=[K.Vtok[i]])
            pb = self.abank()
            dto = O_DT + 16 * P.dd; gto = O_GT + 16 * P.dd
            for k in range(8):
                self.mm(pb, pb[:, 0:16], K.uT[:, k, i * 128:(i + 1) * 128], K.W[:, k, dto:dto + 16], R=[K.W, K.uT], start=(k == 0), stop=(k == 7))
            for k in range(8):
                self.mm(pb, pb[:, 16:32], K.uT[:, k, i * 128:(i + 1) * 128], K.W[:, k, gto:gto + 16], R=[K.W, K.uT], start=(k == 0), stop=(k == 7))
            SM = K.SM[i]
            C.op("dve", lambda e: e.tensor_tensor(out=K.sm[:, 0:16], in0=pb[:, 0:16], in1=K.small[:, 0:16], op=ALU.add), R=[pb, K.small], W=[K.sm])
            C.op("dve", lambda e: e.tensor_tensor(out=K.sm[:, 16:32], in0=pb[:, 16:32], in1=K.small[:, 32:48], op=ALU.add), R=[pb, K.small], W=[K.sm])
            C.op("act", lambda e: e.activation(out=K.e1[:, 0:16], in_=K.sm[:, 0:16], func=AF.Exp), R=[K.sm], W=[K.e1])
            C.op("act", lambda e: e.activation(out=K.e1[:, 16:24], in_=K.sm[:, 24:32], func=AF.Exp, scale=-1.0), R=[K.sm], W=[K.e1])
            C.op("act", lambda e: e.activation(out=SM[:, 24:40], in_=K.e1[:, 0:16], func=AF.Ln, bias=1.0, scale=1.0), R=[K.e1], W=[SM])
            C.op("act", lambda e: e.activation(out=K.e1[:, 24:32], in_=K.e1[:, 16:24], func=AF.Ln, bias=1.0, scale=1.0), R=[K.e1], W=[K.e1])
            C.op("dve", lambda e: e.tensor_scalar(out=SM[:, 16:24], in0=K.e1[:, 24:32], scalar1=-1.0, scalar2=None, op0=ALU.mult), R=[K.e1], W=[SM])
            C.op("dve", lambda e: e.tensor_tensor(out=SM[:, 0:16], in0=SM[:, 24:40], in1=K.aneg[:, :], op=ALU.mult), R=[SM, K.aneg], W=[SM])
            C.op("dve", lambda e: e.tensor_copy(out=SM[:, 40:48], in_=K.sm[:, 16:24]), R=[K.sm], W=[SM])
            if "SM" in self.dbg and P.name == "ownF" and sc == 0 and i == 0:
                self.dump_now("SM", SM, [128, 48])
        if "cv" in self.dbg and P.name == "ownF" and sc == 0:
            self.dump_bf("cv", K.cv, K.cv[:, :, :].rearrange("p k n -> p (k n)"), [128, 12 * 512])
            self.dump_bf("cvq", K.cvq, K.cvq[:, :, :].rearrange("p k n -> p (k n)"), [128, 8 * 512])

    def dump_bf(self, name, buf, ap, shape):
        C = self.C
        o = self.outp("dbg_" + name, shape)
        n = shape[1]
        for c0 in range(0, n, 2048):
            c1 = min(n, c0 + 2048)
            t = C.sb([128, 2048], F32, "dmp")
            C.op("dve", lambda e: e.tensor_copy(out=t[:, 0:c1 - c0], in_=ap[:, c0:c1]), R=[buf], W=[t])
            C.dma(lambda e: e.dma_start(out=o.t[:, c0:c1], in_=t[:, 0:c1 - c0]), R=[t], W=[o])

    def scan_chunk(self, P, sc, i):
        C = self.C; K = self.K
        SKIP = os.environ.get('KSKIP', '').split(',')
        if 'scan' in SKIP:
            return
        tok = slice(i * 128, (i + 1) * 128)
        Uf = self.Lf if P.rev else self.Uf
        Ub = self.Lb if P.rev else self.Ub
        NEGM = self.NEGL if P.rev else self.NEGU
        SM = K.SM[i]
        full = P.full
        K.nchunk += 1
        par = K.nchunk % 2
        sl = self.dslot()
        o = sl.off
        self.mm(sl, sl.t[:, o:o + 24], Uf[:, :], SM[:, 0:24], R=[Uf, SM])
        self.mm(sl, sl.t[:, o + 24:o + 48], self.onesf[:, :], SM[:, 0:24], R=[self.onesf, SM])
        C.op("dve", lambda e: e.tensor_copy(out=K.cs[:, :], in_=sl.t[:, o:o + 48]), R=[sl], W=[K.cs])
        pt = self.tbank()
        for c in range(8):
            self.tr(pt, pt[:, c * 128:(c + 1) * 128], K.cv[:, c, tok], self.identb[:, :], R=[K.cv, self.identb])
        C.op("act", lambda e: e.activation(out=K.Xtok[:, :], in_=pt[:, :], func=AF.Copy), R=[pt], W=[K.Xtok])
        pt = self.tbank()
        for c in range(2):
            self.tr(pt, pt[:, c * 128:(c + 1) * 128], K.cv[:, 8 + c, tok], self.identb[:, :], R=[K.cv, self.identb])
        for c in range(4):
            self.tr(pt, pt[:, 256 + c * 128:256 + (c + 1) * 128], K.cvq[:, 4 + c, tok], self.identb[:, :], R=[K.cvq, self.identb])
        C.op("dve", lambda e: e.tensor_copy(out=K.BK[:, :], in_=pt[:, 0:768]), R=[pt], W=[K.BK])
        C.op("dve", lambda e: e.tensor_tensor(out=K.t16[:, :], in0=K.cs[:, 24:40], in1=K.cs[:, 0:16], op=ALU.subtract), R=[K.cs], W=[K.t16])
        C.op("act", lambda e: e.activation(out=K.t16[:, :], in_=K.t16[:, :], func=AF.Exp), R=[K.t16], W=[K.t16])
        C.op("dve", lambda e: e.tensor_tensor(out=K.wS[:, :], in0=K.t16[:, :], in1=SM[:, 24:40], op=ALU.mult), R=[K.t16, SM], W=[K.wS])
        X3 = K.Xtok[:, :].rearrange("p (h d) -> p h d", h=16)
        C.op("dve", lambda e: e.tensor_tensor(out=K.Xw[:, :].rearrange("p (h d) -> p h d", h=16), in0=X3, in1=bc(K.wS, 0, 16, 64), op=ALU.mult), R=[K.Xtok, K.wS], W=[K.Xw])
        C.op("act", lambda e: e.activation(out=K.exptot[:, :], in_=K.cs[:, 24:40], func=AF.Exp), R=[K.cs], W=[K.exptot])
        if full:
            C.op("act", lambda e: e.activation(out=K.expcum[:, :], in_=K.cs[:, 0:16], func=AF.Exp), R=[K.cs], W=[K.expcum])
            C.op("dve", lambda e: e.tensor_scalar(out=K.ncum[:, :], in0=K.cs[:, 0:16], scalar1=-1.0, scalar2=None, op0=ALU.mult), R=[K.cs], W=[K.ncum])
            C.op("pool", lambda e: e.tensor_tensor(out=K.Xdt[:, :].rearrange("p (h d) -> p h d", h=16), in0=X3, in1=bc(SM, 24, 16, 64), op=ALU.mult), R=[K.Xtok, SM], W=[K.Xdt])
            if P.dd == 0 and self.addD:
                C.op("pool", lambda e: e.tensor_tensor(out=K.XD[:, :].rearrange("p (h d) -> p h d", h=16), in0=X3, in1=bc(K.Dh, 0, 16, 64), op=ALU.mult), R=[K.Xtok, K.Dh], W=[K.XD])
            yout = K.yout[par]
            for g in range(2):
                sl = self.dslot(); o = sl.off
                self.mm(sl, sl.t[:, o:o + 128], K.cv[:, 8 + g, tok], K.cv[:, 10 + g, tok], R=[K.cv])
                C.op("act", lambda e: e.activation(out=K.CBT[g][:, :], in_=sl.t[:, o:o + 128], func=AF.Copy), R=[sl], W=[K.CBT[g]])
                self.mm(self.PYI, self.PYI[:, :], K.cv[:, 10 + g, tok], K.Sbf[g][:, :], R=[K.cv, K.Sbf[g]])
                addD = (P.dd == 0) and self.addD
                if addD:
                    self.mm(self.PY, self.PY[:, :], self.identb[:, :], K.XD[:, g * 512:(g + 1) * 512], R=[self.identb, K.XD], start=True, stop=False)
                pend = None
                for hh in range(8):
                    h = g * 8 + hh
                    sl = self.dslot(); o = sl.off
                    self.mm(sl, sl.t[:, o:o + 128], colbc(SM, h, 128), Uf[:, :], R=[SM, Uf], start=True, stop=False)
                    self.mm(sl, sl.t[:, o:o + 128], self.identf[:, :], NEGM[:, :], R=[self.identf, NEGM], start=False, stop=True)
                    E = K.E[hh % 2]; M = K.M[hh % 2]
                    C.op("act", lambda e: e.activation(out=E[:, :], in_=sl.t[:, o:o + 128], func=AF.Exp, bias=K.ncum[:, h:h + 1], scale=1.0), R=[sl, K.ncum], W=[E])
                    C.op("dve", lambda e: e.tensor_tensor(out=M[:, :], in0=E[:, :], in1=K.CBT[g][:, :], op=ALU.mult), R=[E, K.CBT[g]], W=[M])
                    if pend is not None:
                        pM, phh, ph = pend
                        self.mm(self.PY, self.PY[:, phh * 64:(phh + 1) * 64], pM[:, :], K.Xdt[:, ph * 64:(ph + 1) * 64], R=[pM, K.Xdt], start=(not addD), stop=True)
                    pend = (M, hh, h)
                pM, phh, ph = pend
                self.mm(self.PY, self.PY[:, phh * 64:(phh + 1) * 64], pM[:, :], K.Xdt[:, ph * 64:(ph + 1) * 64], R=[pM, K.Xdt], start=(not addD), stop=True)
                yg = yout[:, g * 512:(g + 1) * 512]
                C.op("dve", lambda e: e.tensor_tensor(out=yg.rearrange("p (h d) -> p h d", h=8), in0=self.PYI[:, :].rearrange("p (h d) -> p h d", h=8), in1=bc(K.expcum, g * 8, 8, 64), op=ALU.mult), R=[self.PYI, K.expcum], W=[yout])
                C.op("dve", lambda e: e.tensor_tensor(out=yg, in0=yg, in1=self.PY[:, :], op=ALU.add), R=[self.PY, yout], W=[yout])
            r0 = sc * P.sc_tok + i * 128
            yd = self.y_d[P.dd]
            C.dma(lambda e: e.dma_start(out=yd.t[r0:r0 + 128, :], in_=yout[:, :]), R=[yout], W=[yd])
        for g in range(2):
            pb = self.gbank()
            self.mm(pb, pb[:, 0:512], K.BK[:, g * 128:(g + 1) * 128], K.Xw[:, g * 512:(g + 1) * 512], R=[K.BK, K.Xw])
            S3 = K.S[g][:, :].rearrange("p (h d) -> p h d", h=8)
            C.op("dve", lambda e: e.tensor_tensor(out=S3, in0=S3, in1=bc(K.exptot, g * 8, 8, 64), op=ALU.mult), R=[K.S[g], K.exptot], W=[K.S[g]])
            C.op("dve", lambda e: e.tensor_tensor(out=K.S[g][:, :], in0=K.S[g][:, :], in1=pb[:, 0:512], op=ALU.add), R=[K.S[g], pb], W=[K.S[g]])
            C.op("act", lambda e: e.activation(out=K.Sbf[g][:, :], in_=K.S[g][:, :], func=AF.Copy), R=[K.S[g]], W=[K.Sbf[g]])
        if 'mlstm' in SKIP:
            return
        C.op("dve", lambda e: e.tensor_tensor(out=K.a8[:, :], in0=SM[:, 40:48], in1=K.cs[:, 16:24], op=ALU.subtract), R=[SM, K.cs], W=[K.a8])
        sl = self.dslot(); o = sl.off
        self.tr(sl, sl.t[0:8, o:o + 128], K.a8[:, 0:8], self.identf[:, :], R=[K.a8, self.identf])
        C.op("dve", lambda e: e.reduce_max(out=K.amax[:, :], in_=sl.t[0:8, o:o + 128], axis=AX.X), R=[sl], W=[K.amax])
        C.op("dve", lambda e: e.tensor_scalar(out=K.dg[:, :], in0=self.identf[0:8, 0:8], scalar1=K.amax[:, 0:1], scalar2=None, op0=ALU.mult), R=[self.identf, K.amax], W=[K.dg])
        sl = self.dslot(); o = sl.off
        self.mm(sl, sl.t[:, o:o + 8], self.onesf[0:8, :], K.dg[:, :], R=[self.onesf, K.dg])
        C.op("dve", lambda e: e.tensor_tensor(out=K.Mc[:, :], in0=K.mbc[:, :], in1=sl.t[:, o:o + 8], op=ALU.max), R=[K.mbc, sl], W=[K.Mc])
        C.op("dve", lambda e: e.tensor_tensor(out=K.cd[:, :], in0=K.mbc[:, :], in1=K.Mc[:, :], op=ALU.subtract), R=[K.mbc, K.Mc], W=[K.cd])
        C.op("act", lambda e: e.activation(out=K.cd[:, :], in_=K.cd[:, :], func=AF.Exp), R=[K.cd], W=[K.cd])
        C.op("dve", lambda e: e.tensor_tensor(out=K.mbc[:, :], in0=K.cs[:, 40:48], in1=K.Mc[:, :], op=ALU.add), R=[K.cs, K.Mc], W=[K.mbc])
        C.op("dve", lambda e: e.tensor_tensor(out=K.w8[:, :], in0=K.a8[:, :], in1=K.Mc[:, :], op=ALU.subtract), R=[K.a8, K.Mc], W=[K.w8])
        C.op("act", lambda e: e.activation(out=K.w8[:, :], in_=K.w8[:, :], func=AF.Exp), R=[K.w8], W=[K.w8])
        C.op("dve", lambda e: e.tensor_copy(out=K.w8b[:, :], in_=K.w8[:, :]), R=[K.w8], W=[K.w8b])
        C.op("pool", lambda e: e.tensor_tensor(out=K.Vw[:, :, :], in0=K.Vtok[i][:, :].rearrange("p (h d) -> p h d", h=8), in1=bc(K.w8, 0, 8, 128), op=ALU.mult), R=[K.Vtok[i], K.w8], W=[K.Vw])
        for hh in range(2):
            ps_ = slice(hh * 64, (hh + 1) * 64)
            cdv = bass.AP(K.cd.t, hh * 64 * 8 + hh, [[8, 64], [2, 4], [0, 128]])
            C.op("dve", lambda e: e.tensor_tensor(out=K.Cm[ps_, :, :], in0=K.Cm[ps_, :, :], in1=cdv, op=ALU.mult), R=[K.Cm, K.cd], W=[K.Cm])
            cdn = bass.AP(K.cd.t, hh * 64 * 8 + hh, [[8, 64], [2, 4]])
            C.op("dve", lambda e: e.tensor_tensor(out=K.nm[ps_, :], in0=K.nm[ps_, :], in1=cdn, op=ALU.mult), R=[K.nm, K.cd], W=[K.nm])
        if full:
            C.op("act", lambda e: e.activation(out=K.Cbf[:, :, :], in_=K.Cm[:, :, :], func=AF.Copy, scale=0.125), R=[K.Cm], W=[K.Cbf])
            C.op("act", lambda e: e.activation(out=K.nbf[:, :], in_=K.nm[:, :], func=AF.Copy, scale=0.125), R=[K.nm], W=[K.nbf])
            C.op("dve", lambda e: e.tensor_tensor(out=K.t8[:, :], in0=K.cs[:, 16:24], in1=K.Mc[:, :], op=ALU.add), R=[K.cs, K.Mc], W=[K.t8])
            C.op("act", lambda e: e.activation(out=K.fl[:, :], in_=K.t8[:, :], func=AF.Exp, scale=-1.0), R=[K.t8], W=[K.fl])
            sden = self.PYI; od = 0
            for h in range(8):
                c = h // 2; pr = (h % 2) * 64
                KT = K.cvq[pr:pr + 64, 4 + c, tok]; QT = K.cvq[pr:pr + 64, c, tok]
                sl = self.dslot(); o = sl.off
                self.mm(sl, sl.t[:, o:o + 128], KT, QT, R=[K.cvq])
                A = K.A[h]
                C.op("dve", lambda e: e.scalar_tensor_tensor(out=A[:, :], in0=sl.t[:, o:o + 128], scalar=0.125, in1=Ub[:, :], op0=ALU.mult, op1=ALU.mult), R=[sl, Ub], W=[A])
                self.mm(sden, sden.t[:, od + h:od + h + 1], A[:, :], K.w8b[:, h:h + 1], R=[A, K.w8b], start=True, stop=False)
                self.mm(sden, sden.t[:, od + h:od + h + 1], QT, K.nbf[pr:pr + 64, c:c + 1], R=[K.cvq, K.nbf], start=False, stop=True)
            C.op("dve", lambda e: e.tensor_copy(out=K.den[:, :], in_=sden.t[:, od:od + 8]), R=[sden], W=[K.den])
            C.op("dve", lambda e: e.scalar_tensor_tensor(out=K.den[:, :], in0=K.den[:, :], scalar=-1.0, in1=K.den[:, :], op0=ALU.mult, op1=ALU.max), R=[K.den], W=[K.den])
            C.op("dve", lambda e: e.tensor_tensor(out=K.den[:, :], in0=K.den[:, :], in1=K.fl[:, :], op=ALU.max), R=[K.den, K.fl], W=[K.den])
            C.op("dve", lambda e: e.reciprocal(out=K.rc[:, :], in_=K.den[:, :]), R=[K.den], W=[K.rc])
            hout = K.hout[par]
            for h in range(8):
                c = h // 2; pr = (h % 2) * 64
                QT = K.cvq[pr:pr + 64, c, tok]
                sl = self.dslot(); o = sl.off
                self.mm(sl, sl.t[:, o:o + 128], K.A[h][:, :], K.Vw[:, h, :], R=[K.A[h], K.Vw], start=True, stop=False)
                self.mm(sl, sl.t[:, o:o + 128], QT, K.Cbf[pr:pr + 64, c, :], R=[K.cvq, K.Cbf], start=False, stop=True)
                C.op("act", lambda e: e.activation(out=hout[:, h * 128:(h + 1) * 128], in_=sl.t[:, o:o + 128], func=AF.Copy, scale=K.rc[:, h:h + 1]), R=[sl, K.rc], W=[hout])
            r0 = sc * P.sc_tok + i * 128
            hd = self.h_d[P.dd]
            C.dma(lambda e: e.dma_start(out=hd.t[r0:r0 + 128, :], in_=hout[:, :]), R=[hout], W=[hd])
        if 'mupd' in SKIP:
            return
        sn2 = self.PY; on = 0
        for h in range(8):
            c = h // 2; hh = h % 2; ps_ = slice(hh * 64, (hh + 1) * 64)
            Kp = K.BK[:, 256 + c * 128:256 + (c + 1) * 128]
            sl = self.dslot(); o = sl.off
            self.mm(sl, sl.t[:, o:o + 128], Kp, K.Vw[:, h, :], R=[K.BK, K.Vw])
            C.op("dve", lambda e: e.tensor_tensor(out=K.Cm[ps_, c, :], in0=K.Cm[ps_, c, :], in1=sl.t[ps_, o:o + 128], op=ALU.add), R=[K.Cm, sl], W=[K.Cm])
            self.mm(sn2, sn2.t[:, on + h:on + h + 1], Kp, K.w8b[:, h:h + 1], R=[K.BK, K.w8b])
        for hh in range(2):
            ps_ = slice(hh * 64, (hh + 1) * 64)
            src = bass.AP(sn2.t, hh * 64 * 512 + on + hh, [[512, 64], [2, 4]])
            C.op("dve", lambda e: e.tensor_tensor(out=K.nm[ps_, :], in0=K.nm[ps_, :], in1=src, op=ALU.add), R=[K.nm, sn2], W=[K.nm])


    def load_w_bf(self, dst, dram, row0, nk, c0, c1, dcol0=0):
        C = self.C
        for k in range(nk):
            C.dma(lambda e: e.dma_start(out=dst[:, k, dcol0:dcol0 + (c1 - c0)], in_=dram.t[row0 + k * 128:row0 + (k + 1) * 128, c0:c1]), R=[dram], W=[dst], q="pool")

    def abank(self):
        self._a = (self._a + 1) % len(self.Gall)
        return self.Gall[self._a]

    def rms_rstd(self, src_ap, srcbuf, junk, ss, rstd, n):
        C = self.C
        C.op("pool", lambda e: e.memset(ss[:, :], 0.0), W=[ss])
        C.op("act", lambda e: e.activation(out=junk[:, :], in_=src_ap, func=AF.Square, accum_out=ss[:, :]), R=[srcbuf, ss], W=[junk, ss])
        C.op("act", lambda e: e.activation(out=rstd[:, :], in_=ss[:, :], func=AF.Sqrt, scale=1.0 / n, bias=self.epsb[:, :]), R=[ss, self.epsb], W=[rstd])
        C.op("dve", lambda e: e.reciprocal(out=rstd[:, :], in_=rstd[:, :]), R=[rstd], W=[rstd])

    def proj(self, lhsT_buf, W, wc0, n512, evac):
        for n in range(n512):
            pb = self.abank()
            for k in range(8):
                self.mm(pb, pb[:, 0:512], lhsT_buf[:, k, :], W[:, k, wc0 + n * 512:wc0 + (n + 1) * 512], R=[lhsT_buf, W], start=(k == 0), stop=(k == 7))
            evac(n, pb)

    def transp8(self, src, dstT):
        C = self.C
        pt = self.tbank()
        for k in range(8):
            self.tr(pt, pt[:, k * 128:(k + 1) * 128], src[:, k * 128:(k + 1) * 128], self.identb[:, :], R=[src, self.identb])
        C.op("act", lambda e: e.activation(out=dstT[:, :, :], in_=pt[:, :].rearrange("p (k n) -> p k n", k=8), func=AF.Copy), R=[pt], W=[dstT])

    def merge_pass(self, xo, w_in, vecs, w_so, w_mo, w_o):
        C = self.C
        self.Gall = self.G + [self.PY, self.PYI] + self.DS
        self._a = 0
        with Scope(C):
            Wz = C.sb([128, 8, 4096], BF16, "Wz")
            self.load_w_bf(Wz, w_in, 0, 8, 0, 1024, 0)
            self.load_w_bf(Wz, w_in, 0, 8, 4672, 7744, 1024)
            Wso = C.sb([128, 8, D], BF16, "Wso"); Wmo = C.sb([128, 8, D], BF16, "Wmo"); Wo = C.sb([128, 8, D], BF16, "Wo")
            self.load_w_bf(Wso, w_so, 0, 8, 0, D); self.load_w_bf(Wmo, w_mo, 0, 8, 0, D); self.load_w_bf(Wo, w_o, 0, 8, 0, D)
            gssd = C.sb([128, D], F32, "gssd"); gml = C.sb([128, D], F32, "gml"); gate = C.sb([128, D], F32, "gate")
            C.dma(lambda e: e.dma_start(out=gssd[:, :], in_=vecs.t[0:128, :]), R=[vecs], W=[gssd])
            C.dma(lambda e: e.dma_start(out=gml[:, :], in_=vecs.t[128:256, :]), R=[vecs], W=[gml])
            self.col_to_bcast(self.gcol, 0, gate)
            xt = C.sb([128, D], F32, "m_xt"); uT = C.sb([128, 8, 128], BF16, "m_uT")
            A1 = C.sb([128, D], F32, "A1"); A2 = C.sb([128, D], F32, "A2"); A3 = C.sb([128, D], F32, "A3"); A4 = C.sb([128, D], F32, "A4")
            Zz = C.sb([128, D], F32, "Zz"); Zo = C.sb([128, D], F32, "Zo"); Zg1 = C.sb([128, D], F32, "Zg1"); Zg2 = C.sb([128, D], F32, "Zg2"); junk = C.sb([128, D], BF16, "m_junk")
            yn = C.sb([128, D], BF16, "yn"); hn = C.sb([128, D], BF16, "hn"); nT = C.sb([128, 8, 128], BF16, "nT"); nT2 = C.sb([128, 8, 128], BF16, "nT2"); nT3 = C.sb([128, 8, 128], BF16, "nT3")
            mg = C.sb([128, D], BF16, "mg"); x1 = C.sb([128, D], F32, "x1t")
            ss = C.sb([128, 1], F32, "m_ss"); rstd = C.sb([128, 1], F32, "m_rstd"); ss8 = C.sb([128, 8], F32, "ss8")
            for t in range(32):
                r0 = t * 128
                sc, i = t // 4, t % 4
                C.dma(lambda e: e.dma_start(out=xt[:, :], in_=xo.t[r0:r0 + 128, :]), R=[xo], W=[xt])
                C.dma(lambda e: e.dma_start(out=uT[:, :, :], in_=self.uT_d.t[sc * 128:(sc + 1) * 128, :].rearrange("p (k n) -> p k n", k=8)[:, :, i * 128:(i + 1) * 128]), R=[self.uT_d], W=[uT])
                C.dma(lambda e: e.dma_start(out=A1[:, :], in_=self.y_d[0].t[r0:r0 + 128, :]), R=[self.y_d[0]], W=[A1])
                C.dma(lambda e: e.dma_start(out=A2[:, :], in_=self.y_d[1].t[r0:r0 + 128, :]), R=[self.y_d[1]], W=[A2])
                C.dma(lambda e: e.dma_start(out=A3[:, :], in_=self.h_d[0].t[r0:r0 + 128, :]), R=[self.h_d[0]], W=[A3])
                C.dma(lambda e: e.dma_start(out=A4[:, :], in_=self.h_d[1].t[r0:r0 + 128, :]), R=[self.h_d[1]], W=[A4])
                def actev(dst, func):
                    return lambda n, pb: C.op("act", lambda e: e.activation(out=dst[:, n * 512:(n + 1) * 512], in_=pb[:, 0:512], func=func), R=[pb], W=[dst])
                self.proj(uT, Wz, 0, 2, actev(Zz, AF.Silu))
                self.proj(uT, Wz, 1024, 2, actev(Zo, AF.Sigmoid))
                self.proj(uT, Wz, 2048, 2, actev(Zg1, AF.Sigmoid))
                self.proj(uT, Wz, 3072, 2, actev(Zg2, AF.Sigmoid))
                C.op("dve", lambda e: e.tensor_tensor(out=A1[:, :], in0=A1[:, :], in1=A2[:, :], op=ALU.add), R=[A1, A2], W=[A1])
                C.op("dve", lambda e: e.tensor_tensor(out=A1[:, :], in0=A1[:, :], in1=Zz[:, :], op=ALU.mult), R=[A1, Zz], W=[A1])
                self.rms_rstd(A1[:, :], A1, junk, ss, rstd, D)
                C.op("dve", lambda e: e.scalar_tensor_tensor(out=yn[:, :], in0=A1[:, :], scalar=rstd[:, 0:1], in1=gssd[:, :], op0=ALU.mult, op1=ALU.mult), R=[A1, rstd, gssd], W=[yn])
                self.transp8(yn, nT)
                C.op("dve", lambda e: e.tensor_tensor(out=A3[:, :], in0=A3[:, :], in1=A4[:, :], op=ALU.add), R=[A3, A4], W=[A3])
                C.op("pool", lambda e: e.tensor_tensor(out=A4[:, :], in0=A3[:, :], in1=A3[:, :], op=ALU.mult), R=[A3], W=[A4])
                C.op("dve", lambda e: e.tensor_reduce(out=ss8[:, :], in_=A4[:, :].rearrange("p (h d) -> p h d", h=8), axis=AX.X, op=ALU.add), R=[A4], W=[ss8])
                C.op("act", lambda e: e.activation(out=ss8[:, :], in_=ss8[:, :], func=AF.Sqrt, scale=1.0 / 128, bias=self.epsb[:, :]), R=[ss8, self.epsb], W=[ss8])
                C.op("dve", lambda e: e.reciprocal(out=ss8[:, :], in_=ss8[:, :]), R=[ss8], W=[ss8])
                C.op("dve", lambda e: e.tensor_tensor(out=A3[:, :].rearrange("p (h d) -> p h d", h=8), in0=A3[:, :].rearrange("p (h d) -> p h d", h=8), in1=bc(ss8, 0, 8, 128), op=ALU.mult), R=[A3, ss8], W=[A3])
                C.op("pool", lambda e: e.tensor_tensor(out=A3[:, :], in0=A3[:, :], in1=gml[:, :], op=ALU.mult), R=[A3, gml], W=[A3])
                C.op("dve", lambda e: e.tensor_tensor(out=hn[:, :], in0=A3[:, :], in1=Zo[:, :], op=ALU.mult), R=[A3, Zo], W=[hn])
                self.transp8(hn, nT2)
                self.proj(nT, Wso, 0, 2, lambda n, pb: C.op("dve", lambda e: e.tensor_tensor(out=A1[:, n * 512:(n + 1) * 512], in0=pb[:, 0:512], in1=Zg1[:, n * 512:(n + 1) * 512], op=ALU.mult), R=[pb, Zg1], W=[A1]))
                self.proj(nT2, Wmo, 0, 2, lambda n, pb: C.op("dve", lambda e: e.tensor_tensor(out=A2[:, n * 512:(n + 1) * 512], in0=pb[:, 0:512], in1=Zg2[:, n * 512:(n + 1) * 512], op=ALU.mult), R=[pb, Zg2], W=[A2]))
                C.op("dve", lambda e: e.tensor_tensor(out=mg[:, :], in0=A1[:, :], in1=A2[:, :], op=ALU.add), R=[A1, A2], W=[mg])
                self.transp8(mg, nT3)
                self.proj(nT3, Wo, 0, 2, lambda n, pb: C.op("dve", lambda e: e.tensor_tensor(out=x1[:, n * 512:(n + 1) * 512], in0=pb[:, 0:512], in1=gate[:, n * 512:(n + 1) * 512], op=ALU.mult), R=[pb, gate], W=[x1]))
                C.op("pool", lambda e: e.tensor_tensor(out=x1[:, :], in0=x1[:, :], in1=xt[:, :], op=ALU.add), R=[x1, xt], W=[x1])
                C.dma(lambda e: e.dma_start(out=self.x1_d.t[r0:r0 + 128, :], in_=x1[:, :]), R=[x1], W=[self.x1_d])

    def ffn_prep(self, vecs, router_w, router_b, sh_g, sh_u, sh_d):
        C = self.C
        with Scope(C):
            if self.Y_d is not None:
                zt = C.sb([128, 2048], F32, "zt")
                C.op("pool", lambda e: e.memset(zt[:, :], 0.0), W=[zt])
                for i in range(SEG * 8 // 256):
                    C.dma(lambda e: e.dma_start(out=self.Y_d.t[i * 256:(i + 1) * 256, :].rearrange("(p a) n -> p (a n)", p=128), in_=zt[:, :]), R=[zt], W=[])
            GS2 = C.sb([128, D], F32, "GS2"); SH2 = C.sb([128, D], F32, "SH2")
            self.col_to_bcast(self.cols, 32, GS2); self.col_to_bcast(self.cols, 40, SH2)
            rw = C.sb([128, 8, NEXP], F32, "rw"); rb = C.sb([128, NEXP], F32, "rb")
            C.dma(lambda e: e.dma_start(out=rw[:, :, :], in_=router_w.t.ap().rearrange("(k p) n -> p k n", p=128)), R=[router_w], W=[rw])
            C.dma(lambda e: e.dma_start(out=rb[:, :], in_=router_b.t[:, :]), R=[router_b], W=[rb])
            Wsgu = C.sb([128, 8, 512], BF16, "Wsgu"); Wsd = C.sb([128, 2, D], BF16, "Wsd")
            self.load_w_bf(Wsgu, sh_g, 0, 8, 0, 256, 0); self.load_w_bf(Wsgu, sh_u, 0, 8, 0, 256, 256); self.load_w_bf(Wsd, sh_d, 0, 2, 0, D)
            iotaE = C.sb([128, NEXP], F32, "iotaE")
            C.op("pool", lambda e: e.iota(iotaE[:, :], pattern=[[1, NEXP]], base=0, channel_multiplier=0, allow_small_or_imprecise_dtypes=True), W=[iotaE])
            C.op("dve", lambda e: e.tensor_scalar(out=iotaE[:, :], in0=iotaE[:, :], scalar1=float(CAP), scalar2=None, op0=ALU.mult), R=[iotaE], W=[iotaE])
            cnt = C.sb([128, NEXP], F32, "cnt")
            C.op("pool", lambda e: e.memset(cnt[:, :], 0.0), W=[cnt])
            SUF = C.sb([128, 2, NEXP], BF16, "SUF")
            C.op("pool", lambda e: e.memset(SUF[:, :, :], 0.0), W=[SUF])
            C.op("dve", lambda e: e.tensor_copy(out=SUF[:, 0, 0:128], in_=self.SUb[:, :]), R=[self.SUb, SUF], W=[SUF])
            C.op("dve", lambda e: e.tensor_copy(out=SUF[:, 0, 128:256], in_=self.onesb[:, :]), R=[self.onesb, SUF], W=[SUF])
            C.op("dve", lambda e: e.tensor_copy(out=SUF[:, 1, 128:256], in_=self.SUb[:, :]), R=[self.SUb, SUF], W=[SUF])
            jv = C.sb([128, 8, NEXP], F32, "jv")
            C.op("pool", lambda e: e.iota(jv[:, :, :], pattern=[[1, 8], [0, NEXP]], base=0, channel_multiplier=0, allow_small_or_imprecise_dtypes=True), W=[jv])
            emT = C.sb([128, 2, 128], BF16, "emT"); jr = C.sb([128, NEXP], F32, "jr")
            Li = C.sb([128, NEXP * CAP // 128, 4], F32, "Li")
            C.op("pool", lambda e: e.memset(Li[:, :, :], 0.0), W=[Li])
            C.op("pool", lambda e: e.memset(Li[:, :, 0:1], 1.0e9), R=[Li], W=[Li])
            C.op("pool", lambda e: e.memset(Li[:, :, 1:2], 1.0e9), R=[Li], W=[Li])
            C.dma(lambda e: e.dma_start(out=self.L_d.t.ap().rearrange("(p a) n -> p a n", p=128), in_=Li[:, :, :]), R=[Li], W=[self.L_d])
            zb = C.sb([128, D], BF16, "zb")
            C.op("pool", lambda e: e.memset(zb[:, :], 0.0), W=[zb])
            C.dma(lambda e: e.dma_start(out=self.u2_d.t[SEG:SEG + 128, :], in_=zb[:, :]), R=[zb], W=[self.u2_d])
            x1 = C.sb([128, D], F32, "f_x1"); junk = C.sb([128, D], BF16, "f_junk"); ss = C.sb([128, 1], F32, "f_ss"); rstd = C.sb([128, 1], F32, "f_rstd")
            u2 = C.sb([128, D], F32, "u2"); u2b = C.sb([128, D], BF16, "u2b")
            u2Tf = C.sb([128, 8, 128], F32, "u2Tf"); u2Tb = C.sb([128, 8, 128], BF16, "u2Tb")
            sc_ = C.sb([128, NEXP], F32, "scores"); gr = C.sb([128, NEXP], F32, "grouped"); sel = C.sb([128, NEXP], F32, "sel")
            m8g = C.sb([128, 8, 8], F32, "m8g"); gs = C.sb([128, 8], F32, "gs"); g8 = C.sb([128, 8], F32, "g8"); pen = C.sb([128, 8], F32, "pen")
            e8 = C.sb([128, 8], F32, "e8"); em = C.sb([128, NEXP], F32, "em"); emb = C.sb([128, NEXP], BF16, "emb")
            Wc = C.sb([128, NEXP], F32, "Wc"); wsum = C.sb([128, 1], F32, "wsum")
            pos = C.sb([128, NEXP], F32, "pos"); dst = C.sb([128, NEXP], F32, "dst"); ov = C.sb([128, NEXP], F32, "ov")
            oh = C.sb([128, 8, NEXP], F32, "oh"); tmp = C.sb([128, 8, NEXP], F32, "ohtmp")
            dj = C.sb([128, 8], F32, "dj"); dji = C.sb([128, 8], I32, "dji"); rowd = C.sb([128, 8, 4], F32, "rowd")
            hs = C.sb([128, 2, 128], BF16, "hs"); sg = C.sb([128, 256], F32, "sgs"); sho = C.sb([128, D], F32, "sho")
            for t in range(32):
                r0 = t * 128
                C.dma(lambda e: e.dma_start(out=x1[:, :], in_=self.x1_d.t[r0:r0 + 128, :]), R=[self.x1_d], W=[x1])
                self.rms_rstd(x1[:, :], x1, junk, ss, rstd, D)
                C.op("dve", lambda e: e.scalar_tensor_tensor(out=u2[:, :], in0=x1[:, :], scalar=rstd[:, 0:1], in1=GS2[:, :], op0=ALU.mult, op1=ALU.mult), R=[x1, rstd, GS2], W=[u2])
                C.op("pool", lambda e: e.tensor_tensor(out=u2[:, :], in0=u2[:, :], in1=SH2[:, :], op=ALU.add), R=[u2, SH2], W=[u2])
                C.op("act", lambda e: e.activation(out=u2b[:, :], in_=u2[:, :], func=AF.Copy), R=[u2], W=[u2b])
                C.dma(lambda e: e.dma_start(out=self.u2_d.t[r0:r0 + 128, :], in_=u2b[:, :]), R=[u2b], W=[self.u2_d])
                for half in range(2):
                    pb = self.abank()
                    for kk in range(4):
                        k = half * 4 + kk
                        self.tr(pb, pb[:, kk * 128:(kk + 1) * 128], u2[:, k * 128:(k + 1) * 128], self.identf[:, :], R=[u2, self.identf])
                    C.op("act", lambda e: e.activation(out=u2Tf[:, half * 4:(half + 1) * 4, :], in_=pb[:, 0:512].rearrange("p (k n) -> p k n", k=4), func=AF.Copy), R=[pb], W=[u2Tf])
                C.op("dve", lambda e: e.tensor_copy(out=u2Tb[:, :, :], in_=u2Tf[:, :, :]), R=[u2Tf], W=[u2Tb])
                pb = self.abank()
                for k in range(8):
                    self.mm(pb, pb[:, 0:NEXP], u2Tf[:, k, :], rw[:, k, :], R=[u2Tf, rw], start=(k == 0), stop=(k == 7))
                C.op("act", lambda e: e.activation(out=sc_[:, :], in_=pb[:, 0:NEXP], func=AF.Sigmoid), R=[pb], W=[sc_])
                C.op("dve", lambda e: e.tensor_tensor(out=gr[:, :], in0=sc_[:, :], in1=rb[:, :], op=ALU.add), R=[sc_, rb], W=[gr])
                for g in range(8):
                    C.op("dve", lambda e: e.max(out=m8g[:, g, :], in_=gr[:, g * 32:(g + 1) * 32]), R=[gr], W=[m8g])
                C.op("dve", lambda e: e.tensor_tensor(out=gs[:, :], in0=m8g[:, :, 0], in1=m8g[:, :, 1], op=ALU.add), R=[m8g], W=[gs])
                C.op("dve", lambda e: e.max(out=g8[:, :], in_=gs[:, :]), R=[gs], W=[g8])
                C.op("dve", lambda e: e.tensor_scalar(out=pen[:, :], in0=gs[:, :], scalar1=g8[:, 3:4], scalar2=None, op0=ALU.is_ge), R=[gs, g8], W=[pen])
                C.op("dve", lambda e: e.tensor_scalar(out=pen[:, :], in0=pen[:, :], scalar1=-1.0, scalar2=1.0e4, op0=ALU.add, op1=ALU.mult), R=[pen], W=[pen])
                C.op("dve", lambda e: e.tensor_tensor(out=sel[:, :].rearrange("p (g e) -> p g e", g=8), in0=gr[:, :].rearrange("p (g e) -> p g e", g=8), in1=bc(pen, 0, 8, 32), op=ALU.add), R=[gr, pen], W=[sel])
                C.op("dve", lambda e: e.max(out=e8[:, :], in_=sel[:, :]), R=[sel], W=[e8])
                C.op("dve", lambda e: e.tensor_scalar(out=em[:, :], in0=sel[:, :], scalar1=e8[:, 7:8], scalar2=None, op0=ALU.is_ge), R=[sel, e8], W=[em])
                C.op("act", lambda e: e.activation(out=emb[:, :], in_=em[:, :], func=AF.Copy), R=[em], W=[emb])
                C.op("dve", lambda e: e.tensor_tensor(out=Wc[:, :], in0=sc_[:, :], in1=em[:, :], op=ALU.mult), R=[sc_, em], W=[Wc])
                C.op("dve", lambda e: e.reduce_sum(out=wsum[:, :], in_=Wc[:, :], axis=AX.X), R=[Wc], W=[wsum])
                C.op("dve", lambda e: e.reciprocal(out=wsum[:, :], in_=wsum[:, :]), R=[wsum], W=[wsum])
                C.op("dve", lambda e: e.tensor_scalar(out=Wc[:, :], in0=Wc[:, :], scalar1=wsum[:, 0:1], scalar2=2.5, op0=ALU.mult, op1=ALU.mult), R=[Wc, wsum], W=[Wc])
                pb = self.abank()
                self.mm(pb, pb[:, 0:NEXP], self.SUb[:, :], emb[:, :], R=[self.SUb, emb])
                C.op("dve", lambda e: e.tensor_tensor(out=pos[:, :], in0=pb[:, 0:NEXP], in1=cnt[:, :], op=ALU.add), R=[pb, cnt], W=[pos])
                pb = self.abank()
                self.mm(pb, pb[:, 0:NEXP], self.onesb[:, :], emb[:, :], R=[self.onesb, emb])
                C.op("dve", lambda e: e.tensor_tensor(out=cnt[:, :], in0=cnt[:, :], in1=pb[:, 0:NEXP], op=ALU.add), R=[pb, cnt], W=[cnt])
                C.op("dve", lambda e: e.tensor_scalar(out=ov[:, :], in0=pos[:, :], scalar1=float(CAP), scalar2=1.0e7, op0=ALU.is_ge, op1=ALU.mult), R=[pos], W=[ov])
                C.op("dve", lambda e: e.tensor_tensor(out=dst[:, :], in0=pos[:, :], in1=iotaE[:, :], op=ALU.add), R=[pos, iotaE], W=[dst])
                C.op("dve", lambda e: e.tensor_tensor(out=dst[:, :], in0=dst[:, :], in1=ov[:, :], op=ALU.add), R=[dst, ov], W=[dst])
                sel_b = bass.AP(sel.t, 0, [[NEXP, 128], [0, 8], [1, NEXP]])
                dst_b = bass.AP(dst.t, 0, [[NEXP, 128], [0, 8], [1, NEXP]])
                wc_b = bass.AP(Wc.t, 0, [[NEXP, 128], [0, 8], [1, NEXP]])
                pt = self.tbank()
                for kc in range(2):
                    self.tr(pt, pt[:, kc * 128:(kc + 1) * 128], emb[:, kc * 128:(kc + 1) * 128], self.identb[:, :], R=[emb, self.identb])
                C.op("act", lambda e: e.activation(out=emT[:, :, :].rearrange("p a b -> p (a b)"), in_=pt[:, 0:256], func=AF.Copy), R=[pt], W=[emT])
                pb = self.abank()
                for kc in range(2):
                    self.mm(pb, pb[:, 0:NEXP], emT[:, kc, :], SUF[:, kc, :], R=[emT, SUF], start=(kc == 0), stop=(kc == 1))
                C.op("act", lambda e: e.activation(out=jr[:, :], in_=pb[:, 0:NEXP], func=AF.Copy), R=[pb], W=[jr])
                C.op("dve", lambda e: e.tensor_tensor(out=dst[:, :], in0=dst[:, :], in1=em[:, :], op=ALU.mult), R=[dst, em], W=[dst])
                jr_b = bass.AP(jr.t, 0, [[NEXP, 128], [0, 8], [1, NEXP]])
                C.op("dve", lambda e: e.tensor_tensor(out=oh[:, :, :], in0=jr_b, in1=jv[:, :, :], op=ALU.is_equal), R=[jr, jv], W=[oh])
                C.op("pool", lambda e: e.tensor_tensor(out=tmp[:, :, :], in0=oh[:, :, :], in1=dst_b, op=ALU.mult), R=[oh, dst], W=[tmp])
                C.op("dve", lambda e: e.tensor_reduce(out=dj[:, :], in_=tmp[:, :, :], axis=AX.X, op=ALU.add), R=[tmp], W=[dj])
                C.op("dve", lambda e: e.tensor_copy(out=dji[:, :], in_=dj[:, :]), R=[dj], W=[dji])
                C.op("pool", lambda e: e.tensor_tensor(out=tmp[:, :, :], in0=oh[:, :, :], in1=wc_b, op=ALU.mult), R=[oh, Wc, tmp], W=[tmp])
                C.op("dve", lambda e: e.tensor_reduce(out=rowd[:, :, 2], in_=tmp[:, :, :], axis=AX.X, op=ALU.add), R=[tmp], W=[rowd])
                C.op("pool", lambda e: e.iota(rowd[:, :, 0], pattern=[[0, 8]], base=r0, channel_multiplier=1, allow_small_or_imprecise_dtypes=True), R=[rowd], W=[rowd])
                C.op("pool", lambda e: e.iota(rowd[:, :, 1], pattern=[[1, 8]], base=r0 * 8, channel_multiplier=8, allow_small_or_imprecise_dtypes=True), R=[rowd], W=[rowd])
                C.op("pool", lambda e: e.memset(rowd[:, :, 3], 0.0), R=[rowd], W=[rowd])
                for j in range(8):
                    C.dma(lambda e: e.indirect_dma_start(out=self.L_d.t[:, :], out_offset=bass.IndirectOffsetOnAxis(ap=dji[:, j:j + 1], axis=0), in_=rowd[:, j, :], in_offset=None,
                                                         bounds_check=self.bnd["L"], oob_is_err=False), R=[rowd, dji, self.L_d], W=[], q="pool")
                pb = self.abank()
                for blk in range(4):
                    for k in range(8):
                        self.mm(pb, pb[:, blk * 128:(blk + 1) * 128], Wsgu[:, k, blk * 128:(blk + 1) * 128], u2Tb[:, k, :], R=[Wsgu, u2Tb], start=(k == 0), stop=(k == 7))
                C.op("act", lambda e: e.activation(out=sg[:, :], in_=pb[:, 0:256], func=AF.Silu), R=[pb], W=[sg])
                C.op("dve", lambda e: e.tensor_tensor(out=hs[:, :, :].rearrange("p a b -> p (a b)"), in0=sg[:, :], in1=pb[:, 256:512], op=ALU.mult), R=[sg, pb], W=[hs])
                for n in range(2):
                    pb = self.abank()
                    for fb in range(2):
                        self.mm(pb, pb[:, 0:512], hs[:, fb, :], Wsd[:, fb, n * 512:(n + 1) * 512], R=[hs, Wsd], start=(fb == 0), stop=(fb == 1))
                    C.op("act", lambda e: e.activation(out=sho[:, n * 512:(n + 1) * 512], in_=pb[:, 0:512], func=AF.Copy), R=[pb], W=[sho])
                C.dma(lambda e: e.dma_start(out=self.sh_d.t[r0:r0 + 128, :], in_=sho[:, :]), R=[sho], W=[self.sh_d])

    def experts(self, moe_g, moe_u, moe_d):
        C = self.C
        NB = CAP // 128
        with Scope(C):
            Wf = [C.sb([128, 6144], F32, "Wf%d" % i) for i in range(3)]
            Wgu = [C.sb([128, 8, 512], BF16, "Wgu%d" % i) for i in range(2)]
            Wd = [C.sb([128, 2, D], BF16, "Wd%d" % i) for i in range(2)]
            Lt = [C.sb([128, NB, 4], F32, "Lt%d" % i) for i in range(3)]
            Li = [C.sb([128, NB, 2], I32, "Lti%d" % i) for i in range(4)]
            Lw = [C.sb([128, NB], F32, "Lw%d" % i) for i in range(4)]
            xg = [[C.sb([128, D], BF16, "xg%d_%d" % (i, j)) for j in range(NB)] for i in range(2)]
            xgT = [C.sb([128, 8, CAP], BF16, "xgT%d" % i) for i in range(2)]
            sg = [C.sb([128, CAP], F32, "e_sg%d" % i) for i in range(2)]
            hb = [C.sb([128, 2, CAP], BF16, "e_hb%d" % i) for i in range(2)]
            yo = [[C.sb([128, D], F32, "yo%d_%d" % (i, j)) for j in range(NB)] for i in range(2)]

            def load(e_):
                b = e_ % 3
                C.dma(lambda e: e.dma_start(out=Wf[b][:, 0:2048].rearrange("p (k f) -> p k f", k=8), in_=moe_g.t[e_ * D:(e_ + 1) * D, :].rearrange("(p k) f -> p k f", k=8)), R=[moe_g], W=[Wf[b]])
                C.dma(lambda e: e.dma_start(out=Wf[b][:, 2048:4096].rearrange("p (k f) -> p k f", k=8), in_=moe_u.t[e_ * D:(e_ + 1) * D, :].rearrange("(p k) f -> p k f", k=8)), R=[moe_u], W=[Wf[b]])
                C.dma(lambda e: e.dma_start(out=Wf[b][:, 4096:6144].rearrange("p (k n) -> p k n", k=2), in_=moe_d.t[e_ * 256:(e_ + 1) * 256, :].rearrange("(k p) n -> p k n", p=128)), R=[moe_d], W=[Wf[b]])
                C.dma(lambda e: e.dma_start(out=Lt[b][:, :, :], in_=self.L_d.t[e_ * CAP:(e_ + 1) * CAP, :].rearrange("(a p) n -> p a n", p=128)), R=[self.L_d], W=[Lt[b]])

            def small(e_):
                b4 = e_ % 4; b3 = e_ % 3
                C.op("dve", lambda e: e.tensor_copy(out=Li[b4][:, :, :], in_=Lt[b3][:, :, 0:2]), R=[Lt[b3]], W=[Li[b4]])
                C.op("dve", lambda e: e.tensor_copy(out=Lw[b4][:, :], in_=Lt[b3][:, :, 2]), R=[Lt[b3]], W=[Lw[b4]])

            def castW(e_):
                b = e_ % 2; b3 = e_ % 3
                C.op("act", lambda e: e.activation(out=Wgu[b][:, :, 0:256], in_=Wf[b3][:, 0:2048].rearrange("p (k f) -> p k f", k=8), func=AF.Copy), R=[Wf[b3]], W=[Wgu[b]])
                C.op("act", lambda e: e.activation(out=Wgu[b][:, :, 256:512], in_=Wf[b3][:, 2048:4096].rearrange("p (k f) -> p k f", k=8), func=AF.Copy), R=[Wf[b3]], W=[Wgu[b]])
                C.op("act", lambda e: e.activation(out=Wd[b][:, :, :], in_=Wf[b3][:, 4096:6144].rearrange("p (k n) -> p k n", k=2), func=AF.Copy), R=[Wf[b3]], W=[Wd[b]])

            def gather(e_):
                b = e_ % 2
                for blk in range(NB):
                    C.dma(lambda e: e.indirect_dma_start(out=xg[b][blk][:, :], out_offset=None, in_=self.u2_d.t[:, :], in_offset=bass.IndirectOffsetOnAxis(ap=Li[e_ % 4][:, blk, 0:1], axis=0),
                                                         bounds_check=self.bnd["u2"], oob_is_err=False), R=[self.u2_d, Li[e_ % 4]], W=[xg[b][blk]], q="pool")

            def transp(e_):
                b = e_ % 2
                for blk in range(NB):
                    pt = self.tbank()
                    for k in range(8):
                        self.tr(pt, pt[:, k * 128:(k + 1) * 128], xg[b][blk][:, k:D:8], self.identb[:, :], R=[xg[b][blk], self.identb])
                    C.op("dve", lambda e: e.tensor_copy(out=xgT[b][:, :, blk * 128:(blk + 1) * 128], in_=pt[:, :].rearrange("p (k n) -> p k n", k=8)), R=[pt], W=[xgT[b]])

            load(0); load(1); load(2); small(0); gather(0); castW(0); transp(0)
            for e_ in range(NEXP):
                b = e_ % 2
                if e_ + 1 < NEXP:
                    small(e_ + 1); gather(e_ + 1)
                pbs = []
                for j in range(4):
                    pb = self.abank()
                    for k in range(8):
                        self.mm(pb, pb[:, 0:CAP], Wgu[b][:, k, j * 128:(j + 1) * 128], xgT[b][:, k, :], R=[Wgu[b], xgT[b]], start=(k == 0), stop=(k == 7))
                    pbs.append(pb)
                for fb in range(2):
                    C.op("act", lambda e: e.activation(out=sg[fb][:, :], in_=pbs[fb][:, 0:CAP], func=AF.Silu), R=[pbs[fb]], W=[sg[fb]])
                    C.op("dve", lambda e: e.tensor_tensor(out=hb[b][:, fb, :], in0=sg[fb][:, :], in1=pbs[2 + fb][:, 0:CAP], op=ALU.mult), R=[sg[fb], pbs[2 + fb]], W=[hb[b]])
                if e_ + 1 < NEXP:
                    castW(e_ + 1)
                    transp(e_ + 1)
                for blk in range(NB):
                    for n in range(2):
                        pb = self.abank()
                        for fb in range(2):
                            self.mm(pb, pb[:, 0:512], hb[b][:, fb, blk * 128:(blk + 1) * 128], Wd[b][:, fb, n * 512:(n + 1) * 512], R=[hb[b], Wd[b]], start=(fb == 0), stop=(fb == 1))
                        C.op("dve", lambda e: e.tensor_scalar(out=yo[b][blk][:, n * 512:(n + 1) * 512], in0=pb[:, 0:512], scalar1=Lw[e_ % 4][:, blk:blk + 1], scalar2=None, op0=ALU.mult), R=[pb, Lw[e_ % 4]], W=[yo[b][blk]])
                    C.dma(lambda e: e.indirect_dma_start(out=self.Y_d.t[:, :], out_offset=bass.IndirectOffsetOnAxis(ap=Li[e_ % 4][:, blk, 1:2], axis=0), in_=yo[b][blk][:, :], in_offset=None,
                                                         bounds_check=self.bnd["Y"], oob_is_err=False), R=[yo[b][blk], Li[e_ % 4], self.Y_d], W=[], q="pool")
                if e_ + 3 < NEXP:
                    load(e_ + 3)

    def final(self, vecs, out):
        C = self.C
        with Scope(C):
            gate = C.sb([128, D], F32, "fgate"); gfin = C.sb([128, D], F32, "gfin")
            self.col_to_bcast(self.gcol, 8, gate)
            C.dma(lambda e: e.dma_start(out=gfin[:, :], in_=vecs.t[256:384, :]), R=[vecs], W=[gfin])
            Yt = [C.sb([128, 8, D], F32, "Yt%d" % i) for i in range(2)]
            x1 = [C.sb([128, D], F32, "fx1%d" % i) for i in range(2)]; sh = [C.sb([128, D], F32, "fsh%d" % i) for i in range(2)]
            accs = [C.sb([128, D], F32, "facc%d" % i) for i in range(2)]; junks = [C.sb([128, D], BF16, "fjunk%d" % i) for i in range(2)]; sss = [C.sb([128, 1], F32, "fss%d" % i) for i in range(2)]; rstds = [C.sb([128, 1], F32, "frstd%d" % i) for i in range(2)]
            ot = [C.sb([128, D], F32, "fot%d" % i) for i in range(2)]
            for t in range(32):
                r0 = t * 128; b = t % 2
                acc = accs[b]; junk = junks[b]; ss = sss[b]; rstd = rstds[b]
                C.dma(lambda e: e.dma_start(out=Yt[b][:, :, :], in_=self.Y_d.t[r0 * 8:(r0 + 128) * 8, :].rearrange("(p j) n -> p j n", j=8)), R=[self.Y_d], W=[Yt[b]])
                C.dma(lambda e: e.dma_start(out=x1[b][:, :], in_=self.x1_d.t[r0:r0 + 128, :]), R=[self.x1_d], W=[x1[b]])
                C.dma(lambda e: e.dma_start(out=sh[b][:, :], in_=self.sh_d.t[r0:r0 + 128, :]), R=[self.sh_d], W=[sh[b]])
                C.op("dve", lambda e: e.tensor_reduce(out=acc[:, :], in_=Yt[b][:, :, :].rearrange("p j n -> p n j"), axis=AX.X, op=ALU.add), R=[Yt[b]], W=[acc])
                C.op("pool", lambda e: e.tensor_tensor(out=acc[:, :], in0=acc[:, :], in1=sh[b][:, :], op=ALU.add), R=[acc, sh[b]], W=[acc])
                C.op("dve", lambda e: e.tensor_tensor(out=acc[:, :], in0=acc[:, :], in1=gate[:, :], op=ALU.mult), R=[acc, gate], W=[acc])
                C.op("pool", lambda e: e.tensor_tensor(out=acc[:, :], in0=acc[:, :], in1=x1[b][:, :], op=ALU.add), R=[acc, x1[b]], W=[acc])
                self.rms_rstd(acc[:, :], acc, junk, ss, rstd, D)
                C.op("dve", lambda e: e.scalar_tensor_tensor(out=ot[b][:, :], in0=acc[:, :], scalar=rstd[:, 0:1], in1=gfin[:, :], op0=ALU.mult, op1=ALU.mult), R=[acc, rstd, gfin], W=[ot[b]])
                C.dma(lambda e: e.dma_start(out=out.t[r0:r0 + 128, :], in_=ot[b][:, :]), R=[ot[b]], W=[out])


def col_layout(v):
    return np.ascontiguousarray(np.asarray(v).reshape(8, 128).T)


def rep128(v):
    v = np.asarray(v).reshape(1, -1)
    return np.ascontiguousarray(np.broadcast_to(v, (128, v.shape[1])))


def host_layout(inp):
    f32 = np.float32
    x = np.asarray(inp["x"], f32); ctx = np.asarray(inp["ctx"], f32)
    w_in = np.ascontiguousarray(np.asarray(inp["w_in"], f32)[0])
    wsc = w_in[:, 1024:1024 + WS]
    cxw = np.asarray(inp["conv_xbc_w"], f32)[0]; cxb = np.asarray(inp["conv_xbc_b"], f32)[0]
    cqw = np.asarray(inp["conv_qk_w"], f32)[0]; cqb = np.asarray(inp["conv_qk_b"], f32)[0]
    dtb = np.asarray(inp["ssd_dt_bias"], f32)[0]; alog = np.asarray(inp["ssd_a_log"], f32)[0]
    ib = np.asarray(inp["mlstm_i_bias"], f32)[0]; fb = np.asarray(inp["mlstm_f_bias"], f32)[0]

    def taps(w, nt, flip):
        ww = w[::-1] if flip else w
        return np.ascontiguousarray(ww.reshape(5, nt, 128).transpose(2, 1, 0).reshape(128, nt * 5))

    def bias(b, nt):
        return np.ascontiguousarray(b.reshape(nt, 128).T)

    def wl(d):
        w = wsc.copy()
        if d == 1:
            w[:, O_DT:O_DT + 16] = wsc[:, O_DT + 16:O_DT + 32]
            w[:, O_GT:O_GT + 16] = wsc[:, O_GT + 16:O_GT + 32]
        return w

    def smallset(d):
        return rep128(np.concatenate([dtb[d], alog[d], ib[d], fb[d]]))
    wl_d = [wl(0), wl(1)]
    shared = dict(
        ada_w=np.ascontiguousarray(np.asarray(inp["ada_w"], f32)[0]),
        ada_b=np.ascontiguousarray(np.asarray(inp["ada_b"], f32)[0].reshape(48, 128).T),
        gcols=np.concatenate([col_layout(inp["norm_mix_g"][0]), col_layout(inp["norm_ffn_g"][0])], axis=1).astype(f32),
        w_in=w_in,
        ssd_d=rep128(np.asarray(inp["ssd_d"], f32)[0]),
        vecs=np.concatenate([rep128(inp["ssd_norm_g"][0]), rep128(inp["mlstm_norm_g"][0]), rep128(inp["norm_final_g"])], axis=0).astype(f32),
        w_ssd_out=np.ascontiguousarray(np.asarray(inp["w_ssd_out"], f32)[0]),
        w_mlstm_out=np.ascontiguousarray(np.asarray(inp["w_mlstm_out"], f32)[0]),
        w_out=np.ascontiguousarray(np.asarray(inp["w_out"], f32)[0]),
        router_w=np.ascontiguousarray(np.asarray(inp["router_w"], f32)[0]),
        router_b=rep128(np.asarray(inp["router_bias"], f32)[0]),
        moe_w_gate=np.asarray(inp["moe_w_gate"], f32)[0].reshape(NEXP * D, 256),
        moe_w_up=np.asarray(inp["moe_w_up"], f32)[0].reshape(NEXP * D, 256),
        moe_w_down=np.asarray(inp["moe_w_down"], f32)[0].reshape(NEXP * 256, D),
        shared_w_gate=np.ascontiguousarray(np.asarray(inp["shared_w_gate"], f32)[0]),
        shared_w_up=np.ascontiguousarray(np.asarray(inp["shared_w_up"], f32)[0]),
        shared_w_down=np.ascontiguousarray(np.asarray(inp["shared_w_down"], f32)[0]),
    )
    maps = []
    for c in range(NCORE):
        b, s = c // 4, c % 4
        m = dict(shared)
        m["xo"] = np.ascontiguousarray(x[b, s * SEG:(s + 1) * SEG])
        pre = []; dirs = []
        for j in range(3):
            if j < s:
                pre.append(x[b, j * SEG:(j + 1) * SEG]); dirs.append(0)
            else:
                g = 3 - (j - s)
                pre.append(x[b, g * SEG:(g + 1) * SEG][::-1]); dirs.append(1)
        m["xpre"] = np.ascontiguousarray(np.concatenate(pre, axis=0))
        m["ctx2"] = np.ascontiguousarray(np.concatenate([ctx[b], ctx[b][::-1]], axis=0))
        cv = np.zeros((128, 16), f32)
        cv[:, 0::2] = col_layout(inp["c"][b]); cv[:, 1::2] = col_layout(inp["c_ctx"])
        m["cvec"] = cv
        sd = [0, 1] + dirs
        m["wlite"] = np.ascontiguousarray(np.concatenate([wl_d[d] for d in sd], axis=0))
        m["cwx"] = np.concatenate([taps(cxw, 12, d == 1) for d in sd] + [taps(cxw, 12, False)], axis=0)
        m["cbx"] = np.concatenate([bias(cxb, 12)] * 6, axis=0)
        m["cwq"] = np.concatenate([taps(cqw, 8, d == 1) for d in sd] + [taps(cqw, 8, False)], axis=0)
        m["cbq"] = np.concatenate([bias(cqb, 8)] * 6, axis=0)
        m["small"] = np.concatenate([smallset(d) for d in sd] + [smallset(0), smallset(1)], axis=0)
        fl = np.zeros((128, 8), f32)
        for j in range(3):
            fl[:, j] = 1.0 if dirs[j] == 0 else 0.0
            fl[:, 4 + j] = 1.0 - fl[:, j]
        m["flags"] = fl
        maps.append(m)
    return maps


def kernel(**inputs):
    maps = host_layout(inputs)
    prog = Prog()
    nc = prog.build()
    maps = [{k: v for k, v in m.items() if k in prog.ins} for m in maps]
    res = run_bass_kernel_spmd(nc, maps, core_ids=list(range(NCORE)))
    out = np.zeros((2, 4 * SEG, D), np.float32)
    for c in range(NCORE):
        b, s = c // 4, c % 4
        out[b, s * SEG:(s + 1) * SEG] = res.results[c]["out"]
    return out
```
